# Optimizing a Trainium2 kernel written in Bass

```python
import math
import jax, jax.numpy as jnp
from jax import lax
import numpy as np

D_MODEL = 1024
BATCH = 16
SEQ = 2048
DEPTH = 1

RW_HEADS = 8
RW_HEAD_DIM = 64
RW_DIM = RW_HEADS * RW_HEAD_DIM
W_LORA = 64
A_LORA = 64
G_LORA = 128
GN_EPS = 64e-5
MLA_HEADS = 8
QK_NOPE = 64
QK_ROPE = 32
V_HEAD = 64
Q_LORA = 384
KV_LORA = 256
ROPE_THETA = 10000.0
Q_BLOCK = 128
N_GROUPS = 4
EXPERTS_PER_GROUP = 8
N_EXPERTS = N_GROUPS * EXPERTS_PER_GROUP
TOP_K = 2
D_EXPERT = 256
EXPERT_BLOCK = 256
NORM_EPS = 1e-6

RW_COLS = 3 * RW_DIM + W_LORA + A_LORA + G_LORA
MLA_COLS = Q_LORA + KV_LORA + QK_ROPE
GATE_COLS = 2 * D_MODEL
IN_COLS = RW_COLS + MLA_COLS + GATE_COLS

kernel_name = "hybrid_rwkv7_mla_hiermoe"


def rmsnorm(x, g):
    xf = x.astype(jnp.float32)
    y = xf * lax.rsqrt(jnp.mean(xf * xf, axis=-1, keepdims=True) + NORM_EPS)
    return (y * g.astype(jnp.float32)).astype(x.dtype)


def rope(t, cos, sin):
    t1, t2 = jnp.split(t, 2, axis=-1)
    return jnp.concatenate([t1 * cos - t2 * sin, t2 * cos + t1 * sin], axis=-1)


def rwkv7_mix(z_in, mu, w0, w_up, a0, a_up, g_up, k_k, k_a, r_k, gn_w, gn_b):
    B, S, _ = z_in.shape
    dt = z_in.dtype
    prev = jnp.pad(z_in, ((0, 0), (1, 0), (0, 0)))[:, :-1]
    z = z_in + (prev - z_in) * mu
    r, k, v, wd, ad, gd = jnp.split(
        z, [RW_DIM, 2 * RW_DIM, 3 * RW_DIM, 3 * RW_DIM + W_LORA, 3 * RW_DIM + W_LORA + A_LORA], axis=-1)
    w_log = -jax.nn.softplus(-(w0 + jnp.tanh(wd) @ w_up).astype(jnp.float32)) - 0.5
    decay = jnp.exp(-jnp.exp(w_log))
    a = jax.nn.sigmoid(a0 + ad @ a_up)
    g = jax.nn.sigmoid(gd) @ g_up

    def heads(t):
        return t.reshape(B, S, RW_HEADS, RW_HEAD_DIM).astype(jnp.float32)

    kk = heads(k * k_k)
    kk = kk / jnp.maximum(jnp.sqrt(jnp.sum(kk * kk, axis=-1, keepdims=True)), 1e-12)
    k = k * (1.0 + (a - 1.0) * k_a)
    r_h, k_h, v_h, a_h, w_h = heads(r), heads(k), heads(v), heads(a), heads(decay)

    def step(state, inp):
        r_t, w_t, k_t, v_t, kk_t, a_t = inp
        sa = jnp.einsum('bhvk,bhk->bhv', state, -kk_t)
        state = (state * w_t[:, :, None, :]
                 + sa[..., None] * (kk_t * a_t)[:, :, None, :]
                 + v_t[..., None] * k_t[:, :, None, :])
        return state, jnp.einsum('bhvk,bhk->bhv', state, r_t)

    xs = tuple(jnp.moveaxis(t, 1, 0) for t in (r_h, w_h, k_h, v_h, kk, a_h))
    s0 = jnp.zeros((B, RW_HEADS, RW_HEAD_DIM, RW_HEAD_DIM), jnp.float32)
    _, ys = lax.scan(step, s0, xs)
    y = jnp.moveaxis(ys, 0, 1)
    mean = jnp.mean(y, axis=-1, keepdims=True)
    var = jnp.mean(jnp.square(y - mean), axis=-1, keepdims=True)
    y = ((y - mean) * lax.rsqrt(var + GN_EPS)).reshape(B, S, RW_DIM)
    y = y * gn_w.astype(jnp.float32) + gn_b.astype(jnp.float32)
    bonus = jnp.sum(r_h * k_h * r_k.astype(jnp.float32), axis=-1, keepdims=True) * v_h
    y = y + bonus.reshape(B, S, RW_DIM)
    return (y * g.astype(jnp.float32)).astype(dt)


def mla_mix(q_d, kv_d, kr_raw, cos, sin, g_qa, w_q_up, g_kva, w_kv_up):
    B, S, _ = q_d.shape
    q = (rmsnorm(q_d, g_qa) @ w_q_up).reshape(B, S, MLA_HEADS, QK_NOPE + QK_ROPE)
    q_nope, q_rope = q[..., :QK_NOPE], q[..., QK_NOPE:]
    kv = (rmsnorm(kv_d, g_kva) @ w_kv_up).reshape(B, S, MLA_HEADS, QK_NOPE + V_HEAD)
    k_nope, v = kv[..., :QK_NOPE], kv[..., QK_NOPE:]
    q_rope = rope(q_rope, cos[:, :, None, :], sin[:, :, None, :])
    k_rope = rope(kr_raw, cos, sin)
    scale = 1.0 / math.sqrt(QK_NOPE + QK_ROPE)
    outs = []
    for start in range(0, S, Q_BLOCK):
        end = min(start + Q_BLOCK, S)
        s = (jnp.einsum('bqhd,bkhd->bhqk', q_nope[:, start:end], k_nope[:, :end])
             + jnp.einsum('bqhr,bkr->bhqk', q_rope[:, start:end], k_rope[:, :end]))
        s = s.astype(jnp.float32) * scale
        causal = jnp.arange(end)[None, :] <= jnp.arange(start, end)[:, None]
        p = jax.nn.softmax(jnp.where(causal, s, -jnp.inf), axis=-1).astype(v.dtype)
        outs.append(jnp.einsum('bhqk,bkhd->bqhd', p, v[:, :end]))
    return jnp.concatenate(outs, axis=1).reshape(B, S, MLA_HEADS * V_HEAD)


def hier_moe(h, w_group, b_group, w_router, b_router, w_gu, w_down):
    B, S, D = h.shape
    N = B * S
    hf = h.reshape(N, D)
    p_group = jax.nn.softmax((hf @ w_group).astype(jnp.float32) + b_group.astype(jnp.float32), axis=-1)
    g_sel = jnp.argmax(p_group, axis=-1)
    gate_g = jnp.take_along_axis(p_group, g_sel[:, None], axis=-1)
    fine = ((hf @ w_router).astype(jnp.float32) + b_router.astype(jnp.float32)
            ).reshape(N, N_GROUPS, EXPERTS_PER_GROUP)
    fine_sel = jnp.take_along_axis(fine, g_sel[:, None, None], axis=1)[:, 0]
    top_v, top_i = lax.top_k(fine_sel, TOP_K)
    gate = jax.nn.softmax(top_v, axis=-1) * gate_g
    expert = g_sel[:, None] * EXPERTS_PER_GROUP + top_i

    A = N * TOP_K
    e_flat = expert.reshape(A)
    t_flat = jnp.arange(A) // TOP_K
    w_flat = gate.reshape(A)
    order = jnp.argsort(e_flat)
    e_s, t_s, w_s = e_flat[order], t_flat[order], w_flat[order]
    counts = jnp.bincount(e_flat, length=N_EXPERTS)
    starts = jnp.cumsum(counts) - counts
    padded = (counts + EXPERT_BLOCK - 1) // EXPERT_BLOCK * EXPERT_BLOCK
    pad_end = jnp.cumsum(padded)
    pad_start = pad_end - padded
    dest = pad_start[e_s] + (jnp.arange(A) - starts[e_s])
    n_blocks = -(-A // EXPERT_BLOCK) + N_EXPERTS
    P = n_blocks * EXPERT_BLOCK
    tok_buf = jnp.full((P,), N, jnp.int32).at[dest].set(t_s.astype(jnp.int32))
    w_buf = jnp.zeros((P,), jnp.float32).at[dest].set(w_s)
    blk_expert = jnp.minimum(
        jnp.searchsorted(pad_end, jnp.arange(n_blocks) * EXPERT_BLOCK, side='right'), N_EXPERTS - 1)
    x_pad = jnp.concatenate([hf, jnp.zeros((1, D), hf.dtype)], axis=0)
    xb = x_pad[tok_buf].reshape(n_blocks, EXPERT_BLOCK, D)

    def expert_block(args):
        xblk, e = args
        gt, up = jnp.split(xblk @ w_gu[e], 2, axis=-1)
        return (jax.nn.silu(gt) * up) @ w_down[e]

    yb = lax.map(expert_block, (xb, blk_expert)).reshape(P, D)
    out = jnp.zeros((N + 1, D), h.dtype).at[tok_buf].add(yb * w_buf[:, None].astype(h.dtype))
    return out[:N].reshape(B, S, D)


def setup_inputs(seed: int = 0) -> dict:
    key = jax.random.key(seed)
    ks = jax.random.split(key, 32)
    f32 = jnp.float32

    def nrm(k, shape, scale):
        return jax.random.normal(k, shape, f32) * scale

    L, D = DEPTH, D_MODEL
    x = jax.random.normal(ks[0], (BATCH, SEQ, D), f32)
    offsets = jax.random.randint(ks[1], (BATCH, 1), 0, 1024, dtype=jnp.int32)
    positions = offsets + jnp.arange(SEQ, dtype=jnp.int32)[None, :]
    return {
        "x": x,
        "positions": positions,
        "mix_norm_g": 1.0 + nrm(ks[2], (L, D), 0.02),
        "w_in": nrm(ks[3], (L, D, IN_COLS), D ** -0.5),
        "rw_mu": jax.random.uniform(ks[4], (L, RW_COLS), f32),
        "rw_w0": jax.random.uniform(ks[5], (L, RW_DIM), f32, -6.0, -1.0),
        "rw_w_up": nrm(ks[6], (L, W_LORA, RW_DIM), 0.1 * W_LORA ** -0.5),
        "rw_a0": nrm(ks[7], (L, RW_DIM), 0.1),
        "rw_a_up": nrm(ks[8], (L, A_LORA, RW_DIM), 0.5 * A_LORA ** -0.5),
        "rw_g_up": nrm(ks[9], (L, G_LORA, RW_DIM), G_LORA ** -0.5),
        "rw_k_k": 0.85 + nrm(ks[10], (L, RW_DIM), 0.02),
        "rw_k_a": 1.0 + nrm(ks[11], (L, RW_DIM), 0.02),
        "rw_r_k": nrm(ks[12], (L, RW_HEADS, RW_HEAD_DIM), 0.1),
        "rw_gn_w": 1.0 + nrm(ks[13], (L, RW_DIM), 0.02),
        "rw_gn_b": nrm(ks[14], (L, RW_DIM), 0.02),
        "mla_g_qa": 1.0 + nrm(ks[15], (L, Q_LORA), 0.02),
        "mla_w_q_up": nrm(ks[16], (L, Q_LORA, MLA_HEADS * (QK_NOPE + QK_ROPE)), Q_LORA ** -0.5),
        "mla_g_kva": 1.0 + nrm(ks[17], (L, KV_LORA), 0.02),
        "mla_w_kv_up": nrm(ks[18], (L, KV_LORA, MLA_HEADS * (QK_NOPE + V_HEAD)), KV_LORA ** -0.5),
        "w_branch_rw": nrm(ks[19], (L, RW_DIM, D), RW_DIM ** -0.5),
        "w_branch_mla": nrm(ks[20], (L, MLA_HEADS * V_HEAD, D), (MLA_HEADS * V_HEAD) ** -0.5),
        "w_out": nrm(ks[21], (L, D, D), D ** -0.5),
        "ffn_norm_g": 1.0 + nrm(ks[22], (L, D), 0.02),
        "moe_w_group": nrm(ks[23], (L, D, N_GROUPS), D ** -0.5),
        "moe_b_group": nrm(ks[24], (L, N_GROUPS), 0.01),
        "moe_w_router": nrm(ks[25], (L, D, N_EXPERTS), D ** -0.5),
        "moe_b_router": nrm(ks[26], (L, N_EXPERTS), 0.01),
        "moe_w_gu": nrm(ks[27], (L, N_EXPERTS, D, 2 * D_EXPERT), D ** -0.5),
        "moe_w_down": nrm(ks[28], (L, N_EXPERTS, D_EXPERT, D), D_EXPERT ** -0.5),
        "final_norm_g": 1.0 + nrm(ks[29], (D,), 0.02),
    }


def reference(x, positions, mix_norm_g, w_in, rw_mu, rw_w0, rw_w_up, rw_a0, rw_a_up, rw_g_up,
              rw_k_k, rw_k_a, rw_r_k, rw_gn_w, rw_gn_b, mla_g_qa, mla_w_q_up, mla_g_kva, mla_w_kv_up,
              w_branch_rw, w_branch_mla, w_out, ffn_norm_g, moe_w_group, moe_b_group, moe_w_router,
              moe_b_router, moe_w_gu, moe_w_down, final_norm_g):
    inv_freq = ROPE_THETA ** (-jnp.arange(0, QK_ROPE, 2, dtype=jnp.float32) / QK_ROPE)
    ang = positions.astype(jnp.float32)[..., None] * inv_freq
    cos, sin = jnp.cos(ang).astype(x.dtype), jnp.sin(ang).astype(x.dtype)

    for l in range(DEPTH):
        h = rmsnorm(x, mix_norm_g[l])
        cols = h @ w_in[l]
        c_rw = cols[..., :RW_COLS]
        c_q = cols[..., RW_COLS:RW_COLS + Q_LORA]
        c_kv = cols[..., RW_COLS + Q_LORA:RW_COLS + Q_LORA + KV_LORA]
        c_kr = cols[..., RW_COLS + Q_LORA + KV_LORA:RW_COLS + MLA_COLS]
        gate_rw = jax.nn.sigmoid(cols[..., RW_COLS + MLA_COLS:RW_COLS + MLA_COLS + D_MODEL])
        gate_mla = jax.nn.sigmoid(cols[..., RW_COLS + MLA_COLS + D_MODEL:])

        y_rw = rwkv7_mix(c_rw, rw_mu[l], rw_w0[l], rw_w_up[l], rw_a0[l], rw_a_up[l], rw_g_up[l],
                         rw_k_k[l], rw_k_a[l], rw_r_k[l], rw_gn_w[l], rw_gn_b[l])
        y_mla = mla_mix(c_q, c_kv, c_kr, cos, sin, mla_g_qa[l], mla_w_q_up[l], mla_g_kva[l], mla_w_kv_up[l])
        merged = gate_rw * (y_rw @ w_branch_rw[l]) + gate_mla * (y_mla @ w_branch_mla[l])
        x = x + merged @ w_out[l]

        h2 = rmsnorm(x, ffn_norm_g[l])
        x = x + hier_moe(h2, moe_w_group[l], moe_b_group[l], moe_w_router[l], moe_b_router[l],
                         moe_w_gu[l], moe_w_down[l])

    return rmsnorm(x, final_norm_g)
```

```python
import math
import numpy as np
import concourse.bass as bass
import concourse.mybir as mybir
from concourse.bass_utils import run_bass_kernel_spmd
from contextlib import ExitStack

F32 = mybir.dt.float32
BF16 = mybir.dt.bfloat16
I32 = mybir.dt.int32
ALU = mybir.AluOpType
AF = mybir.ActivationFunctionType
AX = mybir.AxisListType

NB = 2
T = 2048
D = 1024
NCORES = 8
RW_COLS = 1792
CQ0 = 1792
CKV0 = 2176
CKR0 = 2432
GRW0 = 2464
GML0 = 3488
CH = 128
NCH = T // CH
CAP = 512
ATT_SCALE = 1.0 / math.sqrt(96.0)
LOGW_C = -math.exp(-0.5)


class Res:
    __slots__ = ("w", "r", "excl")

    def __init__(self, excl=False):
        self.w = None
        self.r = []
        self.excl = excl


class Op:
    __slots__ = ("eng", "fn", "deps", "sdeps", "signal", "seq", "dma", "sem", "semval", "idx", "fin", "cost")

    def __init__(self, eng, fn):
        self.eng = eng
        self.fn = fn
        self.deps = []
        self.sdeps = []
        self.idx = 0
        self.fin = None
        self.cost = None
        self.signal = False
        self.seq = 0
        self.dma = False
        self.sem = None
        self.semval = 0


class Prog:
    ENGS = ("pe", "act", "dve", "pool", "sp")

    def __init__(self, nc, stack, n_dma_sems=10):
        self.nc = nc
        self.ops = {e: [] for e in self.ENGS}
        self.esem = {e: stack.enter_context(nc.semaphore("prog_" + e)) for e in self.ENGS}
        self.dsems, self.dcount, self.dlast = {}, {}, {}
        for q in ("sp", "pool"):
            self.dsems[q] = [stack.enter_context(nc.semaphore(f"dma_{q}_{i}")) for i in range(n_dma_sems)]
            self.dcount[q] = 0
            self.dlast[q] = [None] * n_dma_sems

    def _track(self, op, reads, writes):
        deps = []
        reads = list(reads)
        writes = list(writes)
        for r in reads:
            if r.excl and r not in writes:
                writes.append(r)
        for r in reads:
            if r.w is not None:
                deps.append(r.w)
        for w in writes:
            if w.w is not None:
                deps.append(w.w)
            deps.extend(w.r)
        seen = set()
        for d in deps:
            if d is op or id(d) in seen:
                continue
            seen.add(id(d))
            if (not d.dma) and d.eng == "pe" and op.eng == "pe" and not op.dma:
                op.sdeps.append(d)
                continue
            op.deps.append(d)
            d.signal = True
        for r in reads:
            r.r.append(op)
        for w in writes:
            w.w = op
            w.r = []

    def op(self, eng, fn, reads=(), writes=(), cost=None):
        o = Op(eng, fn)
        self.nidx = getattr(self, "nidx", 0) + 1
        o.idx = self.nidx
        o.cost = cost
        self._track(o, reads, writes)
        self.ops[eng].append(o)
        return o

    def schedule(self, window=64):
        import heapq
        base = {"pe": 0.25, "act": 0.6, "dve": 0.55, "pool": 1.0, "sp": 0.05}
        pend = {e: list(self.ops[e]) for e in self.ENGS}
        out = {e: [] for e in self.ENGS}
        free_at = {e: 0.0 for e in self.ENGS}
        now = 0.0
        events = []
        remaining = sum(len(v) for v in pend.values())
        cnt = 0
        while remaining:
            progressed = False
            for e in self.ENGS:
                if free_at[e] > now or not pend[e]:
                    continue
                best = None
                for o in pend[e][:window]:
                    ok = True
                    for d in o.deps:
                        if d.fin is None or d.fin > now:
                            ok = False
                            break
                    if ok:
                        for d in o.sdeps:
                            if d.fin is None:
                                ok = False
                                break
                    if ok:
                        best = o
                        break
                if best is None:
                    fill = getattr(self, "filler_fn", None)
                    fdep = getattr(self, "filler_dep", None)
                    if e == "pe" and fill is not None and fdep is not None and fdep.fin is not None and fdep.fin <= now:
                        fo = Op("pe", fill)
                        fo.deps.append(fdep)
                        fdep.signal = True
                        out[e].append(fo)
                        self.nfill = getattr(self, "nfill", 0) + 1
                        free_at[e] = now + 0.1
                        cnt += 1
                        heapq.heappush(events, (free_at[e], cnt))
                        progressed = True
                    continue
                pend[e].remove(best)
                out[e].append(best)
                remaining -= 1
                progressed = True
                if best.dma:
                    free_at[e] = now + 0.06
                    best.fin = now + 2.5
                else:
                    c = best.cost if best.cost is not None else base[e]
                    free_at[e] = now + c
                    best.fin = now + c + 0.25
                cnt += 1
                heapq.heappush(events, (best.fin, cnt))
                heapq.heappush(events, (free_at[e], cnt))
            if not progressed:
                if not events:
                    raise RuntimeError("scheduler stuck")
                t = heapq.heappop(events)[0]
                now = max(now, t)
        self.ops = out

    def dma(self, q, fn, reads=(), writes=(), extra=()):
        o = Op(q, fn)
        self.nidx = getattr(self, "nidx", 0) + 1
        o.idx = self.nidx
        o.dma = True
        self._track(o, reads, writes)
        for d in extra:
            o.deps.append(d)
            d.signal = True
        n = len(self.dsems[q])
        i = self.dcount[q] % n
        self.dcount[q] += 1
        prev = self.dlast[q][i]
        if prev is not None:
            o.deps.append(prev)
        o.sem = self.dsems[q][i]
        o.semval = (prev.semval if prev is not None else 0) + 16
        self.dlast[q][i] = o
        self.ops[q].append(o)
        return o

    def emit(self, block):
        for e in self.ENGS:
            c = 0
            for o in self.ops[e]:
                if (not o.dma) and o.signal:
                    c += 1
                    o.seq = c

        def run(e):
            def body(eng):
                waited = {}
                self.icount = getattr(self, "icount", {})
                self.icount[e] = 0
                for o in self.ops[e]:
                    self.icount[e] += 1
                    need = {}
                    for d in o.deps:
                        if d.dma:
                            k, v = d.sem, d.semval
                        else:
                            k, v = self.esem[d.eng], d.seq
                        if need.get(k, 0) < v:
                            need[k] = v
                    for k, v in need.items():
                        if waited.get(k, 0) < v:
                            eng.wait_ge(k, v)
                            waited[k] = v
                            self.icount[e] += 1
                    ins = o.fn(eng)
                    if o.dma:
                        ins.then_inc(o.sem, 16)
                    elif o.signal:
                        ins.then_inc(self.esem[e], 1)
            return body

        block.tensor(run("pe"))
        block.scalar(run("act"))
        block.vector(run("dve"))
        block.gpsimd(run("pool"))
        block.sync(run("sp"))


class Reg:
    def __init__(self, ar, blk0, nblk):
        self.ar, self.b0, self.n = ar, blk0, nblk

    def f(self, a=0, b=None):
        b = self.n * 512 if b is None else b
        return self.ar.t[:, self.b0 * 512 + a:self.b0 * 512 + b]

    def h(self, a=0, b=None):
        b = self.n * 1024 if b is None else b
        assert a % 2 == 0 and b % 2 == 0
        return self.ar.t[:, self.b0 * 512 + a // 2:self.b0 * 512 + b // 2].bitcast(BF16)

    def i(self, a=0, b=None):
        b = self.n * 512 if b is None else b
        return self.ar.t[:, self.b0 * 512 + a:self.b0 * 512 + b].bitcast(I32)

    def rf(self, a=0, b=None):
        b = self.n * 512 if b is None else b
        return [self.ar.res[self.b0 + j] for j in range(a // 512, (b - 1) // 512 + 1)]

    def rh(self, a=0, b=None):
        b = self.n * 1024 if b is None else b
        return [self.ar.res[self.b0 + j] for j in range(a // 1024, (b - 1) // 1024 + 1)]

    def sub(self, blk, n):
        assert blk + n <= self.n
        return Reg(self.ar, self.b0 + blk, n)


class Arena:
    def __init__(self, nc, stack, nblk):
        self.t = stack.enter_context(nc.sbuf_tensor("arena", [128, nblk * 512], F32))
        self.res = [Res() for _ in range(nblk)]
        self.nblk = nblk

    def reg(self, blk0, nblk):
        assert blk0 + nblk <= self.nblk, (blk0, nblk)
        return Reg(self, blk0, nblk)


class Bump:
    def __init__(self, ar, lo, hi):
        self.ar, self.lo, self.hi, self.cur = ar, lo, hi, lo

    def get(self, n):
        r = self.ar.reg(self.cur, n)
        self.cur += n
        assert self.cur <= self.hi, ("arena overflow", self.cur, self.hi)
        return r


def _consts():
    c = {}
    c["ident"] = np.eye(128, dtype=np.float32)
    p = np.arange(128)
    c["mask_su"] = (p[:, None] < p[None, :]).astype(np.float32)
    c["mask_iu"] = (p[:, None] <= p[None, :]).astype(np.float32)
    c["mask_sl"] = (p[:, None] > p[None, :]).astype(np.float32)
    blk = (p[:, None] // 64 == p[None, :] // 64).astype(np.float32)
    c["blockmask"] = blk
    c["blockones"] = blk.copy()
    c["blockavg"] = blk / 64.0
    sm = np.ones((128, 512), np.float32)
    sm[:, ::CH] = 0.0
    c["scanmask"] = sm
    c["allones"] = np.ones((128, 128), np.float32)
    c["mhalf"] = np.full((128, 512), -0.5, np.float32)
    inv_freq = (10000.0 ** (-np.arange(0, 32, 2, dtype=np.float32) / 32.0)).astype(np.float32)
    fq = np.zeros((128, 3), np.float32)
    ph = np.full((128, 3), 0.25, np.float32)
    for r in range(64, 128):
        f = inv_freq[r % 16] / (2 * np.pi)
        fq[r, :] = f
    ph[64:96, 0] = 0.25
    ph[96:112, 0] = 0.5
    ph[112:128, 0] = 0.0
    ph[64:128, 1] = 0.25
    ph[64:80, 2] = 0.5
    ph[80:96, 2] = 0.0
    ph[96:112, 2] = 0.5
    ph[112:128, 2] = 0.0
    c["ecap"] = np.tile((np.arange(32, dtype=np.float32) * CAP)[None, :], (128, 1))
    c["rope_fq"] = fq
    c["rope_ph"] = ph
    return c


_CONST_SHAPES = {n: v.shape for n, v in _consts().items()}
CONST_NAMES = ["ident", "mask_su", "mask_iu", "mask_sl", "blockmask", "blockones", "blockavg", "scanmask",
               "allones", "mhalf", "ecap", "rope_fq", "rope_ph"]


def _fm(v, nchunk):
    return np.ascontiguousarray(np.asarray(v, np.float32).reshape(nchunk, 128).T)


def _host_layout(inp):
    w = {}
    w_in = inp["w_in"][0]
    w["w_in"] = w_in
    kr = w_in[:, CKR0:CKR0 + 32]
    krs = np.concatenate([kr[:, 16:32], kr[:, 0:16]], axis=1)
    z64 = np.zeros((D, 64), np.float32)
    w["w_kr"] = np.ascontiguousarray(np.concatenate([z64, kr, kr, z64, krs, krs], axis=1))
    wq = inp["mla_w_q_up"][0]
    wqh = np.zeros((384, 8, 128), np.float32)
    for h in range(8):
        blk = wq[:, h * 96:(h + 1) * 96]
        wqh[:, h, 0:96] = blk
        wqh[:, h, 96:112] = blk[:, 80:96]
        wqh[:, h, 112:128] = blk[:, 64:80]
    w["w_q"] = np.ascontiguousarray(wqh.reshape(384, 1024))
    wkv = inp["mla_w_kv_up"][0]
    wk = np.zeros((256, 8, 128), np.float32)
    wv = np.zeros((256, 8, 64), np.float32)
    for h in range(8):
        wk[:, h, 0:64] = wkv[:, h * 128:h * 128 + 64]
        wv[:, h, :] = wkv[:, h * 128 + 64:h * 128 + 128]
    w["w_k"] = np.ascontiguousarray(wk.reshape(256, 1024))
    w["w_v"] = np.ascontiguousarray(wv.reshape(256, 512))
    w["wa_up"] = np.ascontiguousarray(np.concatenate([inp["rw_w_up"][0], inp["rw_a_up"][0]], axis=0))
    w["g_up"] = np.ascontiguousarray(inp["rw_g_up"][0])
    w["w_brw"] = np.ascontiguousarray(inp["w_branch_rw"][0])
    w["w_bml"] = np.ascontiguousarray(inp["w_branch_mla"][0])
    w["w_out"] = np.ascontiguousarray(inp["w_out"][0])
    w["w_rt"] = np.ascontiguousarray(np.concatenate([inp["moe_w_group"][0], inp["moe_w_router"][0]], axis=1))
    w["b_rt"] = np.ascontiguousarray(np.concatenate([inp["moe_b_group"][0], inp["moe_b_router"][0]])[None, :])
    w["w_gu"] = np.ascontiguousarray(inp["moe_w_gu"][0])
    w["w_dn"] = np.ascontiguousarray(inp["moe_w_down"][0])
    w["pv_mu"] = _fm(inp["rw_mu"][0], 14)
    vecs = [inp["rw_w0"][0], inp["rw_a0"][0], inp["rw_k_k"][0], inp["rw_k_a"][0], inp["rw_r_k"][0].reshape(-1),
            inp["rw_gn_w"][0], inp["rw_gn_b"][0]]
    w["pv_rw"] = np.ascontiguousarray(np.concatenate([_fm(v, 4) for v in vecs], axis=1))
    w["pv_g1"] = _fm(inp["mix_norm_g"][0], 8)
    w["pv_gq"] = _fm(inp["mla_g_qa"][0], 3)
    w["pv_gkv"] = _fm(inp["mla_g_kva"][0], 2)
    w["pv_g2"] = _fm(inp["ffn_norm_g"][0], 8)
    w["g_ffn"] = np.ascontiguousarray(inp["ffn_norm_g"][0][None, :])
    w["g_fin"] = np.ascontiguousarray(inp["final_norm_g"][None, :])
    return w


W_SHAPES = {
    "w_in": [1024, 4512], "w_kr": [1024, 256], "w_q": [384, 1024], "w_k": [256, 1024], "w_v": [256, 512],
    "wa_up": [128, 512], "g_up": [128, 512], "w_brw": [512, 1024], "w_bml": [512, 1024], "w_out": [1024, 1024],
    "w_rt": [1024, 36], "b_rt": [1, 36], "w_gu": [32, 1024, 512], "w_dn": [32, 256, 1024],
    "pv_mu": [128, 14], "pv_rw": [128, 28], "pv_g1": [128, 8], "pv_gq": [128, 3], "pv_gkv": [128, 2],
    "pv_g2": [128, 8], "g_ffn": [1, 1024], "g_fin": [1, 1024],
}


class Builder:
    def __init__(self, debug=None):
        self.debug = debug
        nc = bass.Bass("TRN2", target_bir_lowering=False)
        self.nc = nc
        self.st = ExitStack()
        st = self.st
        self.x = nc.dram_tensor("x", [NB * T, D], F32, kind="ExternalInput").ap()
        self.pos = nc.dram_tensor("pos", [NB, T], I32, kind="ExternalInput").ap()
        self.cd = {n: nc.dram_tensor("c_" + n, list(_CONST_SHAPES[n]), F32, kind="ExternalInput").ap() for n in CONST_NAMES}
        self.wd = {n: nc.dram_tensor(n, s, F32, kind="ExternalInput").ap() for n, s in W_SHAPES.items()}
        self.out = nc.dram_tensor("out", [NB * T, D], F32, kind="ExternalOutput").ap()
        self.tokid = nc.dram_tensor("tokid", [128, 32], I32, kind="ExternalInput").ap()
        self.h2s = nc.dram_tensor("h2s", [NB * T, D], BF16, kind="Internal").ap()
        self.x1s = nc.dram_tensor("x1s", [NB * T, D], F32, kind="Internal").ap()
        self.x1_r = [Res() for _ in range(32)]
        self.ysd = nc.dram_tensor("ysd", [32 * CAP, D], BF16, kind="Internal").ap()
        self.tokd = nc.dram_tensor("tokd", [32 * CAP, 1], I32, kind="Internal").ap()
        self.h2s_r = [Res() for _ in range(32)]
        self.ys_r = [Res() for _ in range(32)]
        self.tok_r = Res()
        self.dbg = None
        if debug is not None:
            self.dbg = nc.dram_tensor("dbg", list(debug[1]), F32, kind="ExternalOutput").ap()
        self.p = Prog(nc, st)
        self.ar = Arena(nc, st, 88)
        self.psb = [st.enter_context(nc.psum_tensor(f"ps{i}", [128, 512], F32)) for i in range(8)]
        self.psr = [Res(excl=True) for _ in range(8)]
        self.psi = 0
        import os as _os
        self.nrot = 7 if _os.environ.get("KFILL", "1") == "1" else 8
        self.small = {}
        self.out_res = Res()

    def sb(self, name, shape, dt=F32):
        t = self.st.enter_context(self.nc.sbuf_tensor("sb_" + name, shape, dt))
        r = Res()
        self.small[name] = (t, r)
        return t, r

    def ps(self, hold=False):
        held = getattr(self, "held", set())
        self.held = held
        nrot = getattr(self, "nrot", 8)
        while self.psi in held:
            self.psi = (self.psi + 1) % nrot
        i = self.psi
        self.psi = (self.psi + 1) % nrot
        if hold:
            held.add(i)
        return self.psb[i], self.psr[i]

    def ps_release(self, t):
        for i in range(8):
            if self.psb[i] is t:
                self.held.discard(i)

    def pe(self, fn, rd, wr):
        return self.p.op("pe", fn, rd, wr)

    def act(self, fn, rd, wr):
        return self.p.op("act", fn, rd, wr)

    def dve(self, fn, rd, wr):
        return self.p.op("dve", fn, rd, wr)

    def pool(self, fn, rd, wr):
        return self.p.op("pool", fn, rd, wr)

    def mm(self, out, lhsT, rhs, start, stop, rd, wr):
        try:
            n = rhs.free_size()
            cost = 0.06 + n / 2400.0 * (4.0 if rhs.dtype == F32 else 1.0)
        except Exception:
            cost = None
        return self.p.op("pe", lambda e: e.matmul(out=out, lhsT=lhsT, rhs=rhs, start=start, stop=stop), rd, wr, cost=cost)

    def loadw(self, dst, src, rd, wr):
        return self.p.dma("pool", lambda e: e.dma_start(out=dst, in_=src), rd, wr)

    def load(self, dst, src, rd, wr):
        return self.p.dma("sp", lambda e: e.dma_start(out=dst, in_=src), rd, wr)

    def setup(self):
        c = {}
        for n in CONST_NAMES:
            t, r = self.sb("k_" + n, list(_CONST_SHAPES[n]))
            self.load(t[:], self.cd[n], [], [r])
            c[n] = (t, r)
        self.c = c
        idb, idb_r = self.sb("identb", [128, 128], BF16)
        self.identb_op = self.dve(lambda e: e.tensor_copy(out=idb[:], in_=c["ident"][0][:]), [c["ident"][1]], [idb_r])
        self.identb, self.identb_r = idb, idb_r
        m2, m2_r = self.sb("mask2", [128, 256])
        self.dve(lambda e: e.tensor_copy(out=m2[:, 0:128], in_=c["mask_su"][0][:]), [c["mask_su"][1]], [m2_r])
        self.dve(lambda e: e.tensor_copy(out=m2[:, 128:256], in_=c["mask_iu"][0][:]), [c["mask_iu"][1]], [m2_r])
        self.mask2, self.mask2_r = m2, m2_r
        pv = {}
        for n in ["pv_mu", "pv_rw", "pv_g1", "pv_gq", "pv_gkv", "pv_g2"]:
            t, r = self.sb("s_" + n, W_SHAPES[n])
            self.load(t[:], self.wd[n], [], [r])
            pv[n] = (t, r)
        self.pv = pv
        omm, omm_r = self.sb("omm", [128, 14])
        self.dve(lambda e: e.tensor_scalar(out=omm[:], in0=pv["pv_mu"][0][:], scalar1=-1.0, scalar2=1.0, op0=ALU.mult, op1=ALU.add),
                 [pv["pv_mu"][1]], [omm_r])
        self.omm, self.omm_r = omm, omm_r
        dv, dv_r = self.sb("rwdv", [128, 12])
        rw = pv["pv_rw"][0]
        self.dve(lambda e: e.tensor_scalar_mul(out=dv[:, 0:8], in0=rw[:, 0:8], scalar1=0.5), [pv["pv_rw"][1]], [dv_r])
        self.dve(lambda e: e.tensor_scalar_mul(out=dv[:, 8:12], in0=rw[:, 12:16], scalar1=-1.0), [pv["pv_rw"][1]], [dv_r])
        self.rwdv, self.rwdv_r = dv, dv_r
        wa_up, wa_up_r = self.sb("wa_up", [128, 512], BF16)
        self.loadw(wa_up[:], self.wd["wa_up"], [], [wa_up_r])
        g_up, g_up_r = self.sb("g_up", [128, 512], BF16)
        self.loadw(g_up[:], self.wd["g_up"], [], [g_up_r])
        self.wa_up, self.wa_up_r, self.g_up, self.g_up_r = wa_up, wa_up_r, g_up, g_up_r
        self.STP, self.STP_r = self.sb("STP", [128, 128])
        self.STB, self.STB_r = self.sb("STB", [128, 128], BF16)
        self.TMS, self.TMS_r = self.sb("TMS", [128, 128])
        self.XB, self.XB_r = self.sb("XB", [128, 128], BF16)
        self.UB, self.UB_r = self.sb("UB", [128, 128], BF16)
        self.UP0, self.UP0_r = self.sb("UP0", [128, 128], BF16)
        self.UP1, self.UP1_r = self.sb("UP1", [128, 128], BF16)
        self.dve(lambda e: e.memset(self.UP0[:], 0.0), [], [self.UP0_r])
        self.dve(lambda e: e.memset(self.UP1[:], 0.0), [], [self.UP1_r])
        self.GC, self.GC_r = self.sb("GC", [128, NCH])
        self.carry = [self.sb(f"carry{i}", [128, 1]) for i in range(3)]
        self.LG, self.LG_r = self.sb("LG", [128, 16, 36])
        self.WT, self.WT_r = self.sb("WT", [128, 16, 32])
        self.R8, self.R8_r = self.sb("R8", [128, 192])
        self.MM, self.MM_r = self.sb("MMem", [128, 32, 32])
        self.SRf, self.SRf_r = self.sb("SRf", [128, 32, 2])
        self.SR, self.SR_r = self.sb("SR", [128, 32, 2], I32)
        self.W12, self.W12_r = self.sb("W12", [128, 32, 2])
        self.TID, self.TID_r = self.sb("TID", [128, 32], I32)
        self.load(self.TID[:], self.tokid, [], [self.TID_r])
        self.ZI, self.ZI_r = self.sb("ZI", [128, 128], I32)
        self.dve(lambda e: e.memset(self.ZI[:], 0), [], [self.ZI_r])
        self.IDX = [self.sb(f"IDX{i}", [128, 1], I32) for i in range(8)]
        self.brt, self.brt_r = self.sb("brt", [128, 36])
        self.load(self.brt[:], self.wd["b_rt"].partition_broadcast(128), [], [self.brt_r])
        self.ssq, self.ssq_r = self.sb("ssq", [128, 16])
        self.rs, self.rs_r = self.sb("rs", [128, 16])

    def phaseA(self, b, hT, scr):
        c = self.c
        xts = [scr.get(2), scr.get(2)]
        junk = scr.get(1)
        hb = scr.get(1)
        g1, g1_r = self.pv["pv_g1"]
        hTv = hT.h().rearrange("p (c t) -> p c t", c=8)
        ssq, ssq_r, rs, rs_r = self.ssq, self.ssq_r, self.rs, self.rs_r
        for i in range(16):
            xt = xts[i % 2]
            row0 = b * T + i * 128
            self.load(xt.f(), self.x[row0:row0 + 128, :], [], xt.rf())
            self.dve(lambda e, i=i: e.memset(ssq[:, i:i + 1], 0.0), [], [ssq_r])
            self.act(lambda e, xt=xt, i=i: e.activation(out=junk.h(), in_=xt.f(), func=AF.Square, accum_out=ssq[:, i:i + 1]),
                     xt.rf() + [ssq_r], junk.rh() + [ssq_r])
            self.dve(lambda e, i=i: e.tensor_scalar(out=rs[:, i:i + 1], in0=ssq[:, i:i + 1], scalar1=1.0 / D, scalar2=1e-6,
                                                    op0=ALU.mult, op1=ALU.add), [ssq_r], [rs_r])
            self.act(lambda e, i=i: e.activation(out=rs[:, i:i + 1], in_=rs[:, i:i + 1], func=AF.Ln), [rs_r], [rs_r])
            self.act(lambda e, i=i: e.activation(out=rs[:, i:i + 1], in_=rs[:, i:i + 1], func=AF.Exp, scale=-0.5), [rs_r], [rs_r])
            self.dve(lambda e, xt=xt, i=i: e.tensor_scalar_mul(out=hb.h(), in0=xt.f(), scalar1=rs[:, i:i + 1]),
                     xt.rf() + [rs_r], hb.rh())
            pt, pr = self.ps()
            ptv = pt[:].bitcast(BF16).rearrange("p (c t) -> p c t", c=8)
            for cc in range(8):
                self.pe(lambda e, cc=cc, ptv=ptv: e.transpose(out=ptv[:, cc, :], in_=hb.h(cc * 128, (cc + 1) * 128), identity=self.identb[:]),
                        hb.rh() + [self.identb_r], [pr])
            wr = []
            for cc in range(8):
                wr += hT.rh(cc * T + i * 128, cc * T + (i + 1) * 128)
            self.dve(lambda e, i=i, ptv=ptv: e.tensor_tensor(out=hTv[:, :, i * 128:(i + 1) * 128], in0=ptv,
                                                            in1=g1[:].unsqueeze(2).broadcast_to([128, 8, 128]), op=ALU.mult),
                     [pr, g1_r], wr)

    def shiftmix(self, sid, wt_ap, wt_res, hT, ci, tt, z, pm):
        mu, mu_r = self.pv["pv_mu"]
        omm, omm_r = self.omm, self.omm_r
        car, car_r = self.carry[sid]
        hTv = hT.h().rearrange("p (c t) -> p c t", c=8)
        pt, pr = self.ps()
        for cc in range(8):
            self.mm(pt[:, :], wt_ap[:, cc, :], hTv[:, cc, tt * 512:(tt + 1) * 512], cc == 0, cc == 7,
                    wt_res + hT.rh(cc * T + tt * 512, cc * T + (tt + 1) * 512), [pr])
        if tt == 0:
            self.dve(lambda e: e.memset(car[:], 0.0), [], [car_r])
        self.act(lambda e: e.activation(out=pm.f(), in_=pt[:, :], func=AF.Copy, scale=mu[:, ci:ci + 1]), [pr, mu_r], pm.rf())
        self.dve(lambda e: e.scalar_tensor_tensor(out=z.f(1, 512), in0=pt[:, 1:512], scalar=omm[:, ci:ci + 1], in1=pm.f(0, 511),
                                                  op0=ALU.mult, op1=ALU.add), [pr, omm_r] + pm.rf(), z.rf())
        self.dve(lambda e: e.scalar_tensor_tensor(out=z.f(0, 1), in0=pt[:, 0:1], scalar=omm[:, ci:ci + 1], in1=car[:, 0:1],
                                                  op0=ALU.mult, op1=ALU.add), [pr, omm_r, car_r], z.rf())
        self.dve(lambda e: e.tensor_copy(out=car[:, 0:1], in_=pm.f(511, 512)), pm.rf(), [car_r])

    def rwkv(self, b, hT, yrwT, scr):
        c = self.c
        w_in = self.wd["w_in"]
        w_in_v = w_in.rearrange("(c p) n -> p c n", p=128)
        rw, rw_r = self.pv["pv_rw"]
        dv, dv_r = self.rwdv, self.rwdv_r
        bones, bones_r = c["blockones"]
        bavg, bavg_r = c["blockavg"]
        smask, smask_r = c["scanmask"]
        bmask, bmask_r = c["blockmask"]
        msl, msl_r = c["mask_sl"]
        idb, idb_r = self.identb, self.identb_r
        m2, m2_r = self.mask2, self.mask2_r
        yv = yrwT.h().rearrange("p (c t) -> p c t", c=4)

        wt = scr.get(1)
        wrkv = scr.get(3)
        WA = scr.get(2)
        SG = scr.get(2)
        AR = scr.get(4)
        BT = scr.get(2)
        KT = scr.get(2)
        BH = scr.get(2)
        KH = scr.get(2)
        VP = scr.get(4)
        G = scr.get(2)
        BON = scr.get(2)
        Rr, Kk0, LOGW, Aa, KK, L, E, tA, tB, PM = [scr.get(1) for _ in range(10)]
        vbt, bht, kht = scr.get(1), scr.get(1), scr.get(1)
        YT = [scr.get(1), scr.get(1)]
        alt = {}
        for nm_, reg_ in (("Rr", Rr), ("Kk0", Kk0), ("tA", tA), ("tB", tB), ("E", E)):
            alt[nm_] = [reg_, scr.get(1)]
        ALL = scr.get(8)
        NSC = scr.get(8)

        wtv = wt.h().rearrange("p (c n) -> p c n", c=8)
        ARv = AR.h().rearrange("p (a t) -> p a t", a=2)
        BHv = BH.h().rearrange("p (c n) -> p c n", c=16)
        KHv = KH.h().rearrange("p (c n) -> p c n", c=16)
        VPv = VP.h().rearrange("p (a c n) -> p a c n", a=2, c=16)

        self.dve(lambda e: e.memset(VP.h(), 0.0), [], VP.rh())

        for ci, which in ((12, "wa"), (13, "sg")):
            col0 = 1536 + (ci - 12) * 128
            self.loadw(wtv, w_in_v[:, :, col0:col0 + 128], [], wt.rh())
            for tt in range(4):
                z = tA
                self.shiftmix(0, wtv, wt.rh(), hT, ci, tt, z, PM)
                sl = slice(tt * 512, (tt + 1) * 512)
                if which == "wa":
                    self.act(lambda e, sl=sl: e.activation(out=WA.h()[0:64, sl], in_=z.f()[0:64, :], func=AF.Tanh), z.rf(), WA.rh(tt * 512, (tt + 1) * 512))
                    self.dve(lambda e, sl=sl: e.tensor_copy(out=WA.h()[64:128, sl], in_=z.f()[64:128, :]), z.rf(), WA.rh(tt * 512, (tt + 1) * 512))
                else:
                    self.act(lambda e: e.activation(out=tB.f(), in_=z.f(), func=AF.Tanh, scale=0.5), z.rf(), tB.rf())
                    self.dve(lambda e, sl=sl: e.tensor_scalar(out=SG.h()[:, sl], in0=tB.f(), scalar1=0.5, scalar2=0.5, op0=ALU.mult, op1=ALU.add),
                             tB.rf(), SG.rh(tt * 512, (tt + 1) * 512))

        import os
        cut = int(os.environ.get("KCUT", "0"))
        if cut == 1:
            return

        def run_hp(hp):
            hs = slice(hp * 128, (hp + 1) * 128)
            w3 = wrkv.h().rearrange("p (j c n) -> p j c n", j=3, c=8)
            for j in range(3):
                col0 = j * 512 + hp * 128
                self.loadw(w3[:, j], w_in_v[:, :, col0:col0 + 128], [], wrkv.rh(j * 1024, (j + 1) * 1024))
            self.dve(lambda e: e.memset(self.STP[:], 0.0), [], [self.STP_r])
            self.dve(lambda e: e.memset(self.STB[:], 0.0), [], [self.STB_r])

            def prep(tt, Rr=Rr, Kk0=Kk0, tA=tA, tB=tB, E=E):
                Rr, Kk0, tA, tB, E = (alt[n_][tt % 2] for n_ in ("Rr", "Kk0", "tA", "tB", "E"))
                sl = slice(tt * 512, (tt + 1) * 512)
                rT = lambda a=tt * 512, bnd=(tt + 1) * 512: None
                self.shiftmix(0, w3[:, 0], wrkv.rh(0, 1024), hT, hp, tt, Rr, PM)
                self.shiftmix(1, w3[:, 1], wrkv.rh(1024, 2048), hT, 4 + hp, tt, Kk0, PM)
                self.shiftmix(2, w3[:, 2], wrkv.rh(2048, 3072), hT, 8 + hp, tt, tA, PM)
                self.act(lambda e: e.copy(out=vbt.h(0, 512), in_=tA.f()), tA.rf(), vbt.rh())
                pt, pr = self.ps()
                self.mm(pt[:, :], self.wa_up[0:64, hs], WA.h()[0:64, sl], True, True, [self.wa_up_r] + WA.rh(tt * 512, (tt + 1) * 512), [pr])
                self.act(lambda e, pt=pt: e.activation(out=tB.f(), in_=pt[:, :], func=AF.Tanh, bias=dv[:, hp:hp + 1], scale=0.5), [pr, dv_r], tB.rf())
                self.dve(lambda e: e.tensor_scalar(out=LOGW.f(), in0=tB.f(), scalar1=0.5 * LOGW_C, scalar2=0.5 * LOGW_C, op0=ALU.mult, op1=ALU.add), tB.rf(), LOGW.rf())
                pt, pr = self.ps()
                self.mm(pt[:, :], self.wa_up[64:128, hs], WA.h()[64:128, sl], True, True, [self.wa_up_r] + WA.rh(tt * 512, (tt + 1) * 512), [pr])
                self.act(lambda e, pt=pt: e.activation(out=tB.f(), in_=pt[:, :], func=AF.Tanh, bias=dv[:, 4 + hp:5 + hp], scale=0.5), [pr, dv_r], tB.rf())
                self.dve(lambda e: e.tensor_scalar(out=Aa.f(), in0=tB.f(), scalar1=0.5, scalar2=0.5, op0=ALU.mult, op1=ALU.add), tB.rf(), Aa.rf())
                pt, pr = self.ps()
                self.mm(pt[:, :], self.g_up[:, hs], SG.h()[:, sl], True, True, [self.g_up_r] + SG.rh(tt * 512, (tt + 1) * 512), [pr])
                self.act(lambda e, pt=pt: e.copy(out=G.h()[:, sl], in_=pt[:, :]), [pr], G.rh(tt * 512, (tt + 1) * 512))
                self.dve(lambda e: e.tensor_scalar_mul(out=tB.f(), in0=Kk0.f(), scalar1=rw[:, 8 + hp:9 + hp]), Kk0.rf() + [rw_r], tB.rf())
                self.act(lambda e: e.activation(out=tA.f(), in_=tB.f(), func=AF.Square), tB.rf(), tA.rf())
                pt, pr = self.ps()
                self.mm(pt[:, :], bones[:], tA.f(), True, True, [bones_r] + tA.rf(), [pr])
                self.act(lambda e, pt=pt: e.copy(out=tA.f(), in_=pt[:, :]), [pr], tA.rf())
                self.act(lambda e: e.activation(out=tA.f(), in_=tA.f(), func=AF.Ln), tA.rf(), tA.rf())
                self.act(lambda e: e.activation(out=tA.f(), in_=tA.f(), func=AF.Exp, scale=-0.5), tA.rf(), tA.rf())
                self.dve(lambda e: e.tensor_tensor(out=KK.f(), in0=tB.f(), in1=tA.f(), op=ALU.mult), tB.rf() + tA.rf(), KK.rf())
                self.dve(lambda e: e.tensor_scalar(out=tA.f(), in0=Aa.f(), scalar1=rw[:, 12 + hp:13 + hp], scalar2=dv[:, 8 + hp:9 + hp],
                                                   op0=ALU.mult, op1=ALU.add), Aa.rf() + [rw_r, dv_r], tA.rf())
                self.dve(lambda e: e.scalar_tensor_tensor(out=Kk0.f(), in0=tA.f(), scalar=1.0, in1=Kk0.f(), op0=ALU.add, op1=ALU.mult),
                         tA.rf() + Kk0.rf(), Kk0.rf())
                self.dve(lambda e: e.tensor_tensor(out=tB.f(), in0=KK.f(), in1=Aa.f(), op=ALU.mult), KK.rf() + Aa.rf(), tB.rf())
                self.dve(lambda e: e.scalar_tensor_tensor(out=tA.f(), in0=Rr.f(), scalar=rw[:, 16 + hp:17 + hp], in1=Kk0.f(), op0=ALU.mult, op1=ALU.mult),
                         Rr.rf() + Kk0.rf() + [rw_r], tA.rf())
                pt, pr = self.ps()
                self.mm(pt[:, :], bones[:], tA.f(), True, True, [bones_r] + tA.rf(), [pr])
                self.dve(lambda e, pt=pt: e.tensor_tensor(out=BON.h()[:, sl], in0=pt[:, :], in1=vbt.h(0, 512), op=ALU.mult), [pr] + vbt.rh(), BON.rh(tt * 512, (tt + 1) * 512))
                self.dve(lambda e: e.tensor_tensor_scan(out=L.f(), data0=smask[:], data1=LOGW.f(), initial=0.0, op0=ALU.mult, op1=ALU.add),
                         [smask_r] + LOGW.rf(), L.rf())
                self.act(lambda e: e.activation(out=E.f(), in_=L.f(), func=AF.Exp), L.rf(), E.rf())
                self.dve(lambda e: e.tensor_tensor(out=ARv[:, 1, sl], in0=Rr.f(), in1=E.f(), op=ALU.mult), Rr.rf() + E.rf(), AR.rh(T + tt * 512, T + (tt + 1) * 512))
                self.dve(lambda e: e.tensor_tensor(out=tA.f(), in0=L.f(), in1=LOGW.f(), op=ALU.subtract), L.rf() + LOGW.rf(), tA.rf())
                self.act(lambda e: e.activation(out=E.f(), in_=tA.f(), func=AF.Exp), tA.rf(), E.rf())
                self.dve(lambda e: e.scalar_tensor_tensor(out=ARv[:, 0, sl], in0=KK.f(), scalar=-1.0, in1=E.f(), op0=ALU.mult, op1=ALU.mult),
                         KK.rf() + E.rf(), AR.rh(tt * 512, (tt + 1) * 512))
                self.act(lambda e: e.activation(out=E.f(), in_=L.f(), func=AF.Exp, scale=-1.0), L.rf(), E.rf())
                self.dve(lambda e: e.tensor_tensor(out=BT.h()[:, sl], in0=tB.f(), in1=E.f(), op=ALU.mult), tB.rf() + E.rf(), BT.rh(tt * 512, (tt + 1) * 512))
                self.dve(lambda e: e.tensor_tensor(out=KT.h()[:, sl], in0=Kk0.f(), in1=E.f(), op=ALU.mult), Kk0.rf() + E.rf(), KT.rh(tt * 512, (tt + 1) * 512))
                Lv = L.f().rearrange("p (c n) -> p c n", c=4)
                self.dve(lambda e: e.tensor_tensor(out=tA.f().rearrange("p (c n) -> p c n", c=4), in0=Lv[:, :, CH - 1:CH].broadcast_to([128, 4, CH]),
                                                   in1=Lv, op=ALU.subtract), L.rf(), tA.rf())
                self.act(lambda e: e.activation(out=E.f(), in_=tA.f(), func=AF.Exp), tA.rf(), E.rf())
                self.act(lambda e: e.activation(out=self.GC[:, tt * 4:(tt + 1) * 4], in_=Lv[:, :, CH - 1], func=AF.Exp), L.rf(), [self.GC_r])
                self.dve(lambda e: e.tensor_tensor(out=bht.h(0, 512), in0=tB.f(), in1=E.f(), op=ALU.mult), tB.rf() + E.rf(), bht.rh())
                self.dve(lambda e: e.tensor_tensor(out=kht.h(0, 512), in0=Kk0.f(), in1=E.f(), op=ALU.mult), Kk0.rf() + E.rf(), kht.rh())
                pt, pr = self.ps()
                ptv = pt[:].bitcast(BF16).rearrange("p (c t) -> p c t", c=8)
                pt2, pr2 = self.ps()
                ptv2 = pt2[:].bitcast(BF16).rearrange("p (c t) -> p c t", c=8)
                for j in range(4):
                    js = slice(j * 128, (j + 1) * 128)
                    self.pe(lambda e, j=j, js=js, ptv=ptv: e.transpose(out=ptv[:, j, :], in_=vbt.h(0, 512)[:, js], identity=idb[:]), vbt.rh() + [idb_r], [pr])
                    self.pe(lambda e, j=j, js=js, ptv=ptv: e.transpose(out=ptv[:, 4 + j, :], in_=bht.h(0, 512)[:, js], identity=idb[:]), bht.rh() + [idb_r], [pr])
                    self.pe(lambda e, j=j, js=js, ptv2=ptv2: e.transpose(out=ptv2[:, j, :], in_=kht.h(0, 512)[:, js], identity=idb[:]), kht.rh() + [idb_r], [pr2])
                cs = slice(tt * 4, (tt + 1) * 4)
                self.act(lambda e, ptv=ptv: e.copy(out=VPv[:, 0, cs, 0:64], in_=ptv[:, 0:4, 0:64]), [pr], VP.rh())
                self.dve(lambda e, ptv=ptv: e.tensor_copy(out=VPv[:, 1, cs, 64:128], in_=ptv[:, 0:4, 64:128]), [pr], VP.rh())
                self.dve(lambda e, ptv=ptv: e.tensor_copy(out=BHv[:, cs, :], in_=ptv[:, 4:8, :]), [pr], BH.rh(tt * 512, (tt + 1) * 512))
                self.act(lambda e, ptv2=ptv2: e.copy(out=KHv[:, cs, :], in_=ptv2[:, 0:4, :]), [pr2], KH.rh(tt * 512, (tt + 1) * 512))

            def unit_regs(u):
                ll = ALL.h(u * 512, (u + 1) * 512)
                llr = ALL.rh(u * 512, (u + 1) * 512)
                s8 = u % 8
                sc = NSC.h(s8 * 1024, (s8 + 1) * 1024)
                scr_ = NSC.rh(s8 * 1024, (s8 + 1) * 1024)
                return ll, llr, sc, scr_

            def aphase(chunks):
                units = [(cidx, hd) for cidx in chunks for hd in range(2)]
                st = {}
                for (cidx, hd) in units:
                    u = (cidx % 8) * 2 + hd
                    ll, llr, sc, scr_ = unit_regs(u)
                    ts = slice(cidx * 128, (cidx + 1) * 128)
                    ps_ = slice(hd * 64, (hd + 1) * 64)
                    rdA = AR.rh(cidx * 128, (cidx + 1) * 128) + AR.rh(T + cidx * 128, T + (cidx + 1) * 128)
                    p1, r1 = self.ps()
                    self.mm(p1[:, 0:256].rearrange("p (a t) -> p a t", a=2), BT.h()[ps_, ts], ARv[ps_, :, ts], True, True, BT.rh(cidx * 128, (cidx + 1) * 128) + rdA, [r1])
                    p2, r2 = self.ps()
                    self.mm(p2[:, 0:256].rearrange("p (a t) -> p a t", a=2), KT.h()[ps_, ts], ARv[ps_, :, ts], True, True, KT.rh(cidx * 128, (cidx + 1) * 128) + rdA, [r2])
                    self.mm(p2[:, 256:384], ARv[ps_, 0, ts], BT.h()[ps_, ts], True, True, BT.rh(cidx * 128, (cidx + 1) * 128) + rdA, [r2])
                    self.dve(lambda e, p1=p1, sc=sc: e.tensor_tensor(out=sc[:, 640:768], in0=p1[:, 0:128], in1=m2[:, 0:128], op=ALU.mult), [r1, m2_r], scr_)
                    self.dve(lambda e, p1=p1, ll=ll: e.tensor_tensor(out=ll[:, 384:512], in0=p1[:, 128:256], in1=m2[:, 128:256], op=ALU.mult), [r1, m2_r], llr)
                    self.dve(lambda e, p2=p2, ll=ll: e.tensor_tensor(out=ll[:, 128:384], in0=p2[:, 0:256], in1=m2[:, :], op=ALU.mult), [r2, m2_r], llr)
                    self.dve(lambda e, p2=p2, sc=sc: e.tensor_tensor(out=sc[:, 0:128], in0=p2[:, 256:384], in1=msl[:], op=ALU.mult), [r2, msl_r], scr_)
                    self.dve(lambda e, ll=ll, sc=sc: e.tensor_tensor(out=ll[:, 0:128], in0=sc[:, 640:768], in1=idb[:], op=ALU.add), scr_ + [idb_r], llr)
                    st[(cidx, hd)] = (ll, llr, sc, scr_)
                def stageA(u_, lvl):
                    ll, llr, sc, scr_ = st[u_]
                    if lvl == 1:
                        Np, NTp = sc[:, 640:768], sc[:, 0:128]
                    else:
                        o = 128 + ((lvl - 1) % 2) * 256
                        Np, NTp = sc[:, o:o + 128], sc[:, o + 128:o + 256]
                    o2 = 128 + (lvl % 2) * 256
                    pn, rn = self.ps()
                    if lvl < 6:
                        self.mm(pn[:, 0:128], NTp, Np, True, True, scr_, [rn])
                    self.mm(pn[:, 128:256], Np, NTp, True, True, scr_, [rn])
                    if lvl < 6:
                        self.act(lambda e, pn=pn, sc=sc, o2=o2: e.copy(out=sc[:, o2:o2 + 256], in_=pn[:, 0:256]), [rn], scr_)
                    else:
                        self.act(lambda e, pn=pn, sc=sc, o2=o2: e.copy(out=sc[:, o2 + 128:o2 + 256], in_=pn[:, 128:256]), [rn], scr_)

                def stageB(u_, lvl):
                    ll, llr, sc, scr_ = st[u_]
                    o2 = 128 + (lvl % 2) * 256
                    pq, rq = self.ps()
                    self.mm(pq[:, 0:128], sc[:, o2 + 128:o2 + 256], ll[:, 0:128], True, True, scr_ + llr, [rq])
                    self.dve(lambda e, pq=pq, ll=ll: e.tensor_tensor(out=ll[:, 0:128], in0=pq[:, 0:128], in1=ll[:, 0:128], op=ALU.add), [rq] + llr, llr)

                SK = 3
                work = [(u_, lvl) for lvl in range(1, 7) for u_ in units]
                for idx in range(len(work) + SK):
                    if idx < len(work):
                        stageA(*work[idx])
                    if idx - SK >= 0:
                        stageB(*work[idx - SK])

            def seq(cidx):
                ts = slice(cidx * 128, (cidx + 1) * 128)
                lls = []
                for hd in range(2):
                    u = (cidx % 8) * 2 + hd
                    ll, llr, _, _ = unit_regs(u)
                    lls.append((ll, llr))
                STP, STB, TMS, XB, UB, UP0, UP1 = self.STP, self.STB, self.TMS, self.XB, self.UB, self.UP0, self.UP1
                rdA0 = AR.rh(cidx * 128, (cidx + 1) * 128)
                rdA1 = AR.rh(T + cidx * 128, T + (cidx + 1) * 128)
                px, rx = self.ps()
                self.mm(px[:, 0:128], ARv[:, 0, ts], STB[:], True, False, rdA0 + [self.STB_r], [rx])
                for hd in range(2):
                    self.mm(px[:, hd * 64:(hd + 1) * 64], lls[hd][0][:, 128:256], VPv[:, hd, cidx, hd * 64:(hd + 1) * 64], False, hd == 1,
                            lls[hd][1] + VP.rh(), [rx])
                self.act(lambda e, px=px: e.copy(out=XB[:], in_=px[:, 0:128]), [rx], [self.XB_r])
                kseq = int(os.environ.get("KSEQ", "99"))
                if kseq <= 1:
                    return
                pu, ru = self.ps()
                kv2 = int(os.environ.get("KV2", "0"))
                for hd in range(2):
                    if kv2 == 1 and hd == 1:
                        continue
                    if kv2 == 2 and hd == 0:
                        continue
                    lq = self.identb[:] if kv2 == 3 else lls[hd][0][:, 0:128]
                    if kv2 == 4:
                        lq = lls[hd][0][:, 384:512]
                    if kv2 == 5:
                        lq = lls[hd][0][:, 256:384]
                    self.mm(pu[:, hd * 64:(hd + 1) * 64], lq, XB[:, hd * 64:(hd + 1) * 64], True, True, lls[hd][1] + [self.XB_r], [ru])
                self.act(lambda e, pu=pu: e.copy(out=UB[:], in_=pu[:, 0:128]), [ru], [self.UB_r])
                kvar = int(os.environ.get("KVAR", "0"))
                if kvar == 3:
                    self.dve(lambda e, pu=pu: e.tensor_copy(out=UP0[:, 0:64], in_=XB[:, 0:64]), [self.XB_r], [self.UP0_r])
                elif kvar == 4:
                    self.dve(lambda e, pu=pu: e.tensor_copy(out=TMS[:, 0:64], in_=pu[:, 0:64]), [ru], [self.TMS_r])
                elif kvar == 5:
                    self.act(lambda e, pu=pu: e.copy(out=UP0[:, 0:64], in_=pu[:, 0:64]), [ru], [self.UP0_r])
                elif kvar != 1:
                    self.dve(lambda e, pu=pu: e.tensor_copy(out=UP0[:, 0:64], in_=pu[:, 0:64]), [ru], [self.UP0_r])
                if kvar == 0:
                    self.dve(lambda e, pu=pu: e.tensor_copy(out=UP1[:, 64:128], in_=pu[:, 64:128]), [ru], [self.UP1_r])
                if kseq <= 2:
                    return
                py, ry = self.ps()
                kv3 = int(os.environ.get("KV3", "7"))
                kv4 = int(os.environ.get("KV4", "0"))
                lst = {0: STB, 1: self.identb, 2: XB, 3: UB}[kv4]
                kv5 = int(os.environ.get("KV5", "0"))
                rr_ = ARv[:, 0, ts] if kv5 == 1 else ARv[:, 1, ts]
                self.mm(py[:, 0:128], lst[:], rr_, True, kv5 == 2, rdA1 + [self.STB_r], [ry])
                ups = [(UP0, self.UP0_r), (UP1, self.UP1_r)]
                for hd in range(2):
                    if kv3 & 2:
                        self.mm(py[:, 0:128], ups[hd][0][:], lls[hd][0][:, 384:512], False, False, [ups[hd][1]] + lls[hd][1], [ry])
                    if kv3 & 4:
                        self.mm(py[:, 0:128], VPv[:, hd, cidx, :], lls[hd][0][:, 256:384], False, hd == 1, VP.rh() + lls[hd][1], [ry])
                yt = YT[(cidx // 4) % 2]
                yo = (cidx % 4) * 128
                self.act(lambda e, py=py: e.copy(out=yt.f(yo, yo + 128), in_=py[:, 0:128]), [ry], yt.rf())
                if kseq <= 3:
                    return
                pS, rS = self.ps()
                self.mm(pS[:, 0:128], BHv[:, cidx, :], UB[:], True, False, BH.rh(cidx * 128, (cidx + 1) * 128) + [self.UB_r], [rS])
                self.mm(pS[:, 0:128], KHv[:, cidx, :], VPv[:, 0, cidx, :], False, False, KH.rh(cidx * 128, (cidx + 1) * 128) + VP.rh(), [rS])
                self.mm(pS[:, 0:128], KHv[:, cidx, :], VPv[:, 1, cidx, :], False, True, KH.rh(cidx * 128, (cidx + 1) * 128) + VP.rh(), [rS])
                self.dve(lambda e, pS=pS: e.scalar_tensor_tensor(out=TMS[:], in0=STP[:], scalar=self.GC[:, cidx:cidx + 1], in1=pS[:, 0:128], op0=ALU.mult, op1=ALU.add),
                         [self.STP_r, self.GC_r, rS], [self.TMS_r])
                self.dve(lambda e: e.tensor_tensor(out=STP[:], in0=TMS[:], in1=bmask[:], op=ALU.mult), [self.TMS_r, bmask_r], [self.STP_r])
                self.dve(lambda e: e.tensor_tensor(out=STB[:], in0=TMS[:], in1=bmask[:], op=ALU.mult), [self.TMS_r, bmask_r], [self.STB_r])

            def post(tt):
                sl = slice(tt * 512, (tt + 1) * 512)
                yt = YT[tt % 2]
                pt, pr = self.ps()
                self.mm(pt[:, :], bavg[:], yt.f(), True, True, [bavg_r] + yt.rf(), [pr])
                self.dve(lambda e, pt=pt: e.tensor_tensor(out=tA.f(), in0=yt.f(), in1=pt[:, :], op=ALU.subtract), yt.rf() + [pr], tA.rf())
                self.act(lambda e: e.activation(out=tB.f(), in_=tA.f(), func=AF.Square), tA.rf(), tB.rf())
                pt, pr = self.ps()
                self.mm(pt[:, :], bavg[:], tB.f(), True, True, [bavg_r] + tB.rf(), [pr])
                self.dve(lambda e, pt=pt: e.tensor_scalar_add(out=tB.f(), in0=pt[:, :], scalar1=64e-5), [pr], tB.rf())
                self.act(lambda e: e.activation(out=tB.f(), in_=tB.f(), func=AF.Ln), tB.rf(), tB.rf())
                self.act(lambda e: e.activation(out=tB.f(), in_=tB.f(), func=AF.Exp, scale=-0.5), tB.rf(), tB.rf())
                self.dve(lambda e: e.tensor_tensor(out=tA.f(), in0=tA.f(), in1=tB.f(), op=ALU.mult), tA.rf() + tB.rf(), tA.rf())
                self.dve(lambda e: e.tensor_scalar(out=tA.f(), in0=tA.f(), scalar1=rw[:, 20 + hp:21 + hp], scalar2=rw[:, 24 + hp:25 + hp], op0=ALU.mult, op1=ALU.add),
                         tA.rf() + [rw_r], tA.rf())
                self.dve(lambda e: e.tensor_tensor(out=tA.f(), in0=tA.f(), in1=BON.h()[:, sl], op=ALU.add), tA.rf() + BON.rh(tt * 512, (tt + 1) * 512), tA.rf())
                self.dve(lambda e: e.tensor_tensor(out=yv[:, hp, sl], in0=tA.f(), in1=G.h()[:, sl], op=ALU.mult), tA.rf() + G.rh(tt * 512, (tt + 1) * 512),
                         yrwT.rh(hp * T + tt * 512, hp * T + (tt + 1) * 512))

            for tt in range(4):
                prep(tt)
            if cut == 2:
                return
            if cut == 3:
                aphase(range(0, 4))
                return
            if cut == 8:
                aphase(range(0, 4))
                self.dve(lambda e: e.tensor_copy(out=tA.f(), in_=ALL.h(0, 512)), ALL.rh(0, 512), tA.rf())
                self.load(self.dbg[0:128, 0:512], tA.f(), tA.rf(), [self.out_res])
                self.dve(lambda e: e.tensor_copy(out=tB.f(), in_=ALL.h(512, 1024)), ALL.rh(512, 1024), tB.rf())
                self.load(self.dbg[0:128, 512:1024], tB.f(), tB.rf(), [self.out_res])
                self.nodump = True
                return
            if cut == 5:
                aphase(range(0, 4))
                aphase(range(4, 8))
                return
            if cut == 6:
                aphase(range(0, 4))
                seq(0)
                return
            if cut == 7:
                aphase(range(0, 4))
                for cidx in range(0, 4):
                    seq(cidx)
                return
            if cut == 4:
                aphase(range(0, 4))
                aphase(range(4, 8))
                for cidx in range(0, 4):
                    seq(cidx)
                post(0)
                return
            for half in range(2):
                for q4 in range(4):
                    aphase(range(half * 8 + q4 * 2, half * 8 + q4 * 2 + 2))
                for cidx in range(half * 8, half * 8 + 8):
                    seq(cidx)
                    if cidx % 4 == 3:
                        post(cidx // 4)

        for hp in range(4):
            run_hp(hp)
            if cut >= 2:
                return

    def mla(self, b, hT, ymlaT, scr):
        c = self.c
        w_in_v = self.wd["w_in"].rearrange("(c p) n -> p c n", p=128)
        hTv = hT.h().rearrange("p (c t) -> p c t", c=8)
        ones, ones_r = c["allones"]
        fq, fq_r = c["rope_fq"]
        ph, ph_r = c["rope_ph"]
        m2, m2_r = self.mask2, self.mask2_r
        yv = ymlaT.h().rearrange("p (c t) -> p c t", c=8)

        TAB = scr.get(6)
        CQN = scr.get(6)
        CKVN = scr.get(4)
        KROT = scr.get(2)
        V = scr.get(9)
        wlat = scr.get(3)
        wv = scr.get(1)
        t1, t2, t3, t4 = [scr.get(1) for _ in range(4)]
        QTs = [scr.get(2), scr.get(2)]
        KTs = [scr.get(2), scr.get(2)]
        PTs = [scr.get(1) for _ in range(3)]
        TABv = TAB.h().rearrange("p (k t) -> p k t", k=3)
        CQNv = CQN.h().rearrange("p (k t) -> p k t", k=3)
        CKVNv = CKVN.h().rearrange("p (k t) -> p k t", k=2)
        Vv = V.h(0, 16 * 8 * 66).rearrange("p (i h d) -> p i h d", i=16, h=8)

        for tt in range(4):
            sl = slice(tt * 512, (tt + 1) * 512)
            self.load(t1.i(), self.pos[b:b + 1, sl].partition_broadcast(128), [], t1.rf())
            self.dve(lambda e: e.tensor_copy(out=t2.f(), in_=t1.i()), t1.rf(), t2.rf())
            for k in range(3):
                self.dve(lambda e, k=k: e.tensor_scalar(out=t3.f(), in0=t2.f(), scalar1=fq[:, k:k + 1], scalar2=ph[:, k:k + 1], op0=ALU.mult, op1=ALU.add),
                         t2.rf() + [fq_r, ph_r], t3.rf())
                self.dve(lambda e: e.tensor_copy(out=t4.i(), in_=t3.f()), t3.rf(), t4.rf())
                self.dve(lambda e: e.tensor_copy(out=t1.f(), in_=t4.i()), t4.rf(), t1.rf())
                self.dve(lambda e: e.tensor_tensor(out=t3.f(), in0=t3.f(), in1=t1.f(), op=ALU.subtract), t3.rf() + t1.rf(), t3.rf())
                self.dve(lambda e: e.tensor_single_scalar(out=t1.f(), in_=t3.f(), scalar=0.5, op=ALU.is_gt), t3.rf(), t1.rf())
                self.dve(lambda e: e.tensor_tensor(out=t3.f(), in0=t3.f(), in1=t1.f(), op=ALU.subtract), t3.rf() + t1.rf(), t3.rf())
                self.dve(lambda e: e.tensor_single_scalar(out=t1.f(), in_=t3.f(), scalar=-0.5, op=ALU.is_lt), t3.rf(), t1.rf())
                self.dve(lambda e: e.tensor_tensor(out=t3.f(), in0=t3.f(), in1=t1.f(), op=ALU.add), t3.rf() + t1.rf(), t3.rf())
                self.act(lambda e, k=k, sl=sl: e.activation(out=TABv[:, k, sl], in_=t3.f(), func=AF.Sin, scale=6.283185), t3.rf(),
                         TAB.rh(k * T + tt * 512, k * T + (tt + 1) * 512))

        def latent(col0, nchunk, gname, OUT, OUTv):
            gq, gq_r = self.pv[gname]
            wl = wlat.h(0, 8 * nchunk * 128).rearrange("p (c n) -> p c n", c=8)
            self.loadw(wl, w_in_v[:, :, col0:col0 + nchunk * 128], [], wlat.rh(0, 8 * nchunk * 128))
            tmps = [t1, t2, t3]
            for tt in range(4):
                sl = slice(tt * 512, (tt + 1) * 512)
                pss, rss = self.ps()
                for j in range(nchunk):
                    pt, pr = self.ps()
                    for cc in range(8):
                        self.mm(pt[:, :], wl[:, cc, j * 128:(j + 1) * 128], hTv[:, cc, sl], cc == 0, cc == 7,
                                wlat.rh(0, 8 * nchunk * 128) + hT.rh(cc * T + tt * 512, cc * T + (tt + 1) * 512), [pr])
                    tj = tmps[j]
                    self.act(lambda e, pt=pt, tj=tj: e.copy(out=tj.f(), in_=pt[:, :]), [pr], tj.rf())
                    self.act(lambda e, tj=tj: e.activation(out=t4.f(), in_=tj.f(), func=AF.Square), tj.rf(), t4.rf())
                    self.mm(pss[:, :], ones[:], t4.f(), j == 0, j == nchunk - 1, [ones_r] + t4.rf(), [rss])
                self.dve(lambda e, pss=pss: e.tensor_scalar(out=t4.f(), in0=pss[:, :], scalar1=1.0 / (nchunk * 128), scalar2=1e-6, op0=ALU.mult, op1=ALU.add),
                         [rss], t4.rf())
                self.act(lambda e: e.activation(out=t4.f(), in_=t4.f(), func=AF.Ln), t4.rf(), t4.rf())
                self.act(lambda e: e.activation(out=t4.f(), in_=t4.f(), func=AF.Exp, scale=-0.5), t4.rf(), t4.rf())
                for j in range(nchunk):
                    tj = tmps[j]
                    self.dve(lambda e, j=j, tj=tj, sl=sl: e.scalar_tensor_tensor(out=OUTv[:, j, sl], in0=tj.f(), scalar=gq[:, j:j + 1], in1=t4.f(), op0=ALU.mult, op1=ALU.mult),
                             tj.rf() + t4.rf() + [gq_r], OUT.rh(j * T + tt * 512, j * T + (tt + 1) * 512))

        latent(CQ0, 3, "pv_gq", CQN, CQNv)
        latent(CKV0, 2, "pv_gkv", CKVN, CKVNv)

        wkr = wlat.h(0, 8 * 256).rearrange("p (c n) -> p c n", c=8)
        self.loadw(wkr, self.wd["w_kr"].rearrange("(c p) n -> p c n", p=128), [], wlat.rh(0, 8 * 256))
        for tt in range(4):
            sl = slice(tt * 512, (tt + 1) * 512)
            pA, rA = self.ps()
            pB, rB = self.ps()
            for cc in range(8):
                rd = wlat.rh(0, 8 * 256) + hT.rh(cc * T + tt * 512, cc * T + (tt + 1) * 512)
                self.mm(pA[:, :], wkr[:, cc, 0:128], hTv[:, cc, sl], cc == 0, cc == 7, rd, [rA])
            for cc in range(8):
                rd = wlat.rh(0, 8 * 256) + hT.rh(cc * T + tt * 512, cc * T + (tt + 1) * 512)
                self.mm(pB[:, :], wkr[:, cc, 128:256], hTv[:, cc, sl], cc == 0, cc == 7, rd, [rB])
            self.dve(lambda e, pA=pA, sl=sl: e.tensor_tensor(out=t1.f(), in0=pA[:, :], in1=TABv[:, 1, sl], op=ALU.mult), [rA] + TAB.rh(T + tt * 512, T + (tt + 1) * 512), t1.rf())
            self.dve(lambda e, pB=pB, sl=sl: e.tensor_tensor(out=t2.f(), in0=pB[:, :], in1=TABv[:, 2, sl], op=ALU.mult), [rB] + TAB.rh(2 * T + tt * 512, 2 * T + (tt + 1) * 512), t2.rf())
            self.dve(lambda e, sl=sl: e.tensor_tensor(out=KROT.h()[:, sl], in0=t1.f(), in1=t2.f(), op=ALU.add), t1.rf() + t2.rf(), KROT.rh(tt * 512, (tt + 1) * 512))

        wvv = wv.h(0, 1024).rearrange("p (c n) -> p c n", c=2)
        self.loadw(wvv, self.wd["w_v"].rearrange("(c p) n -> p c n", p=128), [], wv.rh())
        self.dve(lambda e: e.memset(Vv[:, :, :, 64:66], 1.0), [], V.rh())
        for i in range(16):
            pt, pr = self.ps()
            for kc in range(2):
                self.mm(pt[:, :], CKVNv[:, kc, i * 128:(i + 1) * 128], wvv[:, kc, :], kc == 0, kc == 1,
                        CKVN.rh(kc * T + i * 128, kc * T + (i + 1) * 128) + wv.rh(), [pr])
            self.act(lambda e, pt=pt, i=i: e.copy(out=Vv[:, i, :, 0:64], in_=pt[:, :].rearrange("p (h d) -> p h d", h=8)), [pr], V.rh())

        def head(h):
            QT, KT = QTs[h % 2], KTs[h % 2]
            wq = wlat.h(0, 384).rearrange("p (c n) -> p c n", c=3)
            wk = wlat.h(1024, 1024 + 256).rearrange("p (c n) -> p c n", c=2)
            self.loadw(wq, self.wd["w_q"][:, h * 128:(h + 1) * 128].rearrange("(c p) n -> p c n", p=128), [], wlat.rh(0, 384))
            self.loadw(wk, self.wd["w_k"][:, h * 128:(h + 1) * 128].rearrange("(c p) n -> p c n", p=128), [], wlat.rh(1024, 1280))
            for tt in range(4):
                sl = slice(tt * 512, (tt + 1) * 512)
                pt, pr = self.ps()
                for cc in range(3):
                    self.mm(pt[:, :], wq[:, cc, :], CQNv[:, cc, sl], cc == 0, cc == 2, wlat.rh(0, 384) + CQN.rh(cc * T + tt * 512, cc * T + (tt + 1) * 512), [pr])
                self.dve(lambda e, pt=pt, sl=sl: e.tensor_tensor(out=QT.h()[:, sl], in0=pt[:, :], in1=TABv[:, 0, sl], op=ALU.mult),
                         [pr] + TAB.rh(tt * 512, (tt + 1) * 512), QT.rh(tt * 512, (tt + 1) * 512))
                pt, pr = self.ps()
                for cc in range(2):
                    self.mm(pt[:, :], wk[:, cc, :], CKVNv[:, cc, sl], cc == 0, cc == 1, wlat.rh(1024, 1280) + CKVN.rh(cc * T + tt * 512, cc * T + (tt + 1) * 512), [pr])
                self.dve(lambda e, pt=pt, sl=sl: e.tensor_tensor(out=KT.h()[:, sl], in0=pt[:, :], in1=KROT.h()[:, sl], op=ALU.add),
                         [pr] + KROT.rh(tt * 512, (tt + 1) * 512), KT.rh(tt * 512, (tt + 1) * 512))
            iters = [(qt, kb) for qt in range(4) for kb in range(4 * qt + 4)]
            pos_ = {}
            pts_ = {}
            deferred = []

            def stage_s(n):
                qt, kb = iters[n]
                if kb == 0:
                    pos_[qt] = self.ps(hold=True)
                c0 = max(0, kb - 4 * qt) * 128
                psc, rsc = self.ps()
                self.mm(psc[:, c0:512], KT.h()[:, kb * 128:(kb + 1) * 128], QT.h()[:, qt * 512 + c0:(qt + 1) * 512], True, True,
                        KT.rh(kb * 128, (kb + 1) * 128) + QT.rh(qt * 512, (qt + 1) * 512), [rsc])
                PT = PTs[n % 3]
                pts_[n] = PT
                self.act(lambda e, psc=psc, PT=PT, c0=c0: e.activation(out=PT.h()[:, c0:512], in_=psc[:, c0:512], func=AF.Exp, scale=ATT_SCALE), [rsc], PT.rh())
                if kb >= 4 * qt:
                    self.dve(lambda e, PT=PT, c0=c0: e.tensor_tensor(out=PT.h()[:, c0:c0 + 128], in0=PT.h()[:, c0:c0 + 128], in1=m2[:, 128:256], op=ALU.mult),
                             PT.rh() + [m2_r], PT.rh())

            def epi1(qt):
                po, ro = pos_[qt]
                self.act(lambda e, po=po: e.copy(out=t1.f()[64:65, :], in_=po[64:65, :]), [ro], t1.rf())
                self.dve(lambda e: e.reciprocal(out=t1.f()[64:65, :], in_=t1.f()[64:65, :]), t1.rf(), t1.rf())

            def epi2(qt):
                po, ro = pos_[qt]
                pb_, rb_ = self.ps()
                self.mm(pb_[0:64, :], ones[64:65, 0:64], t1.f()[64:65, :], True, True, [ones_r] + t1.rf(), [rb_])
                self.act(lambda e, pb_=pb_: e.copy(out=t2.f()[0:64, :], in_=pb_[0:64, :]), [rb_], t2.rf())
                self.dve(lambda e, po=po, qt=qt: e.tensor_tensor(out=yv[0:64, h, qt * 512:(qt + 1) * 512], in0=po[0:64, :], in1=t2.f()[0:64, :], op=ALU.mult),
                         [ro] + t2.rf(), ymlaT.rh(h * T + qt * 512, h * T + (qt + 1) * 512))
                self.ps_release(po)

            def stage_p(n):
                qt, kb = iters[n]
                nkb = 4 * qt + 4
                c0 = max(0, kb - 4 * qt) * 128
                po, ro = pos_[qt]
                PT = pts_[n]
                self.mm(po[0:65, c0:512], Vv[:, kb, h, 0:65], PT.h()[:, c0:512], kb == 0, kb == nkb - 1, V.rh() + PT.rh(), [ro])
                if kb == nkb - 1:
                    deferred.append([1, epi1, qt])
                    deferred.append([5, epi2, qt])

            SK = 2
            for idx in range(len(iters) + SK):
                if idx < len(iters):
                    stage_s(idx)
                if idx - SK >= 0:
                    stage_p(idx - SK)
                for d in list(deferred):
                    d[0] -= 1
                    if d[0] <= 0:
                        d[1](d[2])
                        deferred.remove(d)
            for d in list(deferred):
                d[1](d[2])

        for h in range(8):
            head(h)

    def merge(self, b, hT, yrwT, ymlaT, scr, scr2, scr3):
        c = self.c
        w_in_v = self.wd["w_in"].rearrange("(c p) n -> p c n", p=128)
        hTv = hT.h().rearrange("p (c t) -> p c t", c=8)
        yrv = yrwT.h().rearrange("p (c t) -> p c t", c=4)
        ymv = ymlaT.h().rearrange("p (c t) -> p c t", c=8)
        g2, g2_r = self.pv["pv_g2"]
        idf, idf_r = c["ident"]
        MT = scr.get(16)
        MTv = MT.h().rearrange("p (c t) -> p c t", c=8)
        wgr, wgm, wbr, wbm = [scr.get(1) for _ in range(4)]
        t1, t2 = scr.get(1), scr.get(1)
        wgrv = wgr.h().rearrange("p (c n) -> p c n", c=8)
        wgmv = wgm.h().rearrange("p (c n) -> p c n", c=8)
        wbrv = wbr.h(0, 512).rearrange("p (c n) -> p c n", c=4)
        wbmv = wbm.h().rearrange("p (c n) -> p c n", c=8)
        for j in range(8):
            js = slice(j * 128, (j + 1) * 128)
            self.loadw(wgrv, w_in_v[:, :, GRW0 + j * 128:GRW0 + (j + 1) * 128], [], wgr.rh())
            self.loadw(wgmv, w_in_v[:, :, GML0 + j * 128:GML0 + (j + 1) * 128], [], wgm.rh())
            self.loadw(wbrv, self.wd["w_brw"][:, js].rearrange("(c p) n -> p c n", p=128), [], wbr.rh())
            self.loadw(wbmv[0:64], self.wd["w_bml"][:, js].rearrange("(h p) n -> p h n", p=64), [], wbm.rh())
            for tt in range(4):
                sl = slice(tt * 512, (tt + 1) * 512)
                for (wg, wgreg, tdst) in ((wgrv, wgr, t1), (wgmv, wgm, t2)):
                    pg, rg = self.ps()
                    for cc in range(8):
                        self.mm(pg[:, :], wg[:, cc, :], hTv[:, cc, sl], cc == 0, cc == 7, wgreg.rh() + hT.rh(cc * T + tt * 512, cc * T + (tt + 1) * 512), [rg])
                    self.act(lambda e, pg=pg, tdst=tdst: e.activation(out=tdst.f(), in_=pg[:, :], func=AF.Tanh, scale=0.5), [rg], tdst.rf())
                pb1, rb1 = self.ps()
                for cc in range(4):
                    self.mm(pb1[:, :], wbrv[:, cc, :], yrv[:, cc, sl], cc == 0, cc == 3, wbr.rh() + yrwT.rh(cc * T + tt * 512, cc * T + (tt + 1) * 512), [rb1])
                self.dve(lambda e, pb1=pb1: e.scalar_tensor_tensor(out=t1.f(), in0=t1.f(), scalar=1.0, in1=pb1[:, :], op0=ALU.add, op1=ALU.mult), [rb1] + t1.rf(), t1.rf())
                pb2, rb2 = self.ps()
                for hh in range(8):
                    self.mm(pb2[:, :], wbmv[0:64, hh, :], ymv[0:64, hh, sl], hh == 0, hh == 7, wbm.rh() + ymlaT.rh(hh * T + tt * 512, hh * T + (tt + 1) * 512), [rb2])
                self.dve(lambda e, pb2=pb2: e.scalar_tensor_tensor(out=t2.f(), in0=t2.f(), scalar=1.0, in1=pb2[:, :], op0=ALU.add, op1=ALU.mult), [rb2] + t2.rf(), t2.rf())
                self.dve(lambda e: e.tensor_tensor(out=t1.f(), in0=t1.f(), in1=t2.f(), op=ALU.add), t1.rf() + t2.rf(), t1.rf())
                self.act(lambda e, j=j, sl=sl: e.mul(out=MTv[:, j, sl], in_=t1.f(), mul=0.5), t1.rf(), MT.rh(j * T + tt * 512, j * T + (tt + 1) * 512))

        wo = scr.get(8)
        wov = wo.h().rearrange("p (c n) -> p c n", c=8)
        self.loadw(wov, self.wd["w_out"].rearrange("(c p) n -> p c n", p=128), [], wo.rh())
        wrt = scr.get(1)
        wrtv = wrt.f(0, 8 * 36).rearrange("p (c n) -> p c n", c=8)
        self.load(wrtv, self.wd["w_rt"].rearrange("(c p) n -> p c n", p=128), [], wrt.rf())
        xts = [scr2.get(2), scr2.get(2)]
        h2f = scr2.get(2)
        h2Tf = scr2.get(2)
        junk = scr.get(1)
        ACCs = [scr3.get(2), scr3.get(2)]
        gff = scr3.get(2)
        self.load(gff.f(), self.wd["g_ffn"].partition_broadcast(128), [], gff.rf())
        h2gs = [scr3.get(1), scr3.get(1)]
        MM, MM_r, SRf, SRf_r, SR, SR_r, W12, W12_r = self.MM, self.MM_r, self.SRf, self.SRf_r, self.SR, self.SR_r, self.W12, self.W12_r
        msu, msu_r = c["mask_su"]
        ones, ones_r = c["allones"]
        ecap, ecap_r = c["ecap"]
        if b == 0:
            self.tok_init = self.load(self.tokd.rearrange("(p f) o -> p (f o)", p=128), self.ZI[:], [self.ZI_r], [self.tok_r])
            self.scat_ops = []
        tok_init = self.tok_init
        h2Tfv = h2Tf.f().rearrange("p (c t) -> p c t", c=8)
        ssq, ssq_r, rs, rs_r = self.ssq, self.ssq_r, self.rs, self.rs_r
        LG, LG_r = self.LG, self.LG_r
        brt, brt_r = self.brt, self.brt_r
        R8, R8_r = self.R8, self.R8_r
        for i in range(16):
            xt = xts[i % 2]
            row0 = b * T + i * 128
            gi_ = b * 16 + i
            ACCt = ACCs[i % 2]
            self.load(xt.f(), self.x[row0:row0 + 128, :], [], xt.rf())
            for half in range(2):
                hs = slice(half * 512, (half + 1) * 512)
                pt, pr = self.ps()
                for cc in range(8):
                    self.mm(pt[:, :], MTv[:, cc, i * 128:(i + 1) * 128], wov[:, cc, hs], cc == 0, cc == 7,
                            MT.rh(cc * T + i * 128, cc * T + (i + 1) * 128) + wo.rh(), [pr])
                self.dve(lambda e, pt=pt, hs=hs, xt=xt, ACCt=ACCt: e.tensor_tensor(out=ACCt.f()[:, hs], in0=pt[:, :], in1=xt.f()[:, hs], op=ALU.add),
                         [pr] + xt.rf(), ACCt.rf(half * 512, (half + 1) * 512))
            ai = ACCt.rf()
            self.load(self.x1s[row0:row0 + 128, :], ACCt.f(), ai, [self.x1_r[gi_]])
            self.dve(lambda e, i=i: e.memset(ssq[:, i:i + 1], 0.0), [], [ssq_r])
            self.act(lambda e, i=i, ACCt=ACCt: e.activation(out=junk.h(), in_=ACCt.f(), func=AF.Square, accum_out=ssq[:, i:i + 1]), ai + [ssq_r], junk.rh() + [ssq_r])
            self.dve(lambda e, i=i: e.tensor_scalar(out=rs[:, i:i + 1], in0=ssq[:, i:i + 1], scalar1=1.0 / D, scalar2=1e-6, op0=ALU.mult, op1=ALU.add), [ssq_r], [rs_r])
            self.act(lambda e, i=i: e.activation(out=rs[:, i:i + 1], in_=rs[:, i:i + 1], func=AF.Ln), [rs_r], [rs_r])
            self.act(lambda e, i=i: e.activation(out=rs[:, i:i + 1], in_=rs[:, i:i + 1], func=AF.Exp, scale=-0.5), [rs_r], [rs_r])
            self.dve(lambda e, i=i, ACCt=ACCt: e.tensor_scalar_mul(out=h2f.f(), in0=ACCt.f(), scalar1=rs[:, i:i + 1]), ai + [rs_r], h2f.rf())
            h2g = h2gs[i % 2]
            self.dve(lambda e, h2g=h2g: e.tensor_tensor(out=h2g.h(), in0=h2f.f(), in1=gff.f(), op=ALU.mult), h2f.rf() + gff.rf(), h2g.rh())
            self.load(self.h2s[row0:row0 + 128, :], h2g.h(), h2g.rh(), [self.h2s_r[gi_]])
            for grp in range(2):
                pt, pr = self.ps()
                for q in range(4):
                    cc = grp * 4 + q
                    self.pe(lambda e, pt=pt, q=q, cc=cc: e.transpose(out=pt[:, q * 128:(q + 1) * 128], in_=h2f.f(cc * 128, (cc + 1) * 128), identity=idf[:]),
                            h2f.rf() + [idf_r], [pr])
                self.dve(lambda e, pt=pt, grp=grp: e.tensor_tensor(out=h2Tfv[:, grp * 4:(grp + 1) * 4, :], in0=pt[:, :].rearrange("p (c t) -> p c t", c=4),
                                                                 in1=g2[:, grp * 4:(grp + 1) * 4].unsqueeze(2).broadcast_to([128, 4, 128]), op=ALU.mult),
                         [pr, g2_r], h2Tf.rf())
            pl, rl = self.ps()
            for cc in range(8):
                self.mm(pl[:, 0:36], h2Tfv[:, cc, :], wrtv[:, cc, :], cc == 0, cc == 7, h2Tf.rf() + wrt.rf(), [rl])
            self.dve(lambda e, pl=pl, i=i: e.tensor_tensor(out=LG[:, i, :], in0=pl[:, 0:36], in1=brt[:], op=ALU.add), [rl, brt_r], [LG_r])
            lg4 = LG[:, i, 0:4]
            fine = LG[:, i, 4:36]
            r8 = R8
            self.dve(lambda e, lg4=lg4: e.reduce_max(out=r8[:, 0:1], in_=lg4, axis=AX.X), [LG_r], [R8_r])
            self.dve(lambda e, lg4=lg4: e.tensor_scalar(out=r8[:, 8:12], in0=lg4, scalar1=r8[:, 0:1], scalar2=None, op0=ALU.is_equal), [LG_r, R8_r], [R8_r])
            self.dve(lambda e: e.tensor_scalar_mul(out=r8[:, 1:2], in0=r8[:, 0:1], scalar1=-1.0), [R8_r], [R8_r])
            self.dve(lambda e: e.memset(r8[:, 2:3], 0.0), [], [R8_r])
            self.act(lambda e, lg4=lg4: e.activation(out=r8[:, 12:16], in_=lg4, func=AF.Exp, bias=r8[:, 1:2], accum_out=r8[:, 2:3]), [LG_r, R8_r], [R8_r])
            self.dve(lambda e: e.reciprocal(out=r8[:, 3:4], in_=r8[:, 2:3]), [R8_r], [R8_r])
            self.dve(lambda e: e.tensor_scalar(out=r8[:, 16:48].rearrange("p (g k) -> p g k", g=4), in0=r8[:, 8:12].unsqueeze(2).broadcast_to([128, 4, 8]),
                                               scalar1=-1.0, scalar2=1e30, op0=ALU.add, op1=ALU.mult), [R8_r], [R8_r])
            self.dve(lambda e, fine=fine: e.tensor_tensor(out=r8[:, 16:48], in0=r8[:, 16:48], in1=fine, op=ALU.add), [R8_r, LG_r], [R8_r])
            self.dve(lambda e: e.max(out=r8[:, 48:56], in_=r8[:, 16:48]), [R8_r], [R8_r])
            self.dve(lambda e: e.tensor_tensor(out=r8[:, 4:5], in0=r8[:, 49:50], in1=r8[:, 48:49], op=ALU.subtract), [R8_r], [R8_r])
            self.act(lambda e: e.activation(out=r8[:, 4:5], in_=r8[:, 4:5], func=AF.Exp), [R8_r], [R8_r])
            self.dve(lambda e: e.tensor_scalar_add(out=r8[:, 4:5], in0=r8[:, 4:5], scalar1=1.0), [R8_r], [R8_r])
            self.dve(lambda e: e.reciprocal(out=r8[:, 4:5], in_=r8[:, 4:5]), [R8_r], [R8_r])
            self.dve(lambda e: e.tensor_tensor(out=r8[:, 5:6], in0=r8[:, 4:5], in1=r8[:, 3:4], op=ALU.mult), [R8_r], [R8_r])
            self.dve(lambda e: e.tensor_tensor(out=r8[:, 6:7], in0=r8[:, 3:4], in1=r8[:, 5:6], op=ALU.subtract), [R8_r], [R8_r])
            self.dve(lambda e: e.tensor_scalar(out=r8[:, 56:88], in0=r8[:, 16:48], scalar1=r8[:, 48:49], scalar2=None, op0=ALU.is_equal), [R8_r], [R8_r])
            self.dve(lambda e: e.tensor_scalar(out=r8[:, 88:120], in0=r8[:, 16:48], scalar1=r8[:, 49:50], scalar2=None, op0=ALU.is_equal), [R8_r], [R8_r])
            self.dve(lambda e, gi_=gi_: e.tensor_copy(out=W12[:, gi_, :], in_=r8[:, 5:7]), [R8_r], [W12_r])
            self.dve(lambda e, gi_=gi_: e.tensor_tensor(out=MM[:, gi_, :], in0=r8[:, 56:88], in1=r8[:, 88:120], op=ALU.add), [R8_r], [MM_r])
            pR, rR = self.ps()
            for ip in range(gi_):
                self.mm(pR[:, 0:32], ones[:], MM[:, ip, :], ip == 0, False, [ones_r, MM_r], [rR])
            self.mm(pR[:, 0:32], msu[:], MM[:, gi_, :], gi_ == 0, True, [msu_r, MM_r], [rR])
            self.dve(lambda e, pR=pR: e.tensor_tensor(out=r8[:, 120:152], in0=pR[:, 0:32], in1=ecap[:], op=ALU.add), [rR, ecap_r], [R8_r])
            for k in range(2):
                self.dve(lambda e, k=k: e.tensor_tensor(out=r8[:, 152:184], in0=r8[:, 56 + 32 * k:88 + 32 * k], in1=r8[:, 120:152], op=ALU.mult), [R8_r], [R8_r])
                self.dve(lambda e, k=k, gi_=gi_: e.reduce_sum(out=SRf[:, gi_, k:k + 1], in_=r8[:, 152:184], axis=AX.X), [R8_r], [SRf_r])
            self.dve(lambda e, gi_=gi_: e.tensor_copy(out=SR[:, gi_, :], in_=SRf[:, gi_, :]), [SRf_r], [SR_r])
            for k in range(2):
                o = self.p.dma("pool", lambda e, gi_=gi_, k=k: e.indirect_dma_start(
                    out=self.tokd[:, :], out_offset=bass.IndirectOffsetOnAxis(ap=SR[:, gi_, k:k + 1], axis=0),
                    in_=self.TID[:, gi_:gi_ + 1], in_offset=None),
                    [SR_r, self.TID_r], [], extra=[tok_init])
                self.scat_ops.append(o)

    def moe(self, scr):
        idb, idb_r = self.identb, self.identb_r
        wgus = [scr.get(4), scr.get(4)]
        wdns = [scr.get(2), scr.get(2)]
        XGs = [scr.get(1) for _ in range(8)]
        XTs = [scr.get(4), scr.get(4)]
        HTs = [scr.get(1), scr.get(1)]
        YTs = [scr.get(1) for _ in range(4)]
        YKs = [scr.get(1) for _ in range(8)]
        XAs = [scr.get(2) for _ in range(4)]
        gf = scr.get(2)
        self.load(gf.f(), self.wd["g_fin"].partition_broadcast(128), [], gf.rf())
        junk = scr.get(1)
        ots = [scr.get(2) for _ in range(4)]
        ssq, ssq_r, rs, rs_r = self.ssq, self.ssq_r, self.rs, self.rs_r
        t1, t2 = scr.get(1), scr.get(1)
        NBLK = CAP // 128
        SR, SR_r, W12, W12_r = self.SR, self.SR_r, self.W12, self.W12_r

        def loadw_e(ex):
            wgu, wdn = wgus[ex % 2], wdns[ex % 2]
            self.loadw(wgu.h().rearrange("p (c n) -> p c n", c=8), self.wd["w_gu"][ex].rearrange("(c p) n -> p c n", p=128), [], wgu.rh())
            self.loadw(wdn.h().rearrange("p (c n) -> p c n", c=2), self.wd["w_dn"][ex].rearrange("(c p) n -> p c n", p=128), [], wdn.rh())

        loadw_e(0)
        loadw_e(1)
        gi = 0
        yi = 0
        xg_of = {}

        def gather_e(ex):
            nonlocal gi
            lst = []
            for blk in range(NBLK):
                idx, idx_r = self.IDX[gi % 8]
                XG = XGs[gi % 8]
                gi += 1
                r0 = ex * CAP + blk * 128
                self.p.dma("sp", lambda e, idx=idx, r0=r0: e.dma_start(out=idx[:], in_=self.tokd[r0:r0 + 128, :]), [self.tok_r], [idx_r], extra=self.scat_ops)
                self.p.dma("pool", lambda e, idx=idx, XG=XG: e.indirect_dma_start(
                    out=XG.h(), out_offset=None, in_=self.h2s[:, :], in_offset=bass.IndirectOffsetOnAxis(ap=idx[:, 0:1], axis=0)), [idx_r] + self.h2s_r, XG.rh())
                lst.append(XG)
            xg_of[ex] = lst

        gather_e(0)
        gather_e(1)
        for ex in range(32):
            wgu, wdn = wgus[ex % 2], wdns[ex % 2]
            wguv = wgu.h().rearrange("p (c n) -> p c n", c=8)
            wdnv = wdn.h().rearrange("p (c n) -> p c n", c=2)
            XT = XTs[ex % 2]
            XTv = XT.h().rearrange("p (c t) -> p c t", c=8)
            HT = HTs[ex % 2]
            HTv = HT.h(0, 2 * CAP).rearrange("p (c t) -> p c t", c=2)
            for blk in range(NBLK):
                XG = xg_of[ex][blk]
                pt, pr = self.ps()
                ptv = pt[:].bitcast(BF16).rearrange("p (c t) -> p c t", c=8)
                for cc in range(8):
                    self.pe(lambda e, cc=cc, ptv=ptv, XG=XG: e.transpose(out=ptv[:, cc, :], in_=XG.h(cc * 128, (cc + 1) * 128), identity=idb[:]), XG.rh() + [idb_r], [pr])
                self.act(lambda e, ptv=ptv, XTv=XTv, blk=blk: e.copy(out=XTv[:, :, blk * 128:(blk + 1) * 128], in_=ptv), [pr], XT.rh())
            if ex + 2 < 32:
                pass
            for jc in range(2):
                pG, rG = self.ps()
                for cc in range(8):
                    self.mm(pG[:, 0:CAP], wguv[:, cc, jc * 128:(jc + 1) * 128], XTv[:, cc, :], cc == 0, cc == 7, wgu.rh() + XT.rh(), [rG])
                pU, rU = self.ps()
                for cc in range(8):
                    self.mm(pU[:, 0:CAP], wguv[:, cc, 256 + jc * 128:256 + (jc + 1) * 128], XTv[:, cc, :], cc == 0, cc == 7, wgu.rh() + XT.rh(), [rU])
                self.act(lambda e, pG=pG: e.activation(out=t1.f(0, CAP), in_=pG[:, 0:CAP], func=AF.Tanh, scale=0.5), [rG], t1.rf())
                self.dve(lambda e, pG=pG: e.scalar_tensor_tensor(out=t2.f(0, CAP), in0=t1.f(0, CAP), scalar=1.0, in1=pG[:, 0:CAP], op0=ALU.add, op1=ALU.mult), [rG] + t1.rf(), t2.rf())
                self.dve(lambda e, pU=pU, HTv=HTv, jc=jc: e.scalar_tensor_tensor(out=HTv[:, jc, :], in0=t2.f(0, CAP), scalar=0.5, in1=pU[:, 0:CAP], op0=ALU.mult, op1=ALU.mult), [rU] + t2.rf(), HT.rh())
            for blk in range(NBLK):
                YT = YTs[yi % 4]
                yi += 1
                for half in range(2):
                    hs = slice(half * 512, (half + 1) * 512)
                    pY, rY = self.ps()
                    for jc in range(2):
                        self.mm(pY[:, :], HTv[:, jc, blk * 128:(blk + 1) * 128], wdnv[:, jc, hs], jc == 0, jc == 1, HT.rh() + wdn.rh(), [rY])
                    if half == 0:
                        self.act(lambda e, pY=pY, YT=YT, hs=hs: e.copy(out=YT.h()[:, hs], in_=pY[:, :]), [rY], YT.rh())
                    else:
                        self.dve(lambda e, pY=pY, YT=YT, hs=hs: e.tensor_copy(out=YT.h()[:, hs], in_=pY[:, :]), [rY], YT.rh())
                r0 = ex * CAP + blk * 128
                self.load(self.ysd[r0:r0 + 128, :], YT.h(), YT.rh(), [self.ys_r[ex]])
            if ex + 2 < 32:
                gather_e(ex + 2)
                loadw_e(ex + 2)
        ki = 0
        for g in range(32):
            XA = XAs[g % 4]
            self.load(XA.f(), self.x1s[g * 128:(g + 1) * 128, :], [self.x1_r[g]], XA.rf())
            for k in range(2):
                YK = YKs[ki % 8]
                ki += 1
                self.p.dma("pool", lambda e, YK=YK, g=g, k=k: e.indirect_dma_start(
                    out=YK.h(), out_offset=None, in_=self.ysd[:, :], in_offset=bass.IndirectOffsetOnAxis(ap=SR[:, g, k:k + 1], axis=0)), [SR_r] + self.ys_r, YK.rh())
                self.dve(lambda e, YK=YK, g=g, k=k, XA=XA: e.scalar_tensor_tensor(out=XA.f(), in0=YK.h(), scalar=W12[:, g, k:k + 1], in1=XA.f(), op0=ALU.mult, op1=ALU.add),
                         YK.rh() + [W12_r] + XA.rf(), XA.rf())
            i = g % 16
            ot = ots[g % 4]
            self.dve(lambda e, i=i: e.memset(ssq[:, i:i + 1], 0.0), [], [ssq_r])
            self.act(lambda e, i=i, XA=XA: e.activation(out=junk.h(), in_=XA.f(), func=AF.Square, accum_out=ssq[:, i:i + 1]), XA.rf() + [ssq_r], junk.rh() + [ssq_r])
            self.dve(lambda e, i=i: e.tensor_scalar(out=rs[:, i:i + 1], in0=ssq[:, i:i + 1], scalar1=1.0 / D, scalar2=1e-6, op0=ALU.mult, op1=ALU.add), [ssq_r], [rs_r])
            self.act(lambda e, i=i: e.activation(out=rs[:, i:i + 1], in_=rs[:, i:i + 1], func=AF.Ln), [rs_r], [rs_r])
            self.act(lambda e, i=i: e.activation(out=rs[:, i:i + 1], in_=rs[:, i:i + 1], func=AF.Exp, scale=-0.5), [rs_r], [rs_r])
            self.dve(lambda e, i=i, ot=ot, XA=XA: e.scalar_tensor_tensor(out=ot.f(), in0=XA.f(), scalar=rs[:, i:i + 1], in1=gf.f(), op0=ALU.mult, op1=ALU.mult),
                     XA.rf() + [rs_r] + gf.rf(), ot.rf())
            self.load(self.out[g * 128:(g + 1) * 128, :], ot.f(), ot.rf(), [self.out_res])

    def dump_bf16_fm(self, reg, nchunk, scr):
        tmp = scr.get(4)
        v = reg.h().rearrange("p (c t) -> p c t", c=nchunk)
        for cc in range(nchunk):
            self.dve(lambda e, cc=cc: e.tensor_copy(out=tmp.f(), in_=v[:, cc, :]), reg.rh(cc * T, (cc + 1) * T), tmp.rf())
            self.load(self.dbg[cc * 128:(cc + 1) * 128, :], tmp.f(), tmp.rf(), [self.out_res])

    def finish(self):
        fin = self.p.op("sp", lambda e: e.nop(), [self.out_res], [])
        for e in ("pe", "act", "dve", "pool"):
            last = [o for o in self.p.ops[e] if not o.dma]
            if last:
                last[-1].signal = True
                fin.deps.append(last[-1])
        for q in ("sp", "pool"):
            for o in self.p.dlast[q]:
                if o is not None and o is not fin:
                    fin.deps.append(o)
        import os, sys
        if os.environ.get("KFILL", "1") == "1":
            idb = self.identb
            fb = self.psb[7]
            self.p.filler_fn = lambda e: e.matmul(out=fb[:, 0:128], lhsT=idb[:], rhs=idb[:], start=True, stop=True)
            self.p.filler_dep = self.identb_op
        if os.environ.get("KSCHED", "1") == "1":
            self.p.schedule()
            print("NFILL", getattr(self.p, "nfill", 0), file=sys.stderr)
        with self.nc.Block() as block:
            self.p.emit(block)
        self.st.close()
        import sys
        print("ICOUNT", self.p.icount, file=sys.stderr)
        return self.nc


def build(debug=None):
    B = Builder(debug)
    B.setup()
    ar = B.ar
    hT = ar.reg(0, 16)
    yrwT = ar.reg(16, 8)
    ymlaT = ar.reg(24, 16)
    for b in range(NB):
        B.phaseA(b, hT, Bump(ar, 24, 88))
        if debug and debug[0] == "hT":
            B.dump_bf16_fm(hT, 8, Bump(ar, 40, 88))
            return B.finish()
        B.rwkv(b, hT, yrwT, Bump(ar, 24, 88))
        if debug and debug[0] == "rw":
            if not getattr(B, "nodump", False):
                B.dump_bf16_fm(yrwT, 4, Bump(ar, 40, 88))
            return B.finish()
        B.mla(b, hT, ymlaT, Bump(ar, 40, 88))
        if debug and debug[0] == "mla":
            B.dump_bf16_fm(ymlaT, 8, Bump(ar, 40, 88))
            return B.finish()
        B.merge(b, hT, yrwT, ymlaT, Bump(ar, 40, 72), Bump(ar, 72, 80), Bump(ar, 80, 88))
    B.moe(Bump(ar, 0, 88))
    return B.finish()


_NC_CACHE = {}


def _tokid():
    return np.ascontiguousarray((np.arange(32, dtype=np.int32)[None, :] * 128 + np.arange(128, dtype=np.int32)[:, None]).astype(np.int32))


def kernel(**inputs):
    inp = {k: np.asarray(v) for k, v in inputs.items()}
    w = _host_layout(inp)
    consts = _consts()
    if "nc" not in _NC_CACHE:
        _NC_CACHE["nc"] = build()
    nc = _NC_CACHE["nc"]
    x = np.ascontiguousarray(inp["x"], dtype=np.float32)
    pos = np.ascontiguousarray(inp["positions"]).astype(np.int32)
    in_maps = []
    for core in range(NCORES):
        m = {"x": np.ascontiguousarray(x[core * NB:(core + 1) * NB].reshape(NB * T, D)),
             "pos": np.ascontiguousarray(pos[core * NB:(core + 1) * NB])}
        for n in CONST_NAMES:
            m["c_" + n] = consts[n]
        m["tokid"] = _tokid()
        m.update(w)
        in_maps.append(m)
    res = run_bass_kernel_spmd(nc, in_maps, core_ids=list(range(NCORES)))
    out = np.concatenate([np.asarray(r["out"]).reshape(NB, T, D) for r in res.results], axis=0)
    return out.astype(np.float32)
```

```python
import math
import numpy as np
import concourse.bass as bass
import concourse.mybir as mybir
from concourse.bass_utils import run_bass_kernel_spmd
from contextlib import ExitStack

F32 = mybir.dt.float32
BF16 = mybir.dt.bfloat16
I32 = mybir.dt.int32
ALU = mybir.AluOpType
AF = mybir.ActivationFunctionType
AX = mybir.AxisListType

NB = 2
T = 2048
D = 1024
NCORES = 8
RW_COLS = 1792
CQ0 = 1792
CKV0 = 2176
CKR0 = 2432
GRW0 = 2464
GML0 = 3488
CH = 128
NCH = T // CH
CAP = 512
ATT_SCALE = 1.0 / math.sqrt(96.0)
LOGW_C = -math.exp(-0.5)


class Res:
    __slots__ = ("w", "r", "excl")

    def __init__(self, excl=False):
        self.w = None
        self.r = []
        self.excl = excl


class Op:
    __slots__ = ("eng", "fn", "deps", "sdeps", "signal", "seq", "dma", "sem", "semval", "idx", "fin", "cost")

    def __init__(self, eng, fn):
        self.eng = eng
        self.fn = fn
        self.deps = []
        self.sdeps = []
        self.idx = 0
        self.fin = None
        self.cost = None
        self.signal = False
        self.seq = 0
        self.dma = False
        self.sem = None
        self.semval = 0


class Prog:
    ENGS = ("pe", "act", "dve", "pool", "sp")

    def __init__(self, nc, stack, n_dma_sems=10):
        self.nc = nc
        self.ops = {e: [] for e in self.ENGS}
        self.esem = {e: stack.enter_context(nc.semaphore("prog_" + e)) for e in self.ENGS}
        self.dsems, self.dcount, self.dlast = {}, {}, {}
        for q in ("sp", "pool"):
            self.dsems[q] = [stack.enter_context(nc.semaphore(f"dma_{q}_{i}")) for i in range(n_dma_sems)]
            self.dcount[q] = 0
            self.dlast[q] = [None] * n_dma_sems

    def _track(self, op, reads, writes):
        deps = []
        reads = list(reads)
        writes = list(writes)
        for r in reads:
            if r.excl and r not in writes:
                writes.append(r)
        for r in reads:
            if r.w is not None:
                deps.append(r.w)
        for w in writes:
            if w.w is not None:
                deps.append(w.w)
            deps.extend(w.r)
        seen = set()
        for d in deps:
            if d is op or id(d) in seen:
                continue
            seen.add(id(d))
            if (not d.dma) and d.eng == "pe" and op.eng == "pe" and not op.dma:
                op.sdeps.append(d)
                continue
            op.deps.append(d)
            d.signal = True
        for r in reads:
            r.r.append(op)
        for w in writes:
            w.w = op
            w.r = []

    def op(self, eng, fn, reads=(), writes=(), cost=None):
        o = Op(eng, fn)
        self.nidx = getattr(self, "nidx", 0) + 1
        o.idx = self.nidx
        o.cost = cost
        self._track(o, reads, writes)
        self.ops[eng].append(o)
        return o

    def schedule(self, window=64):
        import heapq
        base = {"pe": 0.25, "act": 0.6, "dve": 0.55, "pool": 1.0, "sp": 0.05}
        pend = {e: list(self.ops[e]) for e in self.ENGS}
        out = {e: [] for e in self.ENGS}
        free_at = {e: 0.0 for e in self.ENGS}
        now = 0.0
        events = []
        remaining = sum(len(v) for v in pend.values())
        cnt = 0
        while remaining:
            progressed = False
            for e in self.ENGS:
                if free_at[e] > now or not pend[e]:
                    continue
                best = None
                for o in pend[e][:window]:
                    ok = True
                    for d in o.deps:
                        if d.fin is None or d.fin > now:
                            ok = False
                            break
                    if ok:
                        for d in o.sdeps:
                            if d.fin is None:
                                ok = False
                                break
                    if ok:
                        best = o
                        break
                if best is None:
                    fill = getattr(self, "filler_fn", None)
                    fdep = getattr(self, "filler_dep", None)
                    if e == "pe" and fill is not None and fdep is not None and fdep.fin is not None and fdep.fin <= now:
                        fo = Op("pe", fill)
                        fo.deps.append(fdep)
                        fdep.signal = True
                        out[e].append(fo)
                        self.nfill = getattr(self, "nfill", 0) + 1
                        free_at[e] = now + 0.1
                        cnt += 1
                        heapq.heappush(events, (free_at[e], cnt))
                        progressed = True
                    continue
                pend[e].remove(best)
                out[e].append(best)
                remaining -= 1
                progressed = True
                if best.dma:
                    free_at[e] = now + 0.06
                    best.fin = now + 2.5
                else:
                    c = best.cost if best.cost is not None else base[e]
                    free_at[e] = now + c
                    best.fin = now + c + 0.25
                cnt += 1
                heapq.heappush(events, (best.fin, cnt))
                heapq.heappush(events, (free_at[e], cnt))
            if not progressed:
                if not events:
                    raise RuntimeError("scheduler stuck")
                t = heapq.heappop(events)[0]
                now = max(now, t)
        self.ops = out

    def dma(self, q, fn, reads=(), writes=(), extra=()):
        o = Op(q, fn)
        self.nidx = getattr(self, "nidx", 0) + 1
        o.idx = self.nidx
        o.dma = True
        self._track(o, reads, writes)
        for d in extra:
            o.deps.append(d)
            d.signal = True
        n = len(self.dsems[q])
        i = self.dcount[q] % n
        self.dcount[q] += 1
        prev = self.dlast[q][i]
        if prev is not None:
            o.deps.append(prev)
        o.sem = self.dsems[q][i]
        o.semval = (prev.semval if prev is not None else 0) + 16
        self.dlast[q][i] = o
        self.ops[q].append(o)
        return o

    def emit(self, block):
        for e in self.ENGS:
            c = 0
            for o in self.ops[e]:
                if (not o.dma) and o.signal:
                    c += 1
                    o.seq = c

        def run(e):
            def body(eng):
                waited = {}
                self.icount = getattr(self, "icount", {})
                self.icount[e] = 0
                for o in self.ops[e]:
                    self.icount[e] += 1
                    need = {}
                    for d in o.deps:
                        if d.dma:
                            k, v = d.sem, d.semval
                        else:
                            k, v = self.esem[d.eng], d.seq
                        if need.get(k, 0) < v:
                            need[k] = v
                    for k, v in need.items():
                        if waited.get(k, 0) < v:
                            eng.wait_ge(k, v)
                            waited[k] = v
                            self.icount[e] += 1
                    ins = o.fn(eng)
                    if o.dma:
                        ins.then_inc(o.sem, 16)
                    elif o.signal:
                        ins.then_inc(self.esem[e], 1)
            return body

        block.tensor(run("pe"))
        block.scalar(run("act"))
        block.vector(run("dve"))
        block.gpsimd(run("pool"))
        block.sync(run("sp"))


class Reg:
    def __init__(self, ar, blk0, nblk):
        self.ar, self.b0, self.n = ar, blk0, nblk

    def f(self, a=0, b=None):
        b = self.n * 512 if b is None else b
        return self.ar.t[:, self.b0 * 512 + a:self.b0 * 512 + b]

    def h(self, a=0, b=None):
        b = self.n * 1024 if b is None else b
        assert a % 2 == 0 and b % 2 == 0
        return self.ar.t[:, self.b0 * 512 + a // 2:self.b0 * 512 + b // 2].bitcast(BF16)

    def i(self, a=0, b=None):
        b = self.n * 512 if b is None else b
        return self.ar.t[:, self.b0 * 512 + a:self.b0 * 512 + b].bitcast(I32)

    def rf(self, a=0, b=None):
        b = self.n * 512 if b is None else b
        return [self.ar.res[self.b0 + j] for j in range(a // 512, (b - 1) // 512 + 1)]

    def rh(self, a=0, b=None):
        b = self.n * 1024 if b is None else b
        return [self.ar.res[self.b0 + j] for j in range(a // 1024, (b - 1) // 1024 + 1)]

    def sub(self, blk, n):
        assert blk + n <= self.n
        return Reg(self.ar, self.b0 + blk, n)


class Arena:
    def __init__(self, nc, stack, nblk):
        self.t = stack.enter_context(nc.sbuf_tensor("arena", [128, nblk * 512], F32))
        self.res = [Res() for _ in range(nblk)]
        self.nblk = nblk

    def reg(self, blk0, nblk):
        assert blk0 + nblk <= self.nblk, (blk0, nblk)
        return Reg(self, blk0, nblk)


class Bump:
    def __init__(self, ar, lo, hi):
        self.ar, self.lo, self.hi, self.cur = ar, lo, hi, lo

    def get(self, n):
        r = self.ar.reg(self.cur, n)
        self.cur += n
        assert self.cur <= self.hi, ("arena overflow", self.cur, self.hi)
        return r


def _consts():
    c = {}
    c["ident"] = np.eye(128, dtype=np.float32)
    p = np.arange(128)
    c["mask_su"] = (p[:, None] < p[None, :]).astype(np.float32)
    c["mask_iu"] = (p[:, None] <= p[None, :]).astype(np.float32)
    c["mask_sl"] = (p[:, None] > p[None, :]).astype(np.float32)
    blk = (p[:, None] // 64 == p[None, :] // 64).astype(np.float32)
    c["blockmask"] = blk
    c["blockones"] = blk.copy()
    c["blockavg"] = blk / 64.0
    sm = np.ones((128, 512), np.float32)
    sm[:, ::CH] = 0.0
    c["scanmask"] = sm
    c["allones"] = np.ones((128, 128), np.float32)
    c["mhalf"] = np.full((128, 512), -0.5, np.float32)
    inv_freq = (10000.0 ** (-np.arange(0, 32, 2, dtype=np.float32) / 32.0)).astype(np.float32)
    fq = np.zeros((128, 3), np.float32)
    ph = np.full((128, 3), 0.25, np.float32)
    for r in range(64, 128):
        f = inv_freq[r % 16] / (2 * np.pi)
        fq[r, :] = f
    ph[64:96, 0] = 0.25
    ph[96:112, 0] = 0.5
    ph[112:128, 0] = 0.0
    ph[64:128, 1] = 0.25
    ph[64:80, 2] = 0.5
    ph[80:96, 2] = 0.0
    ph[96:112, 2] = 0.5
    ph[112:128, 2] = 0.0
    c["ecap"] = np.tile((np.arange(32, dtype=np.float32) * CAP)[None, :], (128, 1))
    c["rope_fq"] = fq
    c["rope_ph"] = ph
    return c


_CONST_SHAPES = {n: v.shape for n, v in _consts().items()}
CONST_NAMES = ["ident", "mask_su", "mask_iu", "mask_sl", "blockmask", "blockones", "blockavg", "scanmask",
               "allones", "mhalf", "ecap", "rope_fq", "rope_ph"]


def _fm(v, nchunk):
    return np.ascontiguousarray(np.asarray(v, np.float32).reshape(nchunk, 128).T)


def _host_layout(inp):
    w = {}
    w_in = inp["w_in"][0]
    w["w_in"] = w_in
    kr = w_in[:, CKR0:CKR0 + 32]
    krs = np.concatenate([kr[:, 16:32], kr[:, 0:16]], axis=1)
    z64 = np.zeros((D, 64), np.float32)
    w["w_kr"] = np.ascontiguousarray(np.concatenate([z64, kr, kr, z64, krs, krs], axis=1))
    wq = inp["mla_w_q_up"][0]
    wqh = np.zeros((384, 8, 128), np.float32)
    for h in range(8):
        blk = wq[:, h * 96:(h + 1) * 96]
        wqh[:, h, 0:96] = blk
        wqh[:, h, 96:112] = blk[:, 80:96]
        wqh[:, h, 112:128] = blk[:, 64:80]
    w["w_q"] = np.ascontiguousarray(wqh.reshape(384, 1024))
    wkv = inp["mla_w_kv_up"][0]
    wk = np.zeros((256, 8, 128), np.float32)
    wv = np.zeros((256, 8, 64), np.float32)
    for h in range(8):
        wk[:, h, 0:64] = wkv[:, h * 128:h * 128 + 64]
        wv[:, h, :] = wkv[:, h * 128 + 64:h * 128 + 128]
    w["w_k"] = np.ascontiguousarray(wk.reshape(256, 1024))
    w["w_v"] = np.ascontiguousarray(wv.reshape(256, 512))
    w["wa_up"] = np.ascontiguousarray(np.concatenate([inp["rw_w_up"][0], inp["rw_a_up"][0]], axis=0))
    w["g_up"] = np.ascontiguousarray(inp["rw_g_up"][0])
    w["w_brw"] = np.ascontiguousarray(inp["w_branch_rw"][0])
    w["w_bml"] = np.ascontiguousarray(inp["w_branch_mla"][0])
    w["w_out"] = np.ascontiguousarray(inp["w_out"][0])
    w["w_rt"] = np.ascontiguousarray(np.concatenate([inp["moe_w_group"][0], inp["moe_w_router"][0]], axis=1))
    w["b_rt"] = np.ascontiguousarray(np.concatenate([inp["moe_b_group"][0], inp["moe_b_router"][0]])[None, :])
    w["w_gu"] = np.ascontiguousarray(inp["moe_w_gu"][0])
    w["w_dn"] = np.ascontiguousarray(inp["moe_w_down"][0])
    w["pv_mu"] = _fm(inp["rw_mu"][0], 14)
    vecs = [inp["rw_w0"][0], inp["rw_a0"][0], inp["rw_k_k"][0], inp["rw_k_a"][0], inp["rw_r_k"][0].reshape(-1),
            inp["rw_gn_w"][0], inp["rw_gn_b"][0]]
    w["pv_rw"] = np.ascontiguousarray(np.concatenate([_fm(v, 4) for v in vecs], axis=1))
    w["pv_g1"] = _fm(inp["mix_norm_g"][0], 8)
    w["pv_gq"] = _fm(inp["mla_g_qa"][0], 3)
    w["pv_gkv"] = _fm(inp["mla_g_kva"][0], 2)
    w["pv_g2"] = _fm(inp["ffn_norm_g"][0], 8)
    w["g_ffn"] = np.ascontiguousarray(inp["ffn_norm_g"][0][None, :])
    w["g_fin"] = np.ascontiguousarray(inp["final_norm_g"][None, :])
    return w


W_SHAPES = {
    "w_in": [1024, 4512], "w_kr": [1024, 256], "w_q": [384, 1024], "w_k": [256, 1024], "w_v": [256, 512],
    "wa_up": [128, 512], "g_up": [128, 512], "w_brw": [512, 1024], "w_bml": [512, 1024], "w_out": [1024, 1024],
    "w_rt": [1024, 36], "b_rt": [1, 36], "w_gu": [32, 1024, 512], "w_dn": [32, 256, 1024],
    "pv_mu": [128, 14], "pv_rw": [128, 28], "pv_g1": [128, 8], "pv_gq": [128, 3], "pv_gkv": [128, 2],
    "pv_g2": [128, 8], "g_ffn": [1, 1024], "g_fin": [1, 1024],
}


class Builder:
    def __init__(self, debug=None):
        self.debug = debug
        nc = bass.Bass("TRN2", target_bir_lowering=False)
        self.nc = nc
        self.st = ExitStack()
        st = self.st
        self.x = nc.dram_tensor("x", [NB * T, D], F32, kind="ExternalInput").ap()
        self.pos = nc.dram_tensor("pos", [NB, T], I32, kind="ExternalInput").ap()
        self.cd = {n: nc.dram_tensor("c_" + n, list(_CONST_SHAPES[n]), F32, kind="ExternalInput").ap() for n in CONST_NAMES}
        self.wd = {n: nc.dram_tensor(n, s, F32, kind="ExternalInput").ap() for n, s in W_SHAPES.items()}
        self.out = nc.dram_tensor("out", [NB * T, D], F32, kind="ExternalOutput").ap()
        self.tokid = nc.dram_tensor("tokid", [128, 32], I32, kind="ExternalInput").ap()
        self.h2s = nc.dram_tensor("h2s", [NB * T, D], BF16, kind="Internal").ap()
        self.x1s = nc.dram_tensor("x1s", [NB * T, D], F32, kind="Internal").ap()
        self.x1_r = [Res() for _ in range(32)]
        self.ysd = nc.dram_tensor("ysd", [32 * CAP, D], BF16, kind="Internal").ap()
        self.tokd = nc.dram_tensor("tokd", [32 * CAP, 1], I32, kind="Internal").ap()
        self.h2s_r = [Res() for _ in range(32)]
        self.ys_r = [Res() for _ in range(32)]
        self.tok_r = Res()
        self.dbg = None
        if debug is not None:
            self.dbg = nc.dram_tensor("dbg", list(debug[1]), F32, kind="ExternalOutput").ap()
        self.p = Prog(nc, st)
        self.ar = Arena(nc, st, 88)
        self.psb = [st.enter_context(nc.psum_tensor(f"ps{i}", [128, 512], F32)) for i in range(8)]
        self.psr = [Res(excl=True) for _ in range(8)]
        self.psi = 0
        import os as _os
        self.nrot = 7 if _os.environ.get("KFILL", "1") == "1" else 8
        self.small = {}
        self.out_res = Res()

    def sb(self, name, shape, dt=F32):
        t = self.st.enter_context(self.nc.sbuf_tensor("sb_" + name, shape, dt))
        r = Res()
        self.small[name] = (t, r)
        return t, r

    def ps(self, hold=False):
        held = getattr(self, "held", set())
        self.held = held
        nrot = getattr(self, "nrot", 8)
        while self.psi in held:
            self.psi = (self.psi + 1) % nrot
        i = self.psi
        self.psi = (self.psi + 1) % nrot
        if hold:
            held.add(i)
        return self.psb[i], self.psr[i]

    def ps_release(self, t):
        for i in range(8):
            if self.psb[i] is t:
                self.held.discard(i)

    def pe(self, fn, rd, wr):
        return self.p.op("pe", fn, rd, wr)

    def act(self, fn, rd, wr):
        return self.p.op("act", fn, rd, wr)

    def dve(self, fn, rd, wr):
        return self.p.op("dve", fn, rd, wr)

    def pool(self, fn, rd, wr):
        return self.p.op("pool", fn, rd, wr)

    def mm(self, out, lhsT, rhs, start, stop, rd, wr):
        try:
            n = rhs.free_size()
            cost = 0.06 + n / 2400.0 * (4.0 if rhs.dtype == F32 else 1.0)
        except Exception:
            cost = None
        return self.p.op("pe", lambda e: e.matmul(out=out, lhsT=lhsT, rhs=rhs, start=start, stop=stop), rd, wr, cost=None)

    def loadw(self, dst, src, rd, wr):
        return self.p.dma("pool", lambda e: e.dma_start(out=dst, in_=src), rd, wr)

    def load(self, dst, src, rd, wr):
        return self.p.dma("sp", lambda e: e.dma_start(out=dst, in_=src), rd, wr)

    def setup(self):
        c = {}
        for n in CONST_NAMES:
            t, r = self.sb("k_" + n, list(_CONST_SHAPES[n]))
            self.load(t[:], self.cd[n], [], [r])
            c[n] = (t, r)
        self.c = c
        idb, idb_r = self.sb("identb", [128, 128], BF16)
        self.identb_op = self.dve(lambda e: e.tensor_copy(out=idb[:], in_=c["ident"][0][:]), [c["ident"][1]], [idb_r])
        self.identb, self.identb_r = idb, idb_r
        m2, m2_r = self.sb("mask2", [128, 256])
        self.dve(lambda e: e.tensor_copy(out=m2[:, 0:128], in_=c["mask_su"][0][:]), [c["mask_su"][1]], [m2_r])
        self.dve(lambda e: e.tensor_copy(out=m2[:, 128:256], in_=c["mask_iu"][0][:]), [c["mask_iu"][1]], [m2_r])
        self.mask2, self.mask2_r = m2, m2_r
        pv = {}
        for n in ["pv_mu", "pv_rw", "pv_g1", "pv_gq", "pv_gkv", "pv_g2"]:
            t, r = self.sb("s_" + n, W_SHAPES[n])
            self.load(t[:], self.wd[n], [], [r])
            pv[n] = (t, r)
        self.pv = pv
        omm, omm_r = self.sb("omm", [128, 14])
        self.dve(lambda e: e.tensor_scalar(out=omm[:], in0=pv["pv_mu"][0][:], scalar1=-1.0, scalar2=1.0, op0=ALU.mult, op1=ALU.add),
                 [pv["pv_mu"][1]], [omm_r])
        self.omm, self.omm_r = omm, omm_r
        dv, dv_r = self.sb("rwdv", [128, 12])
        rw = pv["pv_rw"][0]
        self.dve(lambda e: e.tensor_scalar_mul(out=dv[:, 0:8], in0=rw[:, 0:8], scalar1=0.5), [pv["pv_rw"][1]], [dv_r])
        self.dve(lambda e: e.tensor_scalar_mul(out=dv[:, 8:12], in0=rw[:, 12:16], scalar1=-1.0), [pv["pv_rw"][1]], [dv_r])
        self.rwdv, self.rwdv_r = dv, dv_r
        wa_up, wa_up_r = self.sb("wa_up", [128, 512], BF16)
        self.loadw(wa_up[:], self.wd["wa_up"], [], [wa_up_r])
        g_up, g_up_r = self.sb("g_up", [128, 512], BF16)
        self.loadw(g_up[:], self.wd["g_up"], [], [g_up_r])
        self.wa_up, self.wa_up_r, self.g_up, self.g_up_r = wa_up, wa_up_r, g_up, g_up_r
        self.STP, self.STP_r = self.sb("STP", [128, 128])
        self.STB, self.STB_r = self.sb("STB", [128, 128], BF16)
        self.TMS, self.TMS_r = self.sb("TMS", [128, 128])
        self.XB, self.XB_r = self.sb("XB", [128, 128], BF16)
        self.UB, self.UB_r = self.sb("UB", [128, 128], BF16)
        self.UP0, self.UP0_r = self.sb("UP0", [128, 128], BF16)
        self.UP1, self.UP1_r = self.sb("UP1", [128, 128], BF16)
        self.dve(lambda e: e.memset(self.UP0[:], 0.0), [], [self.UP0_r])
        self.dve(lambda e: e.memset(self.UP1[:], 0.0), [], [self.UP1_r])
        self.GC, self.GC_r = self.sb("GC", [128, NCH])
        self.carry = [self.sb(f"carry{i}", [128, 1]) for i in range(3)]
        self.LG, self.LG_r = self.sb("LG", [128, 16, 36])
        self.WT, self.WT_r = self.sb("WT", [128, 16, 32])
        self.R8, self.R8_r = self.sb("R8", [128, 192])
        self.MM, self.MM_r = self.sb("MMem", [128, 32, 32])
        self.SRf, self.SRf_r = self.sb("SRf", [128, 32, 2])
        self.SR, self.SR_r = self.sb("SR", [128, 32, 2], I32)
        self.W12, self.W12_r = self.sb("W12", [128, 32, 2])
        self.TID, self.TID_r = self.sb("TID", [128, 32], I32)
        self.load(self.TID[:], self.tokid, [], [self.TID_r])
        self.ZI, self.ZI_r = self.sb("ZI", [128, 128], I32)
        self.dve(lambda e: e.memset(self.ZI[:], 0), [], [self.ZI_r])
        self.IDX = [self.sb(f"IDX{i}", [128, 1], I32) for i in range(8)]
        self.brt, self.brt_r = self.sb("brt", [128, 36])
        self.load(self.brt[:], self.wd["b_rt"].partition_broadcast(128), [], [self.brt_r])
        self.ssq, self.ssq_r = self.sb("ssq", [128, 16])
        self.rs, self.rs_r = self.sb("rs", [128, 16])

    def phaseA(self, b, hT, scr):
        c = self.c
        xts = [scr.get(2), scr.get(2)]
        junk = scr.get(1)
        hb = scr.get(1)
        g1, g1_r = self.pv["pv_g1"]
        hTv = hT.h().rearrange("p (c t) -> p c t", c=8)
        ssq, ssq_r, rs, rs_r = self.ssq, self.ssq_r, self.rs, self.rs_r
        for i in range(16):
            xt = xts[i % 2]
            row0 = b * T + i * 128
            self.load(xt.f(), self.x[row0:row0 + 128, :], [], xt.rf())
            self.dve(lambda e, i=i: e.memset(ssq[:, i:i + 1], 0.0), [], [ssq_r])
            self.act(lambda e, xt=xt, i=i: e.activation(out=junk.h(), in_=xt.f(), func=AF.Square, accum_out=ssq[:, i:i + 1]),
                     xt.rf() + [ssq_r], junk.rh() + [ssq_r])
            self.dve(lambda e, i=i: e.tensor_scalar(out=rs[:, i:i + 1], in0=ssq[:, i:i + 1], scalar1=1.0 / D, scalar2=1e-6,
                                                    op0=ALU.mult, op1=ALU.add), [ssq_r], [rs_r])
            self.act(lambda e, i=i: e.activation(out=rs[:, i:i + 1], in_=rs[:, i:i + 1], func=AF.Ln), [rs_r], [rs_r])
            self.act(lambda e, i=i: e.activation(out=rs[:, i:i + 1], in_=rs[:, i:i + 1], func=AF.Exp, scale=-0.5), [rs_r], [rs_r])
            self.dve(lambda e, xt=xt, i=i: e.tensor_scalar_mul(out=hb.h(), in0=xt.f(), scalar1=rs[:, i:i + 1]),
                     xt.rf() + [rs_r], hb.rh())
            pt, pr = self.ps()
            ptv = pt[:].bitcast(BF16).rearrange("p (c t) -> p c t", c=8)
            for cc in range(8):
                self.pe(lambda e, cc=cc, ptv=ptv: e.transpose(out=ptv[:, cc, :], in_=hb.h(cc * 128, (cc + 1) * 128), identity=self.identb[:]),
                        hb.rh() + [self.identb_r], [pr])
            wr = []
            for cc in range(8):
                wr += hT.rh(cc * T + i * 128, cc * T + (i + 1) * 128)
            self.dve(lambda e, i=i, ptv=ptv: e.tensor_tensor(out=hTv[:, :, i * 128:(i + 1) * 128], in0=ptv,
                                                            in1=g1[:].unsqueeze(2).broadcast_to([128, 8, 128]), op=ALU.mult),
                     [pr, g1_r], wr)

    def shiftmix(self, sid, wt_ap, wt_res, hT, ci, tt, z, pm):
        mu, mu_r = self.pv["pv_mu"]
        omm, omm_r = self.omm, self.omm_r
        car, car_r = self.carry[sid]
        hTv = hT.h().rearrange("p (c t) -> p c t", c=8)
        pt, pr = self.ps()
        for cc in range(8):
            self.mm(pt[:, :], wt_ap[:, cc, :], hTv[:, cc, tt * 512:(tt + 1) * 512], cc == 0, cc == 7,
                    wt_res + hT.rh(cc * T + tt * 512, cc * T + (tt + 1) * 512), [pr])
        if tt == 0:
            self.dve(lambda e: e.memset(car[:], 0.0), [], [car_r])
        self.act(lambda e: e.activation(out=pm.f(), in_=pt[:, :], func=AF.Copy, scale=mu[:, ci:ci + 1]), [pr, mu_r], pm.rf())
        self.dve(lambda e: e.scalar_tensor_tensor(out=z.f(1, 512), in0=pt[:, 1:512], scalar=omm[:, ci:ci + 1], in1=pm.f(0, 511),
                                                  op0=ALU.mult, op1=ALU.add), [pr, omm_r] + pm.rf(), z.rf())
        self.dve(lambda e: e.scalar_tensor_tensor(out=z.f(0, 1), in0=pt[:, 0:1], scalar=omm[:, ci:ci + 1], in1=car[:, 0:1],
                                                  op0=ALU.mult, op1=ALU.add), [pr, omm_r, car_r], z.rf())
        self.dve(lambda e: e.tensor_copy(out=car[:, 0:1], in_=pm.f(511, 512)), pm.rf(), [car_r])

    def rwkv(self, b, hT, yrwT, scr):
        c = self.c
        w_in = self.wd["w_in"]
        w_in_v = w_in.rearrange("(c p) n -> p c n", p=128)
        rw, rw_r = self.pv["pv_rw"]
        dv, dv_r = self.rwdv, self.rwdv_r
        bones, bones_r = c["blockones"]
        bavg, bavg_r = c["blockavg"]
        smask, smask_r = c["scanmask"]
        bmask, bmask_r = c["blockmask"]
        msl, msl_r = c["mask_sl"]
        idb, idb_r = self.identb, self.identb_r
        m2, m2_r = self.mask2, self.mask2_r
        yv = yrwT.h().rearrange("p (c t) -> p c t", c=4)

        wt = scr.get(1)
        wrkv = scr.get(3)
        WA = scr.get(2)
        SG = scr.get(2)
        AR = scr.get(4)
        BT = scr.get(2)
        KT = scr.get(2)
        BH = scr.get(2)
        KH = scr.get(2)
        VP = scr.get(4)
        G = scr.get(2)
        BON = scr.get(2)
        Rr, Kk0, LOGW, Aa, KK, L, E, tA, tB, PM = [scr.get(1) for _ in range(10)]
        vbt, bht, kht = scr.get(1), scr.get(1), scr.get(1)
        YT = [scr.get(1), scr.get(1)]
        alt = {}
        for nm_, reg_ in (("Rr", Rr), ("Kk0", Kk0), ("tA", tA), ("tB", tB), ("E", E)):
            alt[nm_] = [reg_, scr.get(1)]
        ALL = scr.get(8)
        NSC = scr.get(8)

        wtv = wt.h().rearrange("p (c n) -> p c n", c=8)
        ARv = AR.h().rearrange("p (a t) -> p a t", a=2)
        BHv = BH.h().rearrange("p (c n) -> p c n", c=16)
        KHv = KH.h().rearrange("p (c n) -> p c n", c=16)
        VPv = VP.h().rearrange("p (a c n) -> p a c n", a=2, c=16)

        self.dve(lambda e: e.memset(VP.h(), 0.0), [], VP.rh())

        for ci, which in ((12, "wa"), (13, "sg")):
            col0 = 1536 + (ci - 12) * 128
            self.loadw(wtv, w_in_v[:, :, col0:col0 + 128], [], wt.rh())
            for tt in range(4):
                z = tA
                self.shiftmix(0, wtv, wt.rh(), hT, ci, tt, z, PM)
                sl = slice(tt * 512, (tt + 1) * 512)
                if which == "wa":
                    self.act(lambda e, sl=sl: e.activation(out=WA.h()[0:64, sl], in_=z.f()[0:64, :], func=AF.Tanh), z.rf(), WA.rh(tt * 512, (tt + 1) * 512))
                    self.dve(lambda e, sl=sl: e.tensor_copy(out=WA.h()[64:128, sl], in_=z.f()[64:128, :]), z.rf(), WA.rh(tt * 512, (tt + 1) * 512))
                else:
                    self.act(lambda e: e.activation(out=tB.f(), in_=z.f(), func=AF.Tanh, scale=0.5), z.rf(), tB.rf())
                    self.dve(lambda e, sl=sl: e.tensor_scalar(out=SG.h()[:, sl], in0=tB.f(), scalar1=0.5, scalar2=0.5, op0=ALU.mult, op1=ALU.add),
                             tB.rf(), SG.rh(tt * 512, (tt + 1) * 512))

        import os
        cut = int(os.environ.get("KCUT", "0"))
        if cut == 1:
            return

        def run_hp(hp):
            hs = slice(hp * 128, (hp + 1) * 128)
            w3 = wrkv.h().rearrange("p (j c n) -> p j c n", j=3, c=8)
            for j in range(3):
                col0 = j * 512 + hp * 128
                self.loadw(w3[:, j], w_in_v[:, :, col0:col0 + 128], [], wrkv.rh(j * 1024, (j + 1) * 1024))
            self.dve(lambda e: e.memset(self.STP[:], 0.0), [], [self.STP_r])
            self.dve(lambda e: e.memset(self.STB[:], 0.0), [], [self.STB_r])

            def prep(tt, Rr=Rr, Kk0=Kk0, tA=tA, tB=tB, E=E):
                Rr, Kk0, tA, tB, E = (alt[n_][tt % 2] for n_ in ("Rr", "Kk0", "tA", "tB", "E"))
                sl = slice(tt * 512, (tt + 1) * 512)
                rT = lambda a=tt * 512, bnd=(tt + 1) * 512: None
                self.shiftmix(0, w3[:, 0], wrkv.rh(0, 1024), hT, hp, tt, Rr, PM)
                self.shiftmix(1, w3[:, 1], wrkv.rh(1024, 2048), hT, 4 + hp, tt, Kk0, PM)
                self.shiftmix(2, w3[:, 2], wrkv.rh(2048, 3072), hT, 8 + hp, tt, tA, PM)
                self.act(lambda e: e.copy(out=vbt.h(0, 512), in_=tA.f()), tA.rf(), vbt.rh())
                pt, pr = self.ps()
                self.mm(pt[:, :], self.wa_up[0:64, hs], WA.h()[0:64, sl], True, True, [self.wa_up_r] + WA.rh(tt * 512, (tt + 1) * 512), [pr])
                self.act(lambda e, pt=pt: e.activation(out=tB.f(), in_=pt[:, :], func=AF.Tanh, bias=dv[:, hp:hp + 1], scale=0.5), [pr, dv_r], tB.rf())
                self.dve(lambda e: e.tensor_scalar(out=LOGW.f(), in0=tB.f(), scalar1=0.5 * LOGW_C, scalar2=0.5 * LOGW_C, op0=ALU.mult, op1=ALU.add), tB.rf(), LOGW.rf())
                pt, pr = self.ps()
                self.mm(pt[:, :], self.wa_up[64:128, hs], WA.h()[64:128, sl], True, True, [self.wa_up_r] + WA.rh(tt * 512, (tt + 1) * 512), [pr])
                self.act(lambda e, pt=pt: e.activation(out=tB.f(), in_=pt[:, :], func=AF.Tanh, bias=dv[:, 4 + hp:5 + hp], scale=0.5), [pr, dv_r], tB.rf())
                self.dve(lambda e: e.tensor_scalar(out=Aa.f(), in0=tB.f(), scalar1=0.5, scalar2=0.5, op0=ALU.mult, op1=ALU.add), tB.rf(), Aa.rf())
                pt, pr = self.ps()
                self.mm(pt[:, :], self.g_up[:, hs], SG.h()[:, sl], True, True, [self.g_up_r] + SG.rh(tt * 512, (tt + 1) * 512), [pr])
                self.act(lambda e, pt=pt: e.copy(out=G.h()[:, sl], in_=pt[:, :]), [pr], G.rh(tt * 512, (tt + 1) * 512))
                self.dve(lambda e: e.tensor_scalar_mul(out=tB.f(), in0=Kk0.f(), scalar1=rw[:, 8 + hp:9 + hp]), Kk0.rf() + [rw_r], tB.rf())
                self.act(lambda e: e.activation(out=tA.f(), in_=tB.f(), func=AF.Square), tB.rf(), tA.rf())
                pt, pr = self.ps()
                self.mm(pt[:, :], bones[:], tA.f(), True, True, [bones_r] + tA.rf(), [pr])
                self.act(lambda e, pt=pt: e.copy(out=tA.f(), in_=pt[:, :]), [pr], tA.rf())
                self.act(lambda e: e.activation(out=tA.f(), in_=tA.f(), func=AF.Ln), tA.rf(), tA.rf())
                self.act(lambda e: e.activation(out=tA.f(), in_=tA.f(), func=AF.Exp, scale=-0.5), tA.rf(), tA.rf())
                self.dve(lambda e: e.tensor_tensor(out=KK.f(), in0=tB.f(), in1=tA.f(), op=ALU.mult), tB.rf() + tA.rf(), KK.rf())
                self.dve(lambda e: e.tensor_scalar(out=tA.f(), in0=Aa.f(), scalar1=rw[:, 12 + hp:13 + hp], scalar2=dv[:, 8 + hp:9 + hp],
                                                   op0=ALU.mult, op1=ALU.add), Aa.rf() + [rw_r, dv_r], tA.rf())
                self.dve(lambda e: e.scalar_tensor_tensor(out=Kk0.f(), in0=tA.f(), scalar=1.0, in1=Kk0.f(), op0=ALU.add, op1=ALU.mult),
                         tA.rf() + Kk0.rf(), Kk0.rf())
                self.dve(lambda e: e.tensor_tensor(out=tB.f(), in0=KK.f(), in1=Aa.f(), op=ALU.mult), KK.rf() + Aa.rf(), tB.rf())
                self.dve(lambda e: e.scalar_tensor_tensor(out=tA.f(), in0=Rr.f(), scalar=rw[:, 16 + hp:17 + hp], in1=Kk0.f(), op0=ALU.mult, op1=ALU.mult),
                         Rr.rf() + Kk0.rf() + [rw_r], tA.rf())
                pt, pr = self.ps()
                self.mm(pt[:, :], bones[:], tA.f(), True, True, [bones_r] + tA.rf(), [pr])
                self.dve(lambda e, pt=pt: e.tensor_tensor(out=BON.h()[:, sl], in0=pt[:, :], in1=vbt.h(0, 512), op=ALU.mult), [pr] + vbt.rh(), BON.rh(tt * 512, (tt + 1) * 512))
                self.dve(lambda e: e.tensor_tensor_scan(out=L.f(), data0=smask[:], data1=LOGW.f(), initial=0.0, op0=ALU.mult, op1=ALU.add),
                         [smask_r] + LOGW.rf(), L.rf())
                self.act(lambda e: e.activation(out=E.f(), in_=L.f(), func=AF.Exp), L.rf(), E.rf())
                self.dve(lambda e: e.tensor_tensor(out=ARv[:, 1, sl], in0=Rr.f(), in1=E.f(), op=ALU.mult), Rr.rf() + E.rf(), AR.rh(T + tt * 512, T + (tt + 1) * 512))
                self.dve(lambda e: e.tensor_tensor(out=tA.f(), in0=L.f(), in1=LOGW.f(), op=ALU.subtract), L.rf() + LOGW.rf(), tA.rf())
                self.act(lambda e: e.activation(out=E.f(), in_=tA.f(), func=AF.Exp), tA.rf(), E.rf())
                self.dve(lambda e: e.scalar_tensor_tensor(out=ARv[:, 0, sl], in0=KK.f(), scalar=-1.0, in1=E.f(), op0=ALU.mult, op1=ALU.mult),
                         KK.rf() + E.rf(), AR.rh(tt * 512, (tt + 1) * 512))
                self.act(lambda e: e.activation(out=E.f(), in_=L.f(), func=AF.Exp, scale=-1.0), L.rf(), E.rf())
                self.dve(lambda e: e.tensor_tensor(out=BT.h()[:, sl], in0=tB.f(), in1=E.f(), op=ALU.mult), tB.rf() + E.rf(), BT.rh(tt * 512, (tt + 1) * 512))
                self.dve(lambda e: e.tensor_tensor(out=KT.h()[:, sl], in0=Kk0.f(), in1=E.f(), op=ALU.mult), Kk0.rf() + E.rf(), KT.rh(tt * 512, (tt + 1) * 512))
                Lv = L.f().rearrange("p (c n) -> p c n", c=4)
                self.dve(lambda e: e.tensor_tensor(out=tA.f().rearrange("p (c n) -> p c n", c=4), in0=Lv[:, :, CH - 1:CH].broadcast_to([128, 4, CH]),
                                                   in1=Lv, op=ALU.subtract), L.rf(), tA.rf())
                self.act(lambda e: e.activation(out=E.f(), in_=tA.f(), func=AF.Exp), tA.rf(), E.rf())
                self.act(lambda e: e.activation(out=self.GC[:, tt * 4:(tt + 1) * 4], in_=Lv[:, :, CH - 1], func=AF.Exp), L.rf(), [self.GC_r])
                self.dve(lambda e: e.tensor_tensor(out=bht.h(0, 512), in0=tB.f(), in1=E.f(), op=ALU.mult), tB.rf() + E.rf(), bht.rh())
                self.dve(lambda e: e.tensor_tensor(out=kht.h(0, 512), in0=Kk0.f(), in1=E.f(), op=ALU.mult), Kk0.rf() + E.rf(), kht.rh())
                pt, pr = self.ps()
                ptv = pt[:].bitcast(BF16).rearrange("p (c t) -> p c t", c=8)
                pt2, pr2 = self.ps()
                ptv2 = pt2[:].bitcast(BF16).rearrange("p (c t) -> p c t", c=8)
                for j in range(4):
                    js = slice(j * 128, (j + 1) * 128)
                    self.pe(lambda e, j=j, js=js, ptv=ptv: e.transpose(out=ptv[:, j, :], in_=vbt.h(0, 512)[:, js], identity=idb[:]), vbt.rh() + [idb_r], [pr])
                    self.pe(lambda e, j=j, js=js, ptv=ptv: e.transpose(out=ptv[:, 4 + j, :], in_=bht.h(0, 512)[:, js], identity=idb[:]), bht.rh() + [idb_r], [pr])
                    self.pe(lambda e, j=j, js=js, ptv2=ptv2: e.transpose(out=ptv2[:, j, :], in_=kht.h(0, 512)[:, js], identity=idb[:]), kht.rh() + [idb_r], [pr2])
                cs = slice(tt * 4, (tt + 1) * 4)
                self.act(lambda e, ptv=ptv: e.copy(out=VPv[:, 0, cs, 0:64], in_=ptv[:, 0:4, 0:64]), [pr], VP.rh())
                self.dve(lambda e, ptv=ptv: e.tensor_copy(out=VPv[:, 1, cs, 64:128], in_=ptv[:, 0:4, 64:128]), [pr], VP.rh())
                self.dve(lambda e, ptv=ptv: e.tensor_copy(out=BHv[:, cs, :], in_=ptv[:, 4:8, :]), [pr], BH.rh(tt * 512, (tt + 1) * 512))
                self.act(lambda e, ptv2=ptv2: e.copy(out=KHv[:, cs, :], in_=ptv2[:, 0:4, :]), [pr2], KH.rh(tt * 512, (tt + 1) * 512))

            def unit_regs(u):
                ll = ALL.h(u * 512, (u + 1) * 512)
                llr = ALL.rh(u * 512, (u + 1) * 512)
                s8 = u % 8
                sc = NSC.h(s8 * 1024, (s8 + 1) * 1024)
                scr_ = NSC.rh(s8 * 1024, (s8 + 1) * 1024)
                return ll, llr, sc, scr_

            def aphase(chunks):
                units = [(cidx, hd) for cidx in chunks for hd in range(2)]
                st = {}
                for (cidx, hd) in units:
                    u = (cidx % 8) * 2 + hd
                    ll, llr, sc, scr_ = unit_regs(u)
                    ts = slice(cidx * 128, (cidx + 1) * 128)
                    ps_ = slice(hd * 64, (hd + 1) * 64)
                    rdA = AR.rh(cidx * 128, (cidx + 1) * 128) + AR.rh(T + cidx * 128, T + (cidx + 1) * 128)
                    p1, r1 = self.ps()
                    self.mm(p1[:, 0:256].rearrange("p (a t) -> p a t", a=2), BT.h()[ps_, ts], ARv[ps_, :, ts], True, True, BT.rh(cidx * 128, (cidx + 1) * 128) + rdA, [r1])
                    p2, r2 = self.ps()
                    self.mm(p2[:, 0:256].rearrange("p (a t) -> p a t", a=2), KT.h()[ps_, ts], ARv[ps_, :, ts], True, True, KT.rh(cidx * 128, (cidx + 1) * 128) + rdA, [r2])
                    self.mm(p2[:, 256:384], ARv[ps_, 0, ts], BT.h()[ps_, ts], True, True, BT.rh(cidx * 128, (cidx + 1) * 128) + rdA, [r2])
                    self.dve(lambda e, p1=p1, sc=sc: e.tensor_tensor(out=sc[:, 640:768], in0=p1[:, 0:128], in1=m2[:, 0:128], op=ALU.mult), [r1, m2_r], scr_)
                    self.dve(lambda e, p1=p1, ll=ll: e.tensor_tensor(out=ll[:, 384:512], in0=p1[:, 128:256], in1=m2[:, 128:256], op=ALU.mult), [r1, m2_r], llr)
                    self.dve(lambda e, p2=p2, ll=ll: e.tensor_tensor(out=ll[:, 128:384], in0=p2[:, 0:256], in1=m2[:, :], op=ALU.mult), [r2, m2_r], llr)
                    self.dve(lambda e, p2=p2, sc=sc: e.tensor_tensor(out=sc[:, 0:128], in0=p2[:, 256:384], in1=msl[:], op=ALU.mult), [r2, msl_r], scr_)
                    self.dve(lambda e, ll=ll, sc=sc: e.tensor_tensor(out=ll[:, 0:128], in0=sc[:, 640:768], in1=idb[:], op=ALU.add), scr_ + [idb_r], llr)
                    st[(cidx, hd)] = (ll, llr, sc, scr_)
                def stageA(u_, lvl):
                    ll, llr, sc, scr_ = st[u_]
                    if lvl == 1:
                        Np, NTp = sc[:, 640:768], sc[:, 0:128]
                    else:
                        o = 128 + ((lvl - 1) % 2) * 256
                        Np, NTp = sc[:, o:o + 128], sc[:, o + 128:o + 256]
                    o2 = 128 + (lvl % 2) * 256
                    pn, rn = self.ps()
                    if lvl < 6:
                        self.mm(pn[:, 0:128], NTp, Np, True, True, scr_, [rn])
                    self.mm(pn[:, 128:256], Np, NTp, True, True, scr_, [rn])
                    if lvl < 6:
                        self.act(lambda e, pn=pn, sc=sc, o2=o2: e.copy(out=sc[:, o2:o2 + 256], in_=pn[:, 0:256]), [rn], scr_)
                    else:
                        self.act(lambda e, pn=pn, sc=sc, o2=o2: e.copy(out=sc[:, o2 + 128:o2 + 256], in_=pn[:, 128:256]), [rn], scr_)

                def stageB(u_, lvl):
                    ll, llr, sc, scr_ = st[u_]
                    o2 = 128 + (lvl % 2) * 256
                    pq, rq = self.ps()
                    self.mm(pq[:, 0:128], sc[:, o2 + 128:o2 + 256], ll[:, 0:128], True, True, scr_ + llr, [rq])
                    self.dve(lambda e, pq=pq, ll=ll: e.tensor_tensor(out=ll[:, 0:128], in0=pq[:, 0:128], in1=ll[:, 0:128], op=ALU.add), [rq] + llr, llr)

                SK = 3
                work = [(u_, lvl) for lvl in range(1, 7) for u_ in units]
                for idx in range(len(work) + SK):
                    if idx < len(work):
                        stageA(*work[idx])
                    if idx - SK >= 0:
                        stageB(*work[idx - SK])

            def seq(cidx):
                ts = slice(cidx * 128, (cidx + 1) * 128)
                lls = []
                for hd in range(2):
                    u = (cidx % 8) * 2 + hd
                    ll, llr, _, _ = unit_regs(u)
                    lls.append((ll, llr))
                STP, STB, TMS, XB, UB, UP0, UP1 = self.STP, self.STB, self.TMS, self.XB, self.UB, self.UP0, self.UP1
                rdA0 = AR.rh(cidx * 128, (cidx + 1) * 128)
                rdA1 = AR.rh(T + cidx * 128, T + (cidx + 1) * 128)
                px, rx = self.ps()
                self.mm(px[:, 0:128], ARv[:, 0, ts], STB[:], True, False, rdA0 + [self.STB_r], [rx])
                for hd in range(2):
                    self.mm(px[:, hd * 64:(hd + 1) * 64], lls[hd][0][:, 128:256], VPv[:, hd, cidx, hd * 64:(hd + 1) * 64], False, hd == 1,
                            lls[hd][1] + VP.rh(), [rx])
                self.act(lambda e, px=px: e.copy(out=XB[:], in_=px[:, 0:128]), [rx], [self.XB_r])
                kseq = int(os.environ.get("KSEQ", "99"))
                if kseq <= 1:
                    return
                pu, ru = self.ps()
                kv2 = int(os.environ.get("KV2", "0"))
                for hd in range(2):
                    if kv2 == 1 and hd == 1:
                        continue
                    if kv2 == 2 and hd == 0:
                        continue
                    lq = self.identb[:] if kv2 == 3 else lls[hd][0][:, 0:128]
                    if kv2 == 4:
                        lq = lls[hd][0][:, 384:512]
                    if kv2 == 5:
                        lq = lls[hd][0][:, 256:384]
                    self.mm(pu[:, hd * 64:(hd + 1) * 64], lq, XB[:, hd * 64:(hd + 1) * 64], True, True, lls[hd][1] + [self.XB_r], [ru])
                self.act(lambda e, pu=pu: e.copy(out=UB[:], in_=pu[:, 0:128]), [ru], [self.UB_r])
                kvar = int(os.environ.get("KVAR", "0"))
                if kvar == 3:
                    self.dve(lambda e, pu=pu: e.tensor_copy(out=UP0[:, 0:64], in_=XB[:, 0:64]), [self.XB_r], [self.UP0_r])
                elif kvar == 4:
                    self.dve(lambda e, pu=pu: e.tensor_copy(out=TMS[:, 0:64], in_=pu[:, 0:64]), [ru], [self.TMS_r])
                elif kvar == 5:
                    self.act(lambda e, pu=pu: e.copy(out=UP0[:, 0:64], in_=pu[:, 0:64]), [ru], [self.UP0_r])
                elif kvar != 1:
                    self.dve(lambda e, pu=pu: e.tensor_copy(out=UP0[:, 0:64], in_=pu[:, 0:64]), [ru], [self.UP0_r])
                if kvar == 0:
                    self.dve(lambda e, pu=pu: e.tensor_copy(out=UP1[:, 64:128], in_=pu[:, 64:128]), [ru], [self.UP1_r])
                if kseq <= 2:
                    return
                py, ry = self.ps()
                kv3 = int(os.environ.get("KV3", "7"))
                kv4 = int(os.environ.get("KV4", "0"))
                lst = {0: STB, 1: self.identb, 2: XB, 3: UB}[kv4]
                kv5 = int(os.environ.get("KV5", "0"))
                rr_ = ARv[:, 0, ts] if kv5 == 1 else ARv[:, 1, ts]
                self.mm(py[:, 0:128], lst[:], rr_, True, kv5 == 2, rdA1 + [self.STB_r], [ry])
                ups = [(UP0, self.UP0_r), (UP1, self.UP1_r)]
                for hd in range(2):
                    if kv3 & 2:
                        self.mm(py[:, 0:128], ups[hd][0][:], lls[hd][0][:, 384:512], False, False, [ups[hd][1]] + lls[hd][1], [ry])
                    if kv3 & 4:
                        self.mm(py[:, 0:128], VPv[:, hd, cidx, :], lls[hd][0][:, 256:384], False, hd == 1, VP.rh() + lls[hd][1], [ry])
                yt = YT[(cidx // 4) % 2]
                yo = (cidx % 4) * 128
                self.act(lambda e, py=py: e.copy(out=yt.f(yo, yo + 128), in_=py[:, 0:128]), [ry], yt.rf())
                if kseq <= 3:
                    return
                pS, rS = self.ps()
                self.mm(pS[:, 0:128], BHv[:, cidx, :], UB[:], True, False, BH.rh(cidx * 128, (cidx + 1) * 128) + [self.UB_r], [rS])
                self.mm(pS[:, 0:128], KHv[:, cidx, :], VPv[:, 0, cidx, :], False, False, KH.rh(cidx * 128, (cidx + 1) * 128) + VP.rh(), [rS])
                self.mm(pS[:, 0:128], KHv[:, cidx, :], VPv[:, 1, cidx, :], False, True, KH.rh(cidx * 128, (cidx + 1) * 128) + VP.rh(), [rS])
                self.dve(lambda e, pS=pS: e.scalar_tensor_tensor(out=TMS[:], in0=STP[:], scalar=self.GC[:, cidx:cidx + 1], in1=pS[:, 0:128], op0=ALU.mult, op1=ALU.add),
                         [self.STP_r, self.GC_r, rS], [self.TMS_r])
                self.dve(lambda e: e.tensor_tensor(out=STP[:], in0=TMS[:], in1=bmask[:], op=ALU.mult), [self.TMS_r, bmask_r], [self.STP_r])
                self.dve(lambda e: e.tensor_tensor(out=STB[:], in0=TMS[:], in1=bmask[:], op=ALU.mult), [self.TMS_r, bmask_r], [self.STB_r])

            def post(tt):
                sl = slice(tt * 512, (tt + 1) * 512)
                yt = YT[tt % 2]
                pt, pr = self.ps()
                self.mm(pt[:, :], bavg[:], yt.f(), True, True, [bavg_r] + yt.rf(), [pr])
                self.dve(lambda e, pt=pt: e.tensor_tensor(out=tA.f(), in0=yt.f(), in1=pt[:, :], op=ALU.subtract), yt.rf() + [pr], tA.rf())
                self.act(lambda e: e.activation(out=tB.f(), in_=tA.f(), func=AF.Square), tA.rf(), tB.rf())
                pt, pr = self.ps()
                self.mm(pt[:, :], bavg[:], tB.f(), True, True, [bavg_r] + tB.rf(), [pr])
                self.dve(lambda e, pt=pt: e.tensor_scalar_add(out=tB.f(), in0=pt[:, :], scalar1=64e-5), [pr], tB.rf())
                self.act(lambda e: e.activation(out=tB.f(), in_=tB.f(), func=AF.Ln), tB.rf(), tB.rf())
                self.act(lambda e: e.activation(out=tB.f(), in_=tB.f(), func=AF.Exp, scale=-0.5), tB.rf(), tB.rf())
                self.dve(lambda e: e.tensor_tensor(out=tA.f(), in0=tA.f(), in1=tB.f(), op=ALU.mult), tA.rf() + tB.rf(), tA.rf())
                self.dve(lambda e: e.tensor_scalar(out=tA.f(), in0=tA.f(), scalar1=rw[:, 20 + hp:21 + hp], scalar2=rw[:, 24 + hp:25 + hp], op0=ALU.mult, op1=ALU.add),
                         tA.rf() + [rw_r], tA.rf())
                self.dve(lambda e: e.tensor_tensor(out=tA.f(), in0=tA.f(), in1=BON.h()[:, sl], op=ALU.add), tA.rf() + BON.rh(tt * 512, (tt + 1) * 512), tA.rf())
                self.dve(lambda e: e.tensor_tensor(out=yv[:, hp, sl], in0=tA.f(), in1=G.h()[:, sl], op=ALU.mult), tA.rf() + G.rh(tt * 512, (tt + 1) * 512),
                         yrwT.rh(hp * T + tt * 512, hp * T + (tt + 1) * 512))

            for tt in range(4):
                prep(tt)
            if cut == 2:
                return
            if cut == 3:
                aphase(range(0, 4))
                return
            if cut == 8:
                aphase(range(0, 4))
                self.dve(lambda e: e.tensor_copy(out=tA.f(), in_=ALL.h(0, 512)), ALL.rh(0, 512), tA.rf())
                self.load(self.dbg[0:128, 0:512], tA.f(), tA.rf(), [self.out_res])
                self.dve(lambda e: e.tensor_copy(out=tB.f(), in_=ALL.h(512, 1024)), ALL.rh(512, 1024), tB.rf())
                self.load(self.dbg[0:128, 512:1024], tB.f(), tB.rf(), [self.out_res])
                self.nodump = True
                return
            if cut == 5:
                aphase(range(0, 4))
                aphase(range(4, 8))
                return
            if cut == 6:
                aphase(range(0, 4))
                seq(0)
                return
            if cut == 7:
                aphase(range(0, 4))
                for cidx in range(0, 4):
                    seq(cidx)
                return
            if cut == 4:
                aphase(range(0, 4))
                aphase(range(4, 8))
                for cidx in range(0, 4):
                    seq(cidx)
                post(0)
                return
            for half in range(2):
                for q4 in range(4):
                    aphase(range(half * 8 + q4 * 2, half * 8 + q4 * 2 + 2))
                for cidx in range(half * 8, half * 8 + 8):
                    seq(cidx)
                    if cidx % 4 == 3:
                        post(cidx // 4)

        for hp in range(4):
            run_hp(hp)
            if cut >= 2:
                return

    def mla(self, b, hT, ymlaT, scr):
        c = self.c
        w_in_v = self.wd["w_in"].rearrange("(c p) n -> p c n", p=128)
        hTv = hT.h().rearrange("p (c t) -> p c t", c=8)
        ones, ones_r = c["allones"]
        fq, fq_r = c["rope_fq"]
        ph, ph_r = c["rope_ph"]
        m2, m2_r = self.mask2, self.mask2_r
        yv = ymlaT.h().rearrange("p (c t) -> p c t", c=8)

        TAB = scr.get(6)
        CQN = scr.get(6)
        CKVN = scr.get(4)
        KROT = scr.get(2)
        V = scr.get(9)
        wlat = scr.get(3)
        wv = scr.get(1)
        t1, t2, t3, t4 = [scr.get(1) for _ in range(4)]
        QTs = [scr.get(2), scr.get(2)]
        KTs = [scr.get(2), scr.get(2)]
        PTs = [scr.get(1) for _ in range(3)]
        TABv = TAB.h().rearrange("p (k t) -> p k t", k=3)
        CQNv = CQN.h().rearrange("p (k t) -> p k t", k=3)
        CKVNv = CKVN.h().rearrange("p (k t) -> p k t", k=2)
        Vv = V.h(0, 16 * 8 * 66).rearrange("p (i h d) -> p i h d", i=16, h=8)

        for tt in range(4):
            sl = slice(tt * 512, (tt + 1) * 512)
            self.load(t1.i(), self.pos[b:b + 1, sl].partition_broadcast(128), [], t1.rf())
            self.dve(lambda e: e.tensor_copy(out=t2.f(), in_=t1.i()), t1.rf(), t2.rf())
            for k in range(3):
                self.dve(lambda e, k=k: e.tensor_scalar(out=t3.f(), in0=t2.f(), scalar1=fq[:, k:k + 1], scalar2=ph[:, k:k + 1], op0=ALU.mult, op1=ALU.add),
                         t2.rf() + [fq_r, ph_r], t3.rf())
                self.dve(lambda e: e.tensor_copy(out=t4.i(), in_=t3.f()), t3.rf(), t4.rf())
                self.dve(lambda e: e.tensor_copy(out=t1.f(), in_=t4.i()), t4.rf(), t1.rf())
                self.dve(lambda e: e.tensor_tensor(out=t3.f(), in0=t3.f(), in1=t1.f(), op=ALU.subtract), t3.rf() + t1.rf(), t3.rf())
                self.dve(lambda e: e.tensor_single_scalar(out=t1.f(), in_=t3.f(), scalar=0.5, op=ALU.is_gt), t3.rf(), t1.rf())
                self.dve(lambda e: e.tensor_tensor(out=t3.f(), in0=t3.f(), in1=t1.f(), op=ALU.subtract), t3.rf() + t1.rf(), t3.rf())
                self.dve(lambda e: e.tensor_single_scalar(out=t1.f(), in_=t3.f(), scalar=-0.5, op=ALU.is_lt), t3.rf(), t1.rf())
                self.dve(lambda e: e.tensor_tensor(out=t3.f(), in0=t3.f(), in1=t1.f(), op=ALU.add), t3.rf() + t1.rf(), t3.rf())
                self.act(lambda e, k=k, sl=sl: e.activation(out=TABv[:, k, sl], in_=t3.f(), func=AF.Sin, scale=6.283185), t3.rf(),
                         TAB.rh(k * T + tt * 512, k * T + (tt + 1) * 512))

        def latent(col0, nchunk, gname, OUT, OUTv):
            gq, gq_r = self.pv[gname]
            wl = wlat.h(0, 8 * nchunk * 128).rearrange("p (c n) -> p c n", c=8)
            self.loadw(wl, w_in_v[:, :, col0:col0 + nchunk * 128], [], wlat.rh(0, 8 * nchunk * 128))
            tmps = [t1, t2, t3]
            for tt in range(4):
                sl = slice(tt * 512, (tt + 1) * 512)
                pss, rss = self.ps()
                for j in range(nchunk):
                    pt, pr = self.ps()
                    for cc in range(8):
                        self.mm(pt[:, :], wl[:, cc, j * 128:(j + 1) * 128], hTv[:, cc, sl], cc == 0, cc == 7,
                                wlat.rh(0, 8 * nchunk * 128) + hT.rh(cc * T + tt * 512, cc * T + (tt + 1) * 512), [pr])
                    tj = tmps[j]
                    self.act(lambda e, pt=pt, tj=tj: e.copy(out=tj.f(), in_=pt[:, :]), [pr], tj.rf())
                    self.act(lambda e, tj=tj: e.activation(out=t4.f(), in_=tj.f(), func=AF.Square), tj.rf(), t4.rf())
                    self.mm(pss[:, :], ones[:], t4.f(), j == 0, j == nchunk - 1, [ones_r] + t4.rf(), [rss])
                self.dve(lambda e, pss=pss: e.tensor_scalar(out=t4.f(), in0=pss[:, :], scalar1=1.0 / (nchunk * 128), scalar2=1e-6, op0=ALU.mult, op1=ALU.add),
                         [rss], t4.rf())
                self.act(lambda e: e.activation(out=t4.f(), in_=t4.f(), func=AF.Ln), t4.rf(), t4.rf())
                self.act(lambda e: e.activation(out=t4.f(), in_=t4.f(), func=AF.Exp, scale=-0.5), t4.rf(), t4.rf())
                for j in range(nchunk):
                    tj = tmps[j]
                    self.dve(lambda e, j=j, tj=tj, sl=sl: e.scalar_tensor_tensor(out=OUTv[:, j, sl], in0=tj.f(), scalar=gq[:, j:j + 1], in1=t4.f(), op0=ALU.mult, op1=ALU.mult),
                             tj.rf() + t4.rf() + [gq_r], OUT.rh(j * T + tt * 512, j * T + (tt + 1) * 512))

        latent(CQ0, 3, "pv_gq", CQN, CQNv)
        latent(CKV0, 2, "pv_gkv", CKVN, CKVNv)

        wkr = wlat.h(0, 8 * 256).rearrange("p (c n) -> p c n", c=8)
        self.loadw(wkr, self.wd["w_kr"].rearrange("(c p) n -> p c n", p=128), [], wlat.rh(0, 8 * 256))
        for tt in range(4):
            sl = slice(tt * 512, (tt + 1) * 512)
            pA, rA = self.ps()
            pB, rB = self.ps()
            for cc in range(8):
                rd = wlat.rh(0, 8 * 256) + hT.rh(cc * T + tt * 512, cc * T + (tt + 1) * 512)
                self.mm(pA[:, :], wkr[:, cc, 0:128], hTv[:, cc, sl], cc == 0, cc == 7, rd, [rA])
            for cc in range(8):
                rd = wlat.rh(0, 8 * 256) + hT.rh(cc * T + tt * 512, cc * T + (tt + 1) * 512)
                self.mm(pB[:, :], wkr[:, cc, 128:256], hTv[:, cc, sl], cc == 0, cc == 7, rd, [rB])
            self.dve(lambda e, pA=pA, sl=sl: e.tensor_tensor(out=t1.f(), in0=pA[:, :], in1=TABv[:, 1, sl], op=ALU.mult), [rA] + TAB.rh(T + tt * 512, T + (tt + 1) * 512), t1.rf())
            self.dve(lambda e, pB=pB, sl=sl: e.tensor_tensor(out=t2.f(), in0=pB[:, :], in1=TABv[:, 2, sl], op=ALU.mult), [rB] + TAB.rh(2 * T + tt * 512, 2 * T + (tt + 1) * 512), t2.rf())
            self.dve(lambda e, sl=sl: e.tensor_tensor(out=KROT.h()[:, sl], in0=t1.f(), in1=t2.f(), op=ALU.add), t1.rf() + t2.rf(), KROT.rh(tt * 512, (tt + 1) * 512))

        wvv = wv.h(0, 1024).rearrange("p (c n) -> p c n", c=2)
        self.loadw(wvv, self.wd["w_v"].rearrange("(c p) n -> p c n", p=128), [], wv.rh())
        self.dve(lambda e: e.memset(Vv[:, :, :, 64:66], 1.0), [], V.rh())
        for i in range(16):
            pt, pr = self.ps()
            for kc in range(2):
                self.mm(pt[:, :], CKVNv[:, kc, i * 128:(i + 1) * 128], wvv[:, kc, :], kc == 0, kc == 1,
                        CKVN.rh(kc * T + i * 128, kc * T + (i + 1) * 128) + wv.rh(), [pr])
            self.act(lambda e, pt=pt, i=i: e.copy(out=Vv[:, i, :, 0:64], in_=pt[:, :].rearrange("p (h d) -> p h d", h=8)), [pr], V.rh())

        def head(h):
            QT, KT = QTs[h % 2], KTs[h % 2]
            wq = wlat.h(0, 384).rearrange("p (c n) -> p c n", c=3)
            wk = wlat.h(1024, 1024 + 256).rearrange("p (c n) -> p c n", c=2)
            self.loadw(wq, self.wd["w_q"][:, h * 128:(h + 1) * 128].rearrange("(c p) n -> p c n", p=128), [], wlat.rh(0, 384))
            self.loadw(wk, self.wd["w_k"][:, h * 128:(h + 1) * 128].rearrange("(c p) n -> p c n", p=128), [], wlat.rh(1024, 1280))
            for tt in range(4):
                sl = slice(tt * 512, (tt + 1) * 512)
                pt, pr = self.ps()
                for cc in range(3):
                    self.mm(pt[:, :], wq[:, cc, :], CQNv[:, cc, sl], cc == 0, cc == 2, wlat.rh(0, 384) + CQN.rh(cc * T + tt * 512, cc * T + (tt + 1) * 512), [pr])
                self.dve(lambda e, pt=pt, sl=sl: e.tensor_tensor(out=QT.h()[:, sl], in0=pt[:, :], in1=TABv[:, 0, sl], op=ALU.mult),
                         [pr] + TAB.rh(tt * 512, (tt + 1) * 512), QT.rh(tt * 512, (tt + 1) * 512))
                pt, pr = self.ps()
                for cc in range(2):
                    self.mm(pt[:, :], wk[:, cc, :], CKVNv[:, cc, sl], cc == 0, cc == 1, wlat.rh(1024, 1280) + CKVN.rh(cc * T + tt * 512, cc * T + (tt + 1) * 512), [pr])
                self.dve(lambda e, pt=pt, sl=sl: e.tensor_tensor(out=KT.h()[:, sl], in0=pt[:, :], in1=KROT.h()[:, sl], op=ALU.add),
                         [pr] + KROT.rh(tt * 512, (tt + 1) * 512), KT.rh(tt * 512, (tt + 1) * 512))
            iters = [(qt, kb) for qt in range(4) for kb in range(4 * qt + 4)]
            pos_ = {}
            pts_ = {}
            deferred = []

            def stage_s(n):
                qt, kb = iters[n]
                if kb == 0:
                    pos_[qt] = self.ps(hold=True)
                c0 = max(0, kb - 4 * qt) * 128
                psc, rsc = self.ps()
                self.mm(psc[:, c0:512], KT.h()[:, kb * 128:(kb + 1) * 128], QT.h()[:, qt * 512 + c0:(qt + 1) * 512], True, True,
                        KT.rh(kb * 128, (kb + 1) * 128) + QT.rh(qt * 512, (qt + 1) * 512), [rsc])
                PT = PTs[n % 3]
                pts_[n] = PT
                self.act(lambda e, psc=psc, PT=PT, c0=c0: e.activation(out=PT.h()[:, c0:512], in_=psc[:, c0:512], func=AF.Exp, scale=ATT_SCALE), [rsc], PT.rh())
                if kb >= 4 * qt:
                    self.dve(lambda e, PT=PT, c0=c0: e.tensor_tensor(out=PT.h()[:, c0:c0 + 128], in0=PT.h()[:, c0:c0 + 128], in1=m2[:, 128:256], op=ALU.mult),
                             PT.rh() + [m2_r], PT.rh())

            def epi1(qt):
                po, ro = pos_[qt]
                self.act(lambda e, po=po: e.copy(out=t1.f()[64:65, :], in_=po[64:65, :]), [ro], t1.rf())
                self.dve(lambda e: e.reciprocal(out=t1.f()[64:65, :], in_=t1.f()[64:65, :]), t1.rf(), t1.rf())

            def epi2(qt):
                po, ro = pos_[qt]
                pb_, rb_ = self.ps()
                self.mm(pb_[0:64, :], ones[64:65, 0:64], t1.f()[64:65, :], True, True, [ones_r] + t1.rf(), [rb_])
                self.act(lambda e, pb_=pb_: e.copy(out=t2.f()[0:64, :], in_=pb_[0:64, :]), [rb_], t2.rf())
                self.dve(lambda e, po=po, qt=qt: e.tensor_tensor(out=yv[0:64, h, qt * 512:(qt + 1) * 512], in0=po[0:64, :], in1=t2.f()[0:64, :], op=ALU.mult),
                         [ro] + t2.rf(), ymlaT.rh(h * T + qt * 512, h * T + (qt + 1) * 512))
                self.ps_release(po)

            def stage_p(n):
                qt, kb = iters[n]
                nkb = 4 * qt + 4
                c0 = max(0, kb - 4 * qt) * 128
                po, ro = pos_[qt]
                PT = pts_[n]
                self.mm(po[0:65, c0:512], Vv[:, kb, h, 0:65], PT.h()[:, c0:512], kb == 0, kb == nkb - 1, V.rh() + PT.rh(), [ro])
                if kb == nkb - 1:
                    deferred.append([1, epi1, qt])
                    deferred.append([5, epi2, qt])

            SK = 2
            for idx in range(len(iters) + SK):
                if idx < len(iters):
                    stage_s(idx)
                if idx - SK >= 0:
                    stage_p(idx - SK)
                for d in list(deferred):
                    d[0] -= 1
                    if d[0] <= 0:
                        d[1](d[2])
                        deferred.remove(d)
            for d in list(deferred):
                d[1](d[2])

        for h in range(8):
            head(h)

    def merge(self, b, hT, yrwT, ymlaT, scr, scr2, scr3):
        c = self.c
        w_in_v = self.wd["w_in"].rearrange("(c p) n -> p c n", p=128)
        hTv = hT.h().rearrange("p (c t) -> p c t", c=8)
        yrv = yrwT.h().rearrange("p (c t) -> p c t", c=4)
        ymv = ymlaT.h().rearrange("p (c t) -> p c t", c=8)
        g2, g2_r = self.pv["pv_g2"]
        idf, idf_r = c["ident"]
        MT = scr.get(16)
        MTv = MT.h().rearrange("p (c t) -> p c t", c=8)
        wgr, wgm, wbr, wbm = [scr.get(1) for _ in range(4)]
        t1, t2 = scr.get(1), scr.get(1)
        wgrv = wgr.h().rearrange("p (c n) -> p c n", c=8)
        wgmv = wgm.h().rearrange("p (c n) -> p c n", c=8)
        wbrv = wbr.h(0, 512).rearrange("p (c n) -> p c n", c=4)
        wbmv = wbm.h().rearrange("p (c n) -> p c n", c=8)
        for j in range(8):
            js = slice(j * 128, (j + 1) * 128)
            self.loadw(wgrv, w_in_v[:, :, GRW0 + j * 128:GRW0 + (j + 1) * 128], [], wgr.rh())
            self.loadw(wgmv, w_in_v[:, :, GML0 + j * 128:GML0 + (j + 1) * 128], [], wgm.rh())
            self.loadw(wbrv, self.wd["w_brw"][:, js].rearrange("(c p) n -> p c n", p=128), [], wbr.rh())
            self.loadw(wbmv[0:64], self.wd["w_bml"][:, js].rearrange("(h p) n -> p h n", p=64), [], wbm.rh())
            for tt in range(4):
                sl = slice(tt * 512, (tt + 1) * 512)
                for (wg, wgreg, tdst) in ((wgrv, wgr, t1), (wgmv, wgm, t2)):
                    pg, rg = self.ps()
                    for cc in range(8):
                        self.mm(pg[:, :], wg[:, cc, :], hTv[:, cc, sl], cc == 0, cc == 7, wgreg.rh() + hT.rh(cc * T + tt * 512, cc * T + (tt + 1) * 512), [rg])
                    self.act(lambda e, pg=pg, tdst=tdst: e.activation(out=tdst.f(), in_=pg[:, :], func=AF.Tanh, scale=0.5), [rg], tdst.rf())
                pb1, rb1 = self.ps()
                for cc in range(4):
                    self.mm(pb1[:, :], wbrv[:, cc, :], yrv[:, cc, sl], cc == 0, cc == 3, wbr.rh() + yrwT.rh(cc * T + tt * 512, cc * T + (tt + 1) * 512), [rb1])
                self.dve(lambda e, pb1=pb1: e.scalar_tensor_tensor(out=t1.f(), in0=t1.f(), scalar=1.0, in1=pb1[:, :], op0=ALU.add, op1=ALU.mult), [rb1] + t1.rf(), t1.rf())
                pb2, rb2 = self.ps()
                for hh in range(8):
                    self.mm(pb2[:, :], wbmv[0:64, hh, :], ymv[0:64, hh, sl], hh == 0, hh == 7, wbm.rh() + ymlaT.rh(hh * T + tt * 512, hh * T + (tt + 1) * 512), [rb2])
                self.dve(lambda e, pb2=pb2: e.scalar_tensor_tensor(out=t2.f(), in0=t2.f(), scalar=1.0, in1=pb2[:, :], op0=ALU.add, op1=ALU.mult), [rb2] + t2.rf(), t2.rf())
                self.dve(lambda e: e.tensor_tensor(out=t1.f(), in0=t1.f(), in1=t2.f(), op=ALU.add), t1.rf() + t2.rf(), t1.rf())
                self.act(lambda e, j=j, sl=sl: e.mul(out=MTv[:, j, sl], in_=t1.f(), mul=0.5), t1.rf(), MT.rh(j * T + tt * 512, j * T + (tt + 1) * 512))

        wo = scr.get(8)
        wov = wo.h().rearrange("p (c n) -> p c n", c=8)
        self.loadw(wov, self.wd["w_out"].rearrange("(c p) n -> p c n", p=128), [], wo.rh())
        wrt = scr.get(1)
        wrtv = wrt.f(0, 8 * 36).rearrange("p (c n) -> p c n", c=8)
        self.load(wrtv, self.wd["w_rt"].rearrange("(c p) n -> p c n", p=128), [], wrt.rf())
        xts = [scr2.get(2), scr2.get(2)]
        h2f = scr2.get(2)
        h2Tf = scr2.get(2)
        junk = scr.get(1)
        ACCs = [scr3.get(2), scr3.get(2)]
        gff = scr3.get(2)
        self.load(gff.f(), self.wd["g_ffn"].partition_broadcast(128), [], gff.rf())
        h2gs = [scr3.get(1), scr3.get(1)]
        MM, MM_r, SRf, SRf_r, SR, SR_r, W12, W12_r = self.MM, self.MM_r, self.SRf, self.SRf_r, self.SR, self.SR_r, self.W12, self.W12_r
        msu, msu_r = c["mask_su"]
        ones, ones_r = c["allones"]
        ecap, ecap_r = c["ecap"]
        if b == 0:
            self.tok_init = self.load(self.tokd.rearrange("(p f) o -> p (f o)", p=128), self.ZI[:], [self.ZI_r], [self.tok_r])
            self.scat_ops = []
        tok_init = self.tok_init
        h2Tfv = h2Tf.f().rearrange("p (c t) -> p c t", c=8)
        ssq, ssq_r, rs, rs_r = self.ssq, self.ssq_r, self.rs, self.rs_r
        LG, LG_r = self.LG, self.LG_r
        brt, brt_r = self.brt, self.brt_r
        R8, R8_r = self.R8, self.R8_r
        for i in range(16):
            xt = xts[i % 2]
            row0 = b * T + i * 128
            gi_ = b * 16 + i
            ACCt = ACCs[i % 2]
            self.load(xt.f(), self.x[row0:row0 + 128, :], [], xt.rf())
            for half in range(2):
                hs = slice(half * 512, (half + 1) * 512)
                pt, pr = self.ps()
                for cc in range(8):
                    self.mm(pt[:, :], MTv[:, cc, i * 128:(i + 1) * 128], wov[:, cc, hs], cc == 0, cc == 7,
                            MT.rh(cc * T + i * 128, cc * T + (i + 1) * 128) + wo.rh(), [pr])
                self.dve(lambda e, pt=pt, hs=hs, xt=xt, ACCt=ACCt: e.tensor_tensor(out=ACCt.f()[:, hs], in0=pt[:, :], in1=xt.f()[:, hs], op=ALU.add),
                         [pr] + xt.rf(), ACCt.rf(half * 512, (half + 1) * 512))
            ai = ACCt.rf()
            self.load(self.x1s[row0:row0 + 128, :], ACCt.f(), ai, [self.x1_r[gi_]])
            self.dve(lambda e, i=i: e.memset(ssq[:, i:i + 1], 0.0), [], [ssq_r])
            self.act(lambda e, i=i, ACCt=ACCt: e.activation(out=junk.h(), in_=ACCt.f(), func=AF.Square, accum_out=ssq[:, i:i + 1]), ai + [ssq_r], junk.rh() + [ssq_r])
            self.dve(lambda e, i=i: e.tensor_scalar(out=rs[:, i:i + 1], in0=ssq[:, i:i + 1], scalar1=1.0 / D, scalar2=1e-6, op0=ALU.mult, op1=ALU.add), [ssq_r], [rs_r])
            self.act(lambda e, i=i: e.activation(out=rs[:, i:i + 1], in_=rs[:, i:i + 1], func=AF.Ln), [rs_r], [rs_r])
            self.act(lambda e, i=i: e.activation(out=rs[:, i:i + 1], in_=rs[:, i:i + 1], func=AF.Exp, scale=-0.5), [rs_r], [rs_r])
            self.dve(lambda e, i=i, ACCt=ACCt: e.tensor_scalar_mul(out=h2f.f(), in0=ACCt.f(), scalar1=rs[:, i:i + 1]), ai + [rs_r], h2f.rf())
            h2g = h2gs[i % 2]
            self.dve(lambda e, h2g=h2g: e.tensor_tensor(out=h2g.h(), in0=h2f.f(), in1=gff.f(), op=ALU.mult), h2f.rf() + gff.rf(), h2g.rh())
            self.load(self.h2s[row0:row0 + 128, :], h2g.h(), h2g.rh(), [self.h2s_r[gi_]])
            for grp in range(2):
                pt, pr = self.ps()
                for q in range(4):
                    cc = grp * 4 + q
                    self.pe(lambda e, pt=pt, q=q, cc=cc: e.transpose(out=pt[:, q * 128:(q + 1) * 128], in_=h2f.f(cc * 128, (cc + 1) * 128), identity=idf[:]),
                            h2f.rf() + [idf_r], [pr])
                self.dve(lambda e, pt=pt, grp=grp: e.tensor_tensor(out=h2Tfv[:, grp * 4:(grp + 1) * 4, :], in0=pt[:, :].rearrange("p (c t) -> p c t", c=4),
                                                                 in1=g2[:, grp * 4:(grp + 1) * 4].unsqueeze(2).broadcast_to([128, 4, 128]), op=ALU.mult),
                         [pr, g2_r], h2Tf.rf())
            pl, rl = self.ps()
            for cc in range(8):
                self.mm(pl[:, 0:36], h2Tfv[:, cc, :], wrtv[:, cc, :], cc == 0, cc == 7, h2Tf.rf() + wrt.rf(), [rl])
            self.dve(lambda e, pl=pl, i=i: e.tensor_tensor(out=LG[:, i, :], in0=pl[:, 0:36], in1=brt[:], op=ALU.add), [rl, brt_r], [LG_r])
            lg4 = LG[:, i, 0:4]
            fine = LG[:, i, 4:36]
            r8 = R8
            self.dve(lambda e, lg4=lg4: e.reduce_max(out=r8[:, 0:1], in_=lg4, axis=AX.X), [LG_r], [R8_r])
            self.dve(lambda e, lg4=lg4: e.tensor_scalar(out=r8[:, 8:12], in0=lg4, scalar1=r8[:, 0:1], scalar2=None, op0=ALU.is_equal), [LG_r, R8_r], [R8_r])
            self.dve(lambda e: e.tensor_scalar_mul(out=r8[:, 1:2], in0=r8[:, 0:1], scalar1=-1.0), [R8_r], [R8_r])
            self.dve(lambda e: e.memset(r8[:, 2:3], 0.0), [], [R8_r])
            self.act(lambda e, lg4=lg4: e.activation(out=r8[:, 12:16], in_=lg4, func=AF.Exp, bias=r8[:, 1:2], accum_out=r8[:, 2:3]), [LG_r, R8_r], [R8_r])
            self.dve(lambda e: e.reciprocal(out=r8[:, 3:4], in_=r8[:, 2:3]), [R8_r], [R8_r])
            self.dve(lambda e: e.tensor_scalar(out=r8[:, 16:48].rearrange("p (g k) -> p g k", g=4), in0=r8[:, 8:12].unsqueeze(2).broadcast_to([128, 4, 8]),
                                               scalar1=-1.0, scalar2=1e30, op0=ALU.add, op1=ALU.mult), [R8_r], [R8_r])
            self.dve(lambda e, fine=fine: e.tensor_tensor(out=r8[:, 16:48], in0=r8[:, 16:48], in1=fine, op=ALU.add), [R8_r, LG_r], [R8_r])
            self.dve(lambda e: e.max(out=r8[:, 48:56], in_=r8[:, 16:48]), [R8_r], [R8_r])
            self.dve(lambda e: e.tensor_tensor(out=r8[:, 4:5], in0=r8[:, 49:50], in1=r8[:, 48:49], op=ALU.subtract), [R8_r], [R8_r])
            self.act(lambda e: e.activation(out=r8[:, 4:5], in_=r8[:, 4:5], func=AF.Exp), [R8_r], [R8_r])
            self.dve(lambda e: e.tensor_scalar_add(out=r8[:, 4:5], in0=r8[:, 4:5], scalar1=1.0), [R8_r], [R8_r])
            self.dve(lambda e: e.reciprocal(out=r8[:, 4:5], in_=r8[:, 4:5]), [R8_r], [R8_r])
            self.dve(lambda e: e.tensor_tensor(out=r8[:, 5:6], in0=r8[:, 4:5], in1=r8[:, 3:4], op=ALU.mult), [R8_r], [R8_r])
            self.dve(lambda e: e.tensor_tensor(out=r8[:, 6:7], in0=r8[:, 3:4], in1=r8[:, 5:6], op=ALU.subtract), [R8_r], [R8_r])
            self.dve(lambda e: e.tensor_scalar(out=r8[:, 56:88], in0=r8[:, 16:48], scalar1=r8[:, 48:49], scalar2=None, op0=ALU.is_equal), [R8_r], [R8_r])
            self.dve(lambda e: e.tensor_scalar(out=r8[:, 88:120], in0=r8[:, 16:48], scalar1=r8[:, 49:50], scalar2=None, op0=ALU.is_equal), [R8_r], [R8_r])
            self.dve(lambda e, gi_=gi_: e.tensor_copy(out=W12[:, gi_, :], in_=r8[:, 5:7]), [R8_r], [W12_r])
            self.dve(lambda e, gi_=gi_: e.tensor_tensor(out=MM[:, gi_, :], in0=r8[:, 56:88], in1=r8[:, 88:120], op=ALU.add), [R8_r], [MM_r])
            pR, rR = self.ps()
            for ip in range(gi_):
                self.mm(pR[:, 0:32], ones[:], MM[:, ip, :], ip == 0, False, [ones_r, MM_r], [rR])
            self.mm(pR[:, 0:32], msu[:], MM[:, gi_, :], gi_ == 0, True, [msu_r, MM_r], [rR])
            self.dve(lambda e, pR=pR: e.tensor_tensor(out=r8[:, 120:152], in0=pR[:, 0:32], in1=ecap[:], op=ALU.add), [rR, ecap_r], [R8_r])
            for k in range(2):
                self.dve(lambda e, k=k: e.tensor_tensor(out=r8[:, 152:184], in0=r8[:, 56 + 32 * k:88 + 32 * k], in1=r8[:, 120:152], op=ALU.mult), [R8_r], [R8_r])
                self.dve(lambda e, k=k, gi_=gi_: e.reduce_sum(out=SRf[:, gi_, k:k + 1], in_=r8[:, 152:184], axis=AX.X), [R8_r], [SRf_r])
            self.dve(lambda e, gi_=gi_: e.tensor_copy(out=SR[:, gi_, :], in_=SRf[:, gi_, :]), [SRf_r], [SR_r])
            for k in range(2):
                o = self.p.dma("pool", lambda e, gi_=gi_, k=k: e.indirect_dma_start(
                    out=self.tokd[:, :], out_offset=bass.IndirectOffsetOnAxis(ap=SR[:, gi_, k:k + 1], axis=0),
                    in_=self.TID[:, gi_:gi_ + 1], in_offset=None),
                    [SR_r, self.TID_r], [], extra=[tok_init])
                self.scat_ops.append(o)

    def moe(self, scr):
        idb, idb_r = self.identb, self.identb_r
        wgus = [scr.get(4), scr.get(4)]
        wdns = [scr.get(2), scr.get(2)]
        XGs = [scr.get(1) for _ in range(8)]
        XTs = [scr.get(4), scr.get(4)]
        HTs = [scr.get(1), scr.get(1)]
        YTs = [scr.get(1) for _ in range(4)]
        YKs = [scr.get(1) for _ in range(8)]
        XAs = [scr.get(2) for _ in range(4)]
        gf = scr.get(2)
        self.load(gf.f(), self.wd["g_fin"].partition_broadcast(128), [], gf.rf())
        junk = scr.get(1)
        ots = [scr.get(2) for _ in range(4)]
        ssq, ssq_r, rs, rs_r = self.ssq, self.ssq_r, self.rs, self.rs_r
        t1, t2 = scr.get(1), scr.get(1)
        NBLK = CAP // 128
        SR, SR_r, W12, W12_r = self.SR, self.SR_r, self.W12, self.W12_r

        def loadw_e(ex):
            wgu, wdn = wgus[ex % 2], wdns[ex % 2]
            self.loadw(wgu.h().rearrange("p (c n) -> p c n", c=8), self.wd["w_gu"][ex].rearrange("(c p) n -> p c n", p=128), [], wgu.rh())
            self.loadw(wdn.h().rearrange("p (c n) -> p c n", c=2), self.wd["w_dn"][ex].rearrange("(c p) n -> p c n", p=128), [], wdn.rh())

        loadw_e(0)
        loadw_e(1)
        gi = 0
        yi = 0
        xg_of = {}

        def gather_e(ex):
            nonlocal gi
            lst = []
            for blk in range(NBLK):
                idx, idx_r = self.IDX[gi % 8]
                XG = XGs[gi % 8]
                gi += 1
                r0 = ex * CAP + blk * 128
                self.p.dma("sp", lambda e, idx=idx, r0=r0: e.dma_start(out=idx[:], in_=self.tokd[r0:r0 + 128, :]), [self.tok_r], [idx_r], extra=self.scat_ops)
                self.p.dma("pool", lambda e, idx=idx, XG=XG: e.indirect_dma_start(
                    out=XG.h(), out_offset=None, in_=self.h2s[:, :], in_offset=bass.IndirectOffsetOnAxis(ap=idx[:, 0:1], axis=0)), [idx_r] + self.h2s_r, XG.rh())
                lst.append(XG)
            xg_of[ex] = lst

        gather_e(0)
        gather_e(1)
        for ex in range(32):
            wgu, wdn = wgus[ex % 2], wdns[ex % 2]
            wguv = wgu.h().rearrange("p (c n) -> p c n", c=8)
            wdnv = wdn.h().rearrange("p (c n) -> p c n", c=2)
            XT = XTs[ex % 2]
            XTv = XT.h().rearrange("p (c t) -> p c t", c=8)
            HT = HTs[ex % 2]
            HTv = HT.h(0, 2 * CAP).rearrange("p (c t) -> p c t", c=2)
            for blk in range(NBLK):
                XG = xg_of[ex][blk]
                pt, pr = self.ps()
                ptv = pt[:].bitcast(BF16).rearrange("p (c t) -> p c t", c=8)
                for cc in range(8):
                    self.pe(lambda e, cc=cc, ptv=ptv, XG=XG: e.transpose(out=ptv[:, cc, :], in_=XG.h(cc * 128, (cc + 1) * 128), identity=idb[:]), XG.rh() + [idb_r], [pr])
                self.act(lambda e, ptv=ptv, XTv=XTv, blk=blk: e.copy(out=XTv[:, :, blk * 128:(blk + 1) * 128], in_=ptv), [pr], XT.rh())
            if ex + 2 < 32:
                pass
            for jc in range(2):
                pG, rG = self.ps()
                for cc in range(8):
                    self.mm(pG[:, 0:CAP], wguv[:, cc, jc * 128:(jc + 1) * 128], XTv[:, cc, :], cc == 0, cc == 7, wgu.rh() + XT.rh(), [rG])
                pU, rU = self.ps()
                for cc in range(8):
                    self.mm(pU[:, 0:CAP], wguv[:, cc, 256 + jc * 128:256 + (jc + 1) * 128], XTv[:, cc, :], cc == 0, cc == 7, wgu.rh() + XT.rh(), [rU])
                self.act(lambda e, pG=pG: e.activation(out=t1.f(0, CAP), in_=pG[:, 0:CAP], func=AF.Tanh, scale=0.5), [rG], t1.rf())
                self.dve(lambda e, pG=pG: e.scalar_tensor_tensor(out=t2.f(0, CAP), in0=t1.f(0, CAP), scalar=1.0, in1=pG[:, 0:CAP], op0=ALU.add, op1=ALU.mult), [rG] + t1.rf(), t2.rf())
                self.dve(lambda e, pU=pU, HTv=HTv, jc=jc: e.scalar_tensor_tensor(out=HTv[:, jc, :], in0=t2.f(0, CAP), scalar=0.5, in1=pU[:, 0:CAP], op0=ALU.mult, op1=ALU.mult), [rU] + t2.rf(), HT.rh())
            for blk in range(NBLK):
                YT = YTs[yi % 4]
                yi += 1
                for half in range(2):
                    hs = slice(half * 512, (half + 1) * 512)
                    pY, rY = self.ps()
                    for jc in range(2):
                        self.mm(pY[:, :], HTv[:, jc, blk * 128:(blk + 1) * 128], wdnv[:, jc, hs], jc == 0, jc == 1, HT.rh() + wdn.rh(), [rY])
                    if half == 0:
                        self.act(lambda e, pY=pY, YT=YT, hs=hs: e.copy(out=YT.h()[:, hs], in_=pY[:, :]), [rY], YT.rh())
                    else:
                        self.dve(lambda e, pY=pY, YT=YT, hs=hs: e.tensor_copy(out=YT.h()[:, hs], in_=pY[:, :]), [rY], YT.rh())
                r0 = ex * CAP + blk * 128
                self.load(self.ysd[r0:r0 + 128, :], YT.h(), YT.rh(), [self.ys_r[ex]])
            if ex + 2 < 32:
                gather_e(ex + 2)
                loadw_e(ex + 2)
        ki = 0
        for g in range(32):
            XA = XAs[g % 4]
            self.load(XA.f(), self.x1s[g * 128:(g + 1) * 128, :], [self.x1_r[g]], XA.rf())
            for k in range(2):
                YK = YKs[ki % 8]
                ki += 1
                self.p.dma("pool", lambda e, YK=YK, g=g, k=k: e.indirect_dma_start(
                    out=YK.h(), out_offset=None, in_=self.ysd[:, :], in_offset=bass.IndirectOffsetOnAxis(ap=SR[:, g, k:k + 1], axis=0)), [SR_r] + self.ys_r, YK.rh())
                self.dve(lambda e, YK=YK, g=g, k=k, XA=XA: e.scalar_tensor_tensor(out=XA.f(), in0=YK.h(), scalar=W12[:, g, k:k + 1], in1=XA.f(), op0=ALU.mult, op1=ALU.add),
                         YK.rh() + [W12_r] + XA.rf(), XA.rf())
            i = g % 16
            ot = ots[g % 4]
            self.dve(lambda e, i=i: e.memset(ssq[:, i:i + 1], 0.0), [], [ssq_r])
            self.act(lambda e, i=i, XA=XA: e.activation(out=junk.h(), in_=XA.f(), func=AF.Square, accum_out=ssq[:, i:i + 1]), XA.rf() + [ssq_r], junk.rh() + [ssq_r])
            self.dve(lambda e, i=i: e.tensor_scalar(out=rs[:, i:i + 1], in0=ssq[:, i:i + 1], scalar1=1.0 / D, scalar2=1e-6, op0=ALU.mult, op1=ALU.add), [ssq_r], [rs_r])
            self.act(lambda e, i=i: e.activation(out=rs[:, i:i + 1], in_=rs[:, i:i + 1], func=AF.Ln), [rs_r], [rs_r])
            self.act(lambda e, i=i: e.activation(out=rs[:, i:i + 1], in_=rs[:, i:i + 1], func=AF.Exp, scale=-0.5), [rs_r], [rs_r])
            self.dve(lambda e, i=i, ot=ot, XA=XA: e.scalar_tensor_tensor(out=ot.f(), in0=XA.f(), scalar=rs[:, i:i + 1], in1=gf.f(), op0=ALU.mult, op1=ALU.mult),
                     XA.rf() + [rs_r] + gf.rf(), ot.rf())
            self.load(self.out[g * 128:(g + 1) * 128, :], ot.f(), ot.rf(), [self.out_res])

    def dump_bf16_fm(self, reg, nchunk, scr):
        tmp = scr.get(4)
        v = reg.h().rearrange("p (c t) -> p c t", c=nchunk)
        for cc in range(nchunk):
            self.dve(lambda e, cc=cc: e.tensor_copy(out=tmp.f(), in_=v[:, cc, :]), reg.rh(cc * T, (cc + 1) * T), tmp.rf())
            self.load(self.dbg[cc * 128:(cc + 1) * 128, :], tmp.f(), tmp.rf(), [self.out_res])

    def finish(self):
        fin = self.p.op("sp", lambda e: e.nop(), [self.out_res], [])
        for e in ("pe", "act", "dve", "pool"):
            last = [o for o in self.p.ops[e] if not o.dma]
            if last:
                last[-1].signal = True
                fin.deps.append(last[-1])
        for q in ("sp", "pool"):
            for o in self.p.dlast[q]:
                if o is not None and o is not fin:
                    fin.deps.append(o)
        import os, sys
        if os.environ.get("KFILL", "1") == "1":
            idb = self.identb
            fb = self.psb[7]
            self.p.filler_fn = lambda e: e.matmul(out=fb[:, 0:128], lhsT=idb[:], rhs=idb[:], start=True, stop=True)
            self.p.filler_dep = self.identb_op
        if os.environ.get("KSCHED", "1") == "1":
            self.p.schedule()
            print("NFILL", getattr(self.p, "nfill", 0), file=sys.stderr)
        with self.nc.Block() as block:
            self.p.emit(block)
        self.st.close()
        import sys
        print("ICOUNT", self.p.icount, file=sys.stderr)
        return self.nc


def build(debug=None):
    B = Builder(debug)
    B.setup()
    ar = B.ar
    hT = ar.reg(0, 16)
    yrwT = ar.reg(16, 8)
    ymlaT = ar.reg(24, 16)
    for b in range(NB):
        B.phaseA(b, hT, Bump(ar, 24, 88))
        if debug and debug[0] == "hT":
            B.dump_bf16_fm(hT, 8, Bump(ar, 40, 88))
            return B.finish()
        B.rwkv(b, hT, yrwT, Bump(ar, 24, 88))
        if debug and debug[0] == "rw":
            if not getattr(B, "nodump", False):
                B.dump_bf16_fm(yrwT, 4, Bump(ar, 40, 88))
            return B.finish()
        B.mla(b, hT, ymlaT, Bump(ar, 40, 88))
        if debug and debug[0] == "mla":
            B.dump_bf16_fm(ymlaT, 8, Bump(ar, 40, 88))
            return B.finish()
        B.merge(b, hT, yrwT, ymlaT, Bump(ar, 40, 72), Bump(ar, 72, 80), Bump(ar, 80, 88))
    B.moe(Bump(ar, 0, 88))
    return B.finish()


_NC_CACHE = {}


def _tokid():
    return np.ascontiguousarray((np.arange(32, dtype=np.int32)[None, :] * 128 + np.arange(128, dtype=np.int32)[:, None]).astype(np.int32))


def kernel(**inputs):
    inp = {k: np.asarray(v) for k, v in inputs.items()}
    w = _host_layout(inp)
    consts = _consts()
    if "nc" not in _NC_CACHE:
        _NC_CACHE["nc"] = build()
    nc = _NC_CACHE["nc"]
    x = np.ascontiguousarray(inp["x"], dtype=np.float32)
    pos = np.ascontiguousarray(inp["positions"]).astype(np.int32)
    in_maps = []
    for core in range(NCORES):
        m = {"x": np.ascontiguousarray(x[core * NB:(core + 1) * NB].reshape(NB * T, D)),
             "pos": np.ascontiguousarray(pos[core * NB:(core + 1) * NB])}
        for n in CONST_NAMES:
            m["c_" + n] = consts[n]
        m["tokid"] = _tokid()
        m.update(w)
        in_maps.append(m)
    res = run_bass_kernel_spmd(nc, in_maps, core_ids=list(range(NCORES)))
    out = np.concatenate([np.asarray(r["out"]).reshape(NB, T, D) for r in res.results], axis=0)
    return out.astype(np.float32)
```

```python
import math
import numpy as np
import concourse.bass as bass
import concourse.mybir as mybir
from concourse.bass_utils import run_bass_kernel_spmd
from contextlib import ExitStack

F32 = mybir.dt.float32
BF16 = mybir.dt.bfloat16
I32 = mybir.dt.int32
ALU = mybir.AluOpType
AF = mybir.ActivationFunctionType
AX = mybir.AxisListType

NB = 2
T = 2048
D = 1024
NCORES = 8
RW_COLS = 1792
CQ0 = 1792
CKV0 = 2176
CKR0 = 2432
GRW0 = 2464
GML0 = 3488
CH = 128
NCH = T // CH
CAP = 512
ATT_SCALE = 1.0 / math.sqrt(96.0)
LOGW_C = -math.exp(-0.5)


class Res:
    __slots__ = ("w", "r", "excl")

    def __init__(self, excl=False):
        self.w = None
        self.r = []
        self.excl = excl


class Op:
    __slots__ = ("eng", "fn", "deps", "sdeps", "signal", "seq", "dma", "sem", "semval", "idx", "fin", "cost")

    def __init__(self, eng, fn):
        self.eng = eng
        self.fn = fn
        self.deps = []
        self.sdeps = []
        self.idx = 0
        self.fin = None
        self.cost = None
        self.signal = False
        self.seq = 0
        self.dma = False
        self.sem = None
        self.semval = 0


class Prog:
    ENGS = ("pe", "act", "dve", "pool", "sp")

    def __init__(self, nc, stack, n_dma_sems=10):
        self.nc = nc
        self.ops = {e: [] for e in self.ENGS}
        self.esem = {e: stack.enter_context(nc.semaphore("prog_" + e)) for e in self.ENGS}
        self.dsems, self.dcount, self.dlast = {}, {}, {}
        for q in ("sp", "pool"):
            self.dsems[q] = [stack.enter_context(nc.semaphore(f"dma_{q}_{i}")) for i in range(n_dma_sems)]
            self.dcount[q] = 0
            self.dlast[q] = [None] * n_dma_sems

    def _track(self, op, reads, writes):
        deps = []
        reads = list(reads)
        writes = list(writes)
        for r in reads:
            if r.excl and r not in writes:
                writes.append(r)
        for r in reads:
            if r.w is not None:
                deps.append(r.w)
        for w in writes:
            if w.w is not None:
                deps.append(w.w)
            deps.extend(w.r)
        seen = set()
        for d in deps:
            if d is op or id(d) in seen:
                continue
            seen.add(id(d))
            if (not d.dma) and d.eng == "pe" and op.eng == "pe" and not op.dma:
                op.sdeps.append(d)
                continue
            op.deps.append(d)
            d.signal = True
        for r in reads:
            r.r.append(op)
        for w in writes:
            w.w = op
            w.r = []

    def op(self, eng, fn, reads=(), writes=(), cost=None):
        o = Op(eng, fn)
        self.nidx = getattr(self, "nidx", 0) + 1
        o.idx = self.nidx
        o.cost = cost
        self._track(o, reads, writes)
        self.ops[eng].append(o)
        return o

    def schedule(self, window=64):
        import heapq
        base = {"pe": 0.25, "act": 0.6, "dve": 0.55, "pool": 1.0, "sp": 0.05}
        pend = {e: list(self.ops[e]) for e in self.ENGS}
        out = {e: [] for e in self.ENGS}
        free_at = {e: 0.0 for e in self.ENGS}
        now = 0.0
        events = []
        remaining = sum(len(v) for v in pend.values())
        cnt = 0
        while remaining:
            progressed = False
            for e in self.ENGS:
                if free_at[e] > now or not pend[e]:
                    continue
                best = None
                for o in pend[e][:window]:
                    ok = True
                    for d in o.deps:
                        if d.fin is None or d.fin > now:
                            ok = False
                            break
                    if ok:
                        for d in o.sdeps:
                            if d.fin is None:
                                ok = False
                                break
                    if ok:
                        best = o
                        break
                if best is None:
                    fill = getattr(self, "filler_fn", None)
                    fdep = getattr(self, "filler_dep", None)
                    if e == "pe" and fill is not None and fdep is not None and fdep.fin is not None and fdep.fin <= now:
                        fo = Op("pe", fill)
                        fo.deps.append(fdep)
                        fdep.signal = True
                        out[e].append(fo)
                        self.nfill = getattr(self, "nfill", 0) + 1
                        free_at[e] = now + 0.1
                        cnt += 1
                        heapq.heappush(events, (free_at[e], cnt))
                        progressed = True
                    continue
                pend[e].remove(best)
                out[e].append(best)
                remaining -= 1
                progressed = True
                if best.dma:
                    free_at[e] = now + 0.06
                    best.fin = now + 2.5
                else:
                    c = best.cost if best.cost is not None else base[e]
                    free_at[e] = now + c
                    best.fin = now + c + 0.25
                cnt += 1
                heapq.heappush(events, (best.fin, cnt))
                heapq.heappush(events, (free_at[e], cnt))
            if not progressed:
                if not events:
                    raise RuntimeError("scheduler stuck")
                t = heapq.heappop(events)[0]
                now = max(now, t)
        self.ops = out

    def dma(self, q, fn, reads=(), writes=(), extra=()):
        o = Op(q, fn)
        self.nidx = getattr(self, "nidx", 0) + 1
        o.idx = self.nidx
        o.dma = True
        self._track(o, reads, writes)
        for d in extra:
            o.deps.append(d)
            d.signal = True
        n = len(self.dsems[q])
        i = self.dcount[q] % n
        self.dcount[q] += 1
        prev = self.dlast[q][i]
        if prev is not None:
            o.deps.append(prev)
        o.sem = self.dsems[q][i]
        o.semval = (prev.semval if prev is not None else 0) + 16
        self.dlast[q][i] = o
        self.ops[q].append(o)
        return o

    def emit(self, block):
        for e in self.ENGS:
            c = 0
            for o in self.ops[e]:
                if (not o.dma) and o.signal:
                    c += 1
                    o.seq = c

        def run(e):
            def body(eng):
                waited = {}
                self.icount = getattr(self, "icount", {})
                self.icount[e] = 0
                for o in self.ops[e]:
                    self.icount[e] += 1
                    need = {}
                    for d in o.deps:
                        if d.dma:
                            k, v = d.sem, d.semval
                        else:
                            k, v = self.esem[d.eng], d.seq
                        if need.get(k, 0) < v:
                            need[k] = v
                    for k, v in need.items():
                        if waited.get(k, 0) < v:
                            eng.wait_ge(k, v)
                            waited[k] = v
                            self.icount[e] += 1
                    ins = o.fn(eng)
                    if o.dma:
                        ins.then_inc(o.sem, 16)
                    elif o.signal:
                        ins.then_inc(self.esem[e], 1)
            return body

        block.tensor(run("pe"))
        block.scalar(run("act"))
        block.vector(run("dve"))
        block.gpsimd(run("pool"))
        block.sync(run("sp"))


class Reg:
    def __init__(self, ar, blk0, nblk):
        self.ar, self.b0, self.n = ar, blk0, nblk

    def f(self, a=0, b=None):
        b = self.n * 512 if b is None else b
        return self.ar.t[:, self.b0 * 512 + a:self.b0 * 512 + b]

    def h(self, a=0, b=None):
        b = self.n * 1024 if b is None else b
        assert a % 2 == 0 and b % 2 == 0
        return self.ar.t[:, self.b0 * 512 + a // 2:self.b0 * 512 + b // 2].bitcast(BF16)

    def i(self, a=0, b=None):
        b = self.n * 512 if b is None else b
        return self.ar.t[:, self.b0 * 512 + a:self.b0 * 512 + b].bitcast(I32)

    def rf(self, a=0, b=None):
        b = self.n * 512 if b is None else b
        return [self.ar.res[self.b0 + j] for j in range(a // 512, (b - 1) // 512 + 1)]

    def rh(self, a=0, b=None):
        b = self.n * 1024 if b is None else b
        return [self.ar.res[self.b0 + j] for j in range(a // 1024, (b - 1) // 1024 + 1)]

    def sub(self, blk, n):
        assert blk + n <= self.n
        return Reg(self.ar, self.b0 + blk, n)


class Arena:
    def __init__(self, nc, stack, nblk):
        self.t = stack.enter_context(nc.sbuf_tensor("arena", [128, nblk * 512], F32))
        self.res = [Res() for _ in range(nblk)]
        self.nblk = nblk

    def reg(self, blk0, nblk):
        assert blk0 + nblk <= self.nblk, (blk0, nblk)
        return Reg(self, blk0, nblk)


class Bump:
    def __init__(self, ar, lo, hi):
        self.ar, self.lo, self.hi, self.cur = ar, lo, hi, lo

    def get(self, n):
        r = self.ar.reg(self.cur, n)
        self.cur += n
        assert self.cur <= self.hi, ("arena overflow", self.cur, self.hi)
        return r


def _consts():
    c = {}
    c["ident"] = np.eye(128, dtype=np.float32)
    p = np.arange(128)
    c["mask_su"] = (p[:, None] < p[None, :]).astype(np.float32)
    c["mask_iu"] = (p[:, None] <= p[None, :]).astype(np.float32)
    c["mask_sl"] = (p[:, None] > p[None, :]).astype(np.float32)
    blk = (p[:, None] // 64 == p[None, :] // 64).astype(np.float32)
    c["blockmask"] = blk
    c["blockones"] = blk.copy()
    c["blockavg"] = blk / 64.0
    sm = np.ones((128, 512), np.float32)
    sm[:, ::CH] = 0.0
    c["scanmask"] = sm
    c["allones"] = np.ones((128, 128), np.float32)
    c["mhalf"] = np.full((128, 512), -0.5, np.float32)
    inv_freq = (10000.0 ** (-np.arange(0, 32, 2, dtype=np.float32) / 32.0)).astype(np.float32)
    fq = np.zeros((128, 3), np.float32)
    ph = np.full((128, 3), 0.25, np.float32)
    for r in range(64, 128):
        f = inv_freq[r % 16] / (2 * np.pi)
        fq[r, :] = f
    ph[64:96, 0] = 0.25
    ph[96:112, 0] = 0.5
    ph[112:128, 0] = 0.0
    ph[64:128, 1] = 0.25
    ph[64:80, 2] = 0.5
    ph[80:96, 2] = 0.0
    ph[96:112, 2] = 0.5
    ph[112:128, 2] = 0.0
    c["ecap"] = np.tile((np.arange(32, dtype=np.float32) * CAP)[None, :], (128, 1))
    c["rope_fq"] = fq
    c["rope_ph"] = ph
    return c


_CONST_SHAPES = {n: v.shape for n, v in _consts().items()}
CONST_NAMES = ["ident", "mask_su", "mask_iu", "mask_sl", "blockmask", "blockones", "blockavg", "scanmask",
               "allones", "mhalf", "ecap", "rope_fq", "rope_ph"]


def _fm(v, nchunk):
    return np.ascontiguousarray(np.asarray(v, np.float32).reshape(nchunk, 128).T)


def _host_layout(inp):
    w = {}
    w_in = inp["w_in"][0]
    w["w_in"] = w_in
    kr = w_in[:, CKR0:CKR0 + 32]
    krs = np.concatenate([kr[:, 16:32], kr[:, 0:16]], axis=1)
    z64 = np.zeros((D, 64), np.float32)
    w["w_kr"] = np.ascontiguousarray(np.concatenate([z64, kr, kr, z64, krs, krs], axis=1))
    wq = inp["mla_w_q_up"][0]
    wqh = np.zeros((384, 8, 128), np.float32)
    for h in range(8):
        blk = wq[:, h * 96:(h + 1) * 96]
        wqh[:, h, 0:96] = blk
        wqh[:, h, 96:112] = blk[:, 80:96]
        wqh[:, h, 112:128] = blk[:, 64:80]
    w["w_q"] = np.ascontiguousarray(wqh.reshape(384, 1024))
    wkv = inp["mla_w_kv_up"][0]
    wk = np.zeros((256, 8, 128), np.float32)
    wv = np.zeros((256, 8, 64), np.float32)
    for h in range(8):
        wk[:, h, 0:64] = wkv[:, h * 128:h * 128 + 64]
        wv[:, h, :] = wkv[:, h * 128 + 64:h * 128 + 128]
    w["w_k"] = np.ascontiguousarray(wk.reshape(256, 1024))
    w["w_v"] = np.ascontiguousarray(wv.reshape(256, 512))
    w["wa_up"] = np.ascontiguousarray(np.concatenate([inp["rw_w_up"][0], inp["rw_a_up"][0]], axis=0))
    w["g_up"] = np.ascontiguousarray(inp["rw_g_up"][0])
    w["w_brw"] = np.ascontiguousarray(inp["w_branch_rw"][0])
    w["w_bml"] = np.ascontiguousarray(inp["w_branch_mla"][0])
    w["w_out"] = np.ascontiguousarray(inp["w_out"][0])
    w["w_rt"] = np.ascontiguousarray(np.concatenate([inp["moe_w_group"][0], inp["moe_w_router"][0]], axis=1))
    w["b_rt"] = np.ascontiguousarray(np.concatenate([inp["moe_b_group"][0], inp["moe_b_router"][0]])[None, :])
    w["w_gu"] = np.ascontiguousarray(inp["moe_w_gu"][0])
    w["w_dn"] = np.ascontiguousarray(inp["moe_w_down"][0])
    w["pv_mu"] = _fm(inp["rw_mu"][0], 14)
    vecs = [inp["rw_w0"][0], inp["rw_a0"][0], inp["rw_k_k"][0], inp["rw_k_a"][0], inp["rw_r_k"][0].reshape(-1),
            inp["rw_gn_w"][0], inp["rw_gn_b"][0]]
    w["pv_rw"] = np.ascontiguousarray(np.concatenate([_fm(v, 4) for v in vecs], axis=1))
    w["pv_g1"] = _fm(inp["mix_norm_g"][0], 8)
    w["pv_gq"] = _fm(inp["mla_g_qa"][0], 3)
    w["pv_gkv"] = _fm(inp["mla_g_kva"][0], 2)
    w["pv_g2"] = _fm(inp["ffn_norm_g"][0], 8)
    w["g_ffn"] = np.ascontiguousarray(inp["ffn_norm_g"][0][None, :])
    w["g_fin"] = np.ascontiguousarray(inp["final_norm_g"][None, :])
    return w


W_SHAPES = {
    "w_in": [1024, 4512], "w_kr": [1024, 256], "w_q": [384, 1024], "w_k": [256, 1024], "w_v": [256, 512],
    "wa_up": [128, 512], "g_up": [128, 512], "w_brw": [512, 1024], "w_bml": [512, 1024], "w_out": [1024, 1024],
    "w_rt": [1024, 36], "b_rt": [1, 36], "w_gu": [32, 1024, 512], "w_dn": [32, 256, 1024],
    "pv_mu": [128, 14], "pv_rw": [128, 28], "pv_g1": [128, 8], "pv_gq": [128, 3], "pv_gkv": [128, 2],
    "pv_g2": [128, 8], "g_ffn": [1, 1024], "g_fin": [1, 1024],
}


class Builder:
    def __init__(self, debug=None):
        self.debug = debug
        nc = bass.Bass("TRN2", target_bir_lowering=False)
        self.nc = nc
        self.st = ExitStack()
        st = self.st
        self.x = nc.dram_tensor("x", [NB * T, D], F32, kind="ExternalInput").ap()
        self.pos = nc.dram_tensor("pos", [NB, T], I32, kind="ExternalInput").ap()
        self.cd = {n: nc.dram_tensor("c_" + n, list(_CONST_SHAPES[n]), F32, kind="ExternalInput").ap() for n in CONST_NAMES}
        self.wd = {n: nc.dram_tensor(n, s, F32, kind="ExternalInput").ap() for n, s in W_SHAPES.items()}
        self.out = nc.dram_tensor("out", [NB * T, D], F32, kind="ExternalOutput").ap()
        self.tokid = nc.dram_tensor("tokid", [128, 32], I32, kind="ExternalInput").ap()
        self.h2s = nc.dram_tensor("h2s", [NB * T, D], BF16, kind="Internal").ap()
        self.x1s = nc.dram_tensor("x1s", [NB * T, D], F32, kind="Internal").ap()
        self.x1_r = [Res() for _ in range(32)]
        self.ysd = nc.dram_tensor("ysd", [32 * CAP, D], BF16, kind="Internal").ap()
        self.tokd = nc.dram_tensor("tokd", [32 * CAP, 1], I32, kind="Internal").ap()
        self.h2s_r = [Res() for _ in range(32)]
        self.ys_r = [Res() for _ in range(32)]
        self.tok_r = Res()
        self.dbg = None
        if debug is not None:
            self.dbg = nc.dram_tensor("dbg", list(debug[1]), F32, kind="ExternalOutput").ap()
        self.p = Prog(nc, st)
        self.ar = Arena(nc, st, 88)
        self.psb = [st.enter_context(nc.psum_tensor(f"ps{i}", [128, 512], F32)) for i in range(8)]
        self.psr = [Res(excl=True) for _ in range(8)]
        self.psi = 0
        import os as _os
        self.nrot = 7 if _os.environ.get("KFILL", "1") == "1" else 8
        self.small = {}
        self.out_res = Res()

    def sb(self, name, shape, dt=F32):
        t = self.st.enter_context(self.nc.sbuf_tensor("sb_" + name, shape, dt))
        r = Res()
        self.small[name] = (t, r)
        return t, r

    def ps(self, hold=False):
        held = getattr(self, "held", set())
        self.held = held
        nrot = getattr(self, "nrot", 8)
        while self.psi in held:
            self.psi = (self.psi + 1) % nrot
        i = self.psi
        self.psi = (self.psi + 1) % nrot
        if hold:
            held.add(i)
        return self.psb[i], self.psr[i]

    def ps_release(self, t):
        for i in range(8):
            if self.psb[i] is t:
                self.held.discard(i)

    def pe(self, fn, rd, wr):
        return self.p.op("pe", fn, rd, wr)

    def act(self, fn, rd, wr):
        return self.p.op("act", fn, rd, wr)

    def dve(self, fn, rd, wr):
        return self.p.op("dve", fn, rd, wr)

    def pool(self, fn, rd, wr):
        return self.p.op("pool", fn, rd, wr)

    def mm(self, out, lhsT, rhs, start, stop, rd, wr):
        return self.p.op("pe", lambda e: e.matmul(out=out, lhsT=lhsT, rhs=rhs, start=start, stop=stop), rd, wr)

    def loadw(self, dst, src, rd, wr):
        return self.p.dma("pool", lambda e: e.dma_start(out=dst, in_=src), rd, wr)

    def load(self, dst, src, rd, wr):
        return self.p.dma("sp", lambda e: e.dma_start(out=dst, in_=src), rd, wr)

    def setup(self):
        c = {}
        for n in CONST_NAMES:
            t, r = self.sb("k_" + n, list(_CONST_SHAPES[n]))
            self.load(t[:], self.cd[n], [], [r])
            c[n] = (t, r)
        self.c = c
        idb, idb_r = self.sb("identb", [128, 128], BF16)
        self.identb_op = self.dve(lambda e: e.tensor_copy(out=idb[:], in_=c["ident"][0][:]), [c["ident"][1]], [idb_r])
        self.identb, self.identb_r = idb, idb_r
        m2, m2_r = self.sb("mask2", [128, 256])
        self.dve(lambda e: e.tensor_copy(out=m2[:, 0:128], in_=c["mask_su"][0][:]), [c["mask_su"][1]], [m2_r])
        self.dve(lambda e: e.tensor_copy(out=m2[:, 128:256], in_=c["mask_iu"][0][:]), [c["mask_iu"][1]], [m2_r])
        self.mask2, self.mask2_r = m2, m2_r
        pv = {}
        for n in ["pv_mu", "pv_rw", "pv_g1", "pv_gq", "pv_gkv", "pv_g2"]:
            t, r = self.sb("s_" + n, W_SHAPES[n])
            self.load(t[:], self.wd[n], [], [r])
            pv[n] = (t, r)
        self.pv = pv
        omm, omm_r = self.sb("omm", [128, 14])
        self.dve(lambda e: e.tensor_scalar(out=omm[:], in0=pv["pv_mu"][0][:], scalar1=-1.0, scalar2=1.0, op0=ALU.mult, op1=ALU.add),
                 [pv["pv_mu"][1]], [omm_r])
        self.omm, self.omm_r = omm, omm_r
        dv, dv_r = self.sb("rwdv", [128, 12])
        rw = pv["pv_rw"][0]
        self.dve(lambda e: e.tensor_scalar_mul(out=dv[:, 0:8], in0=rw[:, 0:8], scalar1=0.5), [pv["pv_rw"][1]], [dv_r])
        self.dve(lambda e: e.tensor_scalar_mul(out=dv[:, 8:12], in0=rw[:, 12:16], scalar1=-1.0), [pv["pv_rw"][1]], [dv_r])
        self.rwdv, self.rwdv_r = dv, dv_r
        wa_up, wa_up_r = self.sb("wa_up", [128, 512], BF16)
        self.loadw(wa_up[:], self.wd["wa_up"], [], [wa_up_r])
        g_up, g_up_r = self.sb("g_up", [128, 512], BF16)
        self.loadw(g_up[:], self.wd["g_up"], [], [g_up_r])
        self.wa_up, self.wa_up_r, self.g_up, self.g_up_r = wa_up, wa_up_r, g_up, g_up_r
        self.STP, self.STP_r = self.sb("STP", [128, 128])
        self.STB, self.STB_r = self.sb("STB", [128, 128], BF16)
        self.TMS, self.TMS_r = self.sb("TMS", [128, 128])
        self.XB, self.XB_r = self.sb("XB", [128, 128], BF16)
        self.UB, self.UB_r = self.sb("UB", [128, 128], BF16)
        self.UP0, self.UP0_r = self.sb("UP0", [128, 128], BF16)
        self.UP1, self.UP1_r = self.sb("UP1", [128, 128], BF16)
        self.dve(lambda e: e.memset(self.UP0[:], 0.0), [], [self.UP0_r])
        self.dve(lambda e: e.memset(self.UP1[:], 0.0), [], [self.UP1_r])
        self.GC, self.GC_r = self.sb("GC", [128, NCH])
        self.carry = [self.sb(f"carry{i}", [128, 1]) for i in range(3)]
        self.LG, self.LG_r = self.sb("LG", [128, 16, 36])
        self.WT, self.WT_r = self.sb("WT", [128, 16, 32])
        self.R8, self.R8_r = self.sb("R8", [128, 192])
        self.MM, self.MM_r = self.sb("MMem", [128, 32, 32])
        self.SRf, self.SRf_r = self.sb("SRf", [128, 32, 2])
        self.SR, self.SR_r = self.sb("SR", [128, 32, 2], I32)
        self.W12, self.W12_r = self.sb("W12", [128, 32, 2])
        self.TID, self.TID_r = self.sb("TID", [128, 32], I32)
        self.load(self.TID[:], self.tokid, [], [self.TID_r])
        self.ZI, self.ZI_r = self.sb("ZI", [128, 128], I32)
        self.dve(lambda e: e.memset(self.ZI[:], 0), [], [self.ZI_r])
        self.IDX = [self.sb(f"IDX{i}", [128, 1], I32) for i in range(8)]
        self.brt, self.brt_r = self.sb("brt", [128, 36])
        self.load(self.brt[:], self.wd["b_rt"].partition_broadcast(128), [], [self.brt_r])
        self.ssq, self.ssq_r = self.sb("ssq", [128, 16])
        self.rs, self.rs_r = self.sb("rs", [128, 16])

    def phaseA(self, b, hT, scr):
        c = self.c
        xts = [scr.get(2), scr.get(2)]
        junk = scr.get(1)
        hb = scr.get(1)
        g1, g1_r = self.pv["pv_g1"]
        hTv = hT.h().rearrange("p (c t) -> p c t", c=8)
        ssq, ssq_r, rs, rs_r = self.ssq, self.ssq_r, self.rs, self.rs_r
        for i in range(16):
            xt = xts[i % 2]
            row0 = b * T + i * 128
            self.load(xt.f(), self.x[row0:row0 + 128, :], [], xt.rf())
            self.dve(lambda e, i=i: e.memset(ssq[:, i:i + 1], 0.0), [], [ssq_r])
            self.act(lambda e, xt=xt, i=i: e.activation(out=junk.h(), in_=xt.f(), func=AF.Square, accum_out=ssq[:, i:i + 1]),
                     xt.rf() + [ssq_r], junk.rh() + [ssq_r])
            self.dve(lambda e, i=i: e.tensor_scalar(out=rs[:, i:i + 1], in0=ssq[:, i:i + 1], scalar1=1.0 / D, scalar2=1e-6,
                                                    op0=ALU.mult, op1=ALU.add), [ssq_r], [rs_r])
            self.act(lambda e, i=i: e.activation(out=rs[:, i:i + 1], in_=rs[:, i:i + 1], func=AF.Ln), [rs_r], [rs_r])
            self.act(lambda e, i=i: e.activation(out=rs[:, i:i + 1], in_=rs[:, i:i + 1], func=AF.Exp, scale=-0.5), [rs_r], [rs_r])
            self.dve(lambda e, xt=xt, i=i: e.tensor_scalar_mul(out=hb.h(), in0=xt.f(), scalar1=rs[:, i:i + 1]),
                     xt.rf() + [rs_r], hb.rh())
            pt, pr = self.ps()
            ptv = pt[:].bitcast(BF16).rearrange("p (c t) -> p c t", c=8)
            for cc in range(8):
                self.pe(lambda e, cc=cc, ptv=ptv: e.transpose(out=ptv[:, cc, :], in_=hb.h(cc * 128, (cc + 1) * 128), identity=self.identb[:]),
                        hb.rh() + [self.identb_r], [pr])
            wr = []
            for cc in range(8):
                wr += hT.rh(cc * T + i * 128, cc * T + (i + 1) * 128)
            self.dve(lambda e, i=i, ptv=ptv: e.tensor_tensor(out=hTv[:, :, i * 128:(i + 1) * 128], in0=ptv,
                                                            in1=g1[:].unsqueeze(2).broadcast_to([128, 8, 128]), op=ALU.mult),
                     [pr, g1_r], wr)

    def shiftmix(self, sid, wt_ap, wt_res, hT, ci, tt, z, pm):
        mu, mu_r = self.pv["pv_mu"]
        omm, omm_r = self.omm, self.omm_r
        car, car_r = self.carry[sid]
        hTv = hT.h().rearrange("p (c t) -> p c t", c=8)
        pt, pr = self.ps()
        for cc in range(8):
            self.mm(pt[:, :], wt_ap[:, cc, :], hTv[:, cc, tt * 512:(tt + 1) * 512], cc == 0, cc == 7,
                    wt_res + hT.rh(cc * T + tt * 512, cc * T + (tt + 1) * 512), [pr])
        if tt == 0:
            self.dve(lambda e: e.memset(car[:], 0.0), [], [car_r])
        self.act(lambda e: e.activation(out=pm.f(), in_=pt[:, :], func=AF.Copy, scale=mu[:, ci:ci + 1]), [pr, mu_r], pm.rf())
        self.dve(lambda e: e.scalar_tensor_tensor(out=z.f(1, 512), in0=pt[:, 1:512], scalar=omm[:, ci:ci + 1], in1=pm.f(0, 511),
                                                  op0=ALU.mult, op1=ALU.add), [pr, omm_r] + pm.rf(), z.rf())
        self.dve(lambda e: e.scalar_tensor_tensor(out=z.f(0, 1), in0=pt[:, 0:1], scalar=omm[:, ci:ci + 1], in1=car[:, 0:1],
                                                  op0=ALU.mult, op1=ALU.add), [pr, omm_r, car_r], z.rf())
        self.dve(lambda e: e.tensor_copy(out=car[:, 0:1], in_=pm.f(511, 512)), pm.rf(), [car_r])

    def rwkv(self, b, hT, yrwT, scr):
        c = self.c
        w_in = self.wd["w_in"]
        w_in_v = w_in.rearrange("(c p) n -> p c n", p=128)
        rw, rw_r = self.pv["pv_rw"]
        dv, dv_r = self.rwdv, self.rwdv_r
        bones, bones_r = c["blockones"]
        bavg, bavg_r = c["blockavg"]
        smask, smask_r = c["scanmask"]
        bmask, bmask_r = c["blockmask"]
        msl, msl_r = c["mask_sl"]
        idb, idb_r = self.identb, self.identb_r
        m2, m2_r = self.mask2, self.mask2_r
        yv = yrwT.h().rearrange("p (c t) -> p c t", c=4)

        wt = scr.get(1)
        wrkv = scr.get(3)
        WA = scr.get(2)
        SG = scr.get(2)
        AR = scr.get(4)
        BT = scr.get(2)
        KT = scr.get(2)
        BH = scr.get(2)
        KH = scr.get(2)
        VP = scr.get(4)
        G = scr.get(2)
        BON = scr.get(2)
        Rr, Kk0, LOGW, Aa, KK, L, E, tA, tB, PM = [scr.get(1) for _ in range(10)]
        vbt, bht, kht = scr.get(1), scr.get(1), scr.get(1)
        YT = [scr.get(1), scr.get(1)]
        alt = {}
        for nm_, reg_ in (("Rr", Rr), ("Kk0", Kk0), ("tA", tA), ("tB", tB), ("E", E)):
            alt[nm_] = [reg_, scr.get(1)]
        ALL = scr.get(8)
        NSC = scr.get(8)

        wtv = wt.h().rearrange("p (c n) -> p c n", c=8)
        ARv = AR.h().rearrange("p (a t) -> p a t", a=2)
        BHv = BH.h().rearrange("p (c n) -> p c n", c=16)
        KHv = KH.h().rearrange("p (c n) -> p c n", c=16)
        VPv = VP.h().rearrange("p (a c n) -> p a c n", a=2, c=16)

        self.dve(lambda e: e.memset(VP.h(), 0.0), [], VP.rh())

        for ci, which in ((12, "wa"), (13, "sg")):
            col0 = 1536 + (ci - 12) * 128
            self.loadw(wtv, w_in_v[:, :, col0:col0 + 128], [], wt.rh())
            for tt in range(4):
                z = tA
                self.shiftmix(0, wtv, wt.rh(), hT, ci, tt, z, PM)
                sl = slice(tt * 512, (tt + 1) * 512)
                if which == "wa":
                    self.act(lambda e, sl=sl: e.activation(out=WA.h()[0:64, sl], in_=z.f()[0:64, :], func=AF.Tanh), z.rf(), WA.rh(tt * 512, (tt + 1) * 512))
                    self.dve(lambda e, sl=sl: e.tensor_copy(out=WA.h()[64:128, sl], in_=z.f()[64:128, :]), z.rf(), WA.rh(tt * 512, (tt + 1) * 512))
                else:
                    self.act(lambda e: e.activation(out=tB.f(), in_=z.f(), func=AF.Tanh, scale=0.5), z.rf(), tB.rf())
                    self.dve(lambda e, sl=sl: e.tensor_scalar(out=SG.h()[:, sl], in0=tB.f(), scalar1=0.5, scalar2=0.5, op0=ALU.mult, op1=ALU.add),
                             tB.rf(), SG.rh(tt * 512, (tt + 1) * 512))

        import os
        cut = int(os.environ.get("KCUT", "0"))
        if cut == 1:
            return

        def run_hp(hp):
            hs = slice(hp * 128, (hp + 1) * 128)
            w3 = wrkv.h().rearrange("p (j c n) -> p j c n", j=3, c=8)
            for j in range(3):
                col0 = j * 512 + hp * 128
                self.loadw(w3[:, j], w_in_v[:, :, col0:col0 + 128], [], wrkv.rh(j * 1024, (j + 1) * 1024))
            self.dve(lambda e: e.memset(self.STP[:], 0.0), [], [self.STP_r])
            self.dve(lambda e: e.memset(self.STB[:], 0.0), [], [self.STB_r])

            def prep(tt, Rr=Rr, Kk0=Kk0, tA=tA, tB=tB, E=E):
                Rr, Kk0, tA, tB, E = (alt[n_][tt % 2] for n_ in ("Rr", "Kk0", "tA", "tB", "E"))
                sl = slice(tt * 512, (tt + 1) * 512)
                rT = lambda a=tt * 512, bnd=(tt + 1) * 512: None
                self.shiftmix(0, w3[:, 0], wrkv.rh(0, 1024), hT, hp, tt, Rr, PM)
                self.shiftmix(1, w3[:, 1], wrkv.rh(1024, 2048), hT, 4 + hp, tt, Kk0, PM)
                self.shiftmix(2, w3[:, 2], wrkv.rh(2048, 3072), hT, 8 + hp, tt, tA, PM)
                self.act(lambda e: e.copy(out=vbt.h(0, 512), in_=tA.f()), tA.rf(), vbt.rh())
                pt, pr = self.ps()
                self.mm(pt[:, :], self.wa_up[0:64, hs], WA.h()[0:64, sl], True, True, [self.wa_up_r] + WA.rh(tt * 512, (tt + 1) * 512), [pr])
                self.act(lambda e, pt=pt: e.activation(out=tB.f(), in_=pt[:, :], func=AF.Tanh, bias=dv[:, hp:hp + 1], scale=0.5), [pr, dv_r], tB.rf())
                self.dve(lambda e: e.tensor_scalar(out=LOGW.f(), in0=tB.f(), scalar1=0.5 * LOGW_C, scalar2=0.5 * LOGW_C, op0=ALU.mult, op1=ALU.add), tB.rf(), LOGW.rf())
                pt, pr = self.ps()
                self.mm(pt[:, :], self.wa_up[64:128, hs], WA.h()[64:128, sl], True, True, [self.wa_up_r] + WA.rh(tt * 512, (tt + 1) * 512), [pr])
                self.act(lambda e, pt=pt: e.activation(out=tB.f(), in_=pt[:, :], func=AF.Tanh, bias=dv[:, 4 + hp:5 + hp], scale=0.5), [pr, dv_r], tB.rf())
                self.dve(lambda e: e.tensor_scalar(out=Aa.f(), in0=tB.f(), scalar1=0.5, scalar2=0.5, op0=ALU.mult, op1=ALU.add), tB.rf(), Aa.rf())
                pt, pr = self.ps()
                self.mm(pt[:, :], self.g_up[:, hs], SG.h()[:, sl], True, True, [self.g_up_r] + SG.rh(tt * 512, (tt + 1) * 512), [pr])
                self.act(lambda e, pt=pt: e.copy(out=G.h()[:, sl], in_=pt[:, :]), [pr], G.rh(tt * 512, (tt + 1) * 512))
                self.dve(lambda e: e.tensor_scalar_mul(out=tB.f(), in0=Kk0.f(), scalar1=rw[:, 8 + hp:9 + hp]), Kk0.rf() + [rw_r], tB.rf())
                self.act(lambda e: e.activation(out=tA.f(), in_=tB.f(), func=AF.Square), tB.rf(), tA.rf())
                pt, pr = self.ps()
                self.mm(pt[:, :], bones[:], tA.f(), True, True, [bones_r] + tA.rf(), [pr])
                self.act(lambda e, pt=pt: e.copy(out=tA.f(), in_=pt[:, :]), [pr], tA.rf())
                self.act(lambda e: e.activation(out=tA.f(), in_=tA.f(), func=AF.Ln), tA.rf(), tA.rf())
                self.act(lambda e: e.activation(out=tA.f(), in_=tA.f(), func=AF.Exp, scale=-0.5), tA.rf(), tA.rf())
                self.dve(lambda e: e.tensor_tensor(out=KK.f(), in0=tB.f(), in1=tA.f(), op=ALU.mult), tB.rf() + tA.rf(), KK.rf())
                self.dve(lambda e: e.tensor_scalar(out=tA.f(), in0=Aa.f(), scalar1=rw[:, 12 + hp:13 + hp], scalar2=dv[:, 8 + hp:9 + hp],
                                                   op0=ALU.mult, op1=ALU.add), Aa.rf() + [rw_r, dv_r], tA.rf())
                self.dve(lambda e: e.scalar_tensor_tensor(out=Kk0.f(), in0=tA.f(), scalar=1.0, in1=Kk0.f(), op0=ALU.add, op1=ALU.mult),
                         tA.rf() + Kk0.rf(), Kk0.rf())
                self.dve(lambda e: e.tensor_tensor(out=tB.f(), in0=KK.f(), in1=Aa.f(), op=ALU.mult), KK.rf() + Aa.rf(), tB.rf())
                self.dve(lambda e: e.scalar_tensor_tensor(out=tA.f(), in0=Rr.f(), scalar=rw[:, 16 + hp:17 + hp], in1=Kk0.f(), op0=ALU.mult, op1=ALU.mult),
                         Rr.rf() + Kk0.rf() + [rw_r], tA.rf())
                pt, pr = self.ps()
                self.mm(pt[:, :], bones[:], tA.f(), True, True, [bones_r] + tA.rf(), [pr])
                self.dve(lambda e, pt=pt: e.tensor_tensor(out=BON.h()[:, sl], in0=pt[:, :], in1=vbt.h(0, 512), op=ALU.mult), [pr] + vbt.rh(), BON.rh(tt * 512, (tt + 1) * 512))
                self.dve(lambda e: e.tensor_tensor_scan(out=L.f(), data0=smask[:], data1=LOGW.f(), initial=0.0, op0=ALU.mult, op1=ALU.add),
                         [smask_r] + LOGW.rf(), L.rf())
                self.act(lambda e: e.activation(out=E.f(), in_=L.f(), func=AF.Exp), L.rf(), E.rf())
                self.dve(lambda e: e.tensor_tensor(out=ARv[:, 1, sl], in0=Rr.f(), in1=E.f(), op=ALU.mult), Rr.rf() + E.rf(), AR.rh(T + tt * 512, T + (tt + 1) * 512))
                self.dve(lambda e: e.tensor_tensor(out=tA.f(), in0=L.f(), in1=LOGW.f(), op=ALU.subtract), L.rf() + LOGW.rf(), tA.rf())
                self.act(lambda e: e.activation(out=E.f(), in_=tA.f(), func=AF.Exp), tA.rf(), E.rf())
                self.dve(lambda e: e.scalar_tensor_tensor(out=ARv[:, 0, sl], in0=KK.f(), scalar=-1.0, in1=E.f(), op0=ALU.mult, op1=ALU.mult),
                         KK.rf() + E.rf(), AR.rh(tt * 512, (tt + 1) * 512))
                self.act(lambda e: e.activation(out=E.f(), in_=L.f(), func=AF.Exp, scale=-1.0), L.rf(), E.rf())
                self.dve(lambda e: e.tensor_tensor(out=BT.h()[:, sl], in0=tB.f(), in1=E.f(), op=ALU.mult), tB.rf() + E.rf(), BT.rh(tt * 512, (tt + 1) * 512))
                self.dve(lambda e: e.tensor_tensor(out=KT.h()[:, sl], in0=Kk0.f(), in1=E.f(), op=ALU.mult), Kk0.rf() + E.rf(), KT.rh(tt * 512, (tt + 1) * 512))
                Lv = L.f().rearrange("p (c n) -> p c n", c=4)
                self.dve(lambda e: e.tensor_tensor(out=tA.f().rearrange("p (c n) -> p c n", c=4), in0=Lv[:, :, CH - 1:CH].broadcast_to([128, 4, CH]),
                                                   in1=Lv, op=ALU.subtract), L.rf(), tA.rf())
                self.act(lambda e: e.activation(out=E.f(), in_=tA.f(), func=AF.Exp), tA.rf(), E.rf())
                self.act(lambda e: e.activation(out=self.GC[:, tt * 4:(tt + 1) * 4], in_=Lv[:, :, CH - 1], func=AF.Exp), L.rf(), [self.GC_r])
                self.dve(lambda e: e.tensor_tensor(out=bht.h(0, 512), in0=tB.f(), in1=E.f(), op=ALU.mult), tB.rf() + E.rf(), bht.rh())
                self.dve(lambda e: e.tensor_tensor(out=kht.h(0, 512), in0=Kk0.f(), in1=E.f(), op=ALU.mult), Kk0.rf() + E.rf(), kht.rh())
                pt, pr = self.ps()
                ptv = pt[:].bitcast(BF16).rearrange("p (c t) -> p c t", c=8)
                pt2, pr2 = self.ps()
                ptv2 = pt2[:].bitcast(BF16).rearrange("p (c t) -> p c t", c=8)
                for j in range(4):
                    js = slice(j * 128, (j + 1) * 128)
                    self.pe(lambda e, j=j, js=js, ptv=ptv: e.transpose(out=ptv[:, j, :], in_=vbt.h(0, 512)[:, js], identity=idb[:]), vbt.rh() + [idb_r], [pr])
                    self.pe(lambda e, j=j, js=js, ptv=ptv: e.transpose(out=ptv[:, 4 + j, :], in_=bht.h(0, 512)[:, js], identity=idb[:]), bht.rh() + [idb_r], [pr])
                    self.pe(lambda e, j=j, js=js, ptv2=ptv2: e.transpose(out=ptv2[:, j, :], in_=kht.h(0, 512)[:, js], identity=idb[:]), kht.rh() + [idb_r], [pr2])
                cs = slice(tt * 4, (tt + 1) * 4)
                self.act(lambda e, ptv=ptv: e.copy(out=VPv[:, 0, cs, 0:64], in_=ptv[:, 0:4, 0:64]), [pr], VP.rh())
                self.dve(lambda e, ptv=ptv: e.tensor_copy(out=VPv[:, 1, cs, 64:128], in_=ptv[:, 0:4, 64:128]), [pr], VP.rh())
                self.dve(lambda e, ptv=ptv: e.tensor_copy(out=BHv[:, cs, :], in_=ptv[:, 4:8, :]), [pr], BH.rh(tt * 512, (tt + 1) * 512))
                self.act(lambda e, ptv2=ptv2: e.copy(out=KHv[:, cs, :], in_=ptv2[:, 0:4, :]), [pr2], KH.rh(tt * 512, (tt + 1) * 512))

            def unit_regs(u):
                ll = ALL.h(u * 512, (u + 1) * 512)
                llr = ALL.rh(u * 512, (u + 1) * 512)
                s8 = u % 8
                sc = NSC.h(s8 * 1024, (s8 + 1) * 1024)
                scr_ = NSC.rh(s8 * 1024, (s8 + 1) * 1024)
                return ll, llr, sc, scr_

            def aphase(chunks):
                units = [(cidx, hd) for cidx in chunks for hd in range(2)]
                st = {}
                for (cidx, hd) in units:
                    u = (cidx % 8) * 2 + hd
                    ll, llr, sc, scr_ = unit_regs(u)
                    ts = slice(cidx * 128, (cidx + 1) * 128)
                    ps_ = slice(hd * 64, (hd + 1) * 64)
                    rdA = AR.rh(cidx * 128, (cidx + 1) * 128) + AR.rh(T + cidx * 128, T + (cidx + 1) * 128)
                    p1, r1 = self.ps()
                    self.mm(p1[:, 0:256].rearrange("p (a t) -> p a t", a=2), BT.h()[ps_, ts], ARv[ps_, :, ts], True, True, BT.rh(cidx * 128, (cidx + 1) * 128) + rdA, [r1])
                    p2, r2 = self.ps()
                    self.mm(p2[:, 0:256].rearrange("p (a t) -> p a t", a=2), KT.h()[ps_, ts], ARv[ps_, :, ts], True, True, KT.rh(cidx * 128, (cidx + 1) * 128) + rdA, [r2])
                    self.mm(p2[:, 256:384], ARv[ps_, 0, ts], BT.h()[ps_, ts], True, True, BT.rh(cidx * 128, (cidx + 1) * 128) + rdA, [r2])
                    self.dve(lambda e, p1=p1, sc=sc: e.tensor_tensor(out=sc[:, 640:768], in0=p1[:, 0:128], in1=m2[:, 0:128], op=ALU.mult), [r1, m2_r], scr_)
                    self.dve(lambda e, p1=p1, ll=ll: e.tensor_tensor(out=ll[:, 384:512], in0=p1[:, 128:256], in1=m2[:, 128:256], op=ALU.mult), [r1, m2_r], llr)
                    self.dve(lambda e, p2=p2, ll=ll: e.tensor_tensor(out=ll[:, 128:384], in0=p2[:, 0:256], in1=m2[:, :], op=ALU.mult), [r2, m2_r], llr)
                    self.dve(lambda e, p2=p2, sc=sc: e.tensor_tensor(out=sc[:, 0:128], in0=p2[:, 256:384], in1=msl[:], op=ALU.mult), [r2, msl_r], scr_)
                    self.dve(lambda e, ll=ll, sc=sc: e.tensor_tensor(out=ll[:, 0:128], in0=sc[:, 640:768], in1=idb[:], op=ALU.add), scr_ + [idb_r], llr)
                    st[(cidx, hd)] = (ll, llr, sc, scr_)
                def stageA(u_, lvl):
                    ll, llr, sc, scr_ = st[u_]
                    if lvl == 1:
                        Np, NTp = sc[:, 640:768], sc[:, 0:128]
                    else:
                        o = 128 + ((lvl - 1) % 2) * 256
                        Np, NTp = sc[:, o:o + 128], sc[:, o + 128:o + 256]
                    o2 = 128 + (lvl % 2) * 256
                    pn, rn = self.ps()
                    if lvl < 6:
                        self.mm(pn[:, 0:128], NTp, Np, True, True, scr_, [rn])
                    self.mm(pn[:, 128:256], Np, NTp, True, True, scr_, [rn])
                    if lvl < 6:
                        self.act(lambda e, pn=pn, sc=sc, o2=o2: e.copy(out=sc[:, o2:o2 + 256], in_=pn[:, 0:256]), [rn], scr_)
                    else:
                        self.act(lambda e, pn=pn, sc=sc, o2=o2: e.copy(out=sc[:, o2 + 128:o2 + 256], in_=pn[:, 128:256]), [rn], scr_)

                def stageB(u_, lvl):
                    ll, llr, sc, scr_ = st[u_]
                    o2 = 128 + (lvl % 2) * 256
                    pq, rq = self.ps()
                    self.mm(pq[:, 0:128], idb[:], ll[:, 0:128], True, False, [idb_r] + llr, [rq])
                    self.mm(pq[:, 0:128], sc[:, o2 + 128:o2 + 256], ll[:, 0:128], False, True, scr_ + llr, [rq])
                    self.act(lambda e, pq=pq, ll=ll: e.copy(out=ll[:, 0:128], in_=pq[:, 0:128]), [rq], llr)

                SK = 3
                work = [(u_, lvl) for lvl in range(1, 7) for u_ in units]
                for idx in range(len(work) + SK):
                    if idx < len(work):
                        stageA(*work[idx])
                    if idx - SK >= 0:
                        stageB(*work[idx - SK])

            def seq(cidx):
                ts = slice(cidx * 128, (cidx + 1) * 128)
                lls = []
                for hd in range(2):
                    u = (cidx % 8) * 2 + hd
                    ll, llr, _, _ = unit_regs(u)
                    lls.append((ll, llr))
                STP, STB, TMS, XB, UB, UP0, UP1 = self.STP, self.STB, self.TMS, self.XB, self.UB, self.UP0, self.UP1
                rdA0 = AR.rh(cidx * 128, (cidx + 1) * 128)
                rdA1 = AR.rh(T + cidx * 128, T + (cidx + 1) * 128)
                px, rx = self.ps()
                self.mm(px[:, 0:128], ARv[:, 0, ts], STB[:], True, False, rdA0 + [self.STB_r], [rx])
                for hd in range(2):
                    self.mm(px[:, hd * 64:(hd + 1) * 64], lls[hd][0][:, 128:256], VPv[:, hd, cidx, hd * 64:(hd + 1) * 64], False, hd == 1,
                            lls[hd][1] + VP.rh(), [rx])
                self.act(lambda e, px=px: e.copy(out=XB[:], in_=px[:, 0:128]), [rx], [self.XB_r])
                kseq = int(os.environ.get("KSEQ", "99"))
                if kseq <= 1:
                    return
                pu, ru = self.ps()
                kv2 = int(os.environ.get("KV2", "0"))
                for hd in range(2):
                    if kv2 == 1 and hd == 1:
                        continue
                    if kv2 == 2 and hd == 0:
                        continue
                    lq = self.identb[:] if kv2 == 3 else lls[hd][0][:, 0:128]
                    if kv2 == 4:
                        lq = lls[hd][0][:, 384:512]
                    if kv2 == 5:
                        lq = lls[hd][0][:, 256:384]
                    self.mm(pu[:, hd * 64:(hd + 1) * 64], lq, XB[:, hd * 64:(hd + 1) * 64], True, True, lls[hd][1] + [self.XB_r], [ru])
                self.act(lambda e, pu=pu: e.copy(out=UB[:], in_=pu[:, 0:128]), [ru], [self.UB_r])
                kvar = int(os.environ.get("KVAR", "0"))
                if kvar == 3:
                    self.dve(lambda e, pu=pu: e.tensor_copy(out=UP0[:, 0:64], in_=XB[:, 0:64]), [self.XB_r], [self.UP0_r])
                elif kvar == 4:
                    self.dve(lambda e, pu=pu: e.tensor_copy(out=TMS[:, 0:64], in_=pu[:, 0:64]), [ru], [self.TMS_r])
                elif kvar == 5:
                    self.act(lambda e, pu=pu: e.copy(out=UP0[:, 0:64], in_=pu[:, 0:64]), [ru], [self.UP0_r])
                elif kvar != 1:
                    self.dve(lambda e, pu=pu: e.tensor_copy(out=UP0[:, 0:64], in_=pu[:, 0:64]), [ru], [self.UP0_r])
                if kvar == 0:
                    self.dve(lambda e, pu=pu: e.tensor_copy(out=UP1[:, 64:128], in_=pu[:, 64:128]), [ru], [self.UP1_r])
                if kseq <= 2:
                    return
                py, ry = self.ps()
                kv3 = int(os.environ.get("KV3", "7"))
                kv4 = int(os.environ.get("KV4", "0"))
                lst = {0: STB, 1: self.identb, 2: XB, 3: UB}[kv4]
                kv5 = int(os.environ.get("KV5", "0"))
                rr_ = ARv[:, 0, ts] if kv5 == 1 else ARv[:, 1, ts]
                self.mm(py[:, 0:128], lst[:], rr_, True, kv5 == 2, rdA1 + [self.STB_r], [ry])
                ups = [(UP0, self.UP0_r), (UP1, self.UP1_r)]
                for hd in range(2):
                    if kv3 & 2:
                        self.mm(py[:, 0:128], ups[hd][0][:], lls[hd][0][:, 384:512], False, False, [ups[hd][1]] + lls[hd][1], [ry])
                    if kv3 & 4:
                        self.mm(py[:, 0:128], VPv[:, hd, cidx, :], lls[hd][0][:, 256:384], False, hd == 1, VP.rh() + lls[hd][1], [ry])
                yt = YT[(cidx // 4) % 2]
                yo = (cidx % 4) * 128
                self.act(lambda e, py=py: e.copy(out=yt.f(yo, yo + 128), in_=py[:, 0:128]), [ry], yt.rf())
                if kseq <= 3:
                    return
                pS, rS = self.ps()
                self.mm(pS[:, 0:128], BHv[:, cidx, :], UB[:], True, False, BH.rh(cidx * 128, (cidx + 1) * 128) + [self.UB_r], [rS])
                self.mm(pS[:, 0:128], KHv[:, cidx, :], VPv[:, 0, cidx, :], False, False, KH.rh(cidx * 128, (cidx + 1) * 128) + VP.rh(), [rS])
                self.mm(pS[:, 0:128], KHv[:, cidx, :], VPv[:, 1, cidx, :], False, True, KH.rh(cidx * 128, (cidx + 1) * 128) + VP.rh(), [rS])
                self.dve(lambda e, pS=pS: e.scalar_tensor_tensor(out=TMS[:], in0=STP[:], scalar=self.GC[:, cidx:cidx + 1], in1=pS[:, 0:128], op0=ALU.mult, op1=ALU.add),
                         [self.STP_r, self.GC_r, rS], [self.TMS_r])
                self.dve(lambda e: e.tensor_tensor(out=STP[:], in0=TMS[:], in1=bmask[:], op=ALU.mult), [self.TMS_r, bmask_r], [self.STP_r])
                self.dve(lambda e: e.tensor_tensor(out=STB[:], in0=TMS[:], in1=bmask[:], op=ALU.mult), [self.TMS_r, bmask_r], [self.STB_r])

            def post(tt):
                sl = slice(tt * 512, (tt + 1) * 512)
                yt = YT[tt % 2]
                pt, pr = self.ps()
                self.mm(pt[:, :], bavg[:], yt.f(), True, True, [bavg_r] + yt.rf(), [pr])
                self.dve(lambda e, pt=pt: e.tensor_tensor(out=tA.f(), in0=yt.f(), in1=pt[:, :], op=ALU.subtract), yt.rf() + [pr], tA.rf())
                self.act(lambda e: e.activation(out=tB.f(), in_=tA.f(), func=AF.Square), tA.rf(), tB.rf())
                pt, pr = self.ps()
                self.mm(pt[:, :], bavg[:], tB.f(), True, True, [bavg_r] + tB.rf(), [pr])
                self.dve(lambda e, pt=pt: e.tensor_scalar_add(out=tB.f(), in0=pt[:, :], scalar1=64e-5), [pr], tB.rf())
                self.act(lambda e: e.activation(out=tB.f(), in_=tB.f(), func=AF.Ln), tB.rf(), tB.rf())
                self.act(lambda e: e.activation(out=tB.f(), in_=tB.f(), func=AF.Exp, scale=-0.5), tB.rf(), tB.rf())
                self.dve(lambda e: e.tensor_tensor(out=tA.f(), in0=tA.f(), in1=tB.f(), op=ALU.mult), tA.rf() + tB.rf(), tA.rf())
                self.dve(lambda e: e.tensor_scalar(out=tA.f(), in0=tA.f(), scalar1=rw[:, 20 + hp:21 + hp], scalar2=rw[:, 24 + hp:25 + hp], op0=ALU.mult, op1=ALU.add),
                         tA.rf() + [rw_r], tA.rf())
                self.dve(lambda e: e.tensor_tensor(out=tA.f(), in0=tA.f(), in1=BON.h()[:, sl], op=ALU.add), tA.rf() + BON.rh(tt * 512, (tt + 1) * 512), tA.rf())
                self.dve(lambda e: e.tensor_tensor(out=yv[:, hp, sl], in0=tA.f(), in1=G.h()[:, sl], op=ALU.mult), tA.rf() + G.rh(tt * 512, (tt + 1) * 512),
                         yrwT.rh(hp * T + tt * 512, hp * T + (tt + 1) * 512))

            for tt in range(4):
                prep(tt)
            if cut == 2:
                return
            if cut == 3:
                aphase(range(0, 4))
                return
            if cut == 8:
                aphase(range(0, 4))
                self.dve(lambda e: e.tensor_copy(out=tA.f(), in_=ALL.h(0, 512)), ALL.rh(0, 512), tA.rf())
                self.load(self.dbg[0:128, 0:512], tA.f(), tA.rf(), [self.out_res])
                self.dve(lambda e: e.tensor_copy(out=tB.f(), in_=ALL.h(512, 1024)), ALL.rh(512, 1024), tB.rf())
                self.load(self.dbg[0:128, 512:1024], tB.f(), tB.rf(), [self.out_res])
                self.nodump = True
                return
            if cut == 5:
                aphase(range(0, 4))
                aphase(range(4, 8))
                return
            if cut == 6:
                aphase(range(0, 4))
                seq(0)
                return
            if cut == 7:
                aphase(range(0, 4))
                for cidx in range(0, 4):
                    seq(cidx)
                return
            if cut == 4:
                aphase(range(0, 4))
                aphase(range(4, 8))
                for cidx in range(0, 4):
                    seq(cidx)
                post(0)
                return
            for half in range(2):
                aphase(range(half * 8, half * 8 + 4))
                aphase(range(half * 8 + 4, half * 8 + 8))
                for cidx in range(half * 8, half * 8 + 8):
                    seq(cidx)
                    if cidx % 4 == 3:
                        post(cidx // 4)

        for hp in range(4):
            run_hp(hp)
            if cut >= 2:
                return

    def mla(self, b, hT, ymlaT, scr):
        c = self.c
        w_in_v = self.wd["w_in"].rearrange("(c p) n -> p c n", p=128)
        hTv = hT.h().rearrange("p (c t) -> p c t", c=8)
        ones, ones_r = c["allones"]
        fq, fq_r = c["rope_fq"]
        ph, ph_r = c["rope_ph"]
        m2, m2_r = self.mask2, self.mask2_r
        yv = ymlaT.h().rearrange("p (c t) -> p c t", c=8)

        TAB = scr.get(6)
        CQN = scr.get(6)
        CKVN = scr.get(4)
        KROT = scr.get(2)
        V = scr.get(9)
        wlat = scr.get(3)
        wv = scr.get(1)
        t1, t2, t3, t4 = [scr.get(1) for _ in range(4)]
        QTs = [scr.get(2), scr.get(2)]
        KTs = [scr.get(2), scr.get(2)]
        PTs = [scr.get(1) for _ in range(3)]
        TABv = TAB.h().rearrange("p (k t) -> p k t", k=3)
        CQNv = CQN.h().rearrange("p (k t) -> p k t", k=3)
        CKVNv = CKVN.h().rearrange("p (k t) -> p k t", k=2)
        Vv = V.h(0, 16 * 8 * 66).rearrange("p (i h d) -> p i h d", i=16, h=8)

        for tt in range(4):
            sl = slice(tt * 512, (tt + 1) * 512)
            self.load(t1.i(), self.pos[b:b + 1, sl].partition_broadcast(128), [], t1.rf())
            self.dve(lambda e: e.tensor_copy(out=t2.f(), in_=t1.i()), t1.rf(), t2.rf())
            for k in range(3):
                self.dve(lambda e, k=k: e.tensor_scalar(out=t3.f(), in0=t2.f(), scalar1=fq[:, k:k + 1], scalar2=ph[:, k:k + 1], op0=ALU.mult, op1=ALU.add),
                         t2.rf() + [fq_r, ph_r], t3.rf())
                self.dve(lambda e: e.tensor_copy(out=t4.i(), in_=t3.f()), t3.rf(), t4.rf())
                self.dve(lambda e: e.tensor_copy(out=t1.f(), in_=t4.i()), t4.rf(), t1.rf())
                self.dve(lambda e: e.tensor_tensor(out=t3.f(), in0=t3.f(), in1=t1.f(), op=ALU.subtract), t3.rf() + t1.rf(), t3.rf())
                self.dve(lambda e: e.tensor_single_scalar(out=t1.f(), in_=t3.f(), scalar=0.5, op=ALU.is_gt), t3.rf(), t1.rf())
                self.dve(lambda e: e.tensor_tensor(out=t3.f(), in0=t3.f(), in1=t1.f(), op=ALU.subtract), t3.rf() + t1.rf(), t3.rf())
                self.dve(lambda e: e.tensor_single_scalar(out=t1.f(), in_=t3.f(), scalar=-0.5, op=ALU.is_lt), t3.rf(), t1.rf())
                self.dve(lambda e: e.tensor_tensor(out=t3.f(), in0=t3.f(), in1=t1.f(), op=ALU.add), t3.rf() + t1.rf(), t3.rf())
                self.act(lambda e, k=k, sl=sl: e.activation(out=TABv[:, k, sl], in_=t3.f(), func=AF.Sin, scale=6.283185), t3.rf(),
                         TAB.rh(k * T + tt * 512, k * T + (tt + 1) * 512))

        def latent(col0, nchunk, gname, OUT, OUTv):
            gq, gq_r = self.pv[gname]
            wl = wlat.h(0, 8 * nchunk * 128).rearrange("p (c n) -> p c n", c=8)
            self.loadw(wl, w_in_v[:, :, col0:col0 + nchunk * 128], [], wlat.rh(0, 8 * nchunk * 128))
            tmps = [t1, t2, t3]
            for tt in range(4):
                sl = slice(tt * 512, (tt + 1) * 512)
                pss, rss = self.ps()
                for j in range(nchunk):
                    pt, pr = self.ps()
                    for cc in range(8):
                        self.mm(pt[:, :], wl[:, cc, j * 128:(j + 1) * 128], hTv[:, cc, sl], cc == 0, cc == 7,
                                wlat.rh(0, 8 * nchunk * 128) + hT.rh(cc * T + tt * 512, cc * T + (tt + 1) * 512), [pr])
                    tj = tmps[j]
                    self.act(lambda e, pt=pt, tj=tj: e.copy(out=tj.f(), in_=pt[:, :]), [pr], tj.rf())
                    self.act(lambda e, tj=tj: e.activation(out=t4.f(), in_=tj.f(), func=AF.Square), tj.rf(), t4.rf())
                    self.mm(pss[:, :], ones[:], t4.f(), j == 0, j == nchunk - 1, [ones_r] + t4.rf(), [rss])
                self.dve(lambda e, pss=pss: e.tensor_scalar(out=t4.f(), in0=pss[:, :], scalar1=1.0 / (nchunk * 128), scalar2=1e-6, op0=ALU.mult, op1=ALU.add),
                         [rss], t4.rf())
                self.act(lambda e: e.activation(out=t4.f(), in_=t4.f(), func=AF.Ln), t4.rf(), t4.rf())
                self.act(lambda e: e.activation(out=t4.f(), in_=t4.f(), func=AF.Exp, scale=-0.5), t4.rf(), t4.rf())
                for j in range(nchunk):
                    tj = tmps[j]
                    self.dve(lambda e, j=j, tj=tj, sl=sl: e.scalar_tensor_tensor(out=OUTv[:, j, sl], in0=tj.f(), scalar=gq[:, j:j + 1], in1=t4.f(), op0=ALU.mult, op1=ALU.mult),
                             tj.rf() + t4.rf() + [gq_r], OUT.rh(j * T + tt * 512, j * T + (tt + 1) * 512))

        latent(CQ0, 3, "pv_gq", CQN, CQNv)
        latent(CKV0, 2, "pv_gkv", CKVN, CKVNv)

        wkr = wlat.h(0, 8 * 256).rearrange("p (c n) -> p c n", c=8)
        self.loadw(wkr, self.wd["w_kr"].rearrange("(c p) n -> p c n", p=128), [], wlat.rh(0, 8 * 256))
        for tt in range(4):
            sl = slice(tt * 512, (tt + 1) * 512)
            pA, rA = self.ps()
            pB, rB = self.ps()
            for cc in range(8):
                rd = wlat.rh(0, 8 * 256) + hT.rh(cc * T + tt * 512, cc * T + (tt + 1) * 512)
                self.mm(pA[:, :], wkr[:, cc, 0:128], hTv[:, cc, sl], cc == 0, cc == 7, rd, [rA])
            for cc in range(8):
                rd = wlat.rh(0, 8 * 256) + hT.rh(cc * T + tt * 512, cc * T + (tt + 1) * 512)
                self.mm(pB[:, :], wkr[:, cc, 128:256], hTv[:, cc, sl], cc == 0, cc == 7, rd, [rB])
            self.dve(lambda e, pA=pA, sl=sl: e.tensor_tensor(out=t1.f(), in0=pA[:, :], in1=TABv[:, 1, sl], op=ALU.mult), [rA] + TAB.rh(T + tt * 512, T + (tt + 1) * 512), t1.rf())
            self.dve(lambda e, pB=pB, sl=sl: e.tensor_tensor(out=t2.f(), in0=pB[:, :], in1=TABv[:, 2, sl], op=ALU.mult), [rB] + TAB.rh(2 * T + tt * 512, 2 * T + (tt + 1) * 512), t2.rf())
            self.dve(lambda e, sl=sl: e.tensor_tensor(out=KROT.h()[:, sl], in0=t1.f(), in1=t2.f(), op=ALU.add), t1.rf() + t2.rf(), KROT.rh(tt * 512, (tt + 1) * 512))

        wvv = wv.h(0, 1024).rearrange("p (c n) -> p c n", c=2)
        self.loadw(wvv, self.wd["w_v"].rearrange("(c p) n -> p c n", p=128), [], wv.rh())
        self.dve(lambda e: e.memset(Vv[:, :, :, 64:66], 1.0), [], V.rh())
        for i in range(16):
            pt, pr = self.ps()
            for kc in range(2):
                self.mm(pt[:, :], CKVNv[:, kc, i * 128:(i + 1) * 128], wvv[:, kc, :], kc == 0, kc == 1,
                        CKVN.rh(kc * T + i * 128, kc * T + (i + 1) * 128) + wv.rh(), [pr])
            self.act(lambda e, pt=pt, i=i: e.copy(out=Vv[:, i, :, 0:64], in_=pt[:, :].rearrange("p (h d) -> p h d", h=8)), [pr], V.rh())

        def head(h):
            QT, KT = QTs[h % 2], KTs[h % 2]
            wq = wlat.h(0, 384).rearrange("p (c n) -> p c n", c=3)
            wk = wlat.h(1024, 1024 + 256).rearrange("p (c n) -> p c n", c=2)
            self.loadw(wq, self.wd["w_q"][:, h * 128:(h + 1) * 128].rearrange("(c p) n -> p c n", p=128), [], wlat.rh(0, 384))
            self.loadw(wk, self.wd["w_k"][:, h * 128:(h + 1) * 128].rearrange("(c p) n -> p c n", p=128), [], wlat.rh(1024, 1280))
            for tt in range(4):
                sl = slice(tt * 512, (tt + 1) * 512)
                pt, pr = self.ps()
                for cc in range(3):
                    self.mm(pt[:, :], wq[:, cc, :], CQNv[:, cc, sl], cc == 0, cc == 2, wlat.rh(0, 384) + CQN.rh(cc * T + tt * 512, cc * T + (tt + 1) * 512), [pr])
                self.dve(lambda e, pt=pt, sl=sl: e.tensor_tensor(out=QT.h()[:, sl], in0=pt[:, :], in1=TABv[:, 0, sl], op=ALU.mult),
                         [pr] + TAB.rh(tt * 512, (tt + 1) * 512), QT.rh(tt * 512, (tt + 1) * 512))
                pt, pr = self.ps()
                for cc in range(2):
                    self.mm(pt[:, :], wk[:, cc, :], CKVNv[:, cc, sl], cc == 0, cc == 1, wlat.rh(1024, 1280) + CKVN.rh(cc * T + tt * 512, cc * T + (tt + 1) * 512), [pr])
                self.dve(lambda e, pt=pt, sl=sl: e.tensor_tensor(out=KT.h()[:, sl], in0=pt[:, :], in1=KROT.h()[:, sl], op=ALU.add),
                         [pr] + KROT.rh(tt * 512, (tt + 1) * 512), KT.rh(tt * 512, (tt + 1) * 512))
            iters = [(qt, kb) for qt in range(4) for kb in range(4 * qt + 4)]
            pos_ = {}
            pts_ = {}
            deferred = []

            def stage_s(n):
                qt, kb = iters[n]
                if kb == 0:
                    pos_[qt] = self.ps(hold=True)
                c0 = max(0, kb - 4 * qt) * 128
                psc, rsc = self.ps()
                self.mm(psc[:, c0:512], KT.h()[:, kb * 128:(kb + 1) * 128], QT.h()[:, qt * 512 + c0:(qt + 1) * 512], True, True,
                        KT.rh(kb * 128, (kb + 1) * 128) + QT.rh(qt * 512, (qt + 1) * 512), [rsc])
                PT = PTs[n % 3]
                pts_[n] = PT
                self.act(lambda e, psc=psc, PT=PT, c0=c0: e.activation(out=PT.h()[:, c0:512], in_=psc[:, c0:512], func=AF.Exp, scale=ATT_SCALE), [rsc], PT.rh())
                if kb >= 4 * qt:
                    self.dve(lambda e, PT=PT, c0=c0: e.tensor_tensor(out=PT.h()[:, c0:c0 + 128], in0=PT.h()[:, c0:c0 + 128], in1=m2[:, 128:256], op=ALU.mult),
                             PT.rh() + [m2_r], PT.rh())

            def epi1(qt):
                po, ro = pos_[qt]
                self.act(lambda e, po=po: e.copy(out=t1.f()[64:65, :], in_=po[64:65, :]), [ro], t1.rf())
                self.dve(lambda e: e.reciprocal(out=t1.f()[64:65, :], in_=t1.f()[64:65, :]), t1.rf(), t1.rf())

            def epi2(qt):
                po, ro = pos_[qt]
                pb_, rb_ = self.ps()
                self.mm(pb_[0:64, :], ones[64:65, 0:64], t1.f()[64:65, :], True, True, [ones_r] + t1.rf(), [rb_])
                self.act(lambda e, pb_=pb_: e.copy(out=t2.f()[0:64, :], in_=pb_[0:64, :]), [rb_], t2.rf())
                self.dve(lambda e, po=po, qt=qt: e.tensor_tensor(out=yv[0:64, h, qt * 512:(qt + 1) * 512], in0=po[0:64, :], in1=t2.f()[0:64, :], op=ALU.mult),
                         [ro] + t2.rf(), ymlaT.rh(h * T + qt * 512, h * T + (qt + 1) * 512))
                self.ps_release(po)

            def stage_p(n):
                qt, kb = iters[n]
                nkb = 4 * qt + 4
                c0 = max(0, kb - 4 * qt) * 128
                po, ro = pos_[qt]
                PT = pts_[n]
                self.mm(po[0:65, c0:512], Vv[:, kb, h, 0:65], PT.h()[:, c0:512], kb == 0, kb == nkb - 1, V.rh() + PT.rh(), [ro])
                if kb == nkb - 1:
                    deferred.append([1, epi1, qt])
                    deferred.append([5, epi2, qt])

            SK = 2
            for idx in range(len(iters) + SK):
                if idx < len(iters):
                    stage_s(idx)
                if idx - SK >= 0:
                    stage_p(idx - SK)
                for d in list(deferred):
                    d[0] -= 1
                    if d[0] <= 0:
                        d[1](d[2])
                        deferred.remove(d)
            for d in list(deferred):
                d[1](d[2])

        for h in range(8):
            head(h)

    def merge(self, b, hT, yrwT, ymlaT, scr, scr2, scr3):
        c = self.c
        w_in_v = self.wd["w_in"].rearrange("(c p) n -> p c n", p=128)
        hTv = hT.h().rearrange("p (c t) -> p c t", c=8)
        yrv = yrwT.h().rearrange("p (c t) -> p c t", c=4)
        ymv = ymlaT.h().rearrange("p (c t) -> p c t", c=8)
        g2, g2_r = self.pv["pv_g2"]
        idf, idf_r = c["ident"]
        MT = scr.get(16)
        MTv = MT.h().rearrange("p (c t) -> p c t", c=8)
        wgr, wgm, wbr, wbm = [scr.get(1) for _ in range(4)]
        t1, t2 = scr.get(1), scr.get(1)
        wgrv = wgr.h().rearrange("p (c n) -> p c n", c=8)
        wgmv = wgm.h().rearrange("p (c n) -> p c n", c=8)
        wbrv = wbr.h(0, 512).rearrange("p (c n) -> p c n", c=4)
        wbmv = wbm.h().rearrange("p (c n) -> p c n", c=8)
        for j in range(8):
            js = slice(j * 128, (j + 1) * 128)
            self.loadw(wgrv, w_in_v[:, :, GRW0 + j * 128:GRW0 + (j + 1) * 128], [], wgr.rh())
            self.loadw(wgmv, w_in_v[:, :, GML0 + j * 128:GML0 + (j + 1) * 128], [], wgm.rh())
            self.loadw(wbrv, self.wd["w_brw"][:, js].rearrange("(c p) n -> p c n", p=128), [], wbr.rh())
            self.loadw(wbmv[0:64], self.wd["w_bml"][:, js].rearrange("(h p) n -> p h n", p=64), [], wbm.rh())
            for tt in range(4):
                sl = slice(tt * 512, (tt + 1) * 512)
                for (wg, wgreg, tdst) in ((wgrv, wgr, t1), (wgmv, wgm, t2)):
                    pg, rg = self.ps()
                    for cc in range(8):
                        self.mm(pg[:, :], wg[:, cc, :], hTv[:, cc, sl], cc == 0, cc == 7, wgreg.rh() + hT.rh(cc * T + tt * 512, cc * T + (tt + 1) * 512), [rg])
                    self.act(lambda e, pg=pg, tdst=tdst: e.activation(out=tdst.f(), in_=pg[:, :], func=AF.Tanh, scale=0.5), [rg], tdst.rf())
                pb1, rb1 = self.ps()
                for cc in range(4):
                    self.mm(pb1[:, :], wbrv[:, cc, :], yrv[:, cc, sl], cc == 0, cc == 3, wbr.rh() + yrwT.rh(cc * T + tt * 512, cc * T + (tt + 1) * 512), [rb1])
                self.dve(lambda e, pb1=pb1: e.scalar_tensor_tensor(out=t1.f(), in0=t1.f(), scalar=1.0, in1=pb1[:, :], op0=ALU.add, op1=ALU.mult), [rb1] + t1.rf(), t1.rf())
                pb2, rb2 = self.ps()
                for hh in range(8):
                    self.mm(pb2[:, :], wbmv[0:64, hh, :], ymv[0:64, hh, sl], hh == 0, hh == 7, wbm.rh() + ymlaT.rh(hh * T + tt * 512, hh * T + (tt + 1) * 512), [rb2])
                self.dve(lambda e, pb2=pb2: e.scalar_tensor_tensor(out=t2.f(), in0=t2.f(), scalar=1.0, in1=pb2[:, :], op0=ALU.add, op1=ALU.mult), [rb2] + t2.rf(), t2.rf())
                self.dve(lambda e: e.tensor_tensor(out=t1.f(), in0=t1.f(), in1=t2.f(), op=ALU.add), t1.rf() + t2.rf(), t1.rf())
                self.act(lambda e, j=j, sl=sl: e.mul(out=MTv[:, j, sl], in_=t1.f(), mul=0.5), t1.rf(), MT.rh(j * T + tt * 512, j * T + (tt + 1) * 512))

        wo = scr.get(8)
        wov = wo.h().rearrange("p (c n) -> p c n", c=8)
        self.loadw(wov, self.wd["w_out"].rearrange("(c p) n -> p c n", p=128), [], wo.rh())
        wrt = scr.get(1)
        wrtv = wrt.f(0, 8 * 36).rearrange("p (c n) -> p c n", c=8)
        self.load(wrtv, self.wd["w_rt"].rearrange("(c p) n -> p c n", p=128), [], wrt.rf())
        xts = [scr2.get(2), scr2.get(2)]
        h2f = scr2.get(2)
        h2Tf = scr2.get(2)
        junk = scr.get(1)
        ACCs = [scr3.get(2), scr3.get(2)]
        gff = scr3.get(2)
        self.load(gff.f(), self.wd["g_ffn"].partition_broadcast(128), [], gff.rf())
        h2gs = [scr3.get(1), scr3.get(1)]
        MM, MM_r, SRf, SRf_r, SR, SR_r, W12, W12_r = self.MM, self.MM_r, self.SRf, self.SRf_r, self.SR, self.SR_r, self.W12, self.W12_r
        msu, msu_r = c["mask_su"]
        ones, ones_r = c["allones"]
        ecap, ecap_r = c["ecap"]
        if b == 0:
            self.tok_init = self.load(self.tokd.rearrange("(p f) o -> p (f o)", p=128), self.ZI[:], [self.ZI_r], [self.tok_r])
            self.scat_ops = []
        tok_init = self.tok_init
        h2Tfv = h2Tf.f().rearrange("p (c t) -> p c t", c=8)
        ssq, ssq_r, rs, rs_r = self.ssq, self.ssq_r, self.rs, self.rs_r
        LG, LG_r = self.LG, self.LG_r
        brt, brt_r = self.brt, self.brt_r
        R8, R8_r = self.R8, self.R8_r
        for i in range(16):
            xt = xts[i % 2]
            row0 = b * T + i * 128
            gi_ = b * 16 + i
            ACCt = ACCs[i % 2]
            self.load(xt.f(), self.x[row0:row0 + 128, :], [], xt.rf())
            for half in range(2):
                hs = slice(half * 512, (half + 1) * 512)
                pt, pr = self.ps()
                for cc in range(8):
                    self.mm(pt[:, :], MTv[:, cc, i * 128:(i + 1) * 128], wov[:, cc, hs], cc == 0, cc == 7,
                            MT.rh(cc * T + i * 128, cc * T + (i + 1) * 128) + wo.rh(), [pr])
                self.dve(lambda e, pt=pt, hs=hs, xt=xt, ACCt=ACCt: e.tensor_tensor(out=ACCt.f()[:, hs], in0=pt[:, :], in1=xt.f()[:, hs], op=ALU.add),
                         [pr] + xt.rf(), ACCt.rf(half * 512, (half + 1) * 512))
            ai = ACCt.rf()
            self.load(self.x1s[row0:row0 + 128, :], ACCt.f(), ai, [self.x1_r[gi_]])
            self.dve(lambda e, i=i: e.memset(ssq[:, i:i + 1], 0.0), [], [ssq_r])
            self.act(lambda e, i=i, ACCt=ACCt: e.activation(out=junk.h(), in_=ACCt.f(), func=AF.Square, accum_out=ssq[:, i:i + 1]), ai + [ssq_r], junk.rh() + [ssq_r])
            self.dve(lambda e, i=i: e.tensor_scalar(out=rs[:, i:i + 1], in0=ssq[:, i:i + 1], scalar1=1.0 / D, scalar2=1e-6, op0=ALU.mult, op1=ALU.add), [ssq_r], [rs_r])
            self.act(lambda e, i=i: e.activation(out=rs[:, i:i + 1], in_=rs[:, i:i + 1], func=AF.Ln), [rs_r], [rs_r])
            self.act(lambda e, i=i: e.activation(out=rs[:, i:i + 1], in_=rs[:, i:i + 1], func=AF.Exp, scale=-0.5), [rs_r], [rs_r])
            self.dve(lambda e, i=i, ACCt=ACCt: e.tensor_scalar_mul(out=h2f.f(), in0=ACCt.f(), scalar1=rs[:, i:i + 1]), ai + [rs_r], h2f.rf())
            h2g = h2gs[i % 2]
            self.dve(lambda e, h2g=h2g: e.tensor_tensor(out=h2g.h(), in0=h2f.f(), in1=gff.f(), op=ALU.mult), h2f.rf() + gff.rf(), h2g.rh())
            self.load(self.h2s[row0:row0 + 128, :], h2g.h(), h2g.rh(), [self.h2s_r[gi_]])
            for grp in range(2):
                pt, pr = self.ps()
                for q in range(4):
                    cc = grp * 4 + q
                    self.pe(lambda e, pt=pt, q=q, cc=cc: e.transpose(out=pt[:, q * 128:(q + 1) * 128], in_=h2f.f(cc * 128, (cc + 1) * 128), identity=idf[:]),
                            h2f.rf() + [idf_r], [pr])
                self.dve(lambda e, pt=pt, grp=grp: e.tensor_tensor(out=h2Tfv[:, grp * 4:(grp + 1) * 4, :], in0=pt[:, :].rearrange("p (c t) -> p c t", c=4),
                                                                 in1=g2[:, grp * 4:(grp + 1) * 4].unsqueeze(2).broadcast_to([128, 4, 128]), op=ALU.mult),
                         [pr, g2_r], h2Tf.rf())
            pl, rl = self.ps()
            for cc in range(8):
                self.mm(pl[:, 0:36], h2Tfv[:, cc, :], wrtv[:, cc, :], cc == 0, cc == 7, h2Tf.rf() + wrt.rf(), [rl])
            self.dve(lambda e, pl=pl, i=i: e.tensor_tensor(out=LG[:, i, :], in0=pl[:, 0:36], in1=brt[:], op=ALU.add), [rl, brt_r], [LG_r])
            lg4 = LG[:, i, 0:4]
            fine = LG[:, i, 4:36]
            r8 = R8
            self.dve(lambda e, lg4=lg4: e.reduce_max(out=r8[:, 0:1], in_=lg4, axis=AX.X), [LG_r], [R8_r])
            self.dve(lambda e, lg4=lg4: e.tensor_scalar(out=r8[:, 8:12], in0=lg4, scalar1=r8[:, 0:1], scalar2=None, op0=ALU.is_equal), [LG_r, R8_r], [R8_r])
            self.dve(lambda e: e.tensor_scalar_mul(out=r8[:, 1:2], in0=r8[:, 0:1], scalar1=-1.0), [R8_r], [R8_r])
            self.dve(lambda e: e.memset(r8[:, 2:3], 0.0), [], [R8_r])
            self.act(lambda e, lg4=lg4: e.activation(out=r8[:, 12:16], in_=lg4, func=AF.Exp, bias=r8[:, 1:2], accum_out=r8[:, 2:3]), [LG_r, R8_r], [R8_r])
            self.dve(lambda e: e.reciprocal(out=r8[:, 3:4], in_=r8[:, 2:3]), [R8_r], [R8_r])
            self.dve(lambda e: e.tensor_scalar(out=r8[:, 16:48].rearrange("p (g k) -> p g k", g=4), in0=r8[:, 8:12].unsqueeze(2).broadcast_to([128, 4, 8]),
                                               scalar1=-1.0, scalar2=1e30, op0=ALU.add, op1=ALU.mult), [R8_r], [R8_r])
            self.dve(lambda e, fine=fine: e.tensor_tensor(out=r8[:, 16:48], in0=r8[:, 16:48], in1=fine, op=ALU.add), [R8_r, LG_r], [R8_r])
            self.dve(lambda e: e.max(out=r8[:, 48:56], in_=r8[:, 16:48]), [R8_r], [R8_r])
            self.dve(lambda e: e.tensor_tensor(out=r8[:, 4:5], in0=r8[:, 49:50], in1=r8[:, 48:49], op=ALU.subtract), [R8_r], [R8_r])
            self.act(lambda e: e.activation(out=r8[:, 4:5], in_=r8[:, 4:5], func=AF.Exp), [R8_r], [R8_r])
            self.dve(lambda e: e.tensor_scalar_add(out=r8[:, 4:5], in0=r8[:, 4:5], scalar1=1.0), [R8_r], [R8_r])
            self.dve(lambda e: e.reciprocal(out=r8[:, 4:5], in_=r8[:, 4:5]), [R8_r], [R8_r])
            self.dve(lambda e: e.tensor_tensor(out=r8[:, 5:6], in0=r8[:, 4:5], in1=r8[:, 3:4], op=ALU.mult), [R8_r], [R8_r])
            self.dve(lambda e: e.tensor_tensor(out=r8[:, 6:7], in0=r8[:, 3:4], in1=r8[:, 5:6], op=ALU.subtract), [R8_r], [R8_r])
            self.dve(lambda e: e.tensor_scalar(out=r8[:, 56:88], in0=r8[:, 16:48], scalar1=r8[:, 48:49], scalar2=None, op0=ALU.is_equal), [R8_r], [R8_r])
            self.dve(lambda e: e.tensor_scalar(out=r8[:, 88:120], in0=r8[:, 16:48], scalar1=r8[:, 49:50], scalar2=None, op0=ALU.is_equal), [R8_r], [R8_r])
            self.dve(lambda e, gi_=gi_: e.tensor_copy(out=W12[:, gi_, :], in_=r8[:, 5:7]), [R8_r], [W12_r])
            self.dve(lambda e, gi_=gi_: e.tensor_tensor(out=MM[:, gi_, :], in0=r8[:, 56:88], in1=r8[:, 88:120], op=ALU.add), [R8_r], [MM_r])
            pR, rR = self.ps()
            for ip in range(gi_):
                self.mm(pR[:, 0:32], ones[:], MM[:, ip, :], ip == 0, False, [ones_r, MM_r], [rR])
            self.mm(pR[:, 0:32], msu[:], MM[:, gi_, :], gi_ == 0, True, [msu_r, MM_r], [rR])
            self.dve(lambda e, pR=pR: e.tensor_tensor(out=r8[:, 120:152], in0=pR[:, 0:32], in1=ecap[:], op=ALU.add), [rR, ecap_r], [R8_r])
            for k in range(2):
                self.dve(lambda e, k=k: e.tensor_tensor(out=r8[:, 152:184], in0=r8[:, 56 + 32 * k:88 + 32 * k], in1=r8[:, 120:152], op=ALU.mult), [R8_r], [R8_r])
                self.dve(lambda e, k=k, gi_=gi_: e.reduce_sum(out=SRf[:, gi_, k:k + 1], in_=r8[:, 152:184], axis=AX.X), [R8_r], [SRf_r])
            self.dve(lambda e, gi_=gi_: e.tensor_copy(out=SR[:, gi_, :], in_=SRf[:, gi_, :]), [SRf_r], [SR_r])
            for k in range(2):
                o = self.p.dma("pool", lambda e, gi_=gi_, k=k: e.indirect_dma_start(
                    out=self.tokd[:, :], out_offset=bass.IndirectOffsetOnAxis(ap=SR[:, gi_, k:k + 1], axis=0),
                    in_=self.TID[:, gi_:gi_ + 1], in_offset=None),
                    [SR_r, self.TID_r], [], extra=[tok_init])
                self.scat_ops.append(o)

    def moe(self, scr):
        idb, idb_r = self.identb, self.identb_r
        wgus = [scr.get(4), scr.get(4)]
        wdns = [scr.get(2), scr.get(2)]
        XGs = [scr.get(1) for _ in range(8)]
        XTs = [scr.get(4), scr.get(4)]
        HTs = [scr.get(1), scr.get(1)]
        YTs = [scr.get(1) for _ in range(4)]
        YKs = [scr.get(1) for _ in range(4)]
        XAs = [scr.get(2), scr.get(2)]
        gf = scr.get(2)
        self.load(gf.f(), self.wd["g_fin"].partition_broadcast(128), [], gf.rf())
        junk = scr.get(1)
        ots = [scr.get(2), scr.get(2)]
        ssq, ssq_r, rs, rs_r = self.ssq, self.ssq_r, self.rs, self.rs_r
        t1, t2 = scr.get(1), scr.get(1)
        NBLK = CAP // 128
        SR, SR_r, W12, W12_r = self.SR, self.SR_r, self.W12, self.W12_r

        def loadw_e(ex):
            wgu, wdn = wgus[ex % 2], wdns[ex % 2]
            self.loadw(wgu.h().rearrange("p (c n) -> p c n", c=8), self.wd["w_gu"][ex].rearrange("(c p) n -> p c n", p=128), [], wgu.rh())
            self.loadw(wdn.h().rearrange("p (c n) -> p c n", c=2), self.wd["w_dn"][ex].rearrange("(c p) n -> p c n", p=128), [], wdn.rh())

        loadw_e(0)
        loadw_e(1)
        gi = 0
        yi = 0
        xg_of = {}

        def gather_e(ex):
            nonlocal gi
            lst = []
            for blk in range(NBLK):
                idx, idx_r = self.IDX[gi % 8]
                XG = XGs[gi % 8]
                gi += 1
                r0 = ex * CAP + blk * 128
                self.p.dma("sp", lambda e, idx=idx, r0=r0: e.dma_start(out=idx[:], in_=self.tokd[r0:r0 + 128, :]), [self.tok_r], [idx_r], extra=self.scat_ops)
                self.p.dma("pool", lambda e, idx=idx, XG=XG: e.indirect_dma_start(
                    out=XG.h(), out_offset=None, in_=self.h2s[:, :], in_offset=bass.IndirectOffsetOnAxis(ap=idx[:, 0:1], axis=0)), [idx_r] + self.h2s_r, XG.rh())
                lst.append(XG)
            xg_of[ex] = lst

        gather_e(0)
        gather_e(1)
        for ex in range(32):
            wgu, wdn = wgus[ex % 2], wdns[ex % 2]
            wguv = wgu.h().rearrange("p (c n) -> p c n", c=8)
            wdnv = wdn.h().rearrange("p (c n) -> p c n", c=2)
            XT = XTs[ex % 2]
            XTv = XT.h().rearrange("p (c t) -> p c t", c=8)
            HT = HTs[ex % 2]
            HTv = HT.h(0, 2 * CAP).rearrange("p (c t) -> p c t", c=2)
            for blk in range(NBLK):
                XG = xg_of[ex][blk]
                pt, pr = self.ps()
                ptv = pt[:].bitcast(BF16).rearrange("p (c t) -> p c t", c=8)
                for cc in range(8):
                    self.pe(lambda e, cc=cc, ptv=ptv, XG=XG: e.transpose(out=ptv[:, cc, :], in_=XG.h(cc * 128, (cc + 1) * 128), identity=idb[:]), XG.rh() + [idb_r], [pr])
                self.act(lambda e, ptv=ptv, XTv=XTv, blk=blk: e.copy(out=XTv[:, :, blk * 128:(blk + 1) * 128], in_=ptv), [pr], XT.rh())
            if ex + 2 < 32:
                pass
            for jc in range(2):
                pG, rG = self.ps()
                for cc in range(8):
                    self.mm(pG[:, 0:CAP], wguv[:, cc, jc * 128:(jc + 1) * 128], XTv[:, cc, :], cc == 0, cc == 7, wgu.rh() + XT.rh(), [rG])
                pU, rU = self.ps()
                for cc in range(8):
                    self.mm(pU[:, 0:CAP], wguv[:, cc, 256 + jc * 128:256 + (jc + 1) * 128], XTv[:, cc, :], cc == 0, cc == 7, wgu.rh() + XT.rh(), [rU])
                self.act(lambda e, pG=pG: e.activation(out=t1.f(0, CAP), in_=pG[:, 0:CAP], func=AF.Tanh, scale=0.5), [rG], t1.rf())
                self.dve(lambda e, pG=pG: e.scalar_tensor_tensor(out=t2.f(0, CAP), in0=t1.f(0, CAP), scalar=1.0, in1=pG[:, 0:CAP], op0=ALU.add, op1=ALU.mult), [rG] + t1.rf(), t2.rf())
                self.dve(lambda e, pU=pU, HTv=HTv, jc=jc: e.scalar_tensor_tensor(out=HTv[:, jc, :], in0=t2.f(0, CAP), scalar=0.5, in1=pU[:, 0:CAP], op0=ALU.mult, op1=ALU.mult), [rU] + t2.rf(), HT.rh())
            for blk in range(NBLK):
                YT = YTs[yi % 4]
                yi += 1
                for half in range(2):
                    hs = slice(half * 512, (half + 1) * 512)
                    pY, rY = self.ps()
                    for jc in range(2):
                        self.mm(pY[:, :], HTv[:, jc, blk * 128:(blk + 1) * 128], wdnv[:, jc, hs], jc == 0, jc == 1, HT.rh() + wdn.rh(), [rY])
                    if half == 0:
                        self.act(lambda e, pY=pY, YT=YT, hs=hs: e.copy(out=YT.h()[:, hs], in_=pY[:, :]), [rY], YT.rh())
                    else:
                        self.dve(lambda e, pY=pY, YT=YT, hs=hs: e.tensor_copy(out=YT.h()[:, hs], in_=pY[:, :]), [rY], YT.rh())
                r0 = ex * CAP + blk * 128
                self.load(self.ysd[r0:r0 + 128, :], YT.h(), YT.rh(), [self.ys_r[ex]])
            if ex + 2 < 32:
                gather_e(ex + 2)
                loadw_e(ex + 2)
        ki = 0
        for g in range(32):
            XA = XAs[g % 2]
            self.load(XA.f(), self.x1s[g * 128:(g + 1) * 128, :], [self.x1_r[g]], XA.rf())
            for k in range(2):
                YK = YKs[ki % 4]
                ki += 1
                self.p.dma("pool", lambda e, YK=YK, g=g, k=k: e.indirect_dma_start(
                    out=YK.h(), out_offset=None, in_=self.ysd[:, :], in_offset=bass.IndirectOffsetOnAxis(ap=SR[:, g, k:k + 1], axis=0)), [SR_r] + self.ys_r, YK.rh())
                self.dve(lambda e, YK=YK, g=g, k=k, XA=XA: e.scalar_tensor_tensor(out=XA.f(), in0=YK.h(), scalar=W12[:, g, k:k + 1], in1=XA.f(), op0=ALU.mult, op1=ALU.add),
                         YK.rh() + [W12_r] + XA.rf(), XA.rf())
            i = g % 16
            ot = ots[g % 2]
            self.dve(lambda e, i=i: e.memset(ssq[:, i:i + 1], 0.0), [], [ssq_r])
            self.act(lambda e, i=i, XA=XA: e.activation(out=junk.h(), in_=XA.f(), func=AF.Square, accum_out=ssq[:, i:i + 1]), XA.rf() + [ssq_r], junk.rh() + [ssq_r])
            self.dve(lambda e, i=i: e.tensor_scalar(out=rs[:, i:i + 1], in0=ssq[:, i:i + 1], scalar1=1.0 / D, scalar2=1e-6, op0=ALU.mult, op1=ALU.add), [ssq_r], [rs_r])
            self.act(lambda e, i=i: e.activation(out=rs[:, i:i + 1], in_=rs[:, i:i + 1], func=AF.Ln), [rs_r], [rs_r])
            self.act(lambda e, i=i: e.activation(out=rs[:, i:i + 1], in_=rs[:, i:i + 1], func=AF.Exp, scale=-0.5), [rs_r], [rs_r])
            self.dve(lambda e, i=i, ot=ot, XA=XA: e.scalar_tensor_tensor(out=ot.f(), in0=XA.f(), scalar=rs[:, i:i + 1], in1=gf.f(), op0=ALU.mult, op1=ALU.mult),
                     XA.rf() + [rs_r] + gf.rf(), ot.rf())
            self.load(self.out[g * 128:(g + 1) * 128, :], ot.f(), ot.rf(), [self.out_res])

    def final(self, b, ACC, scr):
        ACCv = ACC.f().rearrange("p (i d) -> p i d", i=16)
        gf = scr.get(2)
        self.load(gf.f(), self.wd["g_fin"].partition_broadcast(128), [], gf.rf())
        junk = scr.get(1)
        ots = [scr.get(2), scr.get(2)]
        ssq, ssq_r, rs, rs_r = self.ssq, self.ssq_r, self.rs, self.rs_r
        for i in range(16):
            ai = ACC.rf(i * 1024, (i + 1) * 1024)
            ot = ots[i % 2]
            self.dve(lambda e, i=i: e.memset(ssq[:, i:i + 1], 0.0), [], [ssq_r])
            self.act(lambda e, i=i: e.activation(out=junk.h(), in_=ACCv[:, i, :], func=AF.Square, accum_out=ssq[:, i:i + 1]), ai + [ssq_r], junk.rh() + [ssq_r])
            self.dve(lambda e, i=i: e.tensor_scalar(out=rs[:, i:i + 1], in0=ssq[:, i:i + 1], scalar1=1.0 / D, scalar2=1e-6, op0=ALU.mult, op1=ALU.add), [ssq_r], [rs_r])
            self.act(lambda e, i=i: e.activation(out=rs[:, i:i + 1], in_=rs[:, i:i + 1], func=AF.Ln), [rs_r], [rs_r])
            self.act(lambda e, i=i: e.activation(out=rs[:, i:i + 1], in_=rs[:, i:i + 1], func=AF.Exp, scale=-0.5), [rs_r], [rs_r])
            self.dve(lambda e, i=i, ot=ot: e.scalar_tensor_tensor(out=ot.f(), in0=ACCv[:, i, :], scalar=rs[:, i:i + 1], in1=gf.f(), op0=ALU.mult, op1=ALU.mult),
                     ai + [rs_r] + gf.rf(), ot.rf())
            row0 = b * T + i * 128
            self.load(self.out[row0:row0 + 128, :], ot.f(), ot.rf(), [self.out_res])

    def dump_bf16_fm(self, reg, nchunk, scr):
        tmp = scr.get(4)
        v = reg.h().rearrange("p (c t) -> p c t", c=nchunk)
        for cc in range(nchunk):
            self.dve(lambda e, cc=cc: e.tensor_copy(out=tmp.f(), in_=v[:, cc, :]), reg.rh(cc * T, (cc + 1) * T), tmp.rf())
            self.load(self.dbg[cc * 128:(cc + 1) * 128, :], tmp.f(), tmp.rf(), [self.out_res])

    def finish(self):
        fin = self.p.op("sp", lambda e: e.nop(), [self.out_res], [])
        for e in ("pe", "act", "dve", "pool"):
            last = [o for o in self.p.ops[e] if not o.dma]
            if last:
                last[-1].signal = True
                fin.deps.append(last[-1])
        for q in ("sp", "pool"):
            for o in self.p.dlast[q]:
                if o is not None and o is not fin:
                    fin.deps.append(o)
        import os, sys
        if os.environ.get("KFILL", "1") == "1":
            idb = self.identb
            fb = self.psb[7]
            self.p.filler_fn = lambda e: e.matmul(out=fb[:, 0:128], lhsT=idb[:], rhs=idb[:], start=True, stop=True)
            self.p.filler_dep = self.identb_op
        if os.environ.get("KSCHED", "1") == "1":
            self.p.schedule()
            print("NFILL", getattr(self.p, "nfill", 0), file=sys.stderr)
        with self.nc.Block() as block:
            self.p.emit(block)
        self.st.close()
        import sys
        print("ICOUNT", self.p.icount, file=sys.stderr)
        return self.nc


def build(debug=None):
    B = Builder(debug)
    B.setup()
    ar = B.ar
    hT = ar.reg(0, 16)
    yrwT = ar.reg(16, 8)
    ymlaT = ar.reg(24, 16)
    for b in range(NB):
        B.phaseA(b, hT, Bump(ar, 24, 88))
        if debug and debug[0] == "hT":
            B.dump_bf16_fm(hT, 8, Bump(ar, 40, 88))
            return B.finish()
        B.rwkv(b, hT, yrwT, Bump(ar, 24, 88))
        if debug and debug[0] == "rw":
            if not getattr(B, "nodump", False):
                B.dump_bf16_fm(yrwT, 4, Bump(ar, 40, 88))
            return B.finish()
        B.mla(b, hT, ymlaT, Bump(ar, 40, 88))
        if debug and debug[0] == "mla":
            B.dump_bf16_fm(ymlaT, 8, Bump(ar, 40, 88))
            return B.finish()
        B.merge(b, hT, yrwT, ymlaT, Bump(ar, 40, 72), Bump(ar, 72, 80), Bump(ar, 80, 88))
    B.moe(Bump(ar, 0, 88))
    return B.finish()


_NC_CACHE = {}


def _tokid():
    return np.ascontiguousarray((np.arange(32, dtype=np.int32)[None, :] * 128 + np.arange(128, dtype=np.int32)[:, None]).astype(np.int32))


def kernel(**inputs):
    inp = {k: np.asarray(v) for k, v in inputs.items()}
    w = _host_layout(inp)
    consts = _consts()
    if "nc" not in _NC_CACHE:
        _NC_CACHE["nc"] = build()
    nc = _NC_CACHE["nc"]
    x = np.ascontiguousarray(inp["x"], dtype=np.float32)
    pos = np.ascontiguousarray(inp["positions"]).astype(np.int32)
    in_maps = []
    for core in range(NCORES):
        m = {"x": np.ascontiguousarray(x[core * NB:(core + 1) * NB].reshape(NB * T, D)),
             "pos": np.ascontiguousarray(pos[core * NB:(core + 1) * NB])}
        for n in CONST_NAMES:
            m["c_" + n] = consts[n]
        m["tokid"] = _tokid()
        m.update(w)
        in_maps.append(m)
    res = run_bass_kernel_spmd(nc, in_maps, core_ids=list(range(NCORES)))
    out = np.concatenate([np.asarray(r["out"]).reshape(NB, T, D) for r in res.results], axis=0)
    return out.astype(np.float32)
```

```python
import math
import numpy as np
import concourse.bass as bass
import concourse.mybir as mybir
from concourse.bass_utils import run_bass_kernel_spmd
from contextlib import ExitStack

F32 = mybir.dt.float32
BF16 = mybir.dt.bfloat16
I32 = mybir.dt.int32
ALU = mybir.AluOpType
AF = mybir.ActivationFunctionType
AX = mybir.AxisListType

NB = 2
T = 2048
D = 1024
NCORES = 8
RW_COLS = 1792
CQ0 = 1792
CKV0 = 2176
CKR0 = 2432
GRW0 = 2464
GML0 = 3488
CH = 128
NCH = T // CH
CAP = 512
ATT_SCALE = 1.0 / math.sqrt(96.0)
LOGW_C = -math.exp(-0.5)


class Res:
    __slots__ = ("w", "r", "excl")

    def __init__(self, excl=False):
        self.w = None
        self.r = []
        self.excl = excl


class Op:
    __slots__ = ("eng", "fn", "deps", "sdeps", "signal", "seq", "dma", "sem", "semval", "idx", "fin", "cost")

    def __init__(self, eng, fn):
        self.eng = eng
        self.fn = fn
        self.deps = []
        self.sdeps = []
        self.idx = 0
        self.fin = None
        self.cost = None
        self.signal = False
        self.seq = 0
        self.dma = False
        self.sem = None
        self.semval = 0


class Prog:
    ENGS = ("pe", "act", "dve", "pool", "sp")

    def __init__(self, nc, stack, n_dma_sems=10):
        self.nc = nc
        self.ops = {e: [] for e in self.ENGS}
        self.esem = {e: stack.enter_context(nc.semaphore("prog_" + e)) for e in self.ENGS}
        self.dsems, self.dcount, self.dlast = {}, {}, {}
        for q in ("sp", "pool"):
            self.dsems[q] = [stack.enter_context(nc.semaphore(f"dma_{q}_{i}")) for i in range(n_dma_sems)]
            self.dcount[q] = 0
            self.dlast[q] = [None] * n_dma_sems

    def _track(self, op, reads, writes):
        deps = []
        reads = list(reads)
        writes = list(writes)
        for r in reads:
            if r.excl and r not in writes:
                writes.append(r)
        for r in reads:
            if r.w is not None:
                deps.append(r.w)
        for w in writes:
            if w.w is not None:
                deps.append(w.w)
            deps.extend(w.r)
        seen = set()
        for d in deps:
            if d is op or id(d) in seen:
                continue
            seen.add(id(d))
            if (not d.dma) and d.eng == "pe" and op.eng == "pe" and not op.dma:
                op.sdeps.append(d)
                continue
            op.deps.append(d)
            d.signal = True
        for r in reads:
            r.r.append(op)
        for w in writes:
            w.w = op
            w.r = []

    def op(self, eng, fn, reads=(), writes=(), cost=None):
        o = Op(eng, fn)
        self.nidx = getattr(self, "nidx", 0) + 1
        o.idx = self.nidx
        o.cost = cost
        self._track(o, reads, writes)
        self.ops[eng].append(o)
        return o

    def schedule(self, window=64):
        import heapq
        base = {"pe": 0.25, "act": 0.6, "dve": 0.55, "pool": 1.0, "sp": 0.05}
        pend = {e: list(self.ops[e]) for e in self.ENGS}
        out = {e: [] for e in self.ENGS}
        free_at = {e: 0.0 for e in self.ENGS}
        now = 0.0
        events = []
        remaining = sum(len(v) for v in pend.values())
        cnt = 0
        while remaining:
            progressed = False
            for e in self.ENGS:
                if free_at[e] > now or not pend[e]:
                    continue
                best = None
                for o in pend[e][:window]:
                    ok = True
                    for d in o.deps:
                        if d.fin is None or d.fin > now:
                            ok = False
                            break
                    if ok:
                        for d in o.sdeps:
                            if d.fin is None:
                                ok = False
                                break
                    if ok:
                        best = o
                        break
                if best is None:
                    fill = getattr(self, "filler_fn", None)
                    fdep = getattr(self, "filler_dep", None)
                    if e == "pe" and fill is not None and fdep is not None and fdep.fin is not None and fdep.fin <= now:
                        fo = Op("pe", fill)
                        fo.deps.append(fdep)
                        fdep.signal = True
                        out[e].append(fo)
                        self.nfill = getattr(self, "nfill", 0) + 1
                        free_at[e] = now + 0.2
                        cnt += 1
                        heapq.heappush(events, (free_at[e], cnt))
                        progressed = True
                    continue
                pend[e].remove(best)
                out[e].append(best)
                remaining -= 1
                progressed = True
                if best.dma:
                    free_at[e] = now + 0.06
                    best.fin = now + 2.5
                else:
                    c = best.cost if best.cost is not None else base[e]
                    free_at[e] = now + c
                    best.fin = now + c + 0.25
                cnt += 1
                heapq.heappush(events, (best.fin, cnt))
                heapq.heappush(events, (free_at[e], cnt))
            if not progressed:
                if not events:
                    raise RuntimeError("scheduler stuck")
                t = heapq.heappop(events)[0]
                now = max(now, t)
        self.ops = out

    def dma(self, q, fn, reads=(), writes=(), extra=()):
        o = Op(q, fn)
        self.nidx = getattr(self, "nidx", 0) + 1
        o.idx = self.nidx
        o.dma = True
        self._track(o, reads, writes)
        for d in extra:
            o.deps.append(d)
            d.signal = True
        n = len(self.dsems[q])
        i = self.dcount[q] % n
        self.dcount[q] += 1
        prev = self.dlast[q][i]
        if prev is not None:
            o.deps.append(prev)
        o.sem = self.dsems[q][i]
        o.semval = (prev.semval if prev is not None else 0) + 16
        self.dlast[q][i] = o
        self.ops[q].append(o)
        return o

    def emit(self, block):
        for e in self.ENGS:
            c = 0
            for o in self.ops[e]:
                if (not o.dma) and o.signal:
                    c += 1
                    o.seq = c

        def run(e):
            def body(eng):
                waited = {}
                self.icount = getattr(self, "icount", {})
                self.icount[e] = 0
                for o in self.ops[e]:
                    self.icount[e] += 1
                    need = {}
                    for d in o.deps:
                        if d.dma:
                            k, v = d.sem, d.semval
                        else:
                            k, v = self.esem[d.eng], d.seq
                        if need.get(k, 0) < v:
                            need[k] = v
                    for k, v in need.items():
                        if waited.get(k, 0) < v:
                            eng.wait_ge(k, v)
                            waited[k] = v
                            self.icount[e] += 1
                    ins = o.fn(eng)
                    if o.dma:
                        ins.then_inc(o.sem, 16)
                    elif o.signal:
                        ins.then_inc(self.esem[e], 1)
            return body

        block.tensor(run("pe"))
        block.scalar(run("act"))
        block.vector(run("dve"))
        block.gpsimd(run("pool"))
        block.sync(run("sp"))


class Reg:
    def __init__(self, ar, blk0, nblk):
        self.ar, self.b0, self.n = ar, blk0, nblk

    def f(self, a=0, b=None):
        b = self.n * 512 if b is None else b
        return self.ar.t[:, self.b0 * 512 + a:self.b0 * 512 + b]

    def h(self, a=0, b=None):
        b = self.n * 1024 if b is None else b
        assert a % 2 == 0 and b % 2 == 0
        return self.ar.t[:, self.b0 * 512 + a // 2:self.b0 * 512 + b // 2].bitcast(BF16)

    def i(self, a=0, b=None):
        b = self.n * 512 if b is None else b
        return self.ar.t[:, self.b0 * 512 + a:self.b0 * 512 + b].bitcast(I32)

    def rf(self, a=0, b=None):
        b = self.n * 512 if b is None else b
        return [self.ar.res[self.b0 + j] for j in range(a // 512, (b - 1) // 512 + 1)]

    def rh(self, a=0, b=None):
        b = self.n * 1024 if b is None else b
        return [self.ar.res[self.b0 + j] for j in range(a // 1024, (b - 1) // 1024 + 1)]

    def sub(self, blk, n):
        assert blk + n <= self.n
        return Reg(self.ar, self.b0 + blk, n)


class Arena:
    def __init__(self, nc, stack, nblk):
        self.t = stack.enter_context(nc.sbuf_tensor("arena", [128, nblk * 512], F32))
        self.res = [Res() for _ in range(nblk)]
        self.nblk = nblk

    def reg(self, blk0, nblk):
        assert blk0 + nblk <= self.nblk, (blk0, nblk)
        return Reg(self, blk0, nblk)


class Bump:
    def __init__(self, ar, lo, hi):
        self.ar, self.lo, self.hi, self.cur = ar, lo, hi, lo

    def get(self, n):
        r = self.ar.reg(self.cur, n)
        self.cur += n
        assert self.cur <= self.hi, ("arena overflow", self.cur, self.hi)
        return r


def _consts():
    c = {}
    c["ident"] = np.eye(128, dtype=np.float32)
    p = np.arange(128)
    c["mask_su"] = (p[:, None] < p[None, :]).astype(np.float32)
    c["mask_iu"] = (p[:, None] <= p[None, :]).astype(np.float32)
    c["mask_sl"] = (p[:, None] > p[None, :]).astype(np.float32)
    blk = (p[:, None] // 64 == p[None, :] // 64).astype(np.float32)
    c["blockmask"] = blk
    c["blockones"] = blk.copy()
    c["blockavg"] = blk / 64.0
    sm = np.ones((128, 512), np.float32)
    sm[:, ::CH] = 0.0
    c["scanmask"] = sm
    c["allones"] = np.ones((128, 128), np.float32)
    c["mhalf"] = np.full((128, 512), -0.5, np.float32)
    inv_freq = (10000.0 ** (-np.arange(0, 32, 2, dtype=np.float32) / 32.0)).astype(np.float32)
    fq = np.zeros((128, 3), np.float32)
    ph = np.full((128, 3), 0.25, np.float32)
    for r in range(64, 128):
        f = inv_freq[r % 16] / (2 * np.pi)
        fq[r, :] = f
    ph[64:96, 0] = 0.25
    ph[96:112, 0] = 0.5
    ph[112:128, 0] = 0.0
    ph[64:128, 1] = 0.25
    ph[64:80, 2] = 0.5
    ph[80:96, 2] = 0.0
    ph[96:112, 2] = 0.5
    ph[112:128, 2] = 0.0
    c["ecap"] = np.tile((np.arange(32, dtype=np.float32) * CAP)[None, :], (128, 1))
    c["rope_fq"] = fq
    c["rope_ph"] = ph
    return c


_CONST_SHAPES = {n: v.shape for n, v in _consts().items()}
CONST_NAMES = ["ident", "mask_su", "mask_iu", "mask_sl", "blockmask", "blockones", "blockavg", "scanmask",
               "allones", "mhalf", "ecap", "rope_fq", "rope_ph"]


def _fm(v, nchunk):
    return np.ascontiguousarray(np.asarray(v, np.float32).reshape(nchunk, 128).T)


def _host_layout(inp):
    w = {}
    w_in = inp["w_in"][0]
    w["w_in"] = w_in
    kr = w_in[:, CKR0:CKR0 + 32]
    krs = np.concatenate([kr[:, 16:32], kr[:, 0:16]], axis=1)
    z64 = np.zeros((D, 64), np.float32)
    w["w_kr"] = np.ascontiguousarray(np.concatenate([z64, kr, kr, z64, krs, krs], axis=1))
    wq = inp["mla_w_q_up"][0]
    wqh = np.zeros((384, 8, 128), np.float32)
    for h in range(8):
        blk = wq[:, h * 96:(h + 1) * 96]
        wqh[:, h, 0:96] = blk
        wqh[:, h, 96:112] = blk[:, 80:96]
        wqh[:, h, 112:128] = blk[:, 64:80]
    w["w_q"] = np.ascontiguousarray(wqh.reshape(384, 1024))
    wkv = inp["mla_w_kv_up"][0]
    wk = np.zeros((256, 8, 128), np.float32)
    wv = np.zeros((256, 8, 64), np.float32)
    for h in range(8):
        wk[:, h, 0:64] = wkv[:, h * 128:h * 128 + 64]
        wv[:, h, :] = wkv[:, h * 128 + 64:h * 128 + 128]
    w["w_k"] = np.ascontiguousarray(wk.reshape(256, 1024))
    w["w_v"] = np.ascontiguousarray(wv.reshape(256, 512))
    w["wa_up"] = np.ascontiguousarray(np.concatenate([inp["rw_w_up"][0], inp["rw_a_up"][0]], axis=0))
    w["g_up"] = np.ascontiguousarray(inp["rw_g_up"][0])
    w["w_brw"] = np.ascontiguousarray(inp["w_branch_rw"][0])
    w["w_bml"] = np.ascontiguousarray(inp["w_branch_mla"][0])
    w["w_out"] = np.ascontiguousarray(inp["w_out"][0])
    w["w_rt"] = np.ascontiguousarray(np.concatenate([inp["moe_w_group"][0], inp["moe_w_router"][0]], axis=1))
    w["b_rt"] = np.ascontiguousarray(np.concatenate([inp["moe_b_group"][0], inp["moe_b_router"][0]])[None, :])
    w["w_gu"] = np.ascontiguousarray(inp["moe_w_gu"][0])
    w["w_dn"] = np.ascontiguousarray(inp["moe_w_down"][0])
    w["pv_mu"] = _fm(inp["rw_mu"][0], 14)
    vecs = [inp["rw_w0"][0], inp["rw_a0"][0], inp["rw_k_k"][0], inp["rw_k_a"][0], inp["rw_r_k"][0].reshape(-1),
            inp["rw_gn_w"][0], inp["rw_gn_b"][0]]
    w["pv_rw"] = np.ascontiguousarray(np.concatenate([_fm(v, 4) for v in vecs], axis=1))
    w["pv_g1"] = _fm(inp["mix_norm_g"][0], 8)
    w["pv_gq"] = _fm(inp["mla_g_qa"][0], 3)
    w["pv_gkv"] = _fm(inp["mla_g_kva"][0], 2)
    w["pv_g2"] = _fm(inp["ffn_norm_g"][0], 8)
    w["g_ffn"] = np.ascontiguousarray(inp["ffn_norm_g"][0][None, :])
    w["g_fin"] = np.ascontiguousarray(inp["final_norm_g"][None, :])
    return w


W_SHAPES = {
    "w_in": [1024, 4512], "w_kr": [1024, 256], "w_q": [384, 1024], "w_k": [256, 1024], "w_v": [256, 512],
    "wa_up": [128, 512], "g_up": [128, 512], "w_brw": [512, 1024], "w_bml": [512, 1024], "w_out": [1024, 1024],
    "w_rt": [1024, 36], "b_rt": [1, 36], "w_gu": [32, 1024, 512], "w_dn": [32, 256, 1024],
    "pv_mu": [128, 14], "pv_rw": [128, 28], "pv_g1": [128, 8], "pv_gq": [128, 3], "pv_gkv": [128, 2],
    "pv_g2": [128, 8], "g_ffn": [1, 1024], "g_fin": [1, 1024],
}


class Builder:
    def __init__(self, debug=None):
        self.debug = debug
        nc = bass.Bass("TRN2", target_bir_lowering=False)
        self.nc = nc
        self.st = ExitStack()
        st = self.st
        self.x = nc.dram_tensor("x", [NB * T, D], F32, kind="ExternalInput").ap()
        self.pos = nc.dram_tensor("pos", [NB, T], I32, kind="ExternalInput").ap()
        self.cd = {n: nc.dram_tensor("c_" + n, list(_CONST_SHAPES[n]), F32, kind="ExternalInput").ap() for n in CONST_NAMES}
        self.wd = {n: nc.dram_tensor(n, s, F32, kind="ExternalInput").ap() for n, s in W_SHAPES.items()}
        self.out = nc.dram_tensor("out", [NB * T, D], F32, kind="ExternalOutput").ap()
        self.tokid = nc.dram_tensor("tokid", [128, 32], I32, kind="ExternalInput").ap()
        self.h2s = nc.dram_tensor("h2s", [NB * T, D], BF16, kind="Internal").ap()
        self.x1s = nc.dram_tensor("x1s", [NB * T, D], F32, kind="Internal").ap()
        self.x1_r = [Res() for _ in range(32)]
        self.ysd = nc.dram_tensor("ysd", [32 * CAP, D], BF16, kind="Internal").ap()
        self.tokd = nc.dram_tensor("tokd", [32 * CAP, 1], I32, kind="Internal").ap()
        self.h2s_r = [Res() for _ in range(32)]
        self.ys_r = [Res() for _ in range(32)]
        self.tok_r = Res()
        self.dbg = None
        if debug is not None:
            self.dbg = nc.dram_tensor("dbg", list(debug[1]), F32, kind="ExternalOutput").ap()
        self.p = Prog(nc, st)
        self.ar = Arena(nc, st, 88)
        self.psb = [st.enter_context(nc.psum_tensor(f"ps{i}", [128, 512], F32)) for i in range(8)]
        self.psr = [Res(excl=True) for _ in range(8)]
        self.psi = 0
        import os as _os
        self.nrot = 7 if _os.environ.get("KFILL", "1") == "1" else 8
        self.small = {}
        self.out_res = Res()

    def sb(self, name, shape, dt=F32):
        t = self.st.enter_context(self.nc.sbuf_tensor("sb_" + name, shape, dt))
        r = Res()
        self.small[name] = (t, r)
        return t, r

    def ps(self, hold=False):
        held = getattr(self, "held", set())
        self.held = held
        nrot = getattr(self, "nrot", 8)
        while self.psi in held:
            self.psi = (self.psi + 1) % nrot
        i = self.psi
        self.psi = (self.psi + 1) % nrot
        if hold:
            held.add(i)
        return self.psb[i], self.psr[i]

    def ps_release(self, t):
        for i in range(8):
            if self.psb[i] is t:
                self.held.discard(i)

    def pe(self, fn, rd, wr):
        return self.p.op("pe", fn, rd, wr)

    def act(self, fn, rd, wr):
        return self.p.op("act", fn, rd, wr)

    def dve(self, fn, rd, wr):
        return self.p.op("dve", fn, rd, wr)

    def pool(self, fn, rd, wr):
        return self.p.op("pool", fn, rd, wr)

    def mm(self, out, lhsT, rhs, start, stop, rd, wr):
        return self.p.op("pe", lambda e: e.matmul(out=out, lhsT=lhsT, rhs=rhs, start=start, stop=stop), rd, wr)

    def loadw(self, dst, src, rd, wr):
        return self.p.dma("pool", lambda e: e.dma_start(out=dst, in_=src), rd, wr)

    def load(self, dst, src, rd, wr):
        return self.p.dma("sp", lambda e: e.dma_start(out=dst, in_=src), rd, wr)

    def setup(self):
        c = {}
        for n in CONST_NAMES:
            t, r = self.sb("k_" + n, list(_CONST_SHAPES[n]))
            self.load(t[:], self.cd[n], [], [r])
            c[n] = (t, r)
        self.c = c
        idb, idb_r = self.sb("identb", [128, 128], BF16)
        self.identb_op = self.dve(lambda e: e.tensor_copy(out=idb[:], in_=c["ident"][0][:]), [c["ident"][1]], [idb_r])
        self.identb, self.identb_r = idb, idb_r
        m2, m2_r = self.sb("mask2", [128, 256])
        self.dve(lambda e: e.tensor_copy(out=m2[:, 0:128], in_=c["mask_su"][0][:]), [c["mask_su"][1]], [m2_r])
        self.dve(lambda e: e.tensor_copy(out=m2[:, 128:256], in_=c["mask_iu"][0][:]), [c["mask_iu"][1]], [m2_r])
        self.mask2, self.mask2_r = m2, m2_r
        pv = {}
        for n in ["pv_mu", "pv_rw", "pv_g1", "pv_gq", "pv_gkv", "pv_g2"]:
            t, r = self.sb("s_" + n, W_SHAPES[n])
            self.load(t[:], self.wd[n], [], [r])
            pv[n] = (t, r)
        self.pv = pv
        omm, omm_r = self.sb("omm", [128, 14])
        self.dve(lambda e: e.tensor_scalar(out=omm[:], in0=pv["pv_mu"][0][:], scalar1=-1.0, scalar2=1.0, op0=ALU.mult, op1=ALU.add),
                 [pv["pv_mu"][1]], [omm_r])
        self.omm, self.omm_r = omm, omm_r
        dv, dv_r = self.sb("rwdv", [128, 12])
        rw = pv["pv_rw"][0]
        self.dve(lambda e: e.tensor_scalar_mul(out=dv[:, 0:8], in0=rw[:, 0:8], scalar1=0.5), [pv["pv_rw"][1]], [dv_r])
        self.dve(lambda e: e.tensor_scalar_mul(out=dv[:, 8:12], in0=rw[:, 12:16], scalar1=-1.0), [pv["pv_rw"][1]], [dv_r])
        self.rwdv, self.rwdv_r = dv, dv_r
        wa_up, wa_up_r = self.sb("wa_up", [128, 512], BF16)
        self.loadw(wa_up[:], self.wd["wa_up"], [], [wa_up_r])
        g_up, g_up_r = self.sb("g_up", [128, 512], BF16)
        self.loadw(g_up[:], self.wd["g_up"], [], [g_up_r])
        self.wa_up, self.wa_up_r, self.g_up, self.g_up_r = wa_up, wa_up_r, g_up, g_up_r
        self.STP, self.STP_r = self.sb("STP", [128, 128])
        self.STB, self.STB_r = self.sb("STB", [128, 128], BF16)
        self.TMS, self.TMS_r = self.sb("TMS", [128, 128])
        self.XB, self.XB_r = self.sb("XB", [128, 128], BF16)
        self.UB, self.UB_r = self.sb("UB", [128, 128], BF16)
        self.UP0, self.UP0_r = self.sb("UP0", [128, 128], BF16)
        self.UP1, self.UP1_r = self.sb("UP1", [128, 128], BF16)
        self.dve(lambda e: e.memset(self.UP0[:], 0.0), [], [self.UP0_r])
        self.dve(lambda e: e.memset(self.UP1[:], 0.0), [], [self.UP1_r])
        self.GC, self.GC_r = self.sb("GC", [128, NCH])
        self.carry = [self.sb(f"carry{i}", [128, 1]) for i in range(3)]
        self.LG, self.LG_r = self.sb("LG", [128, 16, 36])
        self.WT, self.WT_r = self.sb("WT", [128, 16, 32])
        self.R8, self.R8_r = self.sb("R8", [128, 192])
        self.MM, self.MM_r = self.sb("MMem", [128, 32, 32])
        self.SRf, self.SRf_r = self.sb("SRf", [128, 32, 2])
        self.SR, self.SR_r = self.sb("SR", [128, 32, 2], I32)
        self.W12, self.W12_r = self.sb("W12", [128, 32, 2])
        self.TID, self.TID_r = self.sb("TID", [128, 32], I32)
        self.load(self.TID[:], self.tokid, [], [self.TID_r])
        self.ZI, self.ZI_r = self.sb("ZI", [128, 128], I32)
        self.dve(lambda e: e.memset(self.ZI[:], 0), [], [self.ZI_r])
        self.IDX = [self.sb(f"IDX{i}", [128, 1], I32) for i in range(8)]
        self.brt, self.brt_r = self.sb("brt", [128, 36])
        self.load(self.brt[:], self.wd["b_rt"].partition_broadcast(128), [], [self.brt_r])
        self.ssq, self.ssq_r = self.sb("ssq", [128, 16])
        self.rs, self.rs_r = self.sb("rs", [128, 16])

    def phaseA(self, b, hT, scr):
        c = self.c
        xts = [scr.get(2), scr.get(2)]
        junk = scr.get(1)
        hb = scr.get(1)
        g1, g1_r = self.pv["pv_g1"]
        hTv = hT.h().rearrange("p (c t) -> p c t", c=8)
        ssq, ssq_r, rs, rs_r = self.ssq, self.ssq_r, self.rs, self.rs_r
        for i in range(16):
            xt = xts[i % 2]
            row0 = b * T + i * 128
            self.load(xt.f(), self.x[row0:row0 + 128, :], [], xt.rf())
            self.dve(lambda e, i=i: e.memset(ssq[:, i:i + 1], 0.0), [], [ssq_r])
            self.act(lambda e, xt=xt, i=i: e.activation(out=junk.h(), in_=xt.f(), func=AF.Square, accum_out=ssq[:, i:i + 1]),
                     xt.rf() + [ssq_r], junk.rh() + [ssq_r])
            self.dve(lambda e, i=i: e.tensor_scalar(out=rs[:, i:i + 1], in0=ssq[:, i:i + 1], scalar1=1.0 / D, scalar2=1e-6,
                                                    op0=ALU.mult, op1=ALU.add), [ssq_r], [rs_r])
            self.act(lambda e, i=i: e.activation(out=rs[:, i:i + 1], in_=rs[:, i:i + 1], func=AF.Ln), [rs_r], [rs_r])
            self.act(lambda e, i=i: e.activation(out=rs[:, i:i + 1], in_=rs[:, i:i + 1], func=AF.Exp, scale=-0.5), [rs_r], [rs_r])
            self.dve(lambda e, xt=xt, i=i: e.tensor_scalar_mul(out=hb.h(), in0=xt.f(), scalar1=rs[:, i:i + 1]),
                     xt.rf() + [rs_r], hb.rh())
            pt, pr = self.ps()
            ptv = pt[:].bitcast(BF16).rearrange("p (c t) -> p c t", c=8)
            for cc in range(8):
                self.pe(lambda e, cc=cc, ptv=ptv: e.transpose(out=ptv[:, cc, :], in_=hb.h(cc * 128, (cc + 1) * 128), identity=self.identb[:]),
                        hb.rh() + [self.identb_r], [pr])
            wr = []
            for cc in range(8):
                wr += hT.rh(cc * T + i * 128, cc * T + (i + 1) * 128)
            self.dve(lambda e, i=i, ptv=ptv: e.tensor_tensor(out=hTv[:, :, i * 128:(i + 1) * 128], in0=ptv,
                                                            in1=g1[:].unsqueeze(2).broadcast_to([128, 8, 128]), op=ALU.mult),
                     [pr, g1_r], wr)

    def shiftmix(self, sid, wt_ap, wt_res, hT, ci, tt, z, pm):
        mu, mu_r = self.pv["pv_mu"]
        omm, omm_r = self.omm, self.omm_r
        car, car_r = self.carry[sid]
        hTv = hT.h().rearrange("p (c t) -> p c t", c=8)
        pt, pr = self.ps()
        for cc in range(8):
            self.mm(pt[:, :], wt_ap[:, cc, :], hTv[:, cc, tt * 512:(tt + 1) * 512], cc == 0, cc == 7,
                    wt_res + hT.rh(cc * T + tt * 512, cc * T + (tt + 1) * 512), [pr])
        if tt == 0:
            self.dve(lambda e: e.memset(car[:], 0.0), [], [car_r])
        self.act(lambda e: e.activation(out=pm.f(), in_=pt[:, :], func=AF.Copy, scale=mu[:, ci:ci + 1]), [pr, mu_r], pm.rf())
        self.dve(lambda e: e.scalar_tensor_tensor(out=z.f(1, 512), in0=pt[:, 1:512], scalar=omm[:, ci:ci + 1], in1=pm.f(0, 511),
                                                  op0=ALU.mult, op1=ALU.add), [pr, omm_r] + pm.rf(), z.rf())
        self.dve(lambda e: e.scalar_tensor_tensor(out=z.f(0, 1), in0=pt[:, 0:1], scalar=omm[:, ci:ci + 1], in1=car[:, 0:1],
                                                  op0=ALU.mult, op1=ALU.add), [pr, omm_r, car_r], z.rf())
        self.dve(lambda e: e.tensor_copy(out=car[:, 0:1], in_=pm.f(511, 512)), pm.rf(), [car_r])

    def rwkv(self, b, hT, yrwT, scr):
        c = self.c
        w_in = self.wd["w_in"]
        w_in_v = w_in.rearrange("(c p) n -> p c n", p=128)
        rw, rw_r = self.pv["pv_rw"]
        dv, dv_r = self.rwdv, self.rwdv_r
        bones, bones_r = c["blockones"]
        bavg, bavg_r = c["blockavg"]
        smask, smask_r = c["scanmask"]
        bmask, bmask_r = c["blockmask"]
        msl, msl_r = c["mask_sl"]
        idb, idb_r = self.identb, self.identb_r
        m2, m2_r = self.mask2, self.mask2_r
        yv = yrwT.h().rearrange("p (c t) -> p c t", c=4)

        wt = scr.get(1)
        wrkv = scr.get(3)
        WA = scr.get(2)
        SG = scr.get(2)
        AR = scr.get(4)
        BT = scr.get(2)
        KT = scr.get(2)
        BH = scr.get(2)
        KH = scr.get(2)
        VP = scr.get(4)
        G = scr.get(2)
        BON = scr.get(2)
        Rr, Kk0, LOGW, Aa, KK, L, E, tA, tB, PM = [scr.get(1) for _ in range(10)]
        vbt, bht, kht = scr.get(1), scr.get(1), scr.get(1)
        YT = [scr.get(1), scr.get(1)]
        alt = {}
        for nm_, reg_ in (("Rr", Rr), ("Kk0", Kk0), ("tA", tA), ("tB", tB), ("E", E)):
            alt[nm_] = [reg_, scr.get(1)]
        ALL = scr.get(8)
        NSC = scr.get(8)

        wtv = wt.h().rearrange("p (c n) -> p c n", c=8)
        ARv = AR.h().rearrange("p (a t) -> p a t", a=2)
        BHv = BH.h().rearrange("p (c n) -> p c n", c=16)
        KHv = KH.h().rearrange("p (c n) -> p c n", c=16)
        VPv = VP.h().rearrange("p (a c n) -> p a c n", a=2, c=16)

        self.dve(lambda e: e.memset(VP.h(), 0.0), [], VP.rh())

        for ci, which in ((12, "wa"), (13, "sg")):
            col0 = 1536 + (ci - 12) * 128
            self.loadw(wtv, w_in_v[:, :, col0:col0 + 128], [], wt.rh())
            for tt in range(4):
                z = tA
                self.shiftmix(0, wtv, wt.rh(), hT, ci, tt, z, PM)
                sl = slice(tt * 512, (tt + 1) * 512)
                if which == "wa":
                    self.act(lambda e, sl=sl: e.activation(out=WA.h()[0:64, sl], in_=z.f()[0:64, :], func=AF.Tanh), z.rf(), WA.rh(tt * 512, (tt + 1) * 512))
                    self.dve(lambda e, sl=sl: e.tensor_copy(out=WA.h()[64:128, sl], in_=z.f()[64:128, :]), z.rf(), WA.rh(tt * 512, (tt + 1) * 512))
                else:
                    self.act(lambda e: e.activation(out=tB.f(), in_=z.f(), func=AF.Tanh, scale=0.5), z.rf(), tB.rf())
                    self.dve(lambda e, sl=sl: e.tensor_scalar(out=SG.h()[:, sl], in0=tB.f(), scalar1=0.5, scalar2=0.5, op0=ALU.mult, op1=ALU.add),
                             tB.rf(), SG.rh(tt * 512, (tt + 1) * 512))

        import os
        cut = int(os.environ.get("KCUT", "0"))
        if cut == 1:
            return

        def run_hp(hp):
            hs = slice(hp * 128, (hp + 1) * 128)
            w3 = wrkv.h().rearrange("p (j c n) -> p j c n", j=3, c=8)
            for j in range(3):
                col0 = j * 512 + hp * 128
                self.loadw(w3[:, j], w_in_v[:, :, col0:col0 + 128], [], wrkv.rh(j * 1024, (j + 1) * 1024))
            self.dve(lambda e: e.memset(self.STP[:], 0.0), [], [self.STP_r])
            self.dve(lambda e: e.memset(self.STB[:], 0.0), [], [self.STB_r])

            def prep(tt, Rr=Rr, Kk0=Kk0, tA=tA, tB=tB, E=E):
                Rr, Kk0, tA, tB, E = (alt[n_][tt % 2] for n_ in ("Rr", "Kk0", "tA", "tB", "E"))
                sl = slice(tt * 512, (tt + 1) * 512)
                rT = lambda a=tt * 512, bnd=(tt + 1) * 512: None
                self.shiftmix(0, w3[:, 0], wrkv.rh(0, 1024), hT, hp, tt, Rr, PM)
                self.shiftmix(1, w3[:, 1], wrkv.rh(1024, 2048), hT, 4 + hp, tt, Kk0, PM)
                self.shiftmix(2, w3[:, 2], wrkv.rh(2048, 3072), hT, 8 + hp, tt, tA, PM)
                self.act(lambda e: e.copy(out=vbt.h(0, 512), in_=tA.f()), tA.rf(), vbt.rh())
                pt, pr = self.ps()
                self.mm(pt[:, :], self.wa_up[0:64, hs], WA.h()[0:64, sl], True, True, [self.wa_up_r] + WA.rh(tt * 512, (tt + 1) * 512), [pr])
                self.act(lambda e, pt=pt: e.activation(out=tB.f(), in_=pt[:, :], func=AF.Tanh, bias=dv[:, hp:hp + 1], scale=0.5), [pr, dv_r], tB.rf())
                self.dve(lambda e: e.tensor_scalar(out=LOGW.f(), in0=tB.f(), scalar1=0.5 * LOGW_C, scalar2=0.5 * LOGW_C, op0=ALU.mult, op1=ALU.add), tB.rf(), LOGW.rf())
                pt, pr = self.ps()
                self.mm(pt[:, :], self.wa_up[64:128, hs], WA.h()[64:128, sl], True, True, [self.wa_up_r] + WA.rh(tt * 512, (tt + 1) * 512), [pr])
                self.act(lambda e, pt=pt: e.activation(out=tB.f(), in_=pt[:, :], func=AF.Tanh, bias=dv[:, 4 + hp:5 + hp], scale=0.5), [pr, dv_r], tB.rf())
                self.dve(lambda e: e.tensor_scalar(out=Aa.f(), in0=tB.f(), scalar1=0.5, scalar2=0.5, op0=ALU.mult, op1=ALU.add), tB.rf(), Aa.rf())
                pt, pr = self.ps()
                self.mm(pt[:, :], self.g_up[:, hs], SG.h()[:, sl], True, True, [self.g_up_r] + SG.rh(tt * 512, (tt + 1) * 512), [pr])
                self.act(lambda e, pt=pt: e.copy(out=G.h()[:, sl], in_=pt[:, :]), [pr], G.rh(tt * 512, (tt + 1) * 512))
                self.dve(lambda e: e.tensor_scalar_mul(out=tB.f(), in0=Kk0.f(), scalar1=rw[:, 8 + hp:9 + hp]), Kk0.rf() + [rw_r], tB.rf())
                self.act(lambda e: e.activation(out=tA.f(), in_=tB.f(), func=AF.Square), tB.rf(), tA.rf())
                pt, pr = self.ps()
                self.mm(pt[:, :], bones[:], tA.f(), True, True, [bones_r] + tA.rf(), [pr])
                self.act(lambda e, pt=pt: e.copy(out=tA.f(), in_=pt[:, :]), [pr], tA.rf())
                self.act(lambda e: e.activation(out=tA.f(), in_=tA.f(), func=AF.Ln), tA.rf(), tA.rf())
                self.act(lambda e: e.activation(out=tA.f(), in_=tA.f(), func=AF.Exp, scale=-0.5), tA.rf(), tA.rf())
                self.dve(lambda e: e.tensor_tensor(out=KK.f(), in0=tB.f(), in1=tA.f(), op=ALU.mult), tB.rf() + tA.rf(), KK.rf())
                self.dve(lambda e: e.tensor_scalar(out=tA.f(), in0=Aa.f(), scalar1=rw[:, 12 + hp:13 + hp], scalar2=dv[:, 8 + hp:9 + hp],
                                                   op0=ALU.mult, op1=ALU.add), Aa.rf() + [rw_r, dv_r], tA.rf())
                self.dve(lambda e: e.scalar_tensor_tensor(out=Kk0.f(), in0=tA.f(), scalar=1.0, in1=Kk0.f(), op0=ALU.add, op1=ALU.mult),
                         tA.rf() + Kk0.rf(), Kk0.rf())
                self.dve(lambda e: e.tensor_tensor(out=tB.f(), in0=KK.f(), in1=Aa.f(), op=ALU.mult), KK.rf() + Aa.rf(), tB.rf())
                self.dve(lambda e: e.scalar_tensor_tensor(out=tA.f(), in0=Rr.f(), scalar=rw[:, 16 + hp:17 + hp], in1=Kk0.f(), op0=ALU.mult, op1=ALU.mult),
                         Rr.rf() + Kk0.rf() + [rw_r], tA.rf())
                pt, pr = self.ps()
                self.mm(pt[:, :], bones[:], tA.f(), True, True, [bones_r] + tA.rf(), [pr])
                self.dve(lambda e, pt=pt: e.tensor_tensor(out=BON.h()[:, sl], in0=pt[:, :], in1=vbt.h(0, 512), op=ALU.mult), [pr] + vbt.rh(), BON.rh(tt * 512, (tt + 1) * 512))
                self.dve(lambda e: e.tensor_tensor_scan(out=L.f(), data0=smask[:], data1=LOGW.f(), initial=0.0, op0=ALU.mult, op1=ALU.add),
                         [smask_r] + LOGW.rf(), L.rf())
                self.act(lambda e: e.activation(out=E.f(), in_=L.f(), func=AF.Exp), L.rf(), E.rf())
                self.dve(lambda e: e.tensor_tensor(out=ARv[:, 1, sl], in0=Rr.f(), in1=E.f(), op=ALU.mult), Rr.rf() + E.rf(), AR.rh(T + tt * 512, T + (tt + 1) * 512))
                self.dve(lambda e: e.tensor_tensor(out=tA.f(), in0=L.f(), in1=LOGW.f(), op=ALU.subtract), L.rf() + LOGW.rf(), tA.rf())
                self.act(lambda e: e.activation(out=E.f(), in_=tA.f(), func=AF.Exp), tA.rf(), E.rf())
                self.dve(lambda e: e.scalar_tensor_tensor(out=ARv[:, 0, sl], in0=KK.f(), scalar=-1.0, in1=E.f(), op0=ALU.mult, op1=ALU.mult),
                         KK.rf() + E.rf(), AR.rh(tt * 512, (tt + 1) * 512))
                self.act(lambda e: e.activation(out=E.f(), in_=L.f(), func=AF.Exp, scale=-1.0), L.rf(), E.rf())
                self.dve(lambda e: e.tensor_tensor(out=BT.h()[:, sl], in0=tB.f(), in1=E.f(), op=ALU.mult), tB.rf() + E.rf(), BT.rh(tt * 512, (tt + 1) * 512))
                self.dve(lambda e: e.tensor_tensor(out=KT.h()[:, sl], in0=Kk0.f(), in1=E.f(), op=ALU.mult), Kk0.rf() + E.rf(), KT.rh(tt * 512, (tt + 1) * 512))
                Lv = L.f().rearrange("p (c n) -> p c n", c=4)
                self.dve(lambda e: e.tensor_tensor(out=tA.f().rearrange("p (c n) -> p c n", c=4), in0=Lv[:, :, CH - 1:CH].broadcast_to([128, 4, CH]),
                                                   in1=Lv, op=ALU.subtract), L.rf(), tA.rf())
                self.act(lambda e: e.activation(out=E.f(), in_=tA.f(), func=AF.Exp), tA.rf(), E.rf())
                self.act(lambda e: e.activation(out=self.GC[:, tt * 4:(tt + 1) * 4], in_=Lv[:, :, CH - 1], func=AF.Exp), L.rf(), [self.GC_r])
                self.dve(lambda e: e.tensor_tensor(out=bht.h(0, 512), in0=tB.f(), in1=E.f(), op=ALU.mult), tB.rf() + E.rf(), bht.rh())
                self.dve(lambda e: e.tensor_tensor(out=kht.h(0, 512), in0=Kk0.f(), in1=E.f(), op=ALU.mult), Kk0.rf() + E.rf(), kht.rh())
                pt, pr = self.ps()
                ptv = pt[:].bitcast(BF16).rearrange("p (c t) -> p c t", c=8)
                pt2, pr2 = self.ps()
                ptv2 = pt2[:].bitcast(BF16).rearrange("p (c t) -> p c t", c=8)
                for j in range(4):
                    js = slice(j * 128, (j + 1) * 128)
                    self.pe(lambda e, j=j, js=js, ptv=ptv: e.transpose(out=ptv[:, j, :], in_=vbt.h(0, 512)[:, js], identity=idb[:]), vbt.rh() + [idb_r], [pr])
                    self.pe(lambda e, j=j, js=js, ptv=ptv: e.transpose(out=ptv[:, 4 + j, :], in_=bht.h(0, 512)[:, js], identity=idb[:]), bht.rh() + [idb_r], [pr])
                    self.pe(lambda e, j=j, js=js, ptv2=ptv2: e.transpose(out=ptv2[:, j, :], in_=kht.h(0, 512)[:, js], identity=idb[:]), kht.rh() + [idb_r], [pr2])
                cs = slice(tt * 4, (tt + 1) * 4)
                self.act(lambda e, ptv=ptv: e.copy(out=VPv[:, 0, cs, 0:64], in_=ptv[:, 0:4, 0:64]), [pr], VP.rh())
                self.dve(lambda e, ptv=ptv: e.tensor_copy(out=VPv[:, 1, cs, 64:128], in_=ptv[:, 0:4, 64:128]), [pr], VP.rh())
                self.dve(lambda e, ptv=ptv: e.tensor_copy(out=BHv[:, cs, :], in_=ptv[:, 4:8, :]), [pr], BH.rh(tt * 512, (tt + 1) * 512))
                self.act(lambda e, ptv2=ptv2: e.copy(out=KHv[:, cs, :], in_=ptv2[:, 0:4, :]), [pr2], KH.rh(tt * 512, (tt + 1) * 512))

            def unit_regs(u):
                ll = ALL.h(u * 512, (u + 1) * 512)
                llr = ALL.rh(u * 512, (u + 1) * 512)
                s8 = u % 8
                sc = NSC.h(s8 * 1024, (s8 + 1) * 1024)
                scr_ = NSC.rh(s8 * 1024, (s8 + 1) * 1024)
                return ll, llr, sc, scr_

            def aphase(chunks):
                units = [(cidx, hd) for cidx in chunks for hd in range(2)]
                st = {}
                for (cidx, hd) in units:
                    u = (cidx % 8) * 2 + hd
                    ll, llr, sc, scr_ = unit_regs(u)
                    ts = slice(cidx * 128, (cidx + 1) * 128)
                    ps_ = slice(hd * 64, (hd + 1) * 64)
                    rdA = AR.rh(cidx * 128, (cidx + 1) * 128) + AR.rh(T + cidx * 128, T + (cidx + 1) * 128)
                    p1, r1 = self.ps()
                    self.mm(p1[:, 0:256].rearrange("p (a t) -> p a t", a=2), BT.h()[ps_, ts], ARv[ps_, :, ts], True, True, BT.rh(cidx * 128, (cidx + 1) * 128) + rdA, [r1])
                    p2, r2 = self.ps()
                    self.mm(p2[:, 0:256].rearrange("p (a t) -> p a t", a=2), KT.h()[ps_, ts], ARv[ps_, :, ts], True, True, KT.rh(cidx * 128, (cidx + 1) * 128) + rdA, [r2])
                    self.mm(p2[:, 256:384], ARv[ps_, 0, ts], BT.h()[ps_, ts], True, True, BT.rh(cidx * 128, (cidx + 1) * 128) + rdA, [r2])
                    self.dve(lambda e, p1=p1, sc=sc: e.tensor_tensor(out=sc[:, 640:768], in0=p1[:, 0:128], in1=m2[:, 0:128], op=ALU.mult), [r1, m2_r], scr_)
                    self.dve(lambda e, p1=p1, ll=ll: e.tensor_tensor(out=ll[:, 384:512], in0=p1[:, 128:256], in1=m2[:, 128:256], op=ALU.mult), [r1, m2_r], llr)
                    self.dve(lambda e, p2=p2, ll=ll: e.tensor_tensor(out=ll[:, 128:384], in0=p2[:, 0:256], in1=m2[:, :], op=ALU.mult), [r2, m2_r], llr)
                    self.dve(lambda e, p2=p2, sc=sc: e.tensor_tensor(out=sc[:, 0:128], in0=p2[:, 256:384], in1=msl[:], op=ALU.mult), [r2, msl_r], scr_)
                    self.dve(lambda e, ll=ll, sc=sc: e.tensor_tensor(out=ll[:, 0:128], in0=sc[:, 640:768], in1=idb[:], op=ALU.add), scr_ + [idb_r], llr)
                    st[(cidx, hd)] = (ll, llr, sc, scr_)
                def stageA(u_, lvl):
                    ll, llr, sc, scr_ = st[u_]
                    if lvl == 1:
                        Np, NTp = sc[:, 640:768], sc[:, 0:128]
                    else:
                        o = 128 + ((lvl - 1) % 2) * 256
                        Np, NTp = sc[:, o:o + 128], sc[:, o + 128:o + 256]
                    o2 = 128 + (lvl % 2) * 256
                    pn, rn = self.ps()
                    if lvl < 6:
                        self.mm(pn[:, 0:128], NTp, Np, True, True, scr_, [rn])
                    self.mm(pn[:, 128:256], Np, NTp, True, True, scr_, [rn])
                    if lvl < 6:
                        self.act(lambda e, pn=pn, sc=sc, o2=o2: e.copy(out=sc[:, o2:o2 + 256], in_=pn[:, 0:256]), [rn], scr_)
                    else:
                        self.act(lambda e, pn=pn, sc=sc, o2=o2: e.copy(out=sc[:, o2 + 128:o2 + 256], in_=pn[:, 128:256]), [rn], scr_)

                def stageB(u_, lvl):
                    ll, llr, sc, scr_ = st[u_]
                    o2 = 128 + (lvl % 2) * 256
                    pq, rq = self.ps()
                    self.mm(pq[:, 0:128], sc[:, o2 + 128:o2 + 256], ll[:, 0:128], True, True, scr_ + llr, [rq])
                    self.dve(lambda e, pq=pq, ll=ll: e.tensor_tensor(out=ll[:, 0:128], in0=pq[:, 0:128], in1=ll[:, 0:128], op=ALU.add), [rq] + llr, llr)

                SK = 3
                work = [(u_, lvl) for lvl in range(1, 7) for u_ in units]
                for idx in range(len(work) + SK):
                    if idx < len(work):
                        stageA(*work[idx])
                    if idx - SK >= 0:
                        stageB(*work[idx - SK])

            def seq(cidx):
                ts = slice(cidx * 128, (cidx + 1) * 128)
                lls = []
                for hd in range(2):
                    u = (cidx % 8) * 2 + hd
                    ll, llr, _, _ = unit_regs(u)
                    lls.append((ll, llr))
                STP, STB, TMS, XB, UB, UP0, UP1 = self.STP, self.STB, self.TMS, self.XB, self.UB, self.UP0, self.UP1
                rdA0 = AR.rh(cidx * 128, (cidx + 1) * 128)
                rdA1 = AR.rh(T + cidx * 128, T + (cidx + 1) * 128)
                px, rx = self.ps()
                self.mm(px[:, 0:128], ARv[:, 0, ts], STB[:], True, False, rdA0 + [self.STB_r], [rx])
                for hd in range(2):
                    self.mm(px[:, hd * 64:(hd + 1) * 64], lls[hd][0][:, 128:256], VPv[:, hd, cidx, hd * 64:(hd + 1) * 64], False, hd == 1,
                            lls[hd][1] + VP.rh(), [rx])
                self.act(lambda e, px=px: e.copy(out=XB[:], in_=px[:, 0:128]), [rx], [self.XB_r])
                kseq = int(os.environ.get("KSEQ", "99"))
                if kseq <= 1:
                    return
                pu, ru = self.ps()
                kv2 = int(os.environ.get("KV2", "0"))
                for hd in range(2):
                    if kv2 == 1 and hd == 1:
                        continue
                    if kv2 == 2 and hd == 0:
                        continue
                    lq = self.identb[:] if kv2 == 3 else lls[hd][0][:, 0:128]
                    if kv2 == 4:
                        lq = lls[hd][0][:, 384:512]
                    if kv2 == 5:
                        lq = lls[hd][0][:, 256:384]
                    self.mm(pu[:, hd * 64:(hd + 1) * 64], lq, XB[:, hd * 64:(hd + 1) * 64], True, True, lls[hd][1] + [self.XB_r], [ru])
                self.act(lambda e, pu=pu: e.copy(out=UB[:], in_=pu[:, 0:128]), [ru], [self.UB_r])
                kvar = int(os.environ.get("KVAR", "0"))
                if kvar == 3:
                    self.dve(lambda e, pu=pu: e.tensor_copy(out=UP0[:, 0:64], in_=XB[:, 0:64]), [self.XB_r], [self.UP0_r])
                elif kvar == 4:
                    self.dve(lambda e, pu=pu: e.tensor_copy(out=TMS[:, 0:64], in_=pu[:, 0:64]), [ru], [self.TMS_r])
                elif kvar == 5:
                    self.act(lambda e, pu=pu: e.copy(out=UP0[:, 0:64], in_=pu[:, 0:64]), [ru], [self.UP0_r])
                elif kvar != 1:
                    self.dve(lambda e, pu=pu: e.tensor_copy(out=UP0[:, 0:64], in_=pu[:, 0:64]), [ru], [self.UP0_r])
                if kvar == 0:
                    self.dve(lambda e, pu=pu: e.tensor_copy(out=UP1[:, 64:128], in_=pu[:, 64:128]), [ru], [self.UP1_r])
                if kseq <= 2:
                    return
                py, ry = self.ps()
                kv3 = int(os.environ.get("KV3", "7"))
                kv4 = int(os.environ.get("KV4", "0"))
                lst = {0: STB, 1: self.identb, 2: XB, 3: UB}[kv4]
                kv5 = int(os.environ.get("KV5", "0"))
                rr_ = ARv[:, 0, ts] if kv5 == 1 else ARv[:, 1, ts]
                self.mm(py[:, 0:128], lst[:], rr_, True, kv5 == 2, rdA1 + [self.STB_r], [ry])
                ups = [(UP0, self.UP0_r), (UP1, self.UP1_r)]
                for hd in range(2):
                    if kv3 & 2:
                        self.mm(py[:, 0:128], ups[hd][0][:], lls[hd][0][:, 384:512], False, False, [ups[hd][1]] + lls[hd][1], [ry])
                    if kv3 & 4:
                        self.mm(py[:, 0:128], VPv[:, hd, cidx, :], lls[hd][0][:, 256:384], False, hd == 1, VP.rh() + lls[hd][1], [ry])
                yt = YT[(cidx // 4) % 2]
                yo = (cidx % 4) * 128
                self.act(lambda e, py=py: e.copy(out=yt.f(yo, yo + 128), in_=py[:, 0:128]), [ry], yt.rf())
                if kseq <= 3:
                    return
                pS, rS = self.ps()
                self.mm(pS[:, 0:128], BHv[:, cidx, :], UB[:], True, False, BH.rh(cidx * 128, (cidx + 1) * 128) + [self.UB_r], [rS])
                self.mm(pS[:, 0:128], KHv[:, cidx, :], VPv[:, 0, cidx, :], False, False, KH.rh(cidx * 128, (cidx + 1) * 128) + VP.rh(), [rS])
                self.mm(pS[:, 0:128], KHv[:, cidx, :], VPv[:, 1, cidx, :], False, True, KH.rh(cidx * 128, (cidx + 1) * 128) + VP.rh(), [rS])
                self.dve(lambda e, pS=pS: e.scalar_tensor_tensor(out=TMS[:], in0=STP[:], scalar=self.GC[:, cidx:cidx + 1], in1=pS[:, 0:128], op0=ALU.mult, op1=ALU.add),
                         [self.STP_r, self.GC_r, rS], [self.TMS_r])
                self.dve(lambda e: e.tensor_tensor(out=STP[:], in0=TMS[:], in1=bmask[:], op=ALU.mult), [self.TMS_r, bmask_r], [self.STP_r])
                self.dve(lambda e: e.tensor_tensor(out=STB[:], in0=TMS[:], in1=bmask[:], op=ALU.mult), [self.TMS_r, bmask_r], [self.STB_r])

            def post(tt):
                sl = slice(tt * 512, (tt + 1) * 512)
                yt = YT[tt % 2]
                pt, pr = self.ps()
                self.mm(pt[:, :], bavg[:], yt.f(), True, True, [bavg_r] + yt.rf(), [pr])
                self.dve(lambda e, pt=pt: e.tensor_tensor(out=tA.f(), in0=yt.f(), in1=pt[:, :], op=ALU.subtract), yt.rf() + [pr], tA.rf())
                self.act(lambda e: e.activation(out=tB.f(), in_=tA.f(), func=AF.Square), tA.rf(), tB.rf())
                pt, pr = self.ps()
                self.mm(pt[:, :], bavg[:], tB.f(), True, True, [bavg_r] + tB.rf(), [pr])
                self.dve(lambda e, pt=pt: e.tensor_scalar_add(out=tB.f(), in0=pt[:, :], scalar1=64e-5), [pr], tB.rf())
                self.act(lambda e: e.activation(out=tB.f(), in_=tB.f(), func=AF.Ln), tB.rf(), tB.rf())
                self.act(lambda e: e.activation(out=tB.f(), in_=tB.f(), func=AF.Exp, scale=-0.5), tB.rf(), tB.rf())
                self.dve(lambda e: e.tensor_tensor(out=tA.f(), in0=tA.f(), in1=tB.f(), op=ALU.mult), tA.rf() + tB.rf(), tA.rf())
                self.dve(lambda e: e.tensor_scalar(out=tA.f(), in0=tA.f(), scalar1=rw[:, 20 + hp:21 + hp], scalar2=rw[:, 24 + hp:25 + hp], op0=ALU.mult, op1=ALU.add),
                         tA.rf() + [rw_r], tA.rf())
                self.dve(lambda e: e.tensor_tensor(out=tA.f(), in0=tA.f(), in1=BON.h()[:, sl], op=ALU.add), tA.rf() + BON.rh(tt * 512, (tt + 1) * 512), tA.rf())
                self.dve(lambda e: e.tensor_tensor(out=yv[:, hp, sl], in0=tA.f(), in1=G.h()[:, sl], op=ALU.mult), tA.rf() + G.rh(tt * 512, (tt + 1) * 512),
                         yrwT.rh(hp * T + tt * 512, hp * T + (tt + 1) * 512))

            for tt in range(4):
                prep(tt)
            if cut == 2:
                return
            if cut == 3:
                aphase(range(0, 4))
                return
            if cut == 8:
                aphase(range(0, 4))
                self.dve(lambda e: e.tensor_copy(out=tA.f(), in_=ALL.h(0, 512)), ALL.rh(0, 512), tA.rf())
                self.load(self.dbg[0:128, 0:512], tA.f(), tA.rf(), [self.out_res])
                self.dve(lambda e: e.tensor_copy(out=tB.f(), in_=ALL.h(512, 1024)), ALL.rh(512, 1024), tB.rf())
                self.load(self.dbg[0:128, 512:1024], tB.f(), tB.rf(), [self.out_res])
                self.nodump = True
                return
            if cut == 5:
                aphase(range(0, 4))
                aphase(range(4, 8))
                return
            if cut == 6:
                aphase(range(0, 4))
                seq(0)
                return
            if cut == 7:
                aphase(range(0, 4))
                for cidx in range(0, 4):
                    seq(cidx)
                return
            if cut == 4:
                aphase(range(0, 4))
                aphase(range(4, 8))
                for cidx in range(0, 4):
                    seq(cidx)
                post(0)
                return
            for half in range(2):
                aphase(range(half * 8, half * 8 + 4))
                aphase(range(half * 8 + 4, half * 8 + 8))
                for cidx in range(half * 8, half * 8 + 8):
                    seq(cidx)
                    if cidx % 4 == 3:
                        post(cidx // 4)

        for hp in range(4):
            run_hp(hp)
            if cut >= 2:
                return

    def mla(self, b, hT, ymlaT, scr):
        c = self.c
        w_in_v = self.wd["w_in"].rearrange("(c p) n -> p c n", p=128)
        hTv = hT.h().rearrange("p (c t) -> p c t", c=8)
        ones, ones_r = c["allones"]
        fq, fq_r = c["rope_fq"]
        ph, ph_r = c["rope_ph"]
        m2, m2_r = self.mask2, self.mask2_r
        yv = ymlaT.h().rearrange("p (c t) -> p c t", c=8)

        TAB = scr.get(6)
        CQN = scr.get(6)
        CKVN = scr.get(4)
        KROT = scr.get(2)
        V = scr.get(9)
        wlat = scr.get(3)
        wv = scr.get(1)
        t1, t2, t3, t4 = [scr.get(1) for _ in range(4)]
        QTs = [scr.get(2), scr.get(2)]
        KTs = [scr.get(2), scr.get(2)]
        PTs = [scr.get(1) for _ in range(3)]
        TABv = TAB.h().rearrange("p (k t) -> p k t", k=3)
        CQNv = CQN.h().rearrange("p (k t) -> p k t", k=3)
        CKVNv = CKVN.h().rearrange("p (k t) -> p k t", k=2)
        Vv = V.h(0, 16 * 8 * 66).rearrange("p (i h d) -> p i h d", i=16, h=8)

        for tt in range(4):
            sl = slice(tt * 512, (tt + 1) * 512)
            self.load(t1.i(), self.pos[b:b + 1, sl].partition_broadcast(128), [], t1.rf())
            self.dve(lambda e: e.tensor_copy(out=t2.f(), in_=t1.i()), t1.rf(), t2.rf())
            for k in range(3):
                self.dve(lambda e, k=k: e.tensor_scalar(out=t3.f(), in0=t2.f(), scalar1=fq[:, k:k + 1], scalar2=ph[:, k:k + 1], op0=ALU.mult, op1=ALU.add),
                         t2.rf() + [fq_r, ph_r], t3.rf())
                self.dve(lambda e: e.tensor_copy(out=t4.i(), in_=t3.f()), t3.rf(), t4.rf())
                self.dve(lambda e: e.tensor_copy(out=t1.f(), in_=t4.i()), t4.rf(), t1.rf())
                self.dve(lambda e: e.tensor_tensor(out=t3.f(), in0=t3.f(), in1=t1.f(), op=ALU.subtract), t3.rf() + t1.rf(), t3.rf())
                self.dve(lambda e: e.tensor_single_scalar(out=t1.f(), in_=t3.f(), scalar=0.5, op=ALU.is_gt), t3.rf(), t1.rf())
                self.dve(lambda e: e.tensor_tensor(out=t3.f(), in0=t3.f(), in1=t1.f(), op=ALU.subtract), t3.rf() + t1.rf(), t3.rf())
                self.dve(lambda e: e.tensor_single_scalar(out=t1.f(), in_=t3.f(), scalar=-0.5, op=ALU.is_lt), t3.rf(), t1.rf())
                self.dve(lambda e: e.tensor_tensor(out=t3.f(), in0=t3.f(), in1=t1.f(), op=ALU.add), t3.rf() + t1.rf(), t3.rf())
                self.act(lambda e, k=k, sl=sl: e.activation(out=TABv[:, k, sl], in_=t3.f(), func=AF.Sin, scale=6.283185), t3.rf(),
                         TAB.rh(k * T + tt * 512, k * T + (tt + 1) * 512))

        def latent(col0, nchunk, gname, OUT, OUTv):
            gq, gq_r = self.pv[gname]
            wl = wlat.h(0, 8 * nchunk * 128).rearrange("p (c n) -> p c n", c=8)
            self.loadw(wl, w_in_v[:, :, col0:col0 + nchunk * 128], [], wlat.rh(0, 8 * nchunk * 128))
            tmps = [t1, t2, t3]
            for tt in range(4):
                sl = slice(tt * 512, (tt + 1) * 512)
                pss, rss = self.ps()
                for j in range(nchunk):
                    pt, pr = self.ps()
                    for cc in range(8):
                        self.mm(pt[:, :], wl[:, cc, j * 128:(j + 1) * 128], hTv[:, cc, sl], cc == 0, cc == 7,
                                wlat.rh(0, 8 * nchunk * 128) + hT.rh(cc * T + tt * 512, cc * T + (tt + 1) * 512), [pr])
                    tj = tmps[j]
                    self.act(lambda e, pt=pt, tj=tj: e.copy(out=tj.f(), in_=pt[:, :]), [pr], tj.rf())
                    self.act(lambda e, tj=tj: e.activation(out=t4.f(), in_=tj.f(), func=AF.Square), tj.rf(), t4.rf())
                    self.mm(pss[:, :], ones[:], t4.f(), j == 0, j == nchunk - 1, [ones_r] + t4.rf(), [rss])
                self.dve(lambda e, pss=pss: e.tensor_scalar(out=t4.f(), in0=pss[:, :], scalar1=1.0 / (nchunk * 128), scalar2=1e-6, op0=ALU.mult, op1=ALU.add),
                         [rss], t4.rf())
                self.act(lambda e: e.activation(out=t4.f(), in_=t4.f(), func=AF.Ln), t4.rf(), t4.rf())
                self.act(lambda e: e.activation(out=t4.f(), in_=t4.f(), func=AF.Exp, scale=-0.5), t4.rf(), t4.rf())
                for j in range(nchunk):
                    tj = tmps[j]
                    self.dve(lambda e, j=j, tj=tj, sl=sl: e.scalar_tensor_tensor(out=OUTv[:, j, sl], in0=tj.f(), scalar=gq[:, j:j + 1], in1=t4.f(), op0=ALU.mult, op1=ALU.mult),
                             tj.rf() + t4.rf() + [gq_r], OUT.rh(j * T + tt * 512, j * T + (tt + 1) * 512))

        latent(CQ0, 3, "pv_gq", CQN, CQNv)
        latent(CKV0, 2, "pv_gkv", CKVN, CKVNv)

        wkr = wlat.h(0, 8 * 256).rearrange("p (c n) -> p c n", c=8)
        self.loadw(wkr, self.wd["w_kr"].rearrange("(c p) n -> p c n", p=128), [], wlat.rh(0, 8 * 256))
        for tt in range(4):
            sl = slice(tt * 512, (tt + 1) * 512)
            pA, rA = self.ps()
            pB, rB = self.ps()
            for cc in range(8):
                rd = wlat.rh(0, 8 * 256) + hT.rh(cc * T + tt * 512, cc * T + (tt + 1) * 512)
                self.mm(pA[:, :], wkr[:, cc, 0:128], hTv[:, cc, sl], cc == 0, cc == 7, rd, [rA])
            for cc in range(8):
                rd = wlat.rh(0, 8 * 256) + hT.rh(cc * T + tt * 512, cc * T + (tt + 1) * 512)
                self.mm(pB[:, :], wkr[:, cc, 128:256], hTv[:, cc, sl], cc == 0, cc == 7, rd, [rB])
            self.dve(lambda e, pA=pA, sl=sl: e.tensor_tensor(out=t1.f(), in0=pA[:, :], in1=TABv[:, 1, sl], op=ALU.mult), [rA] + TAB.rh(T + tt * 512, T + (tt + 1) * 512), t1.rf())
            self.dve(lambda e, pB=pB, sl=sl: e.tensor_tensor(out=t2.f(), in0=pB[:, :], in1=TABv[:, 2, sl], op=ALU.mult), [rB] + TAB.rh(2 * T + tt * 512, 2 * T + (tt + 1) * 512), t2.rf())
            self.dve(lambda e, sl=sl: e.tensor_tensor(out=KROT.h()[:, sl], in0=t1.f(), in1=t2.f(), op=ALU.add), t1.rf() + t2.rf(), KROT.rh(tt * 512, (tt + 1) * 512))

        wvv = wv.h(0, 1024).rearrange("p (c n) -> p c n", c=2)
        self.loadw(wvv, self.wd["w_v"].rearrange("(c p) n -> p c n", p=128), [], wv.rh())
        self.dve(lambda e: e.memset(Vv[:, :, :, 64:66], 1.0), [], V.rh())
        for i in range(16):
            pt, pr = self.ps()
            for kc in range(2):
                self.mm(pt[:, :], CKVNv[:, kc, i * 128:(i + 1) * 128], wvv[:, kc, :], kc == 0, kc == 1,
                        CKVN.rh(kc * T + i * 128, kc * T + (i + 1) * 128) + wv.rh(), [pr])
            self.act(lambda e, pt=pt, i=i: e.copy(out=Vv[:, i, :, 0:64], in_=pt[:, :].rearrange("p (h d) -> p h d", h=8)), [pr], V.rh())

        def head(h):
            QT, KT = QTs[h % 2], KTs[h % 2]
            wq = wlat.h(0, 384).rearrange("p (c n) -> p c n", c=3)
            wk = wlat.h(1024, 1024 + 256).rearrange("p (c n) -> p c n", c=2)
            self.loadw(wq, self.wd["w_q"][:, h * 128:(h + 1) * 128].rearrange("(c p) n -> p c n", p=128), [], wlat.rh(0, 384))
            self.loadw(wk, self.wd["w_k"][:, h * 128:(h + 1) * 128].rearrange("(c p) n -> p c n", p=128), [], wlat.rh(1024, 1280))
            for tt in range(4):
                sl = slice(tt * 512, (tt + 1) * 512)
                pt, pr = self.ps()
                for cc in range(3):
                    self.mm(pt[:, :], wq[:, cc, :], CQNv[:, cc, sl], cc == 0, cc == 2, wlat.rh(0, 384) + CQN.rh(cc * T + tt * 512, cc * T + (tt + 1) * 512), [pr])
                self.dve(lambda e, pt=pt, sl=sl: e.tensor_tensor(out=QT.h()[:, sl], in0=pt[:, :], in1=TABv[:, 0, sl], op=ALU.mult),
                         [pr] + TAB.rh(tt * 512, (tt + 1) * 512), QT.rh(tt * 512, (tt + 1) * 512))
                pt, pr = self.ps()
                for cc in range(2):
                    self.mm(pt[:, :], wk[:, cc, :], CKVNv[:, cc, sl], cc == 0, cc == 1, wlat.rh(1024, 1280) + CKVN.rh(cc * T + tt * 512, cc * T + (tt + 1) * 512), [pr])
                self.dve(lambda e, pt=pt, sl=sl: e.tensor_tensor(out=KT.h()[:, sl], in0=pt[:, :], in1=KROT.h()[:, sl], op=ALU.add),
                         [pr] + KROT.rh(tt * 512, (tt + 1) * 512), KT.rh(tt * 512, (tt + 1) * 512))
            iters = [(qt, kb) for qt in range(4) for kb in range(4 * qt + 4)]
            pos_ = {}
            pts_ = {}
            deferred = []

            def stage_s(n):
                qt, kb = iters[n]
                if kb == 0:
                    pos_[qt] = self.ps(hold=True)
                c0 = max(0, kb - 4 * qt) * 128
                psc, rsc = self.ps()
                self.mm(psc[:, c0:512], KT.h()[:, kb * 128:(kb + 1) * 128], QT.h()[:, qt * 512 + c0:(qt + 1) * 512], True, True,
                        KT.rh(kb * 128, (kb + 1) * 128) + QT.rh(qt * 512, (qt + 1) * 512), [rsc])
                PT = PTs[n % 3]
                pts_[n] = PT
                self.act(lambda e, psc=psc, PT=PT, c0=c0: e.activation(out=PT.h()[:, c0:512], in_=psc[:, c0:512], func=AF.Exp, scale=ATT_SCALE), [rsc], PT.rh())
                if kb >= 4 * qt:
                    self.dve(lambda e, PT=PT, c0=c0: e.tensor_tensor(out=PT.h()[:, c0:c0 + 128], in0=PT.h()[:, c0:c0 + 128], in1=m2[:, 128:256], op=ALU.mult),
                             PT.rh() + [m2_r], PT.rh())

            def epi1(qt):
                po, ro = pos_[qt]
                self.act(lambda e, po=po: e.copy(out=t1.f()[64:65, :], in_=po[64:65, :]), [ro], t1.rf())
                self.dve(lambda e: e.reciprocal(out=t1.f()[64:65, :], in_=t1.f()[64:65, :]), t1.rf(), t1.rf())

            def epi2(qt):
                po, ro = pos_[qt]
                pb_, rb_ = self.ps()
                self.mm(pb_[0:64, :], ones[64:65, 0:64], t1.f()[64:65, :], True, True, [ones_r] + t1.rf(), [rb_])
                self.act(lambda e, pb_=pb_: e.copy(out=t2.f()[0:64, :], in_=pb_[0:64, :]), [rb_], t2.rf())
                self.dve(lambda e, po=po, qt=qt: e.tensor_tensor(out=yv[0:64, h, qt * 512:(qt + 1) * 512], in0=po[0:64, :], in1=t2.f()[0:64, :], op=ALU.mult),
                         [ro] + t2.rf(), ymlaT.rh(h * T + qt * 512, h * T + (qt + 1) * 512))
                self.ps_release(po)

            def stage_p(n):
                qt, kb = iters[n]
                nkb = 4 * qt + 4
                c0 = max(0, kb - 4 * qt) * 128
                po, ro = pos_[qt]
                PT = pts_[n]
                self.mm(po[0:65, c0:512], Vv[:, kb, h, 0:65], PT.h()[:, c0:512], kb == 0, kb == nkb - 1, V.rh() + PT.rh(), [ro])
                if kb == nkb - 1:
                    deferred.append([1, epi1, qt])
                    deferred.append([5, epi2, qt])

            SK = 2
            for idx in range(len(iters) + SK):
                if idx < len(iters):
                    stage_s(idx)
                if idx - SK >= 0:
                    stage_p(idx - SK)
                for d in list(deferred):
                    d[0] -= 1
                    if d[0] <= 0:
                        d[1](d[2])
                        deferred.remove(d)
            for d in list(deferred):
                d[1](d[2])

        for h in range(8):
            head(h)

    def merge(self, b, hT, yrwT, ymlaT, scr, scr2, scr3):
        c = self.c
        w_in_v = self.wd["w_in"].rearrange("(c p) n -> p c n", p=128)
        hTv = hT.h().rearrange("p (c t) -> p c t", c=8)
        yrv = yrwT.h().rearrange("p (c t) -> p c t", c=4)
        ymv = ymlaT.h().rearrange("p (c t) -> p c t", c=8)
        g2, g2_r = self.pv["pv_g2"]
        idf, idf_r = c["ident"]
        MT = scr.get(16)
        MTv = MT.h().rearrange("p (c t) -> p c t", c=8)
        wgr, wgm, wbr, wbm = [scr.get(1) for _ in range(4)]
        t1, t2 = scr.get(1), scr.get(1)
        wgrv = wgr.h().rearrange("p (c n) -> p c n", c=8)
        wgmv = wgm.h().rearrange("p (c n) -> p c n", c=8)
        wbrv = wbr.h(0, 512).rearrange("p (c n) -> p c n", c=4)
        wbmv = wbm.h().rearrange("p (c n) -> p c n", c=8)
        for j in range(8):
            js = slice(j * 128, (j + 1) * 128)
            self.loadw(wgrv, w_in_v[:, :, GRW0 + j * 128:GRW0 + (j + 1) * 128], [], wgr.rh())
            self.loadw(wgmv, w_in_v[:, :, GML0 + j * 128:GML0 + (j + 1) * 128], [], wgm.rh())
            self.loadw(wbrv, self.wd["w_brw"][:, js].rearrange("(c p) n -> p c n", p=128), [], wbr.rh())
            self.loadw(wbmv[0:64], self.wd["w_bml"][:, js].rearrange("(h p) n -> p h n", p=64), [], wbm.rh())
            for tt in range(4):
                sl = slice(tt * 512, (tt + 1) * 512)
                for (wg, wgreg, tdst) in ((wgrv, wgr, t1), (wgmv, wgm, t2)):
                    pg, rg = self.ps()
                    for cc in range(8):
                        self.mm(pg[:, :], wg[:, cc, :], hTv[:, cc, sl], cc == 0, cc == 7, wgreg.rh() + hT.rh(cc * T + tt * 512, cc * T + (tt + 1) * 512), [rg])
                    self.act(lambda e, pg=pg, tdst=tdst: e.activation(out=tdst.f(), in_=pg[:, :], func=AF.Tanh, scale=0.5), [rg], tdst.rf())
                pb1, rb1 = self.ps()
                for cc in range(4):
                    self.mm(pb1[:, :], wbrv[:, cc, :], yrv[:, cc, sl], cc == 0, cc == 3, wbr.rh() + yrwT.rh(cc * T + tt * 512, cc * T + (tt + 1) * 512), [rb1])
                self.dve(lambda e, pb1=pb1: e.scalar_tensor_tensor(out=t1.f(), in0=t1.f(), scalar=1.0, in1=pb1[:, :], op0=ALU.add, op1=ALU.mult), [rb1] + t1.rf(), t1.rf())
                pb2, rb2 = self.ps()
                for hh in range(8):
                    self.mm(pb2[:, :], wbmv[0:64, hh, :], ymv[0:64, hh, sl], hh == 0, hh == 7, wbm.rh() + ymlaT.rh(hh * T + tt * 512, hh * T + (tt + 1) * 512), [rb2])
                self.dve(lambda e, pb2=pb2: e.scalar_tensor_tensor(out=t2.f(), in0=t2.f(), scalar=1.0, in1=pb2[:, :], op0=ALU.add, op1=ALU.mult), [rb2] + t2.rf(), t2.rf())
                self.dve(lambda e: e.tensor_tensor(out=t1.f(), in0=t1.f(), in1=t2.f(), op=ALU.add), t1.rf() + t2.rf(), t1.rf())
                self.act(lambda e, j=j, sl=sl: e.mul(out=MTv[:, j, sl], in_=t1.f(), mul=0.5), t1.rf(), MT.rh(j * T + tt * 512, j * T + (tt + 1) * 512))

        wo = scr.get(8)
        wov = wo.h().rearrange("p (c n) -> p c n", c=8)
        self.loadw(wov, self.wd["w_out"].rearrange("(c p) n -> p c n", p=128), [], wo.rh())
        wrt = scr.get(1)
        wrtv = wrt.f(0, 8 * 36).rearrange("p (c n) -> p c n", c=8)
        self.load(wrtv, self.wd["w_rt"].rearrange("(c p) n -> p c n", p=128), [], wrt.rf())
        xts = [scr2.get(2), scr2.get(2)]
        h2f = scr2.get(2)
        h2Tf = scr2.get(2)
        junk = scr.get(1)
        ACCs = [scr3.get(2), scr3.get(2)]
        gff = scr3.get(2)
        self.load(gff.f(), self.wd["g_ffn"].partition_broadcast(128), [], gff.rf())
        h2gs = [scr3.get(1), scr3.get(1)]
        MM, MM_r, SRf, SRf_r, SR, SR_r, W12, W12_r = self.MM, self.MM_r, self.SRf, self.SRf_r, self.SR, self.SR_r, self.W12, self.W12_r
        msu, msu_r = c["mask_su"]
        ones, ones_r = c["allones"]
        ecap, ecap_r = c["ecap"]
        if b == 0:
            self.tok_init = self.load(self.tokd.rearrange("(p f) o -> p (f o)", p=128), self.ZI[:], [self.ZI_r], [self.tok_r])
            self.scat_ops = []
        tok_init = self.tok_init
        h2Tfv = h2Tf.f().rearrange("p (c t) -> p c t", c=8)
        ssq, ssq_r, rs, rs_r = self.ssq, self.ssq_r, self.rs, self.rs_r
        LG, LG_r = self.LG, self.LG_r
        brt, brt_r = self.brt, self.brt_r
        R8, R8_r = self.R8, self.R8_r
        for i in range(16):
            xt = xts[i % 2]
            row0 = b * T + i * 128
            gi_ = b * 16 + i
            ACCt = ACCs[i % 2]
            self.load(xt.f(), self.x[row0:row0 + 128, :], [], xt.rf())
            for half in range(2):
                hs = slice(half * 512, (half + 1) * 512)
                pt, pr = self.ps()
                for cc in range(8):
                    self.mm(pt[:, :], MTv[:, cc, i * 128:(i + 1) * 128], wov[:, cc, hs], cc == 0, cc == 7,
                            MT.rh(cc * T + i * 128, cc * T + (i + 1) * 128) + wo.rh(), [pr])
                self.dve(lambda e, pt=pt, hs=hs, xt=xt, ACCt=ACCt: e.tensor_tensor(out=ACCt.f()[:, hs], in0=pt[:, :], in1=xt.f()[:, hs], op=ALU.add),
                         [pr] + xt.rf(), ACCt.rf(half * 512, (half + 1) * 512))
            ai = ACCt.rf()
            self.load(self.x1s[row0:row0 + 128, :], ACCt.f(), ai, [self.x1_r[gi_]])
            self.dve(lambda e, i=i: e.memset(ssq[:, i:i + 1], 0.0), [], [ssq_r])
            self.act(lambda e, i=i, ACCt=ACCt: e.activation(out=junk.h(), in_=ACCt.f(), func=AF.Square, accum_out=ssq[:, i:i + 1]), ai + [ssq_r], junk.rh() + [ssq_r])
            self.dve(lambda e, i=i: e.tensor_scalar(out=rs[:, i:i + 1], in0=ssq[:, i:i + 1], scalar1=1.0 / D, scalar2=1e-6, op0=ALU.mult, op1=ALU.add), [ssq_r], [rs_r])
            self.act(lambda e, i=i: e.activation(out=rs[:, i:i + 1], in_=rs[:, i:i + 1], func=AF.Ln), [rs_r], [rs_r])
            self.act(lambda e, i=i: e.activation(out=rs[:, i:i + 1], in_=rs[:, i:i + 1], func=AF.Exp, scale=-0.5), [rs_r], [rs_r])
            self.dve(lambda e, i=i, ACCt=ACCt: e.tensor_scalar_mul(out=h2f.f(), in0=ACCt.f(), scalar1=rs[:, i:i + 1]), ai + [rs_r], h2f.rf())
            h2g = h2gs[i % 2]
            self.dve(lambda e, h2g=h2g: e.tensor_tensor(out=h2g.h(), in0=h2f.f(), in1=gff.f(), op=ALU.mult), h2f.rf() + gff.rf(), h2g.rh())
            self.load(self.h2s[row0:row0 + 128, :], h2g.h(), h2g.rh(), [self.h2s_r[gi_]])
            for grp in range(2):
                pt, pr = self.ps()
                for q in range(4):
                    cc = grp * 4 + q
                    self.pe(lambda e, pt=pt, q=q, cc=cc: e.transpose(out=pt[:, q * 128:(q + 1) * 128], in_=h2f.f(cc * 128, (cc + 1) * 128), identity=idf[:]),
                            h2f.rf() + [idf_r], [pr])
                self.dve(lambda e, pt=pt, grp=grp: e.tensor_tensor(out=h2Tfv[:, grp * 4:(grp + 1) * 4, :], in0=pt[:, :].rearrange("p (c t) -> p c t", c=4),
                                                                 in1=g2[:, grp * 4:(grp + 1) * 4].unsqueeze(2).broadcast_to([128, 4, 128]), op=ALU.mult),
                         [pr, g2_r], h2Tf.rf())
            pl, rl = self.ps()
            for cc in range(8):
                self.mm(pl[:, 0:36], h2Tfv[:, cc, :], wrtv[:, cc, :], cc == 0, cc == 7, h2Tf.rf() + wrt.rf(), [rl])
            self.dve(lambda e, pl=pl, i=i: e.tensor_tensor(out=LG[:, i, :], in0=pl[:, 0:36], in1=brt[:], op=ALU.add), [rl, brt_r], [LG_r])
            lg4 = LG[:, i, 0:4]
            fine = LG[:, i, 4:36]
            r8 = R8
            self.dve(lambda e, lg4=lg4: e.reduce_max(out=r8[:, 0:1], in_=lg4, axis=AX.X), [LG_r], [R8_r])
            self.dve(lambda e, lg4=lg4: e.tensor_scalar(out=r8[:, 8:12], in0=lg4, scalar1=r8[:, 0:1], scalar2=None, op0=ALU.is_equal), [LG_r, R8_r], [R8_r])
            self.dve(lambda e: e.tensor_scalar_mul(out=r8[:, 1:2], in0=r8[:, 0:1], scalar1=-1.0), [R8_r], [R8_r])
            self.dve(lambda e: e.memset(r8[:, 2:3], 0.0), [], [R8_r])
            self.act(lambda e, lg4=lg4: e.activation(out=r8[:, 12:16], in_=lg4, func=AF.Exp, bias=r8[:, 1:2], accum_out=r8[:, 2:3]), [LG_r, R8_r], [R8_r])
            self.dve(lambda e: e.reciprocal(out=r8[:, 3:4], in_=r8[:, 2:3]), [R8_r], [R8_r])
            self.dve(lambda e: e.tensor_scalar(out=r8[:, 16:48].rearrange("p (g k) -> p g k", g=4), in0=r8[:, 8:12].unsqueeze(2).broadcast_to([128, 4, 8]),
                                               scalar1=-1.0, scalar2=1e30, op0=ALU.add, op1=ALU.mult), [R8_r], [R8_r])
            self.dve(lambda e, fine=fine: e.tensor_tensor(out=r8[:, 16:48], in0=r8[:, 16:48], in1=fine, op=ALU.add), [R8_r, LG_r], [R8_r])
            self.dve(lambda e: e.max(out=r8[:, 48:56], in_=r8[:, 16:48]), [R8_r], [R8_r])
            self.dve(lambda e: e.tensor_tensor(out=r8[:, 4:5], in0=r8[:, 49:50], in1=r8[:, 48:49], op=ALU.subtract), [R8_r], [R8_r])
            self.act(lambda e: e.activation(out=r8[:, 4:5], in_=r8[:, 4:5], func=AF.Exp), [R8_r], [R8_r])
            self.dve(lambda e: e.tensor_scalar_add(out=r8[:, 4:5], in0=r8[:, 4:5], scalar1=1.0), [R8_r], [R8_r])
            self.dve(lambda e: e.reciprocal(out=r8[:, 4:5], in_=r8[:, 4:5]), [R8_r], [R8_r])
            self.dve(lambda e: e.tensor_tensor(out=r8[:, 5:6], in0=r8[:, 4:5], in1=r8[:, 3:4], op=ALU.mult), [R8_r], [R8_r])
            self.dve(lambda e: e.tensor_tensor(out=r8[:, 6:7], in0=r8[:, 3:4], in1=r8[:, 5:6], op=ALU.subtract), [R8_r], [R8_r])
            self.dve(lambda e: e.tensor_scalar(out=r8[:, 56:88], in0=r8[:, 16:48], scalar1=r8[:, 48:49], scalar2=None, op0=ALU.is_equal), [R8_r], [R8_r])
            self.dve(lambda e: e.tensor_scalar(out=r8[:, 88:120], in0=r8[:, 16:48], scalar1=r8[:, 49:50], scalar2=None, op0=ALU.is_equal), [R8_r], [R8_r])
            self.dve(lambda e, gi_=gi_: e.tensor_copy(out=W12[:, gi_, :], in_=r8[:, 5:7]), [R8_r], [W12_r])
            self.dve(lambda e, gi_=gi_: e.tensor_tensor(out=MM[:, gi_, :], in0=r8[:, 56:88], in1=r8[:, 88:120], op=ALU.add), [R8_r], [MM_r])
            pR, rR = self.ps()
            for ip in range(gi_):
                self.mm(pR[:, 0:32], ones[:], MM[:, ip, :], ip == 0, False, [ones_r, MM_r], [rR])
            self.mm(pR[:, 0:32], msu[:], MM[:, gi_, :], gi_ == 0, True, [msu_r, MM_r], [rR])
            self.dve(lambda e, pR=pR: e.tensor_tensor(out=r8[:, 120:152], in0=pR[:, 0:32], in1=ecap[:], op=ALU.add), [rR, ecap_r], [R8_r])
            for k in range(2):
                self.dve(lambda e, k=k: e.tensor_tensor(out=r8[:, 152:184], in0=r8[:, 56 + 32 * k:88 + 32 * k], in1=r8[:, 120:152], op=ALU.mult), [R8_r], [R8_r])
                self.dve(lambda e, k=k, gi_=gi_: e.reduce_sum(out=SRf[:, gi_, k:k + 1], in_=r8[:, 152:184], axis=AX.X), [R8_r], [SRf_r])
            self.dve(lambda e, gi_=gi_: e.tensor_copy(out=SR[:, gi_, :], in_=SRf[:, gi_, :]), [SRf_r], [SR_r])
            for k in range(2):
                o = self.p.dma("pool", lambda e, gi_=gi_, k=k: e.indirect_dma_start(
                    out=self.tokd[:, :], out_offset=bass.IndirectOffsetOnAxis(ap=SR[:, gi_, k:k + 1], axis=0),
                    in_=self.TID[:, gi_:gi_ + 1], in_offset=None),
                    [SR_r, self.TID_r], [], extra=[tok_init])
                self.scat_ops.append(o)

    def moe(self, scr):
        idb, idb_r = self.identb, self.identb_r
        wgus = [scr.get(4), scr.get(4)]
        wdns = [scr.get(2), scr.get(2)]
        XGs = [scr.get(1) for _ in range(8)]
        XTs = [scr.get(4), scr.get(4)]
        HTs = [scr.get(1), scr.get(1)]
        YTs = [scr.get(1) for _ in range(4)]
        YKs = [scr.get(1) for _ in range(4)]
        XAs = [scr.get(2), scr.get(2)]
        gf = scr.get(2)
        self.load(gf.f(), self.wd["g_fin"].partition_broadcast(128), [], gf.rf())
        junk = scr.get(1)
        ots = [scr.get(2), scr.get(2)]
        ssq, ssq_r, rs, rs_r = self.ssq, self.ssq_r, self.rs, self.rs_r
        t1, t2 = scr.get(1), scr.get(1)
        NBLK = CAP // 128
        SR, SR_r, W12, W12_r = self.SR, self.SR_r, self.W12, self.W12_r

        def loadw_e(ex):
            wgu, wdn = wgus[ex % 2], wdns[ex % 2]
            self.loadw(wgu.h().rearrange("p (c n) -> p c n", c=8), self.wd["w_gu"][ex].rearrange("(c p) n -> p c n", p=128), [], wgu.rh())
            self.loadw(wdn.h().rearrange("p (c n) -> p c n", c=2), self.wd["w_dn"][ex].rearrange("(c p) n -> p c n", p=128), [], wdn.rh())

        loadw_e(0)
        loadw_e(1)
        gi = 0
        yi = 0
        xg_of = {}

        def gather_e(ex):
            nonlocal gi
            lst = []
            for blk in range(NBLK):
                idx, idx_r = self.IDX[gi % 8]
                XG = XGs[gi % 8]
                gi += 1
                r0 = ex * CAP + blk * 128
                self.p.dma("sp", lambda e, idx=idx, r0=r0: e.dma_start(out=idx[:], in_=self.tokd[r0:r0 + 128, :]), [self.tok_r], [idx_r], extra=self.scat_ops)
                self.p.dma("pool", lambda e, idx=idx, XG=XG: e.indirect_dma_start(
                    out=XG.h(), out_offset=None, in_=self.h2s[:, :], in_offset=bass.IndirectOffsetOnAxis(ap=idx[:, 0:1], axis=0)), [idx_r] + self.h2s_r, XG.rh())
                lst.append(XG)
            xg_of[ex] = lst

        gather_e(0)
        gather_e(1)
        for ex in range(32):
            wgu, wdn = wgus[ex % 2], wdns[ex % 2]
            wguv = wgu.h().rearrange("p (c n) -> p c n", c=8)
            wdnv = wdn.h().rearrange("p (c n) -> p c n", c=2)
            XT = XTs[ex % 2]
            XTv = XT.h().rearrange("p (c t) -> p c t", c=8)
            HT = HTs[ex % 2]
            HTv = HT.h(0, 2 * CAP).rearrange("p (c t) -> p c t", c=2)
            for blk in range(NBLK):
                XG = xg_of[ex][blk]
                pt, pr = self.ps()
                ptv = pt[:].bitcast(BF16).rearrange("p (c t) -> p c t", c=8)
                for cc in range(8):
                    self.pe(lambda e, cc=cc, ptv=ptv, XG=XG: e.transpose(out=ptv[:, cc, :], in_=XG.h(cc * 128, (cc + 1) * 128), identity=idb[:]), XG.rh() + [idb_r], [pr])
                self.act(lambda e, ptv=ptv, XTv=XTv, blk=blk: e.copy(out=XTv[:, :, blk * 128:(blk + 1) * 128], in_=ptv), [pr], XT.rh())
            if ex + 2 < 32:
                pass
            for jc in range(2):
                pG, rG = self.ps()
                for cc in range(8):
                    self.mm(pG[:, 0:CAP], wguv[:, cc, jc * 128:(jc + 1) * 128], XTv[:, cc, :], cc == 0, cc == 7, wgu.rh() + XT.rh(), [rG])
                pU, rU = self.ps()
                for cc in range(8):
                    self.mm(pU[:, 0:CAP], wguv[:, cc, 256 + jc * 128:256 + (jc + 1) * 128], XTv[:, cc, :], cc == 0, cc == 7, wgu.rh() + XT.rh(), [rU])
                self.act(lambda e, pG=pG: e.activation(out=t1.f(0, CAP), in_=pG[:, 0:CAP], func=AF.Tanh, scale=0.5), [rG], t1.rf())
                self.dve(lambda e, pG=pG: e.scalar_tensor_tensor(out=t2.f(0, CAP), in0=t1.f(0, CAP), scalar=1.0, in1=pG[:, 0:CAP], op0=ALU.add, op1=ALU.mult), [rG] + t1.rf(), t2.rf())
                self.dve(lambda e, pU=pU, HTv=HTv, jc=jc: e.scalar_tensor_tensor(out=HTv[:, jc, :], in0=t2.f(0, CAP), scalar=0.5, in1=pU[:, 0:CAP], op0=ALU.mult, op1=ALU.mult), [rU] + t2.rf(), HT.rh())
            for blk in range(NBLK):
                YT = YTs[yi % 4]
                yi += 1
                for half in range(2):
                    hs = slice(half * 512, (half + 1) * 512)
                    pY, rY = self.ps()
                    for jc in range(2):
                        self.mm(pY[:, :], HTv[:, jc, blk * 128:(blk + 1) * 128], wdnv[:, jc, hs], jc == 0, jc == 1, HT.rh() + wdn.rh(), [rY])
                    if half == 0:
                        self.act(lambda e, pY=pY, YT=YT, hs=hs: e.copy(out=YT.h()[:, hs], in_=pY[:, :]), [rY], YT.rh())
                    else:
                        self.dve(lambda e, pY=pY, YT=YT, hs=hs: e.tensor_copy(out=YT.h()[:, hs], in_=pY[:, :]), [rY], YT.rh())
                r0 = ex * CAP + blk * 128
                self.load(self.ysd[r0:r0 + 128, :], YT.h(), YT.rh(), [self.ys_r[ex]])
            if ex + 2 < 32:
                gather_e(ex + 2)
                loadw_e(ex + 2)
        ki = 0
        for g in range(32):
            XA = XAs[g % 2]
            self.load(XA.f(), self.x1s[g * 128:(g + 1) * 128, :], [self.x1_r[g]], XA.rf())
            for k in range(2):
                YK = YKs[ki % 4]
                ki += 1
                self.p.dma("pool", lambda e, YK=YK, g=g, k=k: e.indirect_dma_start(
                    out=YK.h(), out_offset=None, in_=self.ysd[:, :], in_offset=bass.IndirectOffsetOnAxis(ap=SR[:, g, k:k + 1], axis=0)), [SR_r] + self.ys_r, YK.rh())
                self.dve(lambda e, YK=YK, g=g, k=k, XA=XA: e.scalar_tensor_tensor(out=XA.f(), in0=YK.h(), scalar=W12[:, g, k:k + 1], in1=XA.f(), op0=ALU.mult, op1=ALU.add),
                         YK.rh() + [W12_r] + XA.rf(), XA.rf())
            i = g % 16
            ot = ots[g % 2]
            self.dve(lambda e, i=i: e.memset(ssq[:, i:i + 1], 0.0), [], [ssq_r])
            self.act(lambda e, i=i, XA=XA: e.activation(out=junk.h(), in_=XA.f(), func=AF.Square, accum_out=ssq[:, i:i + 1]), XA.rf() + [ssq_r], junk.rh() + [ssq_r])
            self.dve(lambda e, i=i: e.tensor_scalar(out=rs[:, i:i + 1], in0=ssq[:, i:i + 1], scalar1=1.0 / D, scalar2=1e-6, op0=ALU.mult, op1=ALU.add), [ssq_r], [rs_r])
            self.act(lambda e, i=i: e.activation(out=rs[:, i:i + 1], in_=rs[:, i:i + 1], func=AF.Ln), [rs_r], [rs_r])
            self.act(lambda e, i=i: e.activation(out=rs[:, i:i + 1], in_=rs[:, i:i + 1], func=AF.Exp, scale=-0.5), [rs_r], [rs_r])
            self.dve(lambda e, i=i, ot=ot, XA=XA: e.scalar_tensor_tensor(out=ot.f(), in0=XA.f(), scalar=rs[:, i:i + 1], in1=gf.f(), op0=ALU.mult, op1=ALU.mult),
                     XA.rf() + [rs_r] + gf.rf(), ot.rf())
            self.load(self.out[g * 128:(g + 1) * 128, :], ot.f(), ot.rf(), [self.out_res])

    def final(self, b, ACC, scr):
        ACCv = ACC.f().rearrange("p (i d) -> p i d", i=16)
        gf = scr.get(2)
        self.load(gf.f(), self.wd["g_fin"].partition_broadcast(128), [], gf.rf())
        junk = scr.get(1)
        ots = [scr.get(2), scr.get(2)]
        ssq, ssq_r, rs, rs_r = self.ssq, self.ssq_r, self.rs, self.rs_r
        for i in range(16):
            ai = ACC.rf(i * 1024, (i + 1) * 1024)
            ot = ots[i % 2]
            self.dve(lambda e, i=i: e.memset(ssq[:, i:i + 1], 0.0), [], [ssq_r])
            self.act(lambda e, i=i: e.activation(out=junk.h(), in_=ACCv[:, i, :], func=AF.Square, accum_out=ssq[:, i:i + 1]), ai + [ssq_r], junk.rh() + [ssq_r])
            self.dve(lambda e, i=i: e.tensor_scalar(out=rs[:, i:i + 1], in0=ssq[:, i:i + 1], scalar1=1.0 / D, scalar2=1e-6, op0=ALU.mult, op1=ALU.add), [ssq_r], [rs_r])
            self.act(lambda e, i=i: e.activation(out=rs[:, i:i + 1], in_=rs[:, i:i + 1], func=AF.Ln), [rs_r], [rs_r])
            self.act(lambda e, i=i: e.activation(out=rs[:, i:i + 1], in_=rs[:, i:i + 1], func=AF.Exp, scale=-0.5), [rs_r], [rs_r])
            self.dve(lambda e, i=i, ot=ot: e.scalar_tensor_tensor(out=ot.f(), in0=ACCv[:, i, :], scalar=rs[:, i:i + 1], in1=gf.f(), op0=ALU.mult, op1=ALU.mult),
                     ai + [rs_r] + gf.rf(), ot.rf())
            row0 = b * T + i * 128
            self.load(self.out[row0:row0 + 128, :], ot.f(), ot.rf(), [self.out_res])

    def dump_bf16_fm(self, reg, nchunk, scr):
        tmp = scr.get(4)
        v = reg.h().rearrange("p (c t) -> p c t", c=nchunk)
        for cc in range(nchunk):
            self.dve(lambda e, cc=cc: e.tensor_copy(out=tmp.f(), in_=v[:, cc, :]), reg.rh(cc * T, (cc + 1) * T), tmp.rf())
            self.load(self.dbg[cc * 128:(cc + 1) * 128, :], tmp.f(), tmp.rf(), [self.out_res])

    def finish(self):
        fin = self.p.op("sp", lambda e: e.nop(), [self.out_res], [])
        for e in ("pe", "act", "dve", "pool"):
            last = [o for o in self.p.ops[e] if not o.dma]
            if last:
                last[-1].signal = True
                fin.deps.append(last[-1])
        for q in ("sp", "pool"):
            for o in self.p.dlast[q]:
                if o is not None and o is not fin:
                    fin.deps.append(o)
        import os, sys
        if os.environ.get("KFILL", "1") == "1":
            idb = self.identb
            fb = self.psb[7]
            self.p.filler_fn = lambda e: e.matmul(out=fb[:, 0:128], lhsT=idb[:], rhs=idb[:], start=True, stop=True)
            self.p.filler_dep = self.identb_op
        if os.environ.get("KSCHED", "1") == "1":
            self.p.schedule()
            print("NFILL", getattr(self.p, "nfill", 0), file=sys.stderr)
        with self.nc.Block() as block:
            self.p.emit(block)
        self.st.close()
        import sys
        print("ICOUNT", self.p.icount, file=sys.stderr)
        return self.nc


def build(debug=None):
    B = Builder(debug)
    B.setup()
    ar = B.ar
    hT = ar.reg(0, 16)
    yrwT = ar.reg(16, 8)
    ymlaT = ar.reg(24, 16)
    for b in range(NB):
        B.phaseA(b, hT, Bump(ar, 24, 88))
        if debug and debug[0] == "hT":
            B.dump_bf16_fm(hT, 8, Bump(ar, 40, 88))
            return B.finish()
        B.rwkv(b, hT, yrwT, Bump(ar, 24, 88))
        if debug and debug[0] == "rw":
            if not getattr(B, "nodump", False):
                B.dump_bf16_fm(yrwT, 4, Bump(ar, 40, 88))
            return B.finish()
        B.mla(b, hT, ymlaT, Bump(ar, 40, 88))
        if debug and debug[0] == "mla":
            B.dump_bf16_fm(ymlaT, 8, Bump(ar, 40, 88))
            return B.finish()
        B.merge(b, hT, yrwT, ymlaT, Bump(ar, 40, 72), Bump(ar, 72, 80), Bump(ar, 80, 88))
    B.moe(Bump(ar, 0, 88))
    return B.finish()


_NC_CACHE = {}


def _tokid():
    return np.ascontiguousarray((np.arange(32, dtype=np.int32)[None, :] * 128 + np.arange(128, dtype=np.int32)[:, None]).astype(np.int32))


def kernel(**inputs):
    inp = {k: np.asarray(v) for k, v in inputs.items()}
    w = _host_layout(inp)
    consts = _consts()
    if "nc" not in _NC_CACHE:
        _NC_CACHE["nc"] = build()
    nc = _NC_CACHE["nc"]
    x = np.ascontiguousarray(inp["x"], dtype=np.float32)
    pos = np.ascontiguousarray(inp["positions"]).astype(np.int32)
    in_maps = []
    for core in range(NCORES):
        m = {"x": np.ascontiguousarray(x[core * NB:(core + 1) * NB].reshape(NB * T, D)),
             "pos": np.ascontiguousarray(pos[core * NB:(core + 1) * NB])}
        for n in CONST_NAMES:
            m["c_" + n] = consts[n]
        m["tokid"] = _tokid()
        m.update(w)
        in_maps.append(m)
    res = run_bass_kernel_spmd(nc, in_maps, core_ids=list(range(NCORES)))
    out = np.concatenate([np.asarray(r["out"]).reshape(NB, T, D) for r in res.results], axis=0)
    return out.astype(np.float32)
```
